# Optimizing a Trainium2 kernel written in Bass

```python
import math
import jax
import jax.numpy as jnp
from jax import lax
import numpy as np


D_MODEL = 1024
BATCH = 8
SEQ = 2048
DEPTH = 4

GRID_W = 64
CTX_LEN = 256
N_MIXERS = 4
N_CONV_LAYERS = (DEPTH + 3) // N_MIXERS
N_SSM_LAYERS = (DEPTH + 2) // N_MIXERS
N_SWA_LAYERS = (DEPTH + 1) // N_MIXERS
N_DIFF_LAYERS = DEPTH // N_MIXERS
NORM_EPS = 1e-6
ROPE_BASE = 10000.0
ADA_CHUNKS = 6

CONV_WIDTH = 31

SSM_D_INNER = 2 * D_MODEL
SSM_HEAD_DIM = 64
SSM_HEADS = SSM_D_INNER // SSM_HEAD_DIM
SSM_GROUPS = 4
SSM_STATE = 128
SSM_CONV = 5
SSM_CHUNK = 128
SSM_BC_DIM = 2 * SSM_GROUPS * SSM_STATE
SSM_CONV_DIM = SSM_D_INNER + SSM_BC_DIM
SSM_IN_DIM = SSM_D_INNER + SSM_CONV_DIM + 2 * SSM_HEADS

SWA_HEAD_DIM = 64
SWA_HEADS = D_MODEL // SWA_HEAD_DIM
SWA_KV_HEADS = 4
SWA_GROUP = SWA_HEADS // SWA_KV_HEADS
SWA_WINDOW = 128
SWA_BLOCK = 128
SWA_QKV_DIM = (SWA_HEADS + 2 * SWA_KV_HEADS) * SWA_HEAD_DIM

DIFF_HEAD_DIM = 64
DIFF_HEADS = D_MODEL // (2 * DIFF_HEAD_DIM)
DIFF_V_DIM = 2 * DIFF_HEAD_DIM
DIFF_BLOCK = 128

N_EXPERTS = 32
TOP_K = 4
D_EXPERT = D_MODEL
SWIGLU_LIMIT = 7.0
SWIGLU_ALPHA = 1.702

kernel_name = 'hybrid_flow_trunk_conv_ssd_swa_diff_moe'


def rms_norm(x, g):
    xf = x.astype(jnp.float32)
    y = xf * lax.rsqrt(jnp.mean(xf * xf, axis=-1, keepdims=True) + NORM_EPS)
    return (y * g.astype(jnp.float32)).astype(x.dtype)


def layer_norm(x, g, b):
    xf = x.astype(jnp.float32)
    mu = jnp.mean(xf, axis=-1, keepdims=True)
    var = jnp.mean(jnp.square(xf - mu), axis=-1, keepdims=True)
    y = (xf - mu) * lax.rsqrt(var + NORM_EPS)
    return (y * g.astype(jnp.float32) + b.astype(jnp.float32)).astype(x.dtype)


def modulate(h, shift, scale):
    return h * (1 + scale[:, None]) + shift[:, None]


def axial_rope(rows, head_dim):
    t = jnp.arange(rows * GRID_W)
    row = (t // GRID_W).astype(jnp.float32)
    col = (t % GRID_W).astype(jnp.float32)
    quarter = head_dim // 4
    inv_freq = ROPE_BASE ** (-jnp.arange(quarter, dtype=jnp.float32) / quarter)
    ang = jnp.concatenate([row[:, None] * inv_freq, col[:, None] * inv_freq], axis=-1)
    return jnp.cos(ang), jnp.sin(ang)


def apply_rope(x, cos, sin):
    shape = (cos.shape[0],) + (1,) * (x.ndim - 3) + (cos.shape[1],)
    c, s = cos.reshape(shape), sin.reshape(shape)
    xf = x.astype(jnp.float32)
    x1, x2 = jnp.split(xf, 2, axis=-1)
    return jnp.concatenate([x1 * c - x2 * s, x2 * c + x1 * s], axis=-1).astype(x.dtype)


def depthwise_conv(x, w, b):
    width = w.shape[0]
    half = (width - 1) // 2
    y = lax.conv_general_dilated(
        x, w[:, None, :].astype(x.dtype), window_strides=(1,),
        padding=[(half, width - 1 - half)],
        dimension_numbers=('NWC', 'WIO', 'NWC'),
        feature_group_count=x.shape[-1])
    return y + b


def conformer_mixer(hc, hl, w_pw1, b_pw1, w_dw, b_dw, ln_g, ln_b, w_pw2, b_pw2, ctx_out):
    def run(h):
        a, g = jnp.split(h @ w_pw1 + b_pw1, 2, axis=-1)
        u = a * jax.nn.sigmoid(g)
        u = depthwise_conv(u, w_dw, b_dw)
        u = jax.nn.silu(layer_norm(u, ln_g, ln_b))
        return u @ w_pw2 + b_pw2
    return (run(hc) if ctx_out else None), run(hl)


def ssd_scan(x, dt, a, bm, cm, h0):
    bsz, n = x.shape[:2]
    L = SSM_CHUNK
    nc = n // L
    G, R = SSM_GROUPS, SSM_HEADS // SSM_GROUPS
    loga = (dt * a).reshape(bsz, nc, L, G, R)
    xdt = (x * dt[..., None].astype(x.dtype)).reshape(bsz, nc, L, G, R, SSM_HEAD_DIM)
    bm = bm.reshape(bsz, nc, L, G, SSM_STATE)
    cm = cm.reshape(bsz, nc, L, G, SSM_STATE)
    acum = jnp.cumsum(loga, axis=2)
    lower = jnp.tril(jnp.ones((L, L), dtype=bool))
    seg = acum[:, :, :, None] - acum[:, :, None, :]
    within = jnp.exp(jnp.where(lower[:, :, None, None], seg, -jnp.inf)).astype(x.dtype)
    cb = jnp.einsum('bclgn,bcsgn->bcgls', cm, bm)
    y_diag = jnp.einsum('bcgls,bclsgr,bcsgrp->bclgrp', cb, within, xdt)
    to_end = jnp.exp(acum[:, :, -1:] - acum).astype(x.dtype)
    states = jnp.einsum('bclgn,bclgr,bclgrp->bcgrpn', bm, to_end, xdt)
    chunk_decay = jnp.exp(acum[:, :, -1])

    def step(h, inp):
        s, d = inp
        return h * d[..., None, None] + s, h

    h_final, h_in = lax.scan(step, h0, (jnp.moveaxis(states, 1, 0), jnp.moveaxis(chunk_decay, 1, 0)))
    h_in = jnp.moveaxis(h_in, 0, 1)
    y_off = jnp.einsum('bclgn,bcgrpn,bclgr->bclgrp', cm, h_in.astype(x.dtype),
                       jnp.exp(acum).astype(x.dtype))
    y = (y_diag + y_off).reshape(bsz, n, SSM_HEADS, SSM_HEAD_DIM)
    return y.astype(x.dtype), h_final


def ssm_mixer(hc, hl, w_in, w_conv, b_conv, a_log, dt_bias, d_skip, norm_g, w_out, ctx_out):
    def project(h):
        bsz, n = h.shape[:2]
        z, xbc, dt = jnp.split(h @ w_in, [SSM_D_INNER, SSM_D_INNER + SSM_CONV_DIM], axis=-1)
        xbc = jax.nn.silu(depthwise_conv(xbc, w_conv, b_conv))
        xs, bm, cm = jnp.split(xbc, [SSM_D_INNER, SSM_D_INNER + SSM_GROUPS * SSM_STATE], axis=-1)
        xs = xs.reshape(bsz, n, SSM_HEADS, SSM_HEAD_DIM)
        bm = bm.reshape(bsz, n, SSM_GROUPS, SSM_STATE)
        cm = cm.reshape(bsz, n, SSM_GROUPS, SSM_STATE)
        dt = jax.nn.softplus(dt.astype(jnp.float32).reshape(bsz, n, 2, SSM_HEADS)
                             + dt_bias.astype(jnp.float32))
        return z, xs, bm, cm, dt

    zc, xc, bc, cc, dtc = project(hc)
    zl, xl, bl, cl, dtl = project(hl)
    decay = -jnp.exp(a_log.astype(jnp.float32))
    bsz = hl.shape[0]
    h0 = jnp.zeros((bsz, SSM_GROUPS, SSM_HEADS // SSM_GROUPS, SSM_HEAD_DIM, SSM_STATE), jnp.float32)

    def flip(t):
        return jnp.flip(t, axis=1)

    yc_f, sc_f = ssd_scan(xc, dtc[:, :, 0], decay[0], bc, cc, h0)
    yl_f, _ = ssd_scan(xl, dtl[:, :, 0], decay[0], bl, cl, sc_f)
    yc_b, sc_b = ssd_scan(flip(xc), flip(dtc[:, :, 1]), decay[1], flip(bc), flip(cc), h0)
    yl_b, _ = ssd_scan(flip(xl), flip(dtl[:, :, 1]), decay[1], flip(bl), flip(cl), sc_b)

    def finish(z, xs, y_f, y_b):
        y = y_f + y_b + xs * d_skip[:, None]
        y = y.reshape(y.shape[0], y.shape[1], SSM_D_INNER)
        return rms_norm(y * jax.nn.silu(z), norm_g) @ w_out

    yl = finish(zl, xl, yl_f, flip(yl_b))
    yc = finish(zc, xc, yc_f, flip(yc_b)) if ctx_out else None
    return yc, yl


def gqa_sink_attend(q, k, v, sink, mask):
    s = jnp.einsum('bqkgd,bskd->bkgqs', q, k).astype(jnp.float32) * (SWA_HEAD_DIM ** -0.5)
    if mask is not None:
        s = jnp.where(mask, s, -jnp.inf)
    sink_col = jnp.broadcast_to(sink[None, :, :, None, None], s.shape[:-1] + (1,))
    p = jax.nn.softmax(jnp.concatenate([s, sink_col], axis=-1), axis=-1)[..., :-1]
    return jnp.einsum('bkgqs,bskd->bqkgd', p.astype(v.dtype), v)


def swa_mixer(hc, hl, cos, sin, w_qkv, b_qkv, sinks, w_o, b_o, ctx_out):
    def project(h):
        bsz, n = h.shape[:2]
        q, k, v = jnp.split(h @ w_qkv + b_qkv,
                            [SWA_HEADS * SWA_HEAD_DIM, (SWA_HEADS + SWA_KV_HEADS) * SWA_HEAD_DIM], axis=-1)
        return (q.reshape(bsz, n, SWA_KV_HEADS, SWA_GROUP, SWA_HEAD_DIM),
                k.reshape(bsz, n, SWA_KV_HEADS, SWA_HEAD_DIM),
                v.reshape(bsz, n, SWA_KV_HEADS, SWA_HEAD_DIM))

    qc, kc, vc = project(hc)
    ql, kl, vl = project(hl)
    ql, kl = apply_rope(ql, cos, sin), apply_rope(kl, cos, sin)
    sink = sinks.astype(jnp.float32).reshape(SWA_KV_HEADS, SWA_GROUP)
    bsz, n = hl.shape[:2]
    n_blocks = n // SWA_BLOCK
    span = SWA_BLOCK + 2 * SWA_WINDOW
    pad = ((0, 0), (SWA_WINDOW, SWA_WINDOW), (0, 0), (0, 0))
    kp, vp = jnp.pad(kl, pad), jnp.pad(vl, pad)
    rel = jnp.arange(span) - SWA_WINDOW
    band = jnp.abs(jnp.arange(SWA_BLOCK)[:, None] - rel[None, :]) <= SWA_WINDOW
    ctx_mask = jnp.ones((SWA_BLOCK, kc.shape[1]), dtype=bool)

    def block(args):
        q_blk, start = args
        k_win = lax.dynamic_slice_in_dim(kp, start, span, axis=1)
        v_win = lax.dynamic_slice_in_dim(vp, start, span, axis=1)
        inside = (start + rel >= 0) & (start + rel < n)
        mask = jnp.concatenate([band & inside[None, :], ctx_mask], axis=1)
        return gqa_sink_attend(q_blk, jnp.concatenate([k_win, kc], axis=1),
                               jnp.concatenate([v_win, vc], axis=1), sink, mask)

    q_blocks = jnp.swapaxes(
        ql.reshape(bsz, n_blocks, SWA_BLOCK, SWA_KV_HEADS, SWA_GROUP, SWA_HEAD_DIM), 0, 1)
    starts = jnp.arange(n_blocks) * SWA_BLOCK
    ol = jnp.swapaxes(lax.map(block, (q_blocks, starts)), 0, 1).reshape(bsz, n, SWA_HEADS * SWA_HEAD_DIM)
    yl = ol @ w_o + b_o
    yc = None
    if ctx_out:
        oc = gqa_sink_attend(qc, kc, vc, sink, None).reshape(bsz, qc.shape[1], SWA_HEADS * SWA_HEAD_DIM)
        yc = oc @ w_o + b_o
    return yc, yl


def diff_attend(q, k, v, lam, subln_g, lambda_init):
    s = jnp.einsum('bqhtd,bkhtd->bhtqk', q, k).astype(jnp.float32) * (DIFF_HEAD_DIM ** -0.5)
    p = jax.nn.softmax(s, axis=-1)
    a = p[:, :, 0] - lam * p[:, :, 1]
    o = jnp.einsum('bhqk,bkhe->bqhe', a.astype(v.dtype), v)
    return rms_norm(o, subln_g) * (1.0 - lambda_init)


def diff_mixer(hc, hl, cos, sin, w_qkv, lq1, lk1, lq2, lk2, subln_g, w_o, lambda_init, ctx_out):
    def project(h):
        bsz, n = h.shape[:2]
        q, k, v = jnp.split(h @ w_qkv, 3, axis=-1)
        return (q.reshape(bsz, n, DIFF_HEADS, 2, DIFF_HEAD_DIM),
                k.reshape(bsz, n, DIFF_HEADS, 2, DIFF_HEAD_DIM),
                v.reshape(bsz, n, DIFF_HEADS, DIFF_V_DIM))

    qc, kc, vc = project(hc)
    ql, kl, vl = project(hl)
    ql, kl = apply_rope(ql, cos, sin), apply_rope(kl, cos, sin)
    f32 = jnp.float32
    lam = (jnp.exp(jnp.sum(lq1.astype(f32) * lk1.astype(f32)))
           - jnp.exp(jnp.sum(lq2.astype(f32) * lk2.astype(f32))) + lambda_init)
    bsz, n = hl.shape[:2]
    k_all = jnp.concatenate([kl, kc], axis=1)
    v_all = jnp.concatenate([vl, vc], axis=1)
    q_blocks = jnp.swapaxes(
        ql.reshape(bsz, n // DIFF_BLOCK, DIFF_BLOCK, DIFF_HEADS, 2, DIFF_HEAD_DIM), 0, 1)
    ol = lax.map(lambda qb: diff_attend(qb, k_all, v_all, lam, subln_g, lambda_init), q_blocks)
    ol = jnp.swapaxes(ol, 0, 1).reshape(bsz, n, DIFF_HEADS * DIFF_V_DIM)
    yl = ol @ w_o
    yc = None
    if ctx_out:
        oc = diff_attend(qc, kc, vc, lam, subln_g, lambda_init)
        yc = oc.reshape(bsz, qc.shape[1], DIFF_HEADS * DIFF_V_DIM) @ w_o
    return yc, yl


def moe_ffn(h, w_router, b_router, w_gu, b_gu, w_down, b_down):
    shape = h.shape
    t = h.reshape(-1, shape[-1])
    logits = (t @ w_router + b_router).astype(jnp.float32)
    top_logit, top_e = lax.top_k(logits, TOP_K)
    gate = jax.nn.softmax(top_logit, axis=-1)
    flat_e = top_e.reshape(-1)
    order = jnp.argsort(flat_e)
    e_sorted = flat_e[order]
    tok = order // TOP_K
    xs = t[tok]
    sizes = jnp.bincount(flat_e, length=N_EXPERTS).astype(jnp.int32)
    gu = lax.ragged_dot(xs, w_gu, sizes) + b_gu[e_sorted]
    glu, lin = jnp.split(gu, 2, axis=-1)
    glu = jnp.minimum(glu, SWIGLU_LIMIT)
    lin = jnp.clip(lin, -SWIGLU_LIMIT, SWIGLU_LIMIT)
    act = glu * jax.nn.sigmoid(SWIGLU_ALPHA * glu) * (lin + 1)
    out = lax.ragged_dot(act, w_down, sizes) + b_down[e_sorted]
    out = out * gate.reshape(-1)[order][:, None].astype(out.dtype)
    y = jax.ops.segment_sum(out, tok, num_segments=t.shape[0])
    return y.reshape(shape)


def setup_inputs(seed: int = 0) -> dict:
    key = jax.random.key(seed)
    keys = iter(jax.random.split(key, 64))
    f32 = jnp.float32

    def normal(shape, scale):
        return jax.random.normal(next(keys), shape, f32) * scale

    def gain(shape):
        return 1.0 + normal(shape, 0.02)

    a_log = jnp.log(jax.random.uniform(next(keys), (N_SSM_LAYERS, 2, SSM_HEADS), f32, 1.0, 16.0))
    dt0 = jnp.exp(jax.random.uniform(next(keys), (N_SSM_LAYERS, 2, SSM_HEADS), f32,
                                     math.log(1e-3), math.log(1e-1)))
    dt_bias = dt0 + jnp.log(-jnp.expm1(-dt0))
    d = D_MODEL
    return {
        'x': normal((BATCH, SEQ, d), 1.0),
        'c': normal((BATCH, d), 1.0),
        'ctx': normal((BATCH, CTX_LEN, d), 1.0),
        'c_ctx': normal((d,), 1.0),
        'ada_w': normal((DEPTH, d, ADA_CHUNKS * d), 0.5 * d ** -0.5),
        'ada_b': normal((DEPTH, ADA_CHUNKS * d), 0.02),
        'g_mix': gain((DEPTH, d)),
        'g_ffn': gain((DEPTH, d)),
        'g_final': gain((d,)),
        'conv_w_pw1': normal((N_CONV_LAYERS, d, 2 * d), d ** -0.5),
        'conv_b_pw1': normal((N_CONV_LAYERS, 2 * d), 0.02),
        'conv_w_dw': normal((N_CONV_LAYERS, CONV_WIDTH, d), CONV_WIDTH ** -0.5),
        'conv_b_dw': normal((N_CONV_LAYERS, d), 0.02),
        'conv_ln_g': gain((N_CONV_LAYERS, d)),
        'conv_ln_b': normal((N_CONV_LAYERS, d), 0.02),
        'conv_w_pw2': normal((N_CONV_LAYERS, d, d), d ** -0.5),
        'conv_b_pw2': normal((N_CONV_LAYERS, d), 0.02),
        'ssm_w_in': normal((N_SSM_LAYERS, d, SSM_IN_DIM), d ** -0.5),
        'ssm_w_conv': normal((N_SSM_LAYERS, SSM_CONV, SSM_CONV_DIM), SSM_CONV ** -0.5),
        'ssm_b_conv': normal((N_SSM_LAYERS, SSM_CONV_DIM), 0.02),
        'ssm_a_log': a_log,
        'ssm_dt_bias': dt_bias,
        'ssm_d': 1.0 + normal((N_SSM_LAYERS, SSM_HEADS), 0.1),
        'ssm_norm_g': gain((N_SSM_LAYERS, SSM_D_INNER)),
        'ssm_w_out': normal((N_SSM_LAYERS, SSM_D_INNER, d), SSM_D_INNER ** -0.5),
        'swa_w_qkv': normal((N_SWA_LAYERS, d, SWA_QKV_DIM), d ** -0.5),
        'swa_b_qkv': normal((N_SWA_LAYERS, SWA_QKV_DIM), 0.02),
        'swa_sinks': normal((N_SWA_LAYERS, SWA_HEADS), 0.5),
        'swa_w_o': normal((N_SWA_LAYERS, SWA_HEADS * SWA_HEAD_DIM, d), (SWA_HEADS * SWA_HEAD_DIM) ** -0.5),
        'swa_b_o': normal((N_SWA_LAYERS, d), 0.02),
        'diff_w_qkv': normal((N_DIFF_LAYERS, d, 3 * d), d ** -0.5),
        'diff_lambda_q1': normal((N_DIFF_LAYERS, DIFF_HEAD_DIM), 0.1),
        'diff_lambda_k1': normal((N_DIFF_LAYERS, DIFF_HEAD_DIM), 0.1),
        'diff_lambda_q2': normal((N_DIFF_LAYERS, DIFF_HEAD_DIM), 0.1),
        'diff_lambda_k2': normal((N_DIFF_LAYERS, DIFF_HEAD_DIM), 0.1),
        'diff_subln_g': gain((N_DIFF_LAYERS, DIFF_V_DIM)),
        'diff_w_o': normal((N_DIFF_LAYERS, DIFF_HEADS * DIFF_V_DIM, d), (DIFF_HEADS * DIFF_V_DIM) ** -0.5),
        'moe_w_router': normal((DEPTH, d, N_EXPERTS), d ** -0.5),
        'moe_b_router': normal((DEPTH, N_EXPERTS), 0.01),
        'moe_w_gu': normal((DEPTH, N_EXPERTS, d, 2 * D_EXPERT), d ** -0.5),
        'moe_b_gu': normal((DEPTH, N_EXPERTS, 2 * D_EXPERT), 0.02),
        'moe_w_down': normal((DEPTH, N_EXPERTS, D_EXPERT, d), D_EXPERT ** -0.5),
        'moe_b_down': normal((DEPTH, N_EXPERTS, d), 0.02),
    }


def reference(x, c, ctx, c_ctx, ada_w, ada_b, g_mix, g_ffn, g_final,
              conv_w_pw1, conv_b_pw1, conv_w_dw, conv_b_dw, conv_ln_g, conv_ln_b, conv_w_pw2, conv_b_pw2,
              ssm_w_in, ssm_w_conv, ssm_b_conv, ssm_a_log, ssm_dt_bias, ssm_d, ssm_norm_g, ssm_w_out,
              swa_w_qkv, swa_b_qkv, swa_sinks, swa_w_o, swa_b_o,
              diff_w_qkv, diff_lambda_q1, diff_lambda_k1, diff_lambda_q2, diff_lambda_k2, diff_subln_g, diff_w_o,
              moe_w_router, moe_b_router, moe_w_gu, moe_b_gu, moe_w_down, moe_b_down):
    rows = x.shape[1] // GRID_W
    cos, sin = axial_rope(rows, SWA_HEAD_DIM)
    cond_lat = jax.nn.silu(c)
    cond_ctx = jax.nn.silu(c_ctx)[None]
    xl, xc = x, ctx
    n_ctx = ctx.shape[1]
    for i in range(DEPTH):
        kind, j = i % N_MIXERS, i // N_MIXERS
        ctx_out = i < DEPTH - 1
        ml = jnp.split(cond_lat @ ada_w[i] + ada_b[i], ADA_CHUNKS, axis=-1)
        mc = jnp.split(cond_ctx @ ada_w[i] + ada_b[i], ADA_CHUNKS, axis=-1)
        hl = modulate(rms_norm(xl, g_mix[i]), ml[0], ml[1])
        hc = modulate(rms_norm(xc, g_mix[i]), mc[0], mc[1])
        if kind == 0:
            yc, yl = conformer_mixer(hc, hl, conv_w_pw1[j], conv_b_pw1[j], conv_w_dw[j], conv_b_dw[j],
                                     conv_ln_g[j], conv_ln_b[j], conv_w_pw2[j], conv_b_pw2[j], ctx_out)
        elif kind == 1:
            yc, yl = ssm_mixer(hc, hl, ssm_w_in[j], ssm_w_conv[j], ssm_b_conv[j], ssm_a_log[j],
                               ssm_dt_bias[j], ssm_d[j], ssm_norm_g[j], ssm_w_out[j], ctx_out)
        elif kind == 2:
            yc, yl = swa_mixer(hc, hl, cos, sin, swa_w_qkv[j], swa_b_qkv[j], swa_sinks[j],
                               swa_w_o[j], swa_b_o[j], ctx_out)
        else:
            lambda_init = 0.8 - 0.6 * math.exp(-0.3 * i)
            yc, yl = diff_mixer(hc, hl, cos, sin, diff_w_qkv[j], diff_lambda_q1[j], diff_lambda_k1[j],
                                diff_lambda_q2[j], diff_lambda_k2[j], diff_subln_g[j], diff_w_o[j],
                                lambda_init, ctx_out)
        xl = xl + ml[2][:, None] * yl
        hl = modulate(rms_norm(xl, g_ffn[i]), ml[3], ml[4])
        if ctx_out:
            xc = xc + mc[2][:, None] * yc
            hc = modulate(rms_norm(xc, g_ffn[i]), mc[3], mc[4])
            f = moe_ffn(jnp.concatenate([hc, hl], axis=1), moe_w_router[i], moe_b_router[i],
                        moe_w_gu[i], moe_b_gu[i], moe_w_down[i], moe_b_down[i])
            xc = xc + mc[5][:, None] * f[:, :n_ctx]
            xl = xl + ml[5][:, None] * f[:, n_ctx:]
        else:
            f = moe_ffn(hl, moe_w_router[i], moe_b_router[i], moe_w_gu[i], moe_b_gu[i],
                        moe_w_down[i], moe_b_down[i])
            xl = xl + ml[5][:, None] * f
    return rms_norm(xl, g_final)
```

```python
import math
from contextlib import ExitStack

import numpy as np
import concourse.bass as bass
import concourse.mybir as mybir
from concourse.bass_utils import run_bass_kernel_spmd

F32 = mybir.dt.float32
BF16 = mybir.dt.bfloat16
AF = mybir.ActivationFunctionType
ALU = mybir.AluOpType
AX = mybir.AxisListType

D = 1024
NCTX = 256
NLAT = 2048
T = NCTX + NLAT
NT = T // 128
BLKS = [(0, 256), (256, 512), (768, 512), (1280, 512), (1792, 512)]
EPS = 1e-6
NE = 32


class Res:
    __slots__ = ("name", "w", "r", "dsem", "dcnt")

    def __init__(self, name):
        self.name = name
        self.w = None
        self.r = []
        self.dsem = None
        self.dcnt = 0


class Sched:
    CE = ("pe", "act", "dve", "pool")
    ALLE = ("pe", "act", "dve", "pool", "sp")

    def __init__(self, nc, es):
        self.nc = nc
        self.es = es
        self.ops = {e: [] for e in self.ALLE}
        self.sems = {}
        for e in self.CE:
            self.sems["c_" + e] = es.enter_context(nc.semaphore("c_" + e))
        self.cnt = {e: 0 for e in self.CE}
        self.waited = {e: {} for e in self.ALLE}
        self.dtot = {}
        self.free_dsems = []
        self.ndsem = 0

    def _dsem(self, res):
        if res.dsem is None:
            if self.free_dsems:
                k = self.free_dsems.pop()
            else:
                k = "d%d" % self.ndsem
                self.ndsem += 1
                self.sems[k] = self.es.enter_context(self.nc.semaphore(k))
                self.dtot[k] = 0
            res.dsem = k
        return res.dsem

    def release(self, ress):
        for r in ress:
            if r.dsem is not None:
                self.free_dsems.append(r.dsem)
                r.dsem = None

    def _collect(self, e, reads, writes, pe_acc=False, skip_key=None):
        deps = {}

        def add(ev):
            if ev is None:
                return
            k, v = ev
            if deps.get(k, 0) < v:
                deps[k] = v
        for r in reads:
            add(r.w)
        for w in writes:
            if not (pe_acc and w.w is not None and w.w[0] == "c_pe") and not (
                    skip_key is not None and w.w is not None and w.w[0] == skip_key):
                add(w.w)
            for ev in w.r:
                add(ev)
        out = []
        wd = self.waited[e]
        for k, v in deps.items():
            if wd.get(k, 0) >= v:
                continue
            wd[k] = v
            out.append((k, v))
        return out

    def op(self, e, fn, reads=(), writes=(), pe_acc=False):
        waits = self._collect(e, reads, writes, pe_acc)
        self.cnt[e] += 1
        ev = ("c_" + e, self.cnt[e])
        for r in reads:
            r.r.append(ev)
        for w in writes:
            w.w = ev
            w.r = []
        self.ops[e].append((waits, fn, ev[0], 1))
        return ev

    def dma(self, q, out, in_, reads=(), writes=(), key=None, **kw):
        k = self._dsem(key)
        waits = self._collect(q, reads, writes, skip_key=k)
        self.dtot[k] += 16
        ev = (k, self.dtot[k])
        for r in reads:
            r.r.append(ev)
        for w in writes:
            w.w = ev
            w.r = []
        self.ops[q].append((waits, lambda eng: eng.dma_start(out=out, in_=in_, **kw), k, 16))
        return ev

    def barrier(self):
        allev = [("c_" + e, self.cnt[e]) for e in self.CE if self.cnt[e] > 0]
        allev += [(k, v) for k, v in self.dtot.items() if v > 0]
        for e in self.ALLE:
            wd = self.waited[e]
            waits = []
            for k, v in allev:
                if wd.get(k, 0) < v:
                    wd[k] = v
                    waits.append((k, v))
            if waits:
                self.ops[e].append((waits, None, None, 0))

    def replay(self):
        nc = self.nc
        sems = self.sems
        ops = self.ops

        def run(eng, lst):
            for waits, fn, sk, n in lst:
                for k, v in waits:
                    eng.wait_ge(sems[k], v)
                if fn is not None:
                    fn(eng).then_inc(sems[sk], n)
        with nc.Block() as block:
            @block.sync
            def _(e):
                run(e, ops["sp"])

            @block.tensor
            def _(e):
                run(e, ops["pe"])

            @block.scalar
            def _(e):
                run(e, ops["act"])

            @block.vector
            def _(e):
                run(e, ops["dve"])

            @block.gpsimd
            def _(e):
                run(e, ops["pool"])


class Mem:
    def __init__(self, nc):
        self.nc = nc
        self.n = 0

    def sb(self, es, shape, dt, name=None):
        self.n += 1
        name = (name or "sb") + "_%d" % self.n
        t = es.enter_context(self.nc.sbuf_tensor(name, list(shape), dt))
        return t, Res(name)

    def ps(self, es, shape, dt, name=None):
        self.n += 1
        name = (name or "ps") + "_%d" % self.n
        t = es.enter_context(self.nc.psum_tensor(name, list(shape), dt))
        return t, Res(name)


class Ring:
    def __init__(self, items):
        self.items = items
        self.i = 0

    def next(self):
        it = self.items[self.i % len(self.items)]
        self.i += 1
        return it


def fm(v):
    v = np.asarray(v, np.float32)
    return np.ascontiguousarray(v.reshape(-1, 128).T)


def rope_tables():
    t = np.arange(NLAT)
    row = (t // 64).astype(np.float32)
    col = (t % 64).astype(np.float32)
    quarter = 16
    inv = (10000.0 ** (-np.arange(quarter, dtype=np.float32) / quarter)).astype(np.float32)
    ang = np.concatenate([row[:, None] * inv, col[:, None] * inv], axis=-1).astype(np.float32)
    cos = np.cos(ang).T.astype(np.float32)
    sin = np.sin(ang).T.astype(np.float32)
    C = np.ones((64, T), np.float32)
    S = np.zeros((64, T), np.float32)
    C[0:32, NCTX:] = cos
    C[32:64, NCTX:] = cos
    S[0:32, NCTX:] = -sin
    S[32:64, NCTX:] = sin
    return C, S


class Prog:
    def __init__(self, steps, raw_out=False):
        self.steps = steps
        self.raw_out = raw_out
        import os as _os
        self.dbg = int(_os.environ.get("KDBG", "0"))
        self.nc = bass.Bass("TRN2", target_bir_lowering=False)
        self.din = {}

    def inp(self, name, shape, dt=F32):
        if name not in self.din:
            self.din[name] = self.nc.dram_tensor(name, list(shape), dt, kind="ExternalInput").ap()
        return self.din[name]

    def build(self):
        nc = self.nc
        with ExitStack() as es:
            self.S = S = Sched(nc, es)
            self.M = M = Mem(nc)
            self.es = es
            self.xT, self.xTr = M.sb(es, [128, 8, T], F32, "xT")
            self.identf, self.identf_r = M.sb(es, [128, 128], F32, "identf")
            self.identb, self.identb_r = M.sb(es, [128, 128], BF16, "identb")
            self.onesf, self.onesf_r = M.sb(es, [128, 128], F32, "onesf")
            self.condT, self.condT_r = M.sb(es, [128, 8, 2], F32, "condT")
            self.mv, self.mv_r = M.sb(es, [128, 2, 6, 8], F32, "mv")
            self.epsb, self.epsb_r = M.sb(es, [128, 1], F32, "epsb")
            self.onesb, self.onesb_r = M.sb(es, [128, 128], BF16, "onesb")
            self.setup()
            for st in self.steps:
                kind = st[0]
                if kind == "mods":
                    self.mods(st[1])
                elif kind == "ffn":
                    self.ffn(st[1], with_ctx=st[2])
                elif kind == "mixer":
                    getattr(self, "mixer%d" % st[1])(st[1])
                S.barrier()
            if self.raw_out:
                self.out_raw()
            else:
                self.out_final()
            S.barrier()
            S.replay()
        return nc

    def setup(self):
        nc, S, M = self.nc, self.S, self.M
        x_in = self.inp("x_in", [T, D])
        cond = self.inp("cond", [128, 8, 2])
        identf, ifr = self.identf, self.identf_r
        S.op("pool", lambda e: e.memset(identf[:], 1.0), writes=[ifr])
        S.op("pool", lambda e: e.affine_select(out=identf[:], in_=identf[:], pattern=[[-1, 128]],
                                               compare_op=ALU.is_equal, fill=0.0, base=0, channel_multiplier=1),
             reads=[ifr], writes=[ifr])
        S.op("dve", lambda e: e.tensor_copy(out=self.identb[:], in_=identf[:]), reads=[ifr], writes=[self.identb_r])
        S.op("pool", lambda e: e.memset(self.onesf[:], 1.0), writes=[self.onesf_r])
        S.op("pool", lambda e: e.memset(self.epsb[:], EPS), writes=[self.epsb_r])
        S.op("pool", lambda e: e.memset(self.onesb[:], 1.0), writes=[self.onesb_r])
        with ExitStack() as ps:
            craw, craw_r = M.sb(ps, [128, 8, 2], F32, "craw")
            S.dma("sp", craw[:], cond, writes=[craw_r], key=craw_r)
            S.op("act", lambda e: e.activation(out=self.condT[:], in_=craw[:], func=AF.Silu),
                 reads=[craw_r], writes=[self.condT_r])
            stg = Ring([M.sb(ps, [128, D], F32, "xstg") for _ in range(2)])
            pst = Ring([M.ps(ps, [128, 4, 128], F32, "xtp") for _ in range(2)])
            for t in range(NT):
                st, st_r = stg.next()
                S.dma("sp", st[:], x_in[t * 128:(t + 1) * 128, :], writes=[st_r], key=st_r)
                for h in range(2):
                    pt, pt_r = pst.next()
                    for k in range(4):
                        S.op("pe", lambda e, pt=pt, st=st, k=k, h=h: e.transpose(
                            out=pt[:, k, :], in_=st[:, (h * 4 + k) * 128:(h * 4 + k + 1) * 128], identity=identf[:]),
                            reads=[st_r, ifr], writes=[pt_r], pe_acc=True)
                    eng = "dve" if h == 0 else "act"
                    if eng == "dve":
                        S.op("dve", lambda e, pt=pt, h=h, t=t: e.tensor_copy(
                            out=self.xT[:, h * 4:(h + 1) * 4, t * 128:(t + 1) * 128], in_=pt[:]),
                            reads=[pt_r], writes=[self.xTr])
                    else:
                        S.op("act", lambda e, pt=pt, h=h, t=t: e.activation(
                            out=self.xT[:, h * 4:(h + 1) * 4, t * 128:(t + 1) * 128], in_=pt[:], func=AF.Copy),
                            reads=[pt_r], writes=[self.xTr])
            S.barrier()
            S.release([craw_r] + [r for _, r in stg.items])

    def mods(self, i):
        nc, S, M = self.nc, self.S, self.M
        ada_w = self.inp("ada_w", [4, D, 6 * D])
        lv = self.inp("lvec", [4, 128, 64])
        with ExitStack() as ps:
            lvt, lvt_r = M.sb(ps, [128, 64], F32, "lvt")
            S.dma("sp", lvt[:], lv[i], writes=[lvt_r], key=lvt_r)
            wring = Ring([M.sb(ps, [128, 8, 512], F32, "adaw") for _ in range(2)])
            pm, pm_r = M.ps(ps, [128, 48, 2], F32, "pm")
            md, md_r = M.sb(ps, [128, 2, 48], F32, "md")
            for pi in range(12):
                wt, wt_r = wring.next()
                S.dma("sp", wt[:], ada_w[i, :, pi * 512:(pi + 1) * 512].rearrange("(k p) n -> p k n", p=128),
                      writes=[wt_r], key=wt_r)
                for o4 in range(4):
                    ob = pi * 4 + o4
                    for k in range(8):
                        S.op("pe", lambda e, wt=wt, k=k, o4=o4, ob=ob: e.matmul(
                            pm[:, ob, :], lhsT=wt[:, k, o4 * 128:(o4 + 1) * 128], rhs=self.condT[:, k, :],
                            start=(k == 0), stop=(k == 7)),
                            reads=[wt_r, self.condT_r], writes=[pm_r], pe_acc=True)
            for c in range(2):
                S.op("dve", lambda e, c=c: e.tensor_tensor(out=md[:, c, :], in0=pm[:, :, c], in1=lvt[:, 0:48], op=ALU.add),
                     reads=[pm_r, lvt_r], writes=[md_r])
            mv, mv_r = self.mv, self.mv_r
            for c in range(2):
                S.op("dve", lambda e, c=c: e.scalar_tensor_tensor(out=mv[:, c, 0, :], in0=md[:, c, 8:16], scalar=1.0,
                                                                  in1=lvt[:, 48:56], op0=ALU.add, op1=ALU.mult),
                     reads=[md_r, lvt_r], writes=[mv_r])
                S.op("dve", lambda e, c=c: e.tensor_copy(out=mv[:, c, 1, :], in_=md[:, c, 0:8]), reads=[md_r], writes=[mv_r])
                S.op("dve", lambda e, c=c: e.tensor_copy(out=mv[:, c, 2, :], in_=md[:, c, 16:24]), reads=[md_r], writes=[mv_r])
                S.op("dve", lambda e, c=c: e.scalar_tensor_tensor(out=mv[:, c, 3, :], in0=md[:, c, 32:40], scalar=1.0,
                                                                  in1=lvt[:, 56:64], op0=ALU.add, op1=ALU.mult),
                     reads=[md_r, lvt_r], writes=[mv_r])
                S.op("dve", lambda e, c=c: e.tensor_copy(out=mv[:, c, 4, :], in_=md[:, c, 24:32]), reads=[md_r], writes=[mv_r])
                S.op("dve", lambda e, c=c: e.tensor_copy(out=mv[:, c, 5, :], in_=md[:, c, 40:48]), reads=[md_r], writes=[mv_r])
            S.barrier()
            S.release([lvt_r] + [r for _, r in wring.items])

    def adanorm(self, ps, hT, hT_r, slotA, with_ctx=True, router=None):
        nc, S, M = self.nc, self.S, self.M
        xT, xTr = self.xT, self.xTr
        sq = Ring([M.sb(ps, [128, 512], F32, "nsq") for _ in range(2)])
        if router is not None:
            h32 = Ring([M.sb(ps, [128, 8, 512], F32, "nh32") for _ in range(1)])
        pss = Ring([M.ps(ps, [128, 512], F32, "nss") for _ in range(2)])
        rst = Ring([M.sb(ps, [128, 512], F32, "nrstd") for _ in range(2)])
        tmp = Ring([M.sb(ps, [128, 512], F32, "ntmp") for _ in range(2)])
        if router is not None:
            plg = Ring([M.ps(ps, [128, 32], F32, "rlg") for _ in range(2)])
            pgt = Ring([M.ps(ps, [32, 128], F32, "rgt") for _ in range(2)])
            rt = Ring([[M.sb(ps, [128, 32], F32, "rt%d" % j) for j in range(4)] for _ in range(2)])
            rs = Ring([[M.sb(ps, [128, 8], F32, "rs%d" % j) for j in range(4)] for _ in range(2)])
        for (s, n) in BLKS:
            if s == 0 and not with_ctx:
                continue
            c = 1 if s == 0 else 0
            A = self.mv[:, c, slotA, :]
            B = self.mv[:, c, slotA + 1, :]
            pp, pp_r = pss.next()
            for k in range(8):
                q, q_r = sq.next()
                S.op("act", lambda e, q=q, s=s, n=n, k=k: e.activation(out=q[:, :n], in_=xT[:, k, s:s + n], func=AF.Square),
                     reads=[xTr], writes=[q_r])
                S.op("pe", lambda e, pp=pp, q=q, k=k, n=n: e.matmul(pp[:, :n], lhsT=self.onesf[:], rhs=q[:, :n],
                                                                    start=(k == 0), stop=(k == 7)),
                     reads=[q_r, self.onesf_r], writes=[pp_r], pe_acc=True)
            r, r_r = rst.next()
            S.op("act", lambda e, r=r, pp=pp, n=n: e.activation(out=r[:, :n], in_=pp[:, :n], func=AF.Sqrt, scale=1.0 / D,
                                                                bias=self.epsb[:, 0:1]),
                 reads=[pp_r, self.epsb_r], writes=[r_r])
            S.op("dve", lambda e, r=r, n=n: e.reciprocal(out=r[:, :n], in_=r[:, :n]), reads=[r_r], writes=[r_r])
            if router is not None:
                hh, hh_r = h32.next()
            for k in range(8):
                t_, t_r = tmp.next()
                S.op("dve", lambda e, t_=t_, k=k, s=s, n=n, r=r: e.tensor_tensor(out=t_[:, :n], in0=xT[:, k, s:s + n], in1=r[:, :n],
                                                                                 op=ALU.mult),
                     reads=[xTr, r_r], writes=[t_r])
                if router is not None:
                    S.op("act", lambda e, t_=t_, hh=hh, k=k, n=n, A=A, B=B: e.activation(
                        out=hh[:, k, :n], in_=t_[:, :n], func=AF.Identity, scale=A[:, k:k + 1], bias=B[:, k:k + 1]),
                        reads=[t_r, self.mv_r], writes=[hh_r])
                    S.op("pool", lambda e, hh=hh, k=k, s=s, n=n: e.tensor_copy(out=hT[:, k, s:s + n], in_=hh[:, k, :n]),
                         reads=[hh_r], writes=[hT_r])
                else:
                    S.op("act", lambda e, t_=t_, k=k, s=s, n=n, A=A, B=B: e.activation(
                        out=hT[:, k, s:s + n], in_=t_[:, :n], func=AF.Identity, scale=A[:, k:k + 1], bias=B[:, k:k + 1]),
                        reads=[t_r, self.mv_r], writes=[hT_r])
            if router is not None:
                R = router
                for tt in range(n // 128):
                    lg, lg_r = plg.next()
                    for k in range(8):
                        S.op("pe", lambda e, lg=lg, hh=hh, k=k, tt=tt: e.matmul(
                            lg[:], lhsT=hh[:, k, tt * 128:(tt + 1) * 128], rhs=R["wr"][:, k, :], start=(k == 0), stop=(k == 7)),
                            reads=[hh_r, R["wr_r"]], writes=[lg_r], pe_acc=True)
                    (l, l_r), (ex, ex_r), (mk, mk_r), (gt, gt_r) = rt.next()
                    (m8, m8_r), (ng, ng_r), (sm, sm_r), (rc, rc_r) = rs.next()
                    S.op("dve", lambda e, l=l, lg=lg: e.tensor_tensor(out=l[:], in0=lg[:], in1=R["brb"][:], op=ALU.add),
                         reads=[lg_r, R["brb_r"]], writes=[l_r])
                    S.op("dve", lambda e, m8=m8, l=l: e.max(out=m8[:], in_=l[:]), reads=[l_r], writes=[m8_r])
                    S.op("dve", lambda e, ng=ng, m8=m8: e.tensor_scalar(out=ng[:, 0:1], in0=m8[:, 0:1], scalar1=-1.0, scalar2=None,
                                                                        op0=ALU.mult),
                         reads=[m8_r], writes=[ng_r])
                    S.op("act", lambda e, ex=ex, l=l, ng=ng: e.activation(out=ex[:], in_=l[:], func=AF.Exp, bias=ng[:, 0:1], scale=1.0),
                         reads=[l_r, ng_r], writes=[ex_r])
                    S.op("dve", lambda e, mk=mk, l=l, m8=m8: e.tensor_scalar(out=mk[:], in0=l[:], scalar1=m8[:, 3:4], scalar2=None,
                                                                            op0=ALU.is_ge),
                         reads=[l_r, m8_r], writes=[mk_r])
                    S.op("dve", lambda e, ex=ex, mk=mk: e.tensor_tensor(out=ex[:], in0=ex[:], in1=mk[:], op=ALU.mult),
                         reads=[ex_r, mk_r], writes=[ex_r])
                    S.op("dve", lambda e, sm=sm, ex=ex: e.reduce_sum(out=sm[:, 0:1], in_=ex[:], axis=AX.X), reads=[ex_r], writes=[sm_r])
                    S.op("dve", lambda e, rc=rc, sm=sm: e.reciprocal(out=rc[:, 0:1], in_=sm[:, 0:1]), reads=[sm_r], writes=[rc_r])
                    S.op("dve", lambda e, gt=gt, ex=ex, rc=rc: e.tensor_scalar(out=gt[:], in0=ex[:], scalar1=rc[:, 0:1], scalar2=None,
                                                                              op0=ALU.mult),
                         reads=[ex_r, rc_r], writes=[gt_r])
                    pg, pg_r = pgt.next()
                    S.op("pe", lambda e, pg=pg, gt=gt: e.transpose(out=pg[:], in_=gt[:], identity=self.identf[:]),
                         reads=[gt_r, self.identf_r], writes=[pg_r], pe_acc=True)
                    S.op("act", lambda e, pg=pg, s=s, tt=tt: e.activation(
                        out=R["gateT"][:, s + tt * 128:s + (tt + 1) * 128], in_=pg[:], func=AF.Copy),
                        reads=[pg_r], writes=[R["gateT_r"]])

    def ffn(self, i, with_ctx=True):
        nc, S, M = self.nc, self.S, self.M
        xT, xTr = self.xT, self.xTr
        w_router = self.inp("moe_w_router", [4, D, NE])
        b_router = self.inp("moe_b_router", [4, NE])
        w_gu = self.inp("moe_w_gu", [4, NE, D, 2 * D])
        w_dn = self.inp("moe_w_down", [4, NE, D, D])
        b_gu = self.inp("moe_b_gu_fm", [4, 128, NE, 16])
        b_dn = self.inp("moe_b_down", [4, NE, D])
        blks = [b for b in BLKS if with_ctx or b[0] != 0]
        with ExitStack() as ps:
            hT, hT_r = M.sb(ps, [128, 8, T], BF16, "hT")
            gateT, gateT_r = M.sb(ps, [NE, T], F32, "gateT")
            bgu, bgu_r = M.sb(ps, [128, NE, 16], F32, "bgu")
            bdn, bdn_r = M.sb(ps, [NE, D], F32, "bdn")
            S.dma("sp", bgu[:], b_gu[i], writes=[bgu_r], key=bgu_r)
            S.dma("sp", bdn[:], b_dn[i], writes=[bdn_r], key=bdn_r)
            with ExitStack() as ps2:
                wr, wr_r = M.sb(ps2, [128, 8, NE], F32, "wr")
                brb, brb_r = M.sb(ps2, [128, NE], F32, "brb")
                S.dma("sp", wr[:], w_router[i].rearrange("(k p) n -> p k n", p=128), writes=[wr_r], key=wr_r)
                S.dma("sp", brb[:], b_router[i].partition_broadcast(128), writes=[brb_r], key=brb_r)
                self.adanorm(ps2, hT, hT_r, 3, with_ctx=with_ctx,
                             router=dict(wr=wr, wr_r=wr_r, brb=brb, brb_r=brb_r, gateT=gateT, gateT_r=gateT_r))
                S.barrier()
                S.release([wr_r, brb_r])
            wg = Ring([M.sb(ps, [128, 8, 2, 512], BF16, "wg") for _ in range(2)])
            wd = Ring([M.sb(ps, [128, 4, D], BF16, "wd") for _ in range(2)])
            pgl = Ring([M.ps(ps, [128, 2, 512], F32, "pgl") for _ in range(2)])
            pout = Ring([M.ps(ps, [128, 512], F32, "pout") for _ in range(2)])
            pgb = Ring([M.ps(ps, [128, 512], F32, "pgb") for _ in range(2)])
            gB = Ring([M.sb(ps, [128, 512], F32, "gB") for _ in range(2)])
            mg = Ring([M.sb(ps, [NE, 512], F32, "mg") for _ in range(2)])
            aT = Ring([M.sb(ps, [128, 4, 512], BF16, "aT") for _ in range(2)])
            tg = Ring([M.sb(ps, [128, 512], F32, "tg") for _ in range(2)])
            tsg = Ring([M.sb(ps, [128, 512], F32, "tsg") for _ in range(2)])
            tl = Ring([M.sb(ps, [128, 512], F32, "tl") for _ in range(2)])
            for (s, n) in blks:
                c = 1 if s == 0 else 0
                for oc in range(8):
                    po, po_r = pout.next()
                    S.op("pe", lambda e, po=po, oc=oc, s=s, n=n: e.matmul(
                        po[:, :n], lhsT=bdn[:, oc * 128:(oc + 1) * 128], rhs=gateT[:, s:s + n], start=True, stop=True),
                        reads=[bdn_r, gateT_r], writes=[po_r], pe_acc=True)
                    S.op("dve", lambda e, po=po, oc=oc, s=s, n=n, c=c: e.scalar_tensor_tensor(
                        out=xT[:, oc, s:s + n], in0=po[:, :n], scalar=self.mv[:, c, 5, oc:oc + 1], in1=xT[:, oc, s:s + n],
                        op0=ALU.mult, op1=ALU.add),
                        reads=[po_r, self.mv_r, xTr], writes=[xTr])
            pieces = [(ex, half) for ex in range(NE) for half in range(2)]
            loaded = {}

            def load_piece(idx):
                ex, half = pieces[idx]
                wgt, wg_r = wg.next()
                wdt, wd_r = wd.next()
                for gl in range(2):
                    S.dma("pool", wgt[:, :, gl, :],
                          w_gu[i, ex, :, gl * D + half * 512: gl * D + half * 512 + 512].rearrange("(k p) n -> p k n", p=128),
                          writes=[wg_r], key=wg_r)
                S.dma("pool", wdt[:], w_dn[i, ex, half * 512:(half + 1) * 512, :].rearrange("(j p) n -> p j n", p=128),
                      writes=[wd_r], key=wd_r)
                loaded[idx] = (wgt, wg_r, wdt, wd_r)

            load_piece(0)
            for idx in range(len(pieces)):
                if True:
                    ex, half = pieces[idx]
                    if idx + 1 < len(pieces):
                        load_piece(idx + 1)
                    wgt, wg_r, wdt, wd_r = loaded.pop(idx)
                    for (s, n) in blks:
                        c = 1 if s == 0 else 0
                        pb, pb_r = pgb.next()
                        mg_, mg_r = mg.next()
                        S.op("pool", lambda e, mg_=mg_, ex=ex, s=s, n=n: e.tensor_scalar(
                            out=mg_[:, :n], in0=gateT[:, s:s + n], scalar1=self.identf[0:NE, ex:ex + 1], scalar2=None, op0=ALU.mult),
                            reads=[gateT_r, self.identf_r], writes=[mg_r])
                        S.op("pe", lambda e, pb=pb, mg_=mg_, n=n: e.matmul(
                            pb[:, :n], lhsT=self.onesf[0:NE, :], rhs=mg_[:, :n], start=True, stop=True),
                            reads=[mg_r, self.onesf_r], writes=[pb_r], pe_acc=True)
                        g_, g_r = gB.next()
                        S.op("act", lambda e, g_=g_, pb=pb, n=n: e.activation(out=g_[:, :n], in_=pb[:, :n], func=AF.Copy),
                             reads=[pb_r], writes=[g_r])
                        a_, a_r = aT.next()
                        for jj in range(4):
                            j = half * 4 + jj
                            pg, pg_r = pgl.next()
                            for gl in range(2):
                                for k in range(8):
                                    S.op("pe", lambda e, pg=pg, gl=gl, k=k, jj=jj, s=s, n=n, wgt=wgt: e.matmul(
                                        pg[:, gl, :n], lhsT=wgt[:, k, gl, jj * 128:(jj + 1) * 128], rhs=hT[:, k, s:s + n],
                                        start=(k == 0), stop=(k == 7)),
                                        reads=[wg_r, hT_r], writes=[pg_r], pe_acc=True)
                            t1, t1_r = tg.next()
                            S.op("dve", lambda e, t1=t1, pg=pg, n=n, ex=ex, j=j: e.tensor_scalar(
                                out=t1[:, :n], in0=pg[:, 0, :n], scalar1=bgu[:, ex, j:j + 1], scalar2=7.0, op0=ALU.add, op1=ALU.min),
                                reads=[pg_r, bgu_r], writes=[t1_r])
                            t2, t2_r = tsg.next()
                            S.op("act", lambda e, t2=t2, t1=t1, n=n: e.activation(out=t2[:, :n], in_=t1[:, :n], func=AF.Sigmoid, scale=1.702),
                                 reads=[t1_r], writes=[t2_r])
                            t3, t3_r = tl.next()
                            S.op("dve", lambda e, t3=t3, pg=pg, n=n, ex=ex, j=j: e.tensor_scalar(
                                out=t3[:, :n], in0=pg[:, 1, :n], scalar1=bgu[:, ex, 8 + j:9 + j], scalar2=-7.0, op0=ALU.add, op1=ALU.max),
                                reads=[pg_r, bgu_r], writes=[t3_r])
                            S.op("pool", lambda e, t3=t3, n=n: e.tensor_scalar(
                                out=t3[:, :n], in0=t3[:, :n], scalar1=7.0, scalar2=1.0, op0=ALU.min, op1=ALU.add),
                                reads=[t3_r], writes=[t3_r])
                            S.op("pool", lambda e, t1=t1, t2=t2, n=n: e.tensor_tensor(out=t1[:, :n], in0=t1[:, :n], in1=t2[:, :n], op=ALU.mult),
                                 reads=[t1_r, t2_r], writes=[t1_r])
                            S.op("dve", lambda e, t1=t1, t3=t3, n=n: e.tensor_tensor(out=t1[:, :n], in0=t1[:, :n], in1=t3[:, :n], op=ALU.mult),
                                 reads=[t1_r, t3_r], writes=[t1_r])
                            S.op("pool", lambda e, a_=a_, t1=t1, g_=g_, jj=jj, n=n: e.tensor_tensor(out=a_[:, jj, :n], in0=t1[:, :n], in1=g_[:, :n], op=ALU.mult),
                                 reads=[t1_r, g_r], writes=[a_r])
                        for oc in range(8):
                            po, po_r = pout.next()
                            for jj in range(4):
                                S.op("pe", lambda e, po=po, oc=oc, jj=jj, n=n, wdt=wdt, a_=a_: e.matmul(
                                    po[:, :n], lhsT=wdt[:, jj, oc * 128:(oc + 1) * 128], rhs=a_[:, jj, :n],
                                    start=(jj == 0), stop=(jj == 3)),
                                    reads=[wd_r, a_r], writes=[po_r], pe_acc=True)
                            S.op("dve", lambda e, po=po, oc=oc, s=s, n=n, c=c: e.scalar_tensor_tensor(
                                out=xT[:, oc, s:s + n], in0=po[:, :n], scalar=self.mv[:, c, 5, oc:oc + 1], in1=xT[:, oc, s:s + n],
                                op0=ALU.mult, op1=ALU.add),
                                reads=[po_r, self.mv_r, xTr], writes=[xTr])
            S.barrier()
            S.release([bgu_r, bdn_r] + [r for _, r in wg.items] + [r for _, r in wd.items])

    def resid(self, po, po_r, n, oc, s, gb=None, tring=None):
        S = self.S
        c = 1 if s < NCTX else 0
        xT, xTr = self.xT, self.xTr
        if gb is None:
            S.op("dve", lambda e: e.scalar_tensor_tensor(
                out=xT[:, oc, s:s + n], in0=po[:, :n], scalar=self.mv[:, c, 2, oc:oc + 1], in1=xT[:, oc, s:s + n],
                op0=ALU.mult, op1=ALU.add), reads=[po_r, self.mv_r, xTr], writes=[xTr])
        else:
            gbt, gbt_r = gb
            t_, t_r = tring.next()
            S.op("act", lambda e: e.activation(out=t_[:, :n], in_=po[:, :n], func=AF.Identity,
                                               scale=self.mv[:, c, 2, oc:oc + 1], bias=gbt[:, c, oc:oc + 1]),
                 reads=[po_r, self.mv_r, gbt_r], writes=[t_r])
            S.op("pool", lambda e: e.tensor_tensor(out=xT[:, oc, s:s + n], in0=xT[:, oc, s:s + n], in1=t_[:, :n], op=ALU.add),
                 reads=[t_r, xTr], writes=[xTr])

    def gate_bias(self, ps, bvec_ap):
        S, M = self.S, self.M
        gb, gb_r = M.sb(ps, [128, 2, 8], F32, "gb")
        for c in range(2):
            S.op("dve", lambda e, c=c: e.tensor_tensor(out=gb[:, c, :], in0=self.mv[:, c, 2, :], in1=bvec_ap, op=ALU.mult),
                 reads=[self.mv_r] + self._vec_deps, writes=[gb_r])
        return gb, gb_r

    def mixer0(self, i):
        nc, S, M = self.nc, self.S, self.M
        w1 = self.inp("conv_w_pw1", [1, D, 2 * D])
        w2 = self.inp("conv_w_pw2", [1, D, D])
        cvd = self.inp("conv_vec", [128, 296])
        UW = 2364

        def ucol(s):
            return 15 if s == 0 else s + 45
        with ExitStack() as pA:
            cv, cv_r = M.sb(pA, [128, 296], F32, "cv")
            S.dma("sp", cv[:], cvd, writes=[cv_r], key=cv_r)
            self._vec_deps = [cv_r]
            V, V_r = M.sb(pA, [128, 8, T], BF16, "V")
            with ExitStack() as pB:
                U, U_r = M.sb(pB, [128, 8, UW], BF16, "U")
                S.op("pool", lambda e: e.memset(U[:], 0.0), writes=[U_r])
                with ExitStack() as pC:
                    hT, hT_r = M.sb(pC, [128, 8, T], BF16, "hT")
                    with ExitStack() as pD:
                        self.adanorm(pD, hT, hT_r, 0)
                        S.barrier()
                    wp = Ring([M.sb(pC, [128, 8, 2, 128], BF16, "wp") for _ in range(2)])
                    pa = Ring([M.ps(pC, [128, 2, 512], F32, "pa") for _ in range(2)])
                    sg = Ring([M.sb(pC, [128, 512], F32, "sg") for _ in range(2)])
                    for oc in range(8):
                        wt, wt_r = wp.next()
                        for gl in range(2):
                            S.dma("pool", wt[:, :, gl, :],
                                  w1[0, :, gl * D + oc * 128: gl * D + (oc + 1) * 128].rearrange("(k p) n -> p k n", p=128),
                                  writes=[wt_r], key=wt_r)
                        for (s, n) in BLKS:
                            p_, p_r = pa.next()
                            for gl in range(2):
                                for k in range(8):
                                    S.op("pe", lambda e, p_=p_, gl=gl, k=k, s=s, n=n, wt=wt: e.matmul(
                                        p_[:, gl, :n], lhsT=wt[:, k, gl, :], rhs=hT[:, k, s:s + n], start=(k == 0), stop=(k == 7)),
                                        reads=[wt_r, hT_r], writes=[p_r], pe_acc=True)
                            g_, g_r = sg.next()
                            S.op("act", lambda e, g_=g_, p_=p_, n=n, oc=oc: e.activation(
                                out=g_[:, :n], in_=p_[:, 1, :n], func=AF.Sigmoid, bias=cv[:, 8 + oc:9 + oc], scale=1.0),
                                reads=[p_r, cv_r], writes=[g_r])
                            S.op("dve", lambda e, g_=g_, p_=p_, n=n, oc=oc, s=s: e.scalar_tensor_tensor(
                                out=U[:, oc, ucol(s):ucol(s) + n], in0=p_[:, 0, :n], scalar=cv[:, oc:oc + 1], in1=g_[:, :n],
                                op0=ALU.add, op1=ALU.mult),
                                reads=[p_r, cv_r, g_r], writes=[U_r])
                    S.barrier()
                    S.release([r for _, r in wp.items])
                dgr = Ring([M.sb(pB, [128, 31, 128], BF16, "dg") for _ in range(2)])
                pc = Ring([M.ps(pB, [128, 512], F32, "pc") for _ in range(2)])
                for oc in range(8):
                    dg, dg_r = dgr.next()
                    for w in range(31):
                        S.op("dve", lambda e, dg=dg, w=w, oc=oc: e.tensor_scalar(
                            out=dg[:, w, :], in0=self.identb[:], scalar1=cv[:, 16 + oc * 31 + w:17 + oc * 31 + w], scalar2=None,
                            op0=ALU.mult), reads=[self.identb_r, cv_r], writes=[dg_r])
                    for (s, n) in BLKS:
                        o0 = 0 if s == 0 else s + 30
                        p_, p_r = pc.next()
                        for w in range(31):
                            S.op("pe", lambda e, p_=p_, w=w, oc=oc, o0=o0, n=n, dg=dg: e.matmul(
                                p_[:, :n], lhsT=dg[:, w, :], rhs=U[:, oc, o0 + w:o0 + w + n], start=(w == 0), stop=(w == 30)),
                                reads=[dg_r, U_r], writes=[p_r], pe_acc=True)
                        S.op("act", lambda e, p_=p_, oc=oc, s=s, n=n: e.activation(
                            out=V[:, oc, s:s + n], in_=p_[:, :n], func=AF.Identity, bias=cv[:, 264 + oc:265 + oc], scale=1.0),
                            reads=[p_r, cv_r], writes=[V_r])
                S.barrier()
            h2, h2_r = M.sb(pA, [128, 8, T], BF16, "h2")
            w2t, w2_r = M.sb(pA, [128, 8, D], BF16, "w2t")
            S.dma("pool", w2t[:], w2[0].rearrange("(k p) n -> p k n", p=128), writes=[w2_r], key=w2_r)
            gb = self.gate_bias(pA, cv[:, 288:296])
            ps1 = Ring([M.ps(pA, [128, 512], F32, "ps1") for _ in range(1)])
            ps2 = Ring([M.ps(pA, [128, 512], F32, "ps2") for _ in range(1)])
            po = Ring([M.ps(pA, [128, 512], F32, "po") for _ in range(2)])
            vsq = Ring([M.sb(pA, [128, 512], BF16, "vsq") for _ in range(2)])
            mu = Ring([M.sb(pA, [128, 512], F32, "mu") for _ in range(1)])
            rs = Ring([M.sb(pA, [128, 512], F32, "rs") for _ in range(1)])
            tt = Ring([M.sb(pA, [128, 512], F32, "tt") for _ in range(2)])
            tr = Ring([M.sb(pA, [128, 512], F32, "tr") for _ in range(2)])
            for (s, n) in BLKS:
                a1, a1_r = ps1.next()
                a2, a2_r = ps2.next()
                for k in range(8):
                    q, q_r = vsq.next()
                    S.op("pool", lambda e, q=q, k=k, s=s, n=n: e.tensor_tensor(out=q[:, :n], in0=V[:, k, s:s + n], in1=V[:, k, s:s + n], op=ALU.mult),
                         reads=[V_r], writes=[q_r])
                    S.op("pe", lambda e, a1=a1, k=k, s=s, n=n: e.matmul(a1[:, :n], lhsT=self.onesb[:], rhs=V[:, k, s:s + n],
                                                                         start=(k == 0), stop=(k == 7)),
                         reads=[V_r, self.onesb_r], writes=[a1_r], pe_acc=True)
                    S.op("pe", lambda e, a2=a2, q=q, k=k, n=n: e.matmul(a2[:, :n], lhsT=self.onesb[:], rhs=q[:, :n],
                                                                        start=(k == 0), stop=(k == 7)),
                         reads=[q_r, self.onesb_r], writes=[a2_r], pe_acc=True)
                m_, m_r = mu.next()
                r_, r_r = rs.next()
                S.op("act", lambda e, m_=m_, a1=a1, n=n: e.activation(out=m_[:, :n], in_=a1[:, :n], func=AF.Copy, scale=1.0 / D),
                     reads=[a1_r], writes=[m_r])
                S.op("dve", lambda e, r_=r_, m_=m_, n=n: e.tensor_tensor(out=r_[:, :n], in0=m_[:, :n], in1=m_[:, :n], op=ALU.mult),
                     reads=[m_r], writes=[r_r])
                S.op("dve", lambda e, r_=r_, a2=a2, n=n: e.scalar_tensor_tensor(out=r_[:, :n], in0=a2[:, :n], scalar=1.0 / D, in1=r_[:, :n],
                                                                               op0=ALU.mult, op1=ALU.subtract),
                     reads=[a2_r, r_r], writes=[r_r])
                S.op("act", lambda e, r_=r_, n=n: e.activation(out=r_[:, :n], in_=r_[:, :n], func=AF.Sqrt, bias=self.epsb[:, 0:1], scale=1.0),
                     reads=[r_r, self.epsb_r], writes=[r_r])
                S.op("dve", lambda e, r_=r_, n=n: e.reciprocal(out=r_[:, :n], in_=r_[:, :n]), reads=[r_r], writes=[r_r])
                for k in range(8):
                    t_, t_r = tt.next()
                    S.op("dve", lambda e, t_=t_, k=k, s=s, n=n, m_=m_: e.tensor_tensor(out=t_[:, :n], in0=V[:, k, s:s + n], in1=m_[:, :n], op=ALU.subtract),
                         reads=[V_r, m_r], writes=[t_r])
                    S.op("pool", lambda e, t_=t_, r_=r_, n=n: e.tensor_tensor(out=t_[:, :n], in0=t_[:, :n], in1=r_[:, :n], op=ALU.mult),
                         reads=[t_r, r_r], writes=[t_r])
                    S.op("act", lambda e, t_=t_, k=k, s=s, n=n: e.activation(
                        out=h2[:, k, s:s + n], in_=t_[:, :n], func=AF.Silu, scale=cv[:, 272 + k:273 + k], bias=cv[:, 280 + k:281 + k]),
                        reads=[t_r, cv_r], writes=[h2_r])
            for (s, n) in BLKS:
                for oc in range(8):
                    p_, p_r = po.next()
                    for k in range(8):
                        S.op("pe", lambda e, p_=p_, k=k, oc=oc, s=s, n=n: e.matmul(
                            p_[:, :n], lhsT=w2t[:, k, oc * 128:(oc + 1) * 128], rhs=h2[:, k, s:s + n], start=(k == 0), stop=(k == 7)),
                            reads=[w2_r, h2_r], writes=[p_r], pe_acc=True)
                    self.resid(p_, p_r, n, oc, s, gb=gb, tring=tr)
            S.barrier()
            S.release([cv_r, w2_r])

    def load_w(self, ps, src2d, kch, n, name, npart=128):
        S, M = self.S, self.M
        t, t_r = M.sb(ps, [npart, kch, n], BF16, name)
        S.dma("pool", t[:], src2d.rearrange("(k p) n -> p k n", p=npart), writes=[t_r], key=t_r)
        return t, t_r

    def swap_halves(self, ps, w, w_r, kch, nh, name):
        S, M = self.S, self.M
        ws, ws_r = M.sb(ps, [128, kch, nh * 64], BF16, name)
        for h in range(nh):
            S.op("pool", lambda e, h=h: e.tensor_copy(out=ws[:, :, h * 64:h * 64 + 32], in_=w[:, :, h * 64 + 32:h * 64 + 64]),
                 reads=[w_r], writes=[ws_r])
            S.op("pool", lambda e, h=h: e.tensor_copy(out=ws[:, :, h * 64 + 32:h * 64 + 64], in_=w[:, :, h * 64:h * 64 + 32]),
                 reads=[w_r], writes=[ws_r])
        return ws, ws_r

    def proj_rope(self, pj, pj_r, w, w_r, ws, ws_r, c0, b, bs, b_r, hT, hT_r, out, out_r, blks, C, Sn, tab_r, t1r, t2r):
        S = self.S
        for (s, n) in blks:
            for a, (ww, ww_r) in enumerate(((w, w_r), (ws, ws_r))):
                for k in range(8):
                    S.op("pe", lambda e, a=a, ww=ww, k=k, s=s, n=n: e.matmul(
                        pj[0:64, a, :n], lhsT=ww[:, k, c0:c0 + 64], rhs=hT[:, k, s:s + n], start=(k == 0), stop=(k == 7)),
                        reads=[ww_r, hT_r], writes=[pj_r], pe_acc=True)
            t1, t1_r = t1r.next()
            t2, t2_r = t2r.next()
            S.op("dve", lambda e, t1=t1, s=s, n=n: e.scalar_tensor_tensor(
                out=t1[0:64, :n], in0=pj[0:64, 0, :n], scalar=b, in1=C[:, s:s + n], op0=ALU.add, op1=ALU.mult),
                reads=[pj_r, b_r, tab_r], writes=[t1_r])
            S.op("dve", lambda e, t2=t2, s=s, n=n: e.scalar_tensor_tensor(
                out=t2[0:64, :n], in0=pj[0:64, 1, :n], scalar=bs, in1=Sn[:, s:s + n], op0=ALU.add, op1=ALU.mult),
                reads=[pj_r, b_r, tab_r], writes=[t2_r])
            S.op("pool", lambda e, t1=t1, t2=t2, s=s, n=n: e.tensor_tensor(out=out[0:64, s:s + n], in0=t1[0:64, :n], in1=t2[0:64, :n], op=ALU.add),
                 reads=[t1_r, t2_r], writes=[out_r])

    def load_tables(self, ps):
        S, M = self.S, self.M
        Cd = self.inp("rope_c", [64, T])
        Sd = self.inp("rope_s", [64, T])
        C, C_r = M.sb(ps, [64, T], F32, "ropeC")
        Sn, _ = M.sb(ps, [64, T], F32, "ropeS")
        S.dma("sp", C[:], Cd, writes=[C_r], key=C_r)
        S.dma("sp", Sn[:], Sd, writes=[C_r], key=C_r)
        return C, Sn, C_r

    def mixer2(self, i):
        nc, S, M = self.nc, self.S, self.M
        wqkv = self.inp("swa_w_qkv", [1, D, 1536])
        bqkv = self.inp("swa_b_qkv", [1, 1536])
        wo = self.inp("swa_w_o", [1, D, D])
        bh = self.inp("swa_bh", [64, 44])
        sinks = self.inp("swa_sinks", [1, 16])
        bo = self.inp("swa_bo_fm", [128, 8])
        NEG = -30000.0
        with ExitStack() as pA:
            hT, hT_r = M.sb(pA, [128, 8, T], BF16, "hT")
            with ExitStack() as pD:
                self.adanorm(pD, hT, hT_r, 0)
                S.barrier()
            C, Sn, tab_r = self.load_tables(pA)
            bht, bht_r = M.sb(pA, [64, 44], F32, "bht")
            S.dma("sp", bht[:], bh, writes=[bht_r], key=bht_r)
            skb, skb_r = M.sb(pA, [128, 16], F32, "skb")
            S.dma("sp", skb[:], sinks[0].partition_broadcast(128), writes=[skb_r], key=skb_r)
            bot, bot_r = M.sb(pA, [128, 8], F32, "bot")
            S.dma("sp", bot[:], bo, writes=[bot_r], key=bot_r)
            self._vec_deps = [bot_r]
            gb = self.gate_bias(pA, bot[:])
            mW, mW_r = M.sb(pA, [128, 384], F32, "mW")
            S.op("pool", lambda e: e.memset(mW[:], 0.0), writes=[mW_r])
            S.op("pool", lambda e: e.affine_select(out=mW[:, 0:128], in_=mW[:, 0:128], pattern=[[1, 128]], compare_op=ALU.is_ge,
                                                   fill=NEG, base=0, channel_multiplier=-1), reads=[mW_r], writes=[mW_r])
            S.op("pool", lambda e: e.affine_select(out=mW[:, 256:384], in_=mW[:, 256:384], pattern=[[-1, 128]], compare_op=ALU.is_ge,
                                                   fill=NEG, base=0, channel_multiplier=1), reads=[mW_r], writes=[mW_r])
            kT, kT_r = M.sb(pA, [64, T], BF16, "kT")
            vs, vs_r = M.sb(pA, [128, NT, 64], BF16, "vs")
            qTr = Ring([M.sb(pA, [64, T], BF16, "qT") for _ in range(2)])
            oTg, oTg_r = M.sb(pA, [64, 4, T], BF16, "oTg")
            bvb, bvb_r = M.sb(pA, [128, 64], F32, "bvb")
            t1r = Ring([M.sb(pA, [64, 512], F32, "rp1") for _ in range(2)])
            t2r = Ring([M.sb(pA, [64, 512], F32, "rp2") for _ in range(2)])
            tr = Ring([M.sb(pA, [128, 512], F32, "tr") for _ in range(2)])
            swr = Ring([M.sb(pA, [128, 384], F32, "sw") for _ in range(2)])
            pwr = Ring([M.sb(pA, [128, 640], BF16, "pw") for _ in range(2)])
            pTsr = Ring([M.sb(pA, [128, 5, 128], BF16, "pTs") for _ in range(2)])
            osr = Ring([M.sb(pA, [128, 64], F32, "osb") for _ in range(2)])
            smr = Ring([M.sb(pA, [128, 8], F32, "sm") for _ in range(3)])
            pj, pj_r = M.ps(pA, [128, 2, 512], F32, "pj")
            pscr = Ring([M.ps(pA, [128, 2, 512], F32, "psc") for _ in range(2)])
            pT, pT_r = M.ps(pA, [128, 5, 128], BF16, "pT")
            pso, pso_r = M.ps(pA, [128, 512], F32, "pso")
            for g in range(4):
                if (self.dbg == 7 and g == 1) or (self.dbg == 17 and g == 2) or (self.dbg == 18 and g == 3):
                    return
                with ExitStack() as pG:
                    wq, wq_r = self.load_w(pG, wqkv[0, :, g * 256:(g + 1) * 256], 8, 256, "wq")
                    wk, wk_r = self.load_w(pG, wqkv[0, :, 1024 + g * 64:1024 + (g + 1) * 64], 8, 64, "wk")
                    wv, wv_r = self.load_w(pG, wqkv[0, :, 1280 + g * 64:1280 + (g + 1) * 64], 8, 64, "wv")
                    wqs, wqs_r = self.swap_halves(pG, wq, wq_r, 8, 4, "wqs")
                    wks, wks_r = self.swap_halves(pG, wk, wk_r, 8, 1, "wks")
                    wog, wog_r = self.load_w(pG, wo[0, g * 256:(g + 1) * 256, :], 4, D, "wog", npart=64)
                    S.dma("sp", bvb[:], bqkv[0, 1280 + g * 64:1280 + (g + 1) * 64].partition_broadcast(128),
                          reads=[], writes=[bvb_r], key=bvb_r)
                    self.proj_rope(pj, pj_r, wk, wk_r, wks, wks_r, 0, bht[:, 16 + g:17 + g], bht[:, 24 + 16 + g:25 + 16 + g], bht_r,
                                   hT, hT_r, kT, kT_r, BLKS, C, Sn, tab_r, t1r, t2r)
                    for t in range(NT):
                        for k in range(8):
                            S.op("pe", lambda e, t=t, k=k: e.matmul(pso[:, 0:64], lhsT=hT[:, k, t * 128:(t + 1) * 128], rhs=wv[:, k, :],
                                                                    start=(k == 0), stop=(k == 7)),
                                 reads=[hT_r, wv_r], writes=[pso_r], pe_acc=True)
                        S.op("dve", lambda e, t=t: e.tensor_tensor(out=vs[:, t, :], in0=pso[:, 0:64], in1=bvb[:], op=ALU.add),
                             reads=[pso_r, bvb_r], writes=[vs_r])
                    if self.dbg == 1 or (self.dbg == 11 and g == 1):
                        return
                    for hh in range(4):
                        h = g * 4 + hh
                        qT, qT_r = qTr.next()
                        self.proj_rope(pj, pj_r, wq, wq_r, wqs, wqs_r, hh * 64, bht[:, h:h + 1], bht[:, 24 + h:25 + h], bht_r,
                                       hT, hT_r, qT, qT_r, BLKS, C, Sn, tab_r, t1r, t2r)
                        if self.dbg == 2 or (self.dbg == 12 and g == 1):
                            return
                        for qt in range(NT):
                            if self.dbg == 3 and qt == 1:
                                return
                            if self.dbg == 4 and qt == 3:
                                return
                            if self.dbg == 5 and hh == 1:
                                return
                            psc, psc_r = pscr.next()
                            sm, sm_r = smr.next()
                            pw, pw_r = pwr.next()
                            lat = qt >= 2
                            S.op("pe", lambda e, psc=psc, qT=qT, qt=qt: e.matmul(
                                psc[:, 1, 0:256], lhsT=qT[:, qt * 128:(qt + 1) * 128], rhs=kT[:, 0:256], start=True, stop=True),
                                reads=[qT_r, kT_r], writes=[psc_r], pe_acc=True)
                            if lat:
                                qb = qt - 2
                                lo = max(0, qb - 1)
                                hi = min(15, qb + 1)
                                c0 = (lo - (qb - 1)) * 128
                                c1 = c0 + (hi - lo + 1) * 128
                                ktiles = list(range(lo + 2, hi + 3))
                                S.op("pe", lambda e, psc=psc, qT=qT, qt=qt, lo=lo, hi=hi, c0=c0, c1=c1: e.matmul(
                                    psc[:, 0, c0:c1], lhsT=qT[:, qt * 128:(qt + 1) * 128], rhs=kT[:, 256 + lo * 128:256 + (hi + 1) * 128],
                                    start=True, stop=True), reads=[qT_r, kT_r], writes=[psc_r], pe_acc=True)
                                sw, sw_r = swr.next()
                                S.op("dve", lambda e, sw=sw, psc=psc, c0=c0, c1=c1: e.tensor_tensor(
                                    out=sw[:, c0:c1], in0=psc[:, 0, c0:c1], in1=mW[:, c0:c1], op=ALU.add),
                                    reads=[psc_r, mW_r], writes=[sw_r])
                                S.op("dve", lambda e, sm=sm, sw=sw, c0=c0, c1=c1: e.reduce_max(out=sm[:, 0:1], in_=sw[:, c0:c1], axis=AX.X),
                                     reads=[sw_r], writes=[sm_r])
                            else:
                                ktiles = []
                            S.op("dve", lambda e, sm=sm, psc=psc: e.reduce_max(out=sm[:, 1:2], in_=psc[:, 1, 0:256], axis=AX.X),
                                 reads=[psc_r], writes=[sm_r])
                            if lat:
                                S.op("dve", lambda e, sm=sm: e.tensor_tensor(out=sm[:, 1:2], in0=sm[:, 0:1], in1=sm[:, 1:2], op=ALU.max),
                                     reads=[sm_r], writes=[sm_r])
                            S.op("dve", lambda e, sm=sm, h=h: e.scalar_tensor_tensor(out=sm[:, 2:3], in0=sm[:, 1:2], scalar=0.125, in1=skb[:, h:h + 1],
                                                                                   op0=ALU.mult, op1=ALU.max),
                                 reads=[sm_r, skb_r], writes=[sm_r])
                            S.op("dve", lambda e, sm=sm: e.tensor_scalar(out=sm[:, 3:4], in0=sm[:, 2:3], scalar1=-1.0, scalar2=None, op0=ALU.mult),
                                 reads=[sm_r], writes=[sm_r])
                            S.op("pool", lambda e, sm=sm: e.memset(sm[:, 4:7], 0.0), reads=[sm_r], writes=[sm_r])
                            if lat:
                                S.op("act", lambda e, pw=pw, sw=sw, sm=sm, c0=c0, c1=c1: e.activation(
                                    out=pw[:, c0:c1], in_=sw[:, c0:c1], func=AF.Exp, scale=0.125, bias=sm[:, 3:4], accum_out=sm[:, 4:5]),
                                    reads=[sw_r, sm_r], writes=[pw_r, sm_r])
                            S.op("act", lambda e, pw=pw, psc=psc, sm=sm: e.activation(
                                out=pw[:, 384:640], in_=psc[:, 1, 0:256], func=AF.Exp, scale=0.125, bias=sm[:, 3:4], accum_out=sm[:, 5:6]),
                                reads=[psc_r, sm_r], writes=[pw_r, sm_r])
                            S.op("act", lambda e, sm=sm, h=h: e.activation(out=sm[:, 6:7], in_=skb[:, h:h + 1], func=AF.Exp, scale=1.0, bias=sm[:, 3:4]),
                                 reads=[skb_r, sm_r], writes=[sm_r])
                            S.op("dve", lambda e, sm=sm: e.tensor_tensor(out=sm[:, 4:5], in0=sm[:, 4:5], in1=sm[:, 5:6], op=ALU.add),
                                 reads=[sm_r], writes=[sm_r])
                            S.op("dve", lambda e, sm=sm: e.tensor_tensor(out=sm[:, 4:5], in0=sm[:, 4:5], in1=sm[:, 6:7], op=ALU.add),
                                 reads=[sm_r], writes=[sm_r])
                            S.op("dve", lambda e, sm=sm: e.reciprocal(out=sm[:, 7:8], in_=sm[:, 4:5]), reads=[sm_r], writes=[sm_r])
                            srcs = []
                            if lat:
                                for j, kt_ in enumerate(ktiles):
                                    srcs.append((c0 + j * 128, kt_))
                            srcs += [(384, 0), (512, 1)]
                            for j, (col, kt_) in enumerate(srcs):
                                S.op("pe", lambda e, j=j, col=col, pw=pw: e.transpose(out=pT[:, j, :], in_=pw[:, col:col + 128], identity=self.identb[:]),
                                     reads=[pw_r, self.identb_r], writes=[pT_r], pe_acc=True)
                            pTs, pTs_r = pTsr.next()
                            nj = len(srcs)
                            S.op("act", lambda e, pTs=pTs, nj=nj: e.activation(out=pTs[:, 0:nj, :], in_=pT[:, 0:nj, :], func=AF.Copy),
                                 reads=[pT_r], writes=[pTs_r])
                            for j, (col, kt_) in enumerate(srcs):
                                S.op("pe", lambda e, j=j, kt_=kt_, pTs=pTs, nj=nj: e.matmul(
                                    pso[:, 64:128], lhsT=pTs[:, j, :], rhs=vs[:, kt_, :], start=(j == 0), stop=(j == nj - 1)),
                                    reads=[pTs_r, vs_r], writes=[pso_r], pe_acc=True)
                            osb, osb_r = osr.next()
                            S.op("dve", lambda e, osb=osb, sm=sm: e.tensor_scalar(out=osb[:], in0=pso[:, 64:128], scalar1=sm[:, 7:8], scalar2=None, op0=ALU.mult),
                                 reads=[pso_r, sm_r], writes=[osb_r])
                            S.op("pe", lambda e, osb=osb: e.transpose(out=pso[0:64, 128:256], in_=osb[:], identity=self.identf[:]),
                                 reads=[osb_r, self.identf_r], writes=[pso_r], pe_acc=True)
                            S.op("act", lambda e, hh=hh, qt=qt: e.activation(out=oTg[:, hh, qt * 128:(qt + 1) * 128], in_=pso[0:64, 128:256], func=AF.Copy),
                                 reads=[pso_r], writes=[oTg_r])
                    if self.dbg == 6 or (self.dbg == 16 and g == 1):
                        return
                    for (s, n) in BLKS:
                        for oc in range(8):
                            for hh in range(4):
                                S.op("pe", lambda e, hh=hh, oc=oc, s=s, n=n: e.matmul(
                                    pj[:, 0, :n], lhsT=wog[:, hh, oc * 128:(oc + 1) * 128], rhs=oTg[:, hh, s:s + n], start=(hh == 0), stop=(hh == 3)),
                                    reads=[wog_r, oTg_r], writes=[pj_r], pe_acc=True)
                            if g == 0:
                                self.resid(pj[:, 0, :], pj_r, n, oc, s, gb=gb, tring=tr)
                            else:
                                self.resid(pj[:, 0, :], pj_r, n, oc, s)
                    S.barrier()
                    S.release([wq_r, wk_r, wv_r, wog_r])

    def mixer3(self, i):
        nc, S, M = self.nc, self.S, self.M
        wqkv = self.inp("diff_w_qkv", [1, D, 3 * D])
        wo = self.inp("diff_w_o", [1, D, D])
        lvec = self.inp("diff_lam", [4, 64])
        sg = self.inp("diff_subln_g", [128, 1])
        lambda_init = 0.8 - 0.6 * math.exp(-0.3 * i)
        LBLK = BLKS[1:]
        with ExitStack() as pA:
            hT, hT_r = M.sb(pA, [128, 8, T], BF16, "hT")
            with ExitStack() as pD:
                self.adanorm(pD, hT, hT_r, 0)
                S.barrier()
            C, Sn, tab_r = self.load_tables(pA)
            zb, zb_r = M.sb(pA, [64, 1], F32, "zb")
            S.op("pool", lambda e: e.memset(zb[:], 0.0), writes=[zb_r])
            sgt, sgt_r = M.sb(pA, [128, 1], F32, "sgt")
            S.dma("sp", sgt[:], sg, writes=[sgt_r], key=sgt_r)
            lv, lv_r = M.sb(pA, [128, 4, 64], F32, "lv")
            for a in range(4):
                S.dma("sp", lv[:, a, :], lvec[a].partition_broadcast(128), writes=[lv_r], key=lv_r)
            lam, lam_r = M.sb(pA, [128, 4], F32, "lam")
            lp, lp_r = M.sb(pA, [128, 2, 64], F32, "lp")
            for a in range(2):
                S.op("dve", lambda e, a=a: e.tensor_tensor(out=lp[:, a, :], in0=lv[:, 2 * a, :], in1=lv[:, 2 * a + 1, :], op=ALU.mult),
                     reads=[lv_r], writes=[lp_r])
                S.op("dve", lambda e, a=a: e.reduce_sum(out=lam[:, a:a + 1], in_=lp[:, a, :], axis=AX.X), reads=[lp_r], writes=[lam_r])
            S.op("act", lambda e: e.activation(out=lam[:, 0:2], in_=lam[:, 0:2], func=AF.Exp), reads=[lam_r], writes=[lam_r])
            S.op("dve", lambda e: e.tensor_tensor(out=lam[:, 2:3], in0=lam[:, 0:1], in1=lam[:, 1:2], op=ALU.subtract), reads=[lam_r], writes=[lam_r])
            S.op("dve", lambda e: e.tensor_scalar(out=lam[:, 3:4], in0=lam[:, 2:3], scalar1=float(lambda_init), scalar2=None, op0=ALU.add),
                 reads=[lam_r], writes=[lam_r])
            kTs = [M.sb(pA, [64, T], BF16, "kT%d" % t) for t in range(2)]
            qTs = [M.sb(pA, [64, T], BF16, "qT%d" % t) for t in range(2)]
            vs, vs_r = M.sb(pA, [128, NT, 128], BF16, "vs")
            oT, oT_r = M.sb(pA, [128, NLAT], BF16, "oT")
            pbr = Ring([M.sb(pA, [128, T], BF16, "pb") for _ in range(2)])
            pTs, pTs_r = M.sb(pA, [128, NT, 128], BF16, "pTs")
            t1r = Ring([M.sb(pA, [64, 512], F32, "rp1") for _ in range(2)])
            t2r = Ring([M.sb(pA, [64, 512], F32, "rp2") for _ in range(2)])
            smr = Ring([M.sb(pA, [128, 16], F32, "sm") for _ in range(4)])
            o1r = Ring([M.sb(pA, [128, 128], F32, "o1") for _ in range(2)])
            o2r = Ring([M.sb(pA, [128, 128], F32, "o2") for _ in range(2)])
            jkr = Ring([M.sb(pA, [128, 128], F32, "jk") for _ in range(2)])
            psc, psc_r = M.ps(pA, [128, 5, 512], F32, "psc")
            pT, pT_r = M.ps(pA, [128, 6, 128], BF16, "pT")
            po, po_r = M.ps(pA, [128, 2, 128], F32, "po")
            pout, pout_r = M.ps(pA, [128, 512], F32, "pout")
            KB = [(j * 512, min(512, T - j * 512)) for j in range(5)]
            for c in range(8):
                with ExitStack() as pG:
                    wq, wq_r = self.load_w(pG, wqkv[0, :, c * 128:(c + 1) * 128], 8, 128, "wq")
                    wk, wk_r = self.load_w(pG, wqkv[0, :, D + c * 128:D + (c + 1) * 128], 8, 128, "wk")
                    wv, wv_r = self.load_w(pG, wqkv[0, :, 2 * D + c * 128:2 * D + (c + 1) * 128], 8, 128, "wv")
                    woc, woc_r = self.load_w(pG, wo[0, c * 128:(c + 1) * 128, :], 1, D, "woc")
                    wqs, wqs_r = self.swap_halves(pG, wq, wq_r, 8, 2, "wqs")
                    wks, wks_r = self.swap_halves(pG, wk, wk_r, 8, 2, "wks")
                    for t in range(2):
                        self.proj_rope(psc, psc_r, wk, wk_r, wks, wks_r, t * 64, zb[:, 0:1], zb[:, 0:1], zb_r,
                                       hT, hT_r, kTs[t][0], kTs[t][1], BLKS, C, Sn, tab_r, t1r, t2r)
                        self.proj_rope(psc, psc_r, wq, wq_r, wqs, wqs_r, t * 64, zb[:, 0:1], zb[:, 0:1], zb_r,
                                       hT, hT_r, qTs[t][0], qTs[t][1], LBLK, C, Sn, tab_r, t1r, t2r)
                    for tt in range(NT):
                        for k in range(8):
                            S.op("pe", lambda e, tt=tt, k=k: e.matmul(pout[:, 0:128], lhsT=hT[:, k, tt * 128:(tt + 1) * 128], rhs=wv[:, k, :],
                                                                      start=(k == 0), stop=(k == 7)),
                                 reads=[hT_r, wv_r], writes=[pout_r], pe_acc=True)
                        S.op("act", lambda e, tt=tt: e.activation(out=vs[:, tt, :], in_=pout[:, 0:128], func=AF.Copy),
                             reads=[pout_r], writes=[vs_r])
                    for qb in range(16):
                        q0 = NCTX + qb * 128
                        sms = []
                        for t in range(2):
                            qT, qT_r = qTs[t]
                            kT, kT_r = kTs[t]
                            sm, sm_r = smr.next()
                            sms.append((sm, sm_r))
                            for j, (k0, kn) in enumerate(KB):
                                S.op("pe", lambda e, j=j, k0=k0, kn=kn, qT=qT, kT=kT, q0=q0: e.matmul(
                                    psc[:, j, :kn], lhsT=qT[:, q0:q0 + 128], rhs=kT[:, k0:k0 + kn], start=True, stop=True),
                                    reads=[qT_r, kT_r], writes=[psc_r], pe_acc=True)
                            for j, (k0, kn) in enumerate(KB):
                                S.op("dve", lambda e, j=j, kn=kn, sm=sm: e.reduce_max(out=sm[:, j:j + 1], in_=psc[:, j, :kn], axis=AX.X),
                                     reads=[psc_r], writes=[sm_r])
                            S.op("dve", lambda e, sm=sm: e.reduce_max(out=sm[:, 5:6], in_=sm[:, 0:5], axis=AX.X), reads=[sm_r], writes=[sm_r])
                            S.op("dve", lambda e, sm=sm: e.tensor_scalar(out=sm[:, 6:7], in0=sm[:, 5:6], scalar1=-0.125, scalar2=None, op0=ALU.mult),
                                 reads=[sm_r], writes=[sm_r])
                            S.op("pool", lambda e, sm=sm: e.memset(sm[:, 8:13], 0.0), reads=[sm_r], writes=[sm_r])
                            pb, pb_r = pbr.next()
                            for j, (k0, kn) in enumerate(KB):
                                S.op("act", lambda e, j=j, k0=k0, kn=kn, sm=sm, pb=pb: e.activation(
                                    out=pb[:, k0:k0 + kn], in_=psc[:, j, :kn], func=AF.Exp, scale=0.125, bias=sm[:, 6:7], accum_out=sm[:, 8 + j:9 + j]),
                                    reads=[psc_r, sm_r], writes=[pb_r, sm_r])
                            S.op("dve", lambda e, sm=sm: e.reduce_sum(out=sm[:, 13:14], in_=sm[:, 8:13], axis=AX.X), reads=[sm_r], writes=[sm_r])
                            S.op("dve", lambda e, sm=sm: e.reciprocal(out=sm[:, 14:15], in_=sm[:, 13:14]), reads=[sm_r], writes=[sm_r])
                            for b3 in range(3):
                                for jj in range(6):
                                    j = b3 * 6 + jj
                                    S.op("pe", lambda e, j=j, jj=jj, pb=pb: e.transpose(out=pT[:, jj, :], in_=pb[:, j * 128:(j + 1) * 128], identity=self.identb[:]),
                                         reads=[pb_r, self.identb_r], writes=[pT_r], pe_acc=True)
                                if b3 % 2 == 0:
                                    S.op("act", lambda e, b3=b3: e.activation(out=pTs[:, b3 * 6:(b3 + 1) * 6, :], in_=pT[:], func=AF.Copy),
                                         reads=[pT_r], writes=[pTs_r])
                                else:
                                    S.op("dve", lambda e, b3=b3: e.tensor_copy(out=pTs[:, b3 * 6:(b3 + 1) * 6, :], in_=pT[:]),
                                         reads=[pT_r], writes=[pTs_r])
                            for j in range(NT):
                                S.op("pe", lambda e, j=j, t=t: e.matmul(po[:, t, :], lhsT=pTs[:, j, :], rhs=vs[:, j, :], start=(j == 0), stop=(j == NT - 1)),
                                     reads=[pTs_r, vs_r], writes=[po_r], pe_acc=True)
                        (sm0, sm0_r), (sm1, sm1_r) = sms
                        o1, o1_r = o1r.next()
                        o2, o2_r = o2r.next()
                        jk, jk_r = jkr.next()
                        S.op("dve", lambda e, sm1=sm1: e.tensor_tensor(out=sm1[:, 15:16], in0=sm1[:, 14:15], in1=lam[:, 3:4], op=ALU.mult),
                             reads=[sm1_r, lam_r], writes=[sm1_r])
                        S.op("dve", lambda e, o1=o1, sm1=sm1: e.tensor_scalar(out=o1[:], in0=po[:, 1, :], scalar1=sm1[:, 15:16], scalar2=None, op0=ALU.mult),
                             reads=[po_r, sm1_r], writes=[o1_r])
                        S.op("dve", lambda e, o1=o1, o2=o2, sm0=sm0: e.scalar_tensor_tensor(out=o2[:], in0=po[:, 0, :], scalar=sm0[:, 14:15], in1=o1[:],
                                                                                         op0=ALU.mult, op1=ALU.subtract),
                             reads=[po_r, sm0_r, o1_r], writes=[o2_r])
                        S.op("pool", lambda e, sm0=sm0: e.memset(sm0[:, 7:8], 0.0), reads=[sm0_r], writes=[sm0_r])
                        S.op("act", lambda e, jk=jk, o2=o2, sm0=sm0: e.activation(out=jk[:], in_=o2[:], func=AF.Square, accum_out=sm0[:, 7:8]),
                             reads=[o2_r, sm0_r], writes=[jk_r, sm0_r])
                        S.op("act", lambda e, sm0=sm0: e.activation(out=sm0[:, 7:8], in_=sm0[:, 7:8], func=AF.Sqrt, scale=1.0 / 128, bias=self.epsb[:, 0:1]),
                             reads=[sm0_r, self.epsb_r], writes=[sm0_r])
                        S.op("dve", lambda e, sm0=sm0: e.reciprocal(out=sm0[:, 7:8], in_=sm0[:, 7:8]), reads=[sm0_r], writes=[sm0_r])
                        S.op("dve", lambda e, o2=o2, sm0=sm0: e.tensor_scalar(out=o2[:], in0=o2[:], scalar1=sm0[:, 7:8], scalar2=float(1.0 - lambda_init),
                                                                            op0=ALU.mult, op1=ALU.mult),
                             reads=[o2_r, sm0_r], writes=[o2_r])
                        S.op("pe", lambda e, o2=o2: e.transpose(out=pout[:, 128:256], in_=o2[:], identity=self.identf[:]),
                             reads=[o2_r, self.identf_r], writes=[pout_r], pe_acc=True)
                        S.op("act", lambda e, qb=qb: e.activation(out=oT[:, qb * 128:(qb + 1) * 128], in_=pout[:, 128:256], func=AF.Identity,
                                                                  scale=sgt[:, 0:1]),
                             reads=[pout_r, sgt_r], writes=[oT_r])
                    for (s, n) in LBLK:
                        for oc in range(8):
                            S.op("pe", lambda e, oc=oc, s=s, n=n: e.matmul(
                                pout[:, :n], lhsT=woc[:, 0, oc * 128:(oc + 1) * 128], rhs=oT[:, s - NCTX:s - NCTX + n], start=True, stop=True),
                                reads=[woc_r, oT_r], writes=[pout_r], pe_acc=True)
                            self.resid(pout, pout_r, n, oc, s)
                    S.barrier()
                    S.release([wq_r, wk_r, wv_r, woc_r])

    def mixer1(self, i):
        nc, S, M = self.nc, self.S, self.M
        w_in = self.inp("ssm_w_in", [1, D, 5184])
        w_out = self.inp("ssm_w_out", [1, 2048, D])
        svec = self.inp("ssm_vec", [128, 160])
        a_log = self.inp("ssm_a_log", [1, 64])
        dt_bias = self.inp("ssm_dt_bias", [1, 64])
        d_skip = self.inp("ssm_d", [1, 32])
        dr = lambda name, shape: (nc.dram_tensor(name, list(shape), BF16, kind="Internal").ap(), Res(name))
        XS, XS_r = dr("scr_xs", [NT, 128, 2048])
        BTd, BTd_r = dr("scr_bt", [NT, 128, 4, 128])
        CTd, CTd_r = dr("scr_ct", [NT, 128, 4, 128])
        BMd, BMd_r = dr("scr_bm", [NT, 128, 512])
        ZS, ZS_r = dr("scr_zs", [NT, 128, 2048])
        HB, HB_r = dr("scr_hb", [NT, 128, 2048])
        PW = 2312
        OW = 2308

        def pcol(s):
            return 2 if s == 0 else s + 6
        with ExitStack() as pA:
            sv, sv_r = M.sb(pA, [128, 160], F32, "sv")
            S.dma("sp", sv[:], svec, writes=[sv_r], key=sv_r)
            msk, msk_r = M.sb(pA, [128, 4, 128], F32, "msk")
            S.op("pool", lambda e: e.memset(msk[:], 1.0), writes=[msk_r])
            for a, (pat, cm, op) in enumerate((([[1, 128]], -1, ALU.is_ge), ([[-1, 128]], 1, ALU.is_ge),
                                              ([[-1, 128]], 1, ALU.is_gt), ([[1, 128]], -1, ALU.is_gt))):
                S.op("pool", lambda e, a=a, pat=pat, cm=cm, op=op: e.affine_select(
                    out=msk[:, a, :], in_=msk[:, a, :], pattern=pat, compare_op=op, fill=0.0, base=0, channel_multiplier=cm),
                    reads=[msk_r], writes=[msk_r])
            triF, triB, mltF, mltB = (msk[:, a, :] for a in range(4))
            dt, dt_r = M.sb(pA, [128, NT, 64], F32, "dt")
            loga, loga_r = M.sb(pA, [128, NT, 64], F32, "loga")
            expA, expA_r = M.sb(pA, [128, NT, 64], F32, "expA")
            dec, dec_r = M.sb(pA, [128, NT, 64], F32, "dec")
            wst, wst_r = M.sb(pA, [128, NT, 64], F32, "wst")
            abc, abc_r = M.sb(pA, [128, 64], F32, "abc")
            dtb, dtb_r = M.sb(pA, [128, 64], F32, "dtb")
            dsk, dsk_r = M.sb(pA, [128, 32], F32, "dsk")
            S.dma("sp", abc[:], a_log[0].partition_broadcast(128), writes=[abc_r], key=abc_r)
            S.dma("sp", dtb[:], dt_bias[0].partition_broadcast(128), writes=[dtb_r], key=dtb_r)
            S.dma("sp", dsk[:], d_skip[0].partition_broadcast(128), writes=[dsk_r], key=dsk_r)
            S.op("act", lambda e: e.activation(out=abc[:], in_=abc[:], func=AF.Exp), reads=[abc_r], writes=[abc_r])
            S.op("dve", lambda e: e.tensor_scalar(out=abc[:], in0=abc[:], scalar1=-1.0, scalar2=None, op0=ALU.mult), reads=[abc_r], writes=[abc_r])
            with ExitStack() as pB:
                hT, hT_r = M.sb(pB, [128, 8, T], BF16, "hT")
                with ExitStack() as pD:
                    self.adanorm(pD, hT, hT_r, 0)
                    S.barrier()
                with ExitStack() as pC:
                    wdt, wdt_r = self.load_w(pC, w_in[0, :, 5120:5184], 8, 64, "wdt")
                    pdt = Ring([M.ps(pC, [128, 64], F32, "pdt") for _ in range(2)])
                    pcs = Ring([M.ps(pC, [128, 2, 64], F32, "pcs") for _ in range(2)])
                    tmr = Ring([[M.sb(pC, [128, 64], F32, "sp%d" % j) for j in range(4)] for _ in range(2)])
                    for t in range(NT):
                        p_, p_r = pdt.next()
                        for k in range(8):
                            S.op("pe", lambda e, p_=p_, t=t, k=k: e.matmul(p_[:], lhsT=hT[:, k, t * 128:(t + 1) * 128], rhs=wdt[:, k, :],
                                                                          start=(k == 0), stop=(k == 7)),
                                 reads=[hT_r, wdt_r], writes=[p_r], pe_acc=True)
                        (x_, x_r), (ax, ax_r), (ex, ex_r), (rl, rl_r) = tmr.next()
                        S.op("dve", lambda e, x_=x_, p_=p_: e.tensor_tensor(out=x_[:], in0=p_[:], in1=dtb[:], op=ALU.add),
                             reads=[p_r, dtb_r], writes=[x_r])
                        S.op("act", lambda e, ax=ax, x_=x_: e.activation(out=ax[:], in_=x_[:], func=AF.Abs),
                             reads=[x_r], writes=[ax_r])
                        S.op("act", lambda e, ex=ex, ax=ax: e.activation(out=ex[:], in_=ax[:], func=AF.Exp, scale=-1.0), reads=[ax_r], writes=[ex_r])
                        S.op("act", lambda e, ex=ex: e.activation(out=ex[:], in_=ex[:], func=AF.Ln, bias=self.onesf[:, 0:1], scale=1.0),
                             reads=[ex_r, self.onesf_r], writes=[ex_r])
                        S.op("dve", lambda e, rl=rl, x_=x_: e.tensor_scalar(out=rl[:], in0=x_[:], scalar1=0.0, scalar2=None, op0=ALU.max),
                             reads=[x_r], writes=[rl_r])
                        S.op("dve", lambda e, rl=rl, ex=ex, t=t: e.tensor_tensor(out=dt[:, t, :], in0=rl[:], in1=ex[:], op=ALU.add),
                             reads=[rl_r, ex_r], writes=[dt_r])
                        S.op("dve", lambda e, t=t: e.tensor_tensor(out=loga[:, t, :], in0=dt[:, t, :], in1=abc[:], op=ALU.mult),
                             reads=[dt_r, abc_r], writes=[loga_r])
                        c_, c_r = pcs.next()
                        S.op("pe", lambda e, c_=c_, t=t: e.matmul(c_[:, 0, 0:32], lhsT=triF, rhs=loga[:, t, 0:32], start=True, stop=True),
                             reads=[msk_r, loga_r], writes=[c_r], pe_acc=True)
                        S.op("pe", lambda e, c_=c_, t=t: e.matmul(c_[:, 0, 32:64], lhsT=triB, rhs=loga[:, t, 32:64], start=True, stop=True),
                             reads=[msk_r, loga_r], writes=[c_r], pe_acc=True)
                        S.op("pe", lambda e, c_=c_, t=t: e.matmul(c_[:, 1, :], lhsT=self.onesf[:], rhs=loga[:, t, :], start=True, stop=True),
                             reads=[self.onesf_r, loga_r], writes=[c_r], pe_acc=True)
                        S.op("act", lambda e, c_=c_, t=t: e.activation(out=expA[:, t, :], in_=c_[:, 0, :], func=AF.Exp), reads=[c_r], writes=[expA_r])
                        S.op("act", lambda e, c_=c_, t=t: e.activation(out=dec[:, t, :], in_=c_[:, 1, :], func=AF.Exp), reads=[c_r], writes=[dec_r])
                        S.op("act", lambda e, c_=c_, ax=ax: e.activation(out=ax[:], in_=c_[:, 0, :], func=AF.Copy), reads=[c_r, ax_r], writes=[ax_r])
                        S.op("dve", lambda e, c_=c_, ax=ax: e.tensor_tensor(out=ax[:], in0=c_[:, 1, :], in1=ax[:], op=ALU.subtract),
                             reads=[c_r, ax_r], writes=[ax_r])
                        S.op("act", lambda e, ax=ax: e.activation(out=ax[:], in_=ax[:], func=AF.Exp), reads=[ax_r], writes=[ax_r])
                        S.op("dve", lambda e, ax=ax, t=t: e.tensor_tensor(out=wst[:, t, :], in0=ax[:], in1=dt[:, t, :], op=ALU.mult),
                             reads=[ax_r, dt_r], writes=[wst_r])
                    S.barrier()
                    S.release([wdt_r])
                with ExitStack() as pC:
                    wz, wz_r = self.load_w(pC, w_in[0, :, 0:2048], 8, 2048, "wz")
                    pz, pz_r = M.ps(pC, [128, 4, 512], F32, "pz")
                    zr = Ring([M.sb(pC, [128, 2048], BF16, "zt") for _ in range(2)])
                    for t in range(NT):
                        for nb in range(4):
                            for k in range(8):
                                S.op("pe", lambda e, t=t, nb=nb, k=k: e.matmul(pz[:, nb, :], lhsT=hT[:, k, t * 128:(t + 1) * 128],
                                                                               rhs=wz[:, k, nb * 512:(nb + 1) * 512], start=(k == 0), stop=(k == 7)),
                                     reads=[hT_r, wz_r], writes=[pz_r], pe_acc=True)
                        z_, z_r = zr.next()
                        S.op("act", lambda e, z_=z_: e.activation(out=z_[:].rearrange("p (a b) -> p a b", b=512), in_=pz[:], func=AF.Silu),
                             reads=[pz_r], writes=[z_r])
                        S.dma("sp", ZS[t], z_[:], reads=[z_r], writes=[ZS_r], key=z_r)
                    S.barrier()
                    S.release([wz_r] + [r for _, r in zr.items])
                with ExitStack() as pC:
                    wpr = Ring([M.sb(pC, [128, 8, 128], BF16, "wxp") for _ in range(3)])
                    upr = Ring([M.sb(pC, [128, PW], F32, "upad") for _ in range(1)])
                    acr = Ring([M.sb(pC, [128, OW], F32, "cacc") for _ in range(1)])
                    scr = Ring([M.sb(pC, [128, T], BF16, "scc") for _ in range(2)])
                    xh, xh_r = M.sb(pC, [128, NT, 512], BF16, "xhalf")
                    btm, btm_r = M.sb(pC, [128, NT, 128], BF16, "btm")
                    pp = Ring([M.ps(pC, [128, 512], F32, "pxp") for _ in range(2)])
                    ptr = Ring([M.ps(pC, [128, 6, 128], BF16, "ptx") for _ in range(2)])
                    for u, u_r in upr.items:
                        S.op("pool", lambda e, u=u: e.memset(u[:], 0.0), writes=[u_r])
                    for cc in range(24):
                        wt, wt_r = wpr.next()
                        S.dma("pool", wt[:], w_in[0, :, 2048 + cc * 128:2048 + (cc + 1) * 128].rearrange("(k p) n -> p k n", p=128),
                              writes=[wt_r], key=wt_r)
                        u, u_r = upr.next()
                        for (s, n) in BLKS:
                            p_, p_r = pp.next()
                            for k in range(8):
                                S.op("pe", lambda e, p_=p_, wt=wt, k=k, s=s, n=n: e.matmul(p_[:, :n], lhsT=wt[:, k, :], rhs=hT[:, k, s:s + n],
                                                                                          start=(k == 0), stop=(k == 7)),
                                     reads=[wt_r, hT_r], writes=[p_r], pe_acc=True)
                            S.op("act", lambda e, p_=p_, u=u, s=s, n=n: e.activation(out=u[:, pcol(s):pcol(s) + n], in_=p_[:, :n], func=AF.Copy),
                                 reads=[p_r], writes=[u_r])
                        ac, ac_r = acr.next()
                        eng = "dve"
                        S.op(eng, lambda e, ac=ac, u=u, cc=cc: e.tensor_scalar(
                            out=ac[:], in0=u[:, 0:OW], scalar1=sv[:, cc * 5:cc * 5 + 1], scalar2=sv[:, 120 + cc:121 + cc], op0=ALU.mult, op1=ALU.add),
                            reads=[u_r, sv_r], writes=[ac_r])
                        for w in range(1, 5):
                            S.op(eng, lambda e, ac=ac, u=u, cc=cc, w=w: e.scalar_tensor_tensor(
                                out=ac[:], in0=u[:, w:w + OW], scalar=sv[:, cc * 5 + w:cc * 5 + w + 1], in1=ac[:], op0=ALU.mult, op1=ALU.add),
                                reads=[u_r, sv_r, ac_r], writes=[ac_r])
                        sc, sc_r = scr.next()
                        S.op("act", lambda e, sc=sc, ac=ac: e.activation(out=sc[:, 0:NCTX], in_=ac[:, 0:NCTX], func=AF.Silu), reads=[ac_r], writes=[sc_r])
                        S.op("act", lambda e, sc=sc, ac=ac: e.activation(out=sc[:, NCTX:T], in_=ac[:, 260:260 + NLAT], func=AF.Silu), reads=[ac_r], writes=[sc_r])
                        if cc < 20:
                            for b3 in range(3):
                                pt, pt_r = ptr.next()
                                for jj in range(6):
                                    t = b3 * 6 + jj
                                    S.op("pe", lambda e, pt=pt, jj=jj, t=t, sc=sc: e.transpose(out=pt[:, jj, :], in_=sc[:, t * 128:(t + 1) * 128], identity=self.identb[:]),
                                         reads=[sc_r, self.identb_r], writes=[pt_r], pe_acc=True)
                                if cc < 16:
                                    c8 = cc % 4
                                    S.op("dve", lambda e, pt=pt, b3=b3, c8=c8: e.tensor_copy(out=xh[:, b3 * 6:(b3 + 1) * 6, c8 * 128:(c8 + 1) * 128], in_=pt[:]),
                                         reads=[pt_r], writes=[xh_r])
                                else:
                                    S.op("dve", lambda e, pt=pt, b3=b3: e.tensor_copy(out=btm[:, b3 * 6:(b3 + 1) * 6, :], in_=pt[:]),
                                         reads=[pt_r], writes=[btm_r])
                        if cc < 16 and cc % 4 == 3:
                            half = cc // 4
                            S.dma("sp", XS[:, :, half * 512:(half + 1) * 512].rearrange("t l f -> l t f"), xh[:],
                                  reads=[xh_r], writes=[XS_r], key=xh_r)
                        if 16 <= cc < 20:
                            g = cc - 16
                            S.dma("sp", BTd[:, :, g, :].rearrange("t n l -> n t l"), sc[:].rearrange("p (t l) -> p t l", l=128),
                                  reads=[sc_r], writes=[BTd_r], key=sc_r)
                            S.dma("sp", BMd[:, :, g * 128:(g + 1) * 128].rearrange("t l n -> l t n"), btm[:],
                                  reads=[btm_r], writes=[BMd_r], key=btm_r)
                        if cc >= 20:
                            g = cc - 20
                            S.dma("sp", CTd[:, :, g, :].rearrange("t n l -> n t l"), sc[:].rearrange("p (t l) -> p t l", l=128),
                                  reads=[sc_r], writes=[CTd_r], key=sc_r)
                    S.barrier()
                    S.release([r for _, r in wpr.items] + [r for _, r in scr.items] + [xh_r, btm_r])
            order_b = [1, 0] + list(range(NT - 1, 1, -1))
            Hf, Hf_r = M.sb(pA, [128, 4, 512], F32, "Hst")
            Hb16, Hb16_r = M.sb(pA, [128, 4, 512], BF16, "Hst16")
            pst, pst_r = M.ps(pA, [128, 512], F32, "pst")
            xsr = Ring([M.sb(pA, [128, 2048], BF16, "xs") for _ in range(2)])
            bmr = Ring([M.sb(pA, [128, 512], BF16, "bm") for _ in range(2)])
            xwr = Ring([M.sb(pA, [128, 2048], BF16, "xw") for _ in range(1)])
            hbo = Ring([M.sb(pA, [128, 2048], BF16, "hbo") for _ in range(2)])

            def bc(ap2d):
                return ap2d.unsqueeze(2).to_broadcast([128, 8, 64])

            def v3(ap2d):
                return ap2d.rearrange("p (h d) -> p h d", d=64)

            def state_update(t, xs, xs_r, bm, bm_r, d0):
                xw, xw_r = xwr.next()
                for g in range(4):
                    S.op("pool" if g % 2 else "dve", lambda e, g=g, xw=xw, xs=xs, t=t: e.tensor_tensor(
                        out=v3(xw[:, g * 512:(g + 1) * 512]), in0=v3(xs[:, g * 512:(g + 1) * 512]),
                        in1=bc(wst[:, t, d0 + g * 8:d0 + (g + 1) * 8]), op=ALU.mult),
                        reads=[xs_r, wst_r], writes=[xw_r])
                for g in range(4):
                    S.op("pe", lambda e, g=g, bm=bm, xw=xw: e.matmul(pst[:], lhsT=bm[:, g * 128:(g + 1) * 128], rhs=xw[:, g * 512:(g + 1) * 512],
                                                                     start=True, stop=True),
                         reads=[bm_r, xw_r], writes=[pst_r], pe_acc=True)
                    S.op("dve", lambda e, g=g, t=t: e.tensor_tensor(out=v3(Hf[:, g, :]), in0=v3(Hf[:, g, :]),
                                                                    in1=bc(dec[:, t, d0 + g * 8:d0 + (g + 1) * 8]), op=ALU.mult),
                         reads=[Hf_r, dec_r], writes=[Hf_r])
                    S.op("dve", lambda e, g=g: e.tensor_tensor(out=Hf[:, g, :], in0=Hf[:, g, :], in1=pst[:], op=ALU.add),
                         reads=[Hf_r, pst_r], writes=[Hf_r])
            S.op("pool", lambda e: e.memset(Hf[:], 0.0), writes=[Hf_r])
            for t in order_b:
                xs, xs_r = xsr.next()
                bm, bm_r = bmr.next()
                S.dma("sp", xs[:], XS[t], reads=[XS_r], writes=[xs_r], key=xs_r)
                S.dma("sp", bm[:], BMd[t], reads=[BMd_r], writes=[bm_r], key=bm_r)
                ho, ho_r = hbo.next()
                S.op("act", lambda e, ho=ho: e.activation(out=ho[:].rearrange("p (g f) -> p g f", f=512), in_=Hf[:], func=AF.Copy),
                     reads=[Hf_r], writes=[ho_r])
                S.dma("sp", HB[t], ho[:], reads=[ho_r], writes=[HB_r], key=ho_r)
                state_update(t, xs, xs_r, bm, bm_r, 32)
            S.barrier()
            S.op("pool", lambda e: e.memset(Hf[:], 0.0), writes=[Hf_r])
            S.op("pool", lambda e: e.memset(Hb16[:], 0.0), writes=[Hb16_r])
            wo_t, wo_r = self.load_w(pA, w_out[0], 16, D, "wout")
            for kc in range(16):
                S.op("pool", lambda e, kc=kc: e.tensor_scalar(out=wo_t[:, kc, :], in0=wo_t[:, kc, :], scalar1=sv[:, 144 + kc:145 + kc], scalar2=None,
                                                             op0=ALU.mult), reads=[wo_r, sv_r], writes=[wo_r])
            btr = Ring([M.sb(pA, [128, 4, 128], BF16, "btc") for _ in range(2)])
            ctr = Ring([M.sb(pA, [128, 4, 128], BF16, "ctc") for _ in range(2)])
            zsr = Ring([M.sb(pA, [128, 2048], BF16, "zsc") for _ in range(2)])
            xdf, xdf_r = M.sb(pA, [128, 2048], BF16, "xdf")
            xdb, xdb_r = M.sb(pA, [128, 2048], BF16, "xdb")
            gm, gm_r = M.sb(pA, [128, 2, 128], F32, "gm")
            lhr = Ring([M.sb(pA, [128, 128], F32, "lh") for _ in range(3)])
            dhr = Ring([M.sb(pA, [128, 128], F32, "dh") for _ in range(3)])
            mtr = Ring([M.sb(pA, [128, 128], BF16, "mt") for _ in range(3)])
            yg, yg_r = M.sb(pA, [128, 512], F32, "yg")
            tq = Ring([M.sb(pA, [128, 512], F32, "tq") for _ in range(2)])
            ybf, ybf_r = M.sb(pA, [128, 2048], BF16, "ybf")
            ynT, ynT_r = M.sb(pA, [128, 16, 128], BF16, "ynT")
            ssq, ssq_r = M.sb(pA, [128, 8], F32, "ssq")
            psg = Ring([M.ps(pA, [128, 128], F32, "psg") for _ in range(2)])
            pd, pd_r = M.ps(pA, [128, 512], F32, "pd")
            pf, pf_r = M.ps(pA, [128, 2, 512], F32, "pf")
            ptT, ptT_r = M.ps(pA, [128, 8, 128], BF16, "ptT")
            pout, pout_r = M.ps(pA, [128, 128], F32, "pout")
            for t in range(NT):
                xs, xs_r = xsr.next()
                bm, bm_r = bmr.next()
                bt, bt_r = btr.next()
                ct, ct_r = ctr.next()
                zs, zs_r = zsr.next()
                hb, hb_r = hbo.next()
                S.dma("sp", xs[:], XS[t], reads=[XS_r], writes=[xs_r], key=xs_r)
                S.dma("sp", bm[:], BMd[t], reads=[BMd_r], writes=[bm_r], key=bm_r)
                S.dma("sp", bt[:], BTd[t], reads=[BTd_r], writes=[bt_r], key=bt_r)
                S.dma("sp", ct[:], CTd[t], reads=[CTd_r], writes=[ct_r], key=ct_r)
                S.dma("sp", zs[:], ZS[t], reads=[ZS_r], writes=[zs_r], key=zs_r)
                S.dma("sp", hb[:], HB[t], reads=[HB_r], writes=[hb_r], key=hb_r)
                for g in range(4):
                    S.op("dve", lambda e, g=g, xs=xs, t=t: e.tensor_tensor(out=v3(xdf[:, g * 512:(g + 1) * 512]), in0=v3(xs[:, g * 512:(g + 1) * 512]),
                                                                        in1=bc(dt[:, t, g * 8:(g + 1) * 8]), op=ALU.mult),
                         reads=[xs_r, dt_r], writes=[xdf_r])
                    S.op("pool", lambda e, g=g, xs=xs, t=t: e.tensor_tensor(out=v3(xdb[:, g * 512:(g + 1) * 512]), in0=v3(xs[:, g * 512:(g + 1) * 512]),
                                                                         in1=bc(dt[:, t, 32 + g * 8:32 + (g + 1) * 8]), op=ALU.mult),
                         reads=[xs_r, dt_r], writes=[xdb_r])
                S.op("pool", lambda e: e.memset(ssq[:], 0.0), reads=[ssq_r], writes=[ssq_r])
                for g in range(4):
                    S.op("pe", lambda e, g=g, bt=bt, ct=ct: e.matmul(pst[:, 0:128], lhsT=bt[:, g, :], rhs=ct[:, g, :], start=True, stop=True),
                         reads=[bt_r, ct_r], writes=[pst_r], pe_acc=True)
                    S.op("dve", lambda e: e.tensor_tensor(out=gm[:, 0, :], in0=pst[:, 0:128], in1=triF, op=ALU.mult), reads=[pst_r, msk_r], writes=[gm_r])
                    S.op("dve", lambda e: e.tensor_tensor(out=gm[:, 1, :], in0=pst[:, 0:128], in1=triB, op=ALU.mult), reads=[pst_r, msk_r], writes=[gm_r])
                    for r in range(8):
                        h = g * 8 + r
                        for d_, (mlt, tri, xd, xd_r) in enumerate(((mltF, triF, xdf, xdf_r), (mltB, triB, xdb, xdb_r))):
                            lh, lh_r = lhr.next()
                            S.op("pool", lambda e, lh=lh, mlt=mlt, t=t, h=h, d_=d_: e.tensor_scalar(
                                out=lh[:], in0=mlt, scalar1=loga[:, t, d_ * 32 + h:d_ * 32 + h + 1], scalar2=None, op0=ALU.mult),
                                reads=[msk_r, loga_r], writes=[lh_r])
                            sg_, sg_r = psg.next()
                            S.op("pe", lambda e, sg_=sg_, lh=lh, tri=tri: e.matmul(sg_[:], lhsT=lh[:], rhs=tri, start=True, stop=True),
                                 reads=[lh_r, msk_r], writes=[sg_r], pe_acc=True)
                            dh, dh_r = dhr.next()
                            S.op("act", lambda e, dh=dh, sg_=sg_: e.activation(out=dh[:], in_=sg_[:], func=AF.Exp), reads=[sg_r], writes=[dh_r])
                            mt, mt_r = mtr.next()
                            S.op("dve", lambda e, mt=mt, dh=dh, d_=d_: e.tensor_tensor(out=mt[:], in0=gm[:, d_, :], in1=dh[:], op=ALU.mult),
                                 reads=[gm_r, dh_r], writes=[mt_r])
                            S.op("pe", lambda e, mt=mt, xd=xd, r=r, h=h, d_=d_: e.matmul(
                                pd[:, r * 64:(r + 1) * 64], lhsT=mt[:], rhs=xd[:, h * 64:(h + 1) * 64], start=(d_ == 0), stop=(d_ == 1)),
                                reads=[mt_r, xd_r], writes=[pd_r], pe_acc=True)
                    S.op("pe", lambda e, g=g, ct=ct: e.matmul(pf[:, 0, :], lhsT=ct[:, g, :], rhs=Hb16[:, g, :], start=True, stop=True),
                         reads=[ct_r, Hb16_r], writes=[pf_r], pe_acc=True)
                    S.op("pe", lambda e, g=g, ct=ct, hb=hb: e.matmul(pf[:, 1, :], lhsT=ct[:, g, :], rhs=hb[:, g * 512:(g + 1) * 512], start=True, stop=True),
                         reads=[ct_r, hb_r], writes=[pf_r], pe_acc=True)
                    S.op("act", lambda e: e.activation(out=yg[:], in_=pd[:], func=AF.Copy), reads=[pd_r], writes=[yg_r])
                    for d_ in range(2):
                        q_, q_r = tq.next()
                        S.op("dve", lambda e, q_=q_, d_=d_, t=t, g=g: e.tensor_tensor(out=v3(q_[:]), in0=v3(pf[:, d_, :]),
                                                                                  in1=bc(expA[:, t, d_ * 32 + g * 8:d_ * 32 + (g + 1) * 8]), op=ALU.mult),
                             reads=[pf_r, expA_r], writes=[q_r])
                        S.op("pool", lambda e, q_=q_: e.tensor_tensor(out=yg[:], in0=yg[:], in1=q_[:], op=ALU.add), reads=[yg_r, q_r], writes=[yg_r])
                    q_, q_r = tq.next()
                    S.op("dve", lambda e, q_=q_, g=g, xs=xs: e.tensor_tensor(out=v3(q_[:]), in0=v3(xs[:, g * 512:(g + 1) * 512]),
                                                                          in1=bc(dsk[:, g * 8:(g + 1) * 8]), op=ALU.mult),
                         reads=[xs_r, dsk_r], writes=[q_r])
                    S.op("pool", lambda e, q_=q_: e.tensor_tensor(out=yg[:], in0=yg[:], in1=q_[:], op=ALU.add), reads=[yg_r, q_r], writes=[yg_r])
                    S.op("dve", lambda e, g=g, zs=zs: e.tensor_tensor(out=yg[:], in0=yg[:], in1=zs[:, g * 512:(g + 1) * 512], op=ALU.mult),
                         reads=[yg_r, zs_r], writes=[yg_r])
                    jk, jk_r = tq.next()
                    S.op("act", lambda e, g=g, jk=jk: e.activation(out=jk[:], in_=yg[:], func=AF.Square, accum_out=ssq[:, g:g + 1]),
                         reads=[yg_r, ssq_r], writes=[jk_r, ssq_r])
                    S.op("pool", lambda e, g=g: e.tensor_copy(out=ybf[:, g * 512:(g + 1) * 512], in_=yg[:]), reads=[yg_r], writes=[ybf_r])
                S.op("dve", lambda e: e.reduce_sum(out=ssq[:, 4:5], in_=ssq[:, 0:4], axis=AX.X), reads=[ssq_r], writes=[ssq_r])
                S.op("act", lambda e: e.activation(out=ssq[:, 5:6], in_=ssq[:, 4:5], func=AF.Sqrt, scale=1.0 / 2048, bias=self.epsb[:, 0:1]),
                     reads=[ssq_r, self.epsb_r], writes=[ssq_r])
                S.op("dve", lambda e: e.reciprocal(out=ssq[:, 6:7], in_=ssq[:, 5:6]), reads=[ssq_r], writes=[ssq_r])
                S.op("dve", lambda e: e.tensor_scalar(out=ybf[:], in0=ybf[:], scalar1=ssq[:, 6:7], scalar2=None, op0=ALU.mult),
                     reads=[ybf_r, ssq_r], writes=[ybf_r])
                for b2 in range(2):
                    for jj in range(8):
                        kc = b2 * 8 + jj
                        S.op("pe", lambda e, jj=jj, kc=kc: e.transpose(out=ptT[:, jj, :], in_=ybf[:, kc * 128:(kc + 1) * 128], identity=self.identb[:]),
                             reads=[ybf_r, self.identb_r], writes=[ptT_r], pe_acc=True)
                    S.op("act", lambda e, b2=b2: e.activation(out=ynT[:, b2 * 8:(b2 + 1) * 8, :], in_=ptT[:], func=AF.Copy), reads=[ptT_r], writes=[ynT_r])
                for oc in range(8):
                    for kc in range(16):
                        S.op("pe", lambda e, oc=oc, kc=kc: e.matmul(pout[:], lhsT=wo_t[:, kc, oc * 128:(oc + 1) * 128], rhs=ynT[:, kc, :],
                                                                    start=(kc == 0), stop=(kc == 15)),
                             reads=[wo_r, ynT_r], writes=[pout_r], pe_acc=True)
                    self.resid(pout, pout_r, 128, oc, t * 128)
                state_update(t, xs, xs_r, bm, bm_r, 0)
                S.op("act", lambda e: e.activation(out=Hb16[:], in_=Hf[:], func=AF.Copy), reads=[Hf_r], writes=[Hb16_r])
            S.barrier()

    def out_raw(self):
        nc, S, M = self.nc, self.S, self.M
        y = nc.dram_tensor("y", [T, D], F32, kind="ExternalOutput").ap()
        y_r = Res("y")
        with ExitStack() as ps:
            pst = Ring([M.ps(ps, [128, 4, 128], F32, "otp") for _ in range(2)])
            stg = Ring([M.sb(ps, [128, D], F32, "ostg") for _ in range(2)])
            for t in range(NT):
                st, st_r = stg.next()
                for h in range(2):
                    pt, pt_r = pst.next()
                    for k in range(4):
                        S.op("pe", lambda e, pt=pt, k=k, h=h, t=t: e.transpose(
                            out=pt[:, k, :], in_=self.xT[:, h * 4 + k, t * 128:(t + 1) * 128], identity=self.identf[:]),
                            reads=[self.xTr, self.identf_r], writes=[pt_r], pe_acc=True)
                    S.op("dve", lambda e, pt=pt, st=st, h=h: e.tensor_copy(out=st[:, h * 512:(h + 1) * 512], in_=pt[:]),
                         reads=[pt_r], writes=[st_r])
                S.dma("sp", y[t * 128:(t + 1) * 128, :], st[:], reads=[st_r], writes=[y_r], key=st_r)
            S.barrier()

    def out_final(self):
        nc, S, M = self.nc, self.S, self.M
        y = nc.dram_tensor("y", [NLAT, D], F32, kind="ExternalOutput").ap()
        gfin = self.inp("g_final", [D])
        y_r = Res("y")
        with ExitStack() as ps:
            gb, gb_r = M.sb(ps, [128, D], F32, "gfin")
            S.dma("sp", gb[:], gfin.partition_broadcast(128), writes=[gb_r], key=gb_r)
            pst = Ring([M.ps(ps, [128, 4, 128], F32, "otp") for _ in range(2)])
            stg = Ring([M.sb(ps, [128, D], F32, "ostg") for _ in range(2)])
            jk = Ring([M.sb(ps, [128, D], F32, "ojk") for _ in range(2)])
            ssq = Ring([M.sb(ps, [128, 1], F32, "ossq") for _ in range(2)])
            for t in range(NCTX // 128, NT):
                st, st_r = stg.next()
                for h in range(2):
                    pt, pt_r = pst.next()
                    for k in range(4):
                        S.op("pe", lambda e, pt=pt, k=k, h=h, t=t: e.transpose(
                            out=pt[:, k, :], in_=self.xT[:, h * 4 + k, t * 128:(t + 1) * 128], identity=self.identf[:]),
                            reads=[self.xTr, self.identf_r], writes=[pt_r], pe_acc=True)
                    S.op("dve", lambda e, pt=pt, st=st, h=h: e.tensor_copy(out=st[:, h * 512:(h + 1) * 512], in_=pt[:]),
                         reads=[pt_r], writes=[st_r])
                j_, j_r = jk.next()
                q, q_r = ssq.next()
                S.op("act", lambda e, j_=j_, st=st, q=q: e.activation(out=j_[:], in_=st[:], func=AF.Square, accum_out=q[:]),
                     reads=[st_r], writes=[j_r, q_r])
                S.op("act", lambda e, q=q: e.activation(out=q[:], in_=q[:], func=AF.Sqrt, scale=1.0 / D, bias=self.epsb[:, 0:1]),
                     reads=[q_r, self.epsb_r], writes=[q_r])
                S.op("dve", lambda e, q=q: e.reciprocal(out=q[:], in_=q[:]), reads=[q_r], writes=[q_r])
                S.op("dve", lambda e, j_=j_, st=st, q=q: e.scalar_tensor_tensor(out=j_[:], in0=st[:], scalar=q[:, 0:1], in1=gb[:],
                                                                              op0=ALU.mult, op1=ALU.mult),
                     reads=[st_r, q_r, gb_r, j_r], writes=[j_r])
                r0 = (t - NCTX // 128) * 128
                S.dma("sp", y[r0:r0 + 128, :], j_[:], reads=[j_r], writes=[y_r], key=j_r)
            S.barrier()


FULL_STEPS = []
for _i in range(4):
    FULL_STEPS += [("mods", _i), ("mixer", _i), ("ffn", _i, _i < 3)]


def make_in_maps(inputs, ncores=8, xs=None, cs=None):
    f32 = np.float32
    shared = {}
    lvec = np.zeros((4, 128, 64), f32)
    for i in range(4):
        lvec[i, :, 0:48] = fm(inputs["ada_b"][i])
        lvec[i, :, 48:56] = fm(inputs["g_mix"][i])
        lvec[i, :, 56:64] = fm(inputs["g_ffn"][i])
    shared["lvec"] = lvec
    shared["ada_w"] = np.ascontiguousarray(inputs["ada_w"], f32)
    shared["g_final"] = np.ascontiguousarray(inputs["g_final"], f32)
    shared["moe_w_router"] = np.ascontiguousarray(inputs["moe_w_router"], f32)
    shared["moe_b_router"] = np.ascontiguousarray(inputs["moe_b_router"], f32)
    shared["moe_w_gu"] = np.ascontiguousarray(inputs["moe_w_gu"], f32)
    shared["moe_w_down"] = np.ascontiguousarray(inputs["moe_w_down"], f32)
    shared["moe_b_down"] = np.ascontiguousarray(inputs["moe_b_down"], f32)
    bgu = np.asarray(inputs["moe_b_gu"], f32)
    shared["moe_b_gu_fm"] = np.ascontiguousarray(bgu.reshape(4, NE, 16, 128).transpose(0, 3, 1, 2))
    if "conv_w_pw1" in inputs:
        shared["conv_w_pw1"] = np.ascontiguousarray(inputs["conv_w_pw1"], f32)
        shared["conv_w_pw2"] = np.ascontiguousarray(inputs["conv_w_pw2"], f32)
        cvec = np.zeros((128, 296), f32)
        cvec[:, 0:16] = fm(inputs["conv_b_pw1"][0])
        wdw = np.asarray(inputs["conv_w_dw"][0], f32)
        cvec[:, 16:264] = wdw.reshape(31, 8, 128).transpose(2, 1, 0).reshape(128, 248)
        cvec[:, 264:272] = fm(inputs["conv_b_dw"][0])
        cvec[:, 272:280] = fm(inputs["conv_ln_g"][0])
        cvec[:, 280:288] = fm(inputs["conv_ln_b"][0])
        cvec[:, 288:296] = fm(inputs["conv_b_pw2"][0])
        shared["conv_vec"] = cvec
    if "swa_w_qkv" in inputs:
        shared["swa_w_qkv"] = np.ascontiguousarray(inputs["swa_w_qkv"], f32)
        shared["swa_b_qkv"] = np.ascontiguousarray(inputs["swa_b_qkv"], f32)
        shared["swa_w_o"] = np.ascontiguousarray(inputs["swa_w_o"], f32)
        shared["swa_sinks"] = np.ascontiguousarray(inputs["swa_sinks"], f32)
        bq = np.asarray(inputs["swa_b_qkv"][0], f32).reshape(24, 64).T
        bh = np.zeros((64, 44), f32)
        bh[:, 0:24] = bq
        bh[:, 24:44] = np.roll(bq[:, 0:20], 32, axis=0)
        shared["swa_bh"] = bh
        shared["swa_bo_fm"] = fm(inputs["swa_b_o"][0])
        Ct, St = rope_tables()
        shared["rope_c"] = Ct
        shared["rope_s"] = St
    if "diff_w_qkv" in inputs:
        shared["diff_w_qkv"] = np.ascontiguousarray(inputs["diff_w_qkv"], f32)
        shared["diff_w_o"] = np.ascontiguousarray(inputs["diff_w_o"], f32)
        shared["diff_lam"] = np.ascontiguousarray(np.stack([inputs["diff_lambda_q1"][0], inputs["diff_lambda_k1"][0],
                                                            inputs["diff_lambda_q2"][0], inputs["diff_lambda_k2"][0]], 0), f32)
        shared["diff_subln_g"] = np.ascontiguousarray(np.asarray(inputs["diff_subln_g"][0], f32).reshape(128, 1))
        if "rope_c" not in shared:
            Ct, St = rope_tables()
            shared["rope_c"] = Ct
            shared["rope_s"] = St
    if "ssm_w_in" in inputs:
        shared["ssm_w_in"] = np.ascontiguousarray(inputs["ssm_w_in"], f32)
        shared["ssm_w_out"] = np.ascontiguousarray(inputs["ssm_w_out"], f32)
        svec = np.zeros((128, 160), f32)
        wc = np.asarray(inputs["ssm_w_conv"][0], f32)
        svec[:, 0:120] = wc.reshape(5, 24, 128).transpose(2, 1, 0).reshape(128, 120)
        svec[:, 120:144] = fm(inputs["ssm_b_conv"][0])
        svec[:, 144:160] = fm(inputs["ssm_norm_g"][0])
        shared["ssm_vec"] = svec
        shared["ssm_a_log"] = np.ascontiguousarray(np.asarray(inputs["ssm_a_log"], f32).reshape(1, 64))
        shared["ssm_dt_bias"] = np.ascontiguousarray(np.asarray(inputs["ssm_dt_bias"], f32).reshape(1, 64))
        shared["ssm_d"] = np.ascontiguousarray(np.asarray(inputs["ssm_d"], f32).reshape(1, 32))
    maps = []
    for b in range(ncores):
        m = dict(shared)
        m["x_in"] = np.ascontiguousarray(np.concatenate([inputs["ctx"][b], inputs["x"][b]], axis=0), f32)
        cond = np.stack([fm(inputs["c"][b]), fm(inputs["c_ctx"])], axis=-1)
        m["cond"] = np.ascontiguousarray(cond, f32)
        maps.append(m)
    return maps


def kernel(**inputs):
    prog = Prog(FULL_STEPS)
    nc = prog.build()
    maps = make_in_maps(inputs)
    maps = [{k: v for k, v in m.items() if k in prog.din} for m in maps]
    res = run_bass_kernel_spmd(nc, maps, core_ids=list(range(8)))
    return np.stack([r["y"] for r in res.results], axis=0).astype(np.float32)
```

```python
import math
import os
from contextlib import ExitStack

import numpy as np
import concourse.bass as bass
import concourse.mybir as mybir
from concourse.bass_utils import run_bass_kernel_spmd

F32 = mybir.dt.float32
BF16 = mybir.dt.bfloat16
AF = mybir.ActivationFunctionType
ALU = mybir.AluOpType
AX = mybir.AxisListType

D = 1024
NCTX = 256
NLAT = 2048
T = NCTX + NLAT
NT = T // 128
BLKS = [(0, 256), (256, 512), (768, 512), (1280, 512), (1792, 512)]
EPS = 1e-6
NE = 32
KMOE_NE = int(os.environ.get("KMOE_NE", NE))


class Res:
    __slots__ = ("name", "w", "r", "dsem", "dcnt")

    def __init__(self, name):
        self.name = name
        self.w = None
        self.r = []
        self.dsem = None
        self.dcnt = 0


class Sched:
    CE = ("pe", "act", "dve", "pool")
    ALLE = ("pe", "act", "dve", "pool", "sp")

    def __init__(self, nc, es):
        self.nc = nc
        self.es = es
        self.ops = {e: [] for e in self.ALLE}
        self.sems = {}
        for e in self.CE:
            self.sems["c_" + e] = es.enter_context(nc.semaphore("c_" + e))
        self.cnt = {e: 0 for e in self.CE}
        self.waited = {e: {} for e in self.ALLE}
        self.dtot = {}
        self.free_dsems = []
        self.ndsem = 0

    def _dsem(self, res):
        if res.dsem is None:
            if self.free_dsems:
                k = self.free_dsems.pop()
            else:
                k = "d%d" % self.ndsem
                self.ndsem += 1
                self.sems[k] = self.es.enter_context(self.nc.semaphore(k))
                self.dtot[k] = 0
            res.dsem = k
        return res.dsem

    def release(self, ress):
        for r in ress:
            if r.dsem is not None:
                self.free_dsems.append(r.dsem)
                r.dsem = None

    def _collect(self, e, reads, writes, pe_acc=False, skip_key=None):
        deps = {}

        def add(ev):
            if ev is None:
                return
            k, v = ev
            if deps.get(k, 0) < v:
                deps[k] = v
        for r in reads:
            add(r.w)
        for w in writes:
            if not (pe_acc and w.w is not None and w.w[0] == "c_pe") and not (
                    skip_key is not None and w.w is not None and w.w[0] == skip_key):
                add(w.w)
            for ev in w.r:
                add(ev)
        out = []
        wd = self.waited[e]
        for k, v in deps.items():
            if wd.get(k, 0) >= v:
                continue
            wd[k] = v
            out.append((k, v))
        return out

    def op(self, e, fn, reads=(), writes=(), pe_acc=False):
        waits = self._collect(e, reads, writes, pe_acc)
        self.cnt[e] += 1
        ev = ("c_" + e, self.cnt[e])
        for r in reads:
            r.r.append(ev)
        for w in writes:
            w.w = ev
            w.r = []
        self.ops[e].append((waits, fn, ev[0], 1))
        return ev

    def dma(self, q, out, in_, reads=(), writes=(), key=None, **kw):
        k = self._dsem(key)
        waits = self._collect(q, reads, writes, skip_key=k)
        self.dtot[k] += 16
        ev = (k, self.dtot[k])
        for r in reads:
            r.r.append(ev)
        for w in writes:
            w.w = ev
            w.r = []
        self.ops[q].append((waits, lambda eng: eng.dma_start(out=out, in_=in_, **kw), k, 16))
        return ev

    def barrier(self):
        allev = [("c_" + e, self.cnt[e]) for e in self.CE if self.cnt[e] > 0]
        allev += [(k, v) for k, v in self.dtot.items() if v > 0]
        for e in self.ALLE:
            wd = self.waited[e]
            waits = []
            for k, v in allev:
                if wd.get(k, 0) < v:
                    wd[k] = v
                    waits.append((k, v))
            if waits:
                self.ops[e].append((waits, None, None, 0))

    def replay(self):
        nc = self.nc
        sems = self.sems
        ops = self.ops

        def run(eng, lst):
            for waits, fn, sk, n in lst:
                for k, v in waits:
                    eng.wait_ge(sems[k], v)
                if fn is not None:
                    fn(eng).then_inc(sems[sk], n)
        with nc.Block() as block:
            @block.sync
            def _(e):
                run(e, ops["sp"])

            @block.tensor
            def _(e):
                run(e, ops["pe"])

            @block.scalar
            def _(e):
                run(e, ops["act"])

            @block.vector
            def _(e):
                run(e, ops["dve"])

            @block.gpsimd
            def _(e):
                run(e, ops["pool"])


class Mem:
    def __init__(self, nc):
        self.nc = nc
        self.n = 0

    def sb(self, es, shape, dt, name=None):
        self.n += 1
        name = (name or "sb") + "_%d" % self.n
        t = es.enter_context(self.nc.sbuf_tensor(name, list(shape), dt))
        return t, Res(name)

    def ps(self, es, shape, dt, name=None):
        self.n += 1
        name = (name or "ps") + "_%d" % self.n
        t = es.enter_context(self.nc.psum_tensor(name, list(shape), dt))
        return t, Res(name)


class Ring:
    def __init__(self, items):
        self.items = items
        self.i = 0

    def next(self):
        it = self.items[self.i % len(self.items)]
        self.i += 1
        return it


def fm(v):
    v = np.asarray(v, np.float32)
    return np.ascontiguousarray(v.reshape(-1, 128).T)


def rope_tables():
    t = np.arange(NLAT)
    row = (t // 64).astype(np.float32)
    col = (t % 64).astype(np.float32)
    quarter = 16
    inv = (10000.0 ** (-np.arange(quarter, dtype=np.float32) / quarter)).astype(np.float32)
    ang = np.concatenate([row[:, None] * inv, col[:, None] * inv], axis=-1).astype(np.float32)
    cos = np.cos(ang).T.astype(np.float32)
    sin = np.sin(ang).T.astype(np.float32)
    C = np.ones((64, T), np.float32)
    S = np.zeros((64, T), np.float32)
    C[0:32, NCTX:] = cos
    C[32:64, NCTX:] = cos
    S[0:32, NCTX:] = -sin
    S[32:64, NCTX:] = sin
    return C, S


class Prog:
    def __init__(self, steps, raw_out=False):
        self.steps = steps
        self.raw_out = raw_out
        self.dbg = int(os.environ.get("KDBG", "0"))
        self.nc = bass.Bass("TRN2", target_bir_lowering=False)
        self.din = {}

    def inp(self, name, shape, dt=F32):
        if name not in self.din:
            self.din[name] = self.nc.dram_tensor(name, list(shape), dt, kind="ExternalInput").ap()
        return self.din[name]

    def build(self):
        nc = self.nc
        with ExitStack() as es:
            self.S = S = Sched(nc, es)
            self.M = M = Mem(nc)
            self.es = es
            self.xT, self.xTr = M.sb(es, [128, 8, T], F32, "xT")
            self.identf, self.identf_r = M.sb(es, [128, 128], F32, "identf")
            self.identb, self.identb_r = M.sb(es, [128, 128], BF16, "identb")
            self.onesf, self.onesf_r = M.sb(es, [128, 128], F32, "onesf")
            self.condT, self.condT_r = M.sb(es, [128, 8, 2], F32, "condT")
            self.mv, self.mv_r = M.sb(es, [128, 2, 6, 8], F32, "mv")
            self.epsb, self.epsb_r = M.sb(es, [128, 1], F32, "epsb")
            self.onesb, self.onesb_r = M.sb(es, [128, 128], BF16, "onesb")
            self.setup()
            for st in self.steps:
                kind = st[0]
                if kind == "mods":
                    self.mods(st[1])
                elif kind == "ffn":
                    self.ffn(st[1], with_ctx=st[2])
                elif kind == "mixer":
                    getattr(self, "mixer%d" % st[1])(st[1])
                S.barrier()
            if self.raw_out:
                self.out_raw()
            else:
                self.out_final()
            S.barrier()
            S.replay()
        return nc

    def setup(self):
        nc, S, M = self.nc, self.S, self.M
        x_in = self.inp("x_in", [T, D])
        cond = self.inp("cond", [128, 8, 2])
        identf, ifr = self.identf, self.identf_r
        S.op("pool", lambda e: e.memset(identf[:], 1.0), writes=[ifr])
        S.op("pool", lambda e: e.affine_select(out=identf[:], in_=identf[:], pattern=[[-1, 128]],
                                               compare_op=ALU.is_equal, fill=0.0, base=0, channel_multiplier=1),
             reads=[ifr], writes=[ifr])
        S.op("dve", lambda e: e.tensor_copy(out=self.identb[:], in_=identf[:]), reads=[ifr], writes=[self.identb_r])
        S.op("pool", lambda e: e.memset(self.onesf[:], 1.0), writes=[self.onesf_r])
        S.op("pool", lambda e: e.memset(self.epsb[:], EPS), writes=[self.epsb_r])
        S.op("pool", lambda e: e.memset(self.onesb[:], 1.0), writes=[self.onesb_r])
        with ExitStack() as ps:
            craw, craw_r = M.sb(ps, [128, 8, 2], F32, "craw")
            S.dma("sp", craw[:], cond, writes=[craw_r], key=craw_r)
            S.op("act", lambda e: e.activation(out=self.condT[:], in_=craw[:], func=AF.Silu),
                 reads=[craw_r], writes=[self.condT_r])
            stg = Ring([M.sb(ps, [128, D], F32, "xstg") for _ in range(2)])
            pst = Ring([M.ps(ps, [128, 4, 128], F32, "xtp") for _ in range(2)])
            for t in range(NT):
                st, st_r = stg.next()
                S.dma("sp", st[:], x_in[t * 128:(t + 1) * 128, :], writes=[st_r], key=st_r)
                for h in range(2):
                    pt, pt_r = pst.next()
                    for k in range(4):
                        S.op("pe", lambda e, pt=pt, st=st, k=k, h=h: e.transpose(
                            out=pt[:, k, :], in_=st[:, (h * 4 + k) * 128:(h * 4 + k + 1) * 128], identity=identf[:]),
                            reads=[st_r, ifr], writes=[pt_r], pe_acc=True)
                    eng = "dve" if h == 0 else "act"
                    if eng == "dve":
                        S.op("dve", lambda e, pt=pt, h=h, t=t: e.tensor_copy(
                            out=self.xT[:, h * 4:(h + 1) * 4, t * 128:(t + 1) * 128], in_=pt[:]),
                            reads=[pt_r], writes=[self.xTr])
                    else:
                        S.op("act", lambda e, pt=pt, h=h, t=t: e.activation(
                            out=self.xT[:, h * 4:(h + 1) * 4, t * 128:(t + 1) * 128], in_=pt[:], func=AF.Copy),
                            reads=[pt_r], writes=[self.xTr])
            S.barrier()
            S.release([craw_r] + [r for _, r in stg.items])

    def mods(self, i):
        nc, S, M = self.nc, self.S, self.M
        ada_w = self.inp("ada_w", [4, D, 6 * D])
        lv = self.inp("lvec", [4, 128, 64])
        with ExitStack() as ps:
            lvt, lvt_r = M.sb(ps, [128, 64], F32, "lvt")
            S.dma("sp", lvt[:], lv[i], writes=[lvt_r], key=lvt_r)
            wring = Ring([M.sb(ps, [128, 8, 512], F32, "adaw") for _ in range(2)])
            pm, pm_r = M.ps(ps, [128, 48, 2], F32, "pm")
            md, md_r = M.sb(ps, [128, 2, 48], F32, "md")
            for pi in range(12):
                wt, wt_r = wring.next()
                S.dma("sp", wt[:], ada_w[i, :, pi * 512:(pi + 1) * 512].rearrange("(k p) n -> p k n", p=128),
                      writes=[wt_r], key=wt_r)
                for o4 in range(4):
                    ob = pi * 4 + o4
                    for k in range(8):
                        S.op("pe", lambda e, wt=wt, k=k, o4=o4, ob=ob: e.matmul(
                            pm[:, ob, :], lhsT=wt[:, k, o4 * 128:(o4 + 1) * 128], rhs=self.condT[:, k, :],
                            start=(k == 0), stop=(k == 7)),
                            reads=[wt_r, self.condT_r], writes=[pm_r], pe_acc=True)
            for c in range(2):
                S.op("dve", lambda e, c=c: e.tensor_tensor(out=md[:, c, :], in0=pm[:, :, c], in1=lvt[:, 0:48], op=ALU.add),
                     reads=[pm_r, lvt_r], writes=[md_r])
            mv, mv_r = self.mv, self.mv_r
            for c in range(2):
                S.op("dve", lambda e, c=c: e.scalar_tensor_tensor(out=mv[:, c, 0, :], in0=md[:, c, 8:16], scalar=1.0,
                                                                  in1=lvt[:, 48:56], op0=ALU.add, op1=ALU.mult),
                     reads=[md_r, lvt_r], writes=[mv_r])
                S.op("dve", lambda e, c=c: e.tensor_copy(out=mv[:, c, 1, :], in_=md[:, c, 0:8]), reads=[md_r], writes=[mv_r])
                S.op("dve", lambda e, c=c: e.tensor_copy(out=mv[:, c, 2, :], in_=md[:, c, 16:24]), reads=[md_r], writes=[mv_r])
                S.op("dve", lambda e, c=c: e.scalar_tensor_tensor(out=mv[:, c, 3, :], in0=md[:, c, 32:40], scalar=1.0,
                                                                  in1=lvt[:, 56:64], op0=ALU.add, op1=ALU.mult),
                     reads=[md_r, lvt_r], writes=[mv_r])
                S.op("dve", lambda e, c=c: e.tensor_copy(out=mv[:, c, 4, :], in_=md[:, c, 24:32]), reads=[md_r], writes=[mv_r])
                S.op("dve", lambda e, c=c: e.tensor_copy(out=mv[:, c, 5, :], in_=md[:, c, 40:48]), reads=[md_r], writes=[mv_r])
            S.barrier()
            S.release([lvt_r] + [r for _, r in wring.items])

    def adanorm(self, ps, hT, hT_r, slotA, with_ctx=True, router=None):
        nc, S, M = self.nc, self.S, self.M
        xT, xTr = self.xT, self.xTr
        sq = Ring([M.sb(ps, [128, 512], F32, "nsq") for _ in range(2)])
        if router is not None:
            h32 = Ring([M.sb(ps, [128, 8, 512], F32, "nh32") for _ in range(1)])
        pss = Ring([M.ps(ps, [128, 512], F32, "nss") for _ in range(2)])
        rst = Ring([M.sb(ps, [128, 512], F32, "nrstd") for _ in range(2)])
        tmp = Ring([M.sb(ps, [128, 512], F32, "ntmp") for _ in range(2)])
        if router is not None:
            plg = Ring([M.ps(ps, [128, 32], F32, "rlg") for _ in range(2)])
            pgt = Ring([M.ps(ps, [32, 128], F32, "rgt") for _ in range(2)])
            rt = Ring([[M.sb(ps, [128, 32], F32, "rt%d" % j) for j in range(4)] for _ in range(2)])
            rs = Ring([[M.sb(ps, [128, 8], F32, "rs%d" % j) for j in range(4)] for _ in range(2)])
        for (s, n) in BLKS:
            if s == 0 and not with_ctx:
                continue
            c = 1 if s == 0 else 0
            A = self.mv[:, c, slotA, :]
            B = self.mv[:, c, slotA + 1, :]
            pp, pp_r = pss.next()
            for k in range(8):
                q, q_r = sq.next()
                S.op("act", lambda e, q=q, s=s, n=n, k=k: e.activation(out=q[:, :n], in_=xT[:, k, s:s + n], func=AF.Square),
                     reads=[xTr], writes=[q_r])
                S.op("pe", lambda e, pp=pp, q=q, k=k, n=n: e.matmul(pp[:, :n], lhsT=self.onesf[:], rhs=q[:, :n],
                                                                    start=(k == 0), stop=(k == 7)),
                     reads=[q_r, self.onesf_r], writes=[pp_r], pe_acc=True)
            r, r_r = rst.next()
            S.op("act", lambda e, r=r, pp=pp, n=n: e.activation(out=r[:, :n], in_=pp[:, :n], func=AF.Sqrt, scale=1.0 / D,
                                                                bias=self.epsb[:, 0:1]),
                 reads=[pp_r, self.epsb_r], writes=[r_r])
            S.op("dve", lambda e, r=r, n=n: e.reciprocal(out=r[:, :n], in_=r[:, :n]), reads=[r_r], writes=[r_r])
            if router is not None:
                hh, hh_r = h32.next()
            for k in range(8):
                t_, t_r = tmp.next()
                S.op("dve", lambda e, t_=t_, k=k, s=s, n=n, r=r: e.tensor_tensor(out=t_[:, :n], in0=xT[:, k, s:s + n], in1=r[:, :n],
                                                                                 op=ALU.mult),
                     reads=[xTr, r_r], writes=[t_r])
                if router is not None:
                    S.op("act", lambda e, t_=t_, hh=hh, k=k, n=n, A=A, B=B: e.activation(
                        out=hh[:, k, :n], in_=t_[:, :n], func=AF.Identity, scale=A[:, k:k + 1], bias=B[:, k:k + 1]),
                        reads=[t_r, self.mv_r], writes=[hh_r])
                    S.op("pool", lambda e, hh=hh, k=k, s=s, n=n: e.tensor_copy(out=hT[:, k, s:s + n], in_=hh[:, k, :n]),
                         reads=[hh_r], writes=[hT_r])
                else:
                    S.op("act", lambda e, t_=t_, k=k, s=s, n=n, A=A, B=B: e.activation(
                        out=hT[:, k, s:s + n], in_=t_[:, :n], func=AF.Identity, scale=A[:, k:k + 1], bias=B[:, k:k + 1]),
                        reads=[t_r, self.mv_r], writes=[hT_r])
            if router is not None:
                R = router
                for tt in range(n // 128):
                    lg, lg_r = plg.next()
                    for k in range(8):
                        S.op("pe", lambda e, lg=lg, hh=hh, k=k, tt=tt: e.matmul(
                            lg[:], lhsT=hh[:, k, tt * 128:(tt + 1) * 128], rhs=R["wr"][:, k, :], start=(k == 0), stop=(k == 7)),
                            reads=[hh_r, R["wr_r"]], writes=[lg_r], pe_acc=True)
                    (l, l_r), (ex, ex_r), (mk, mk_r), (gt, gt_r) = rt.next()
                    (m8, m8_r), (ng, ng_r), (sm, sm_r), (rc, rc_r) = rs.next()
                    S.op("dve", lambda e, l=l, lg=lg: e.tensor_tensor(out=l[:], in0=lg[:], in1=R["brb"][:], op=ALU.add),
                         reads=[lg_r, R["brb_r"]], writes=[l_r])
                    S.op("dve", lambda e, m8=m8, l=l: e.max(out=m8[:], in_=l[:]), reads=[l_r], writes=[m8_r])
                    S.op("dve", lambda e, ng=ng, m8=m8: e.tensor_scalar(out=ng[:, 0:1], in0=m8[:, 0:1], scalar1=-1.0, scalar2=None,
                                                                        op0=ALU.mult),
                         reads=[m8_r], writes=[ng_r])
                    S.op("act", lambda e, ex=ex, l=l, ng=ng: e.activation(out=ex[:], in_=l[:], func=AF.Exp, bias=ng[:, 0:1], scale=1.0),
                         reads=[l_r, ng_r], writes=[ex_r])
                    S.op("dve", lambda e, mk=mk, l=l, m8=m8: e.tensor_scalar(out=mk[:], in0=l[:], scalar1=m8[:, 3:4], scalar2=None,
                                                                            op0=ALU.is_ge),
                         reads=[l_r, m8_r], writes=[mk_r])
                    S.op("dve", lambda e, ex=ex, mk=mk: e.tensor_tensor(out=ex[:], in0=ex[:], in1=mk[:], op=ALU.mult),
                         reads=[ex_r, mk_r], writes=[ex_r])
                    S.op("dve", lambda e, sm=sm, ex=ex: e.reduce_sum(out=sm[:, 0:1], in_=ex[:], axis=AX.X), reads=[ex_r], writes=[sm_r])
                    S.op("dve", lambda e, rc=rc, sm=sm: e.reciprocal(out=rc[:, 0:1], in_=sm[:, 0:1]), reads=[sm_r], writes=[rc_r])
                    S.op("dve", lambda e, gt=gt, ex=ex, rc=rc: e.tensor_scalar(out=gt[:], in0=ex[:], scalar1=rc[:, 0:1], scalar2=None,
                                                                              op0=ALU.mult),
                         reads=[ex_r, rc_r], writes=[gt_r])
                    pg, pg_r = pgt.next()
                    S.op("pe", lambda e, pg=pg, gt=gt: e.transpose(out=pg[:], in_=gt[:], identity=self.identf[:]),
                         reads=[gt_r, self.identf_r], writes=[pg_r], pe_acc=True)
                    S.op("act", lambda e, pg=pg, s=s, tt=tt: e.activation(
                        out=R["gateT"][:, s + tt * 128:s + (tt + 1) * 128], in_=pg[:], func=AF.Copy),
                        reads=[pg_r], writes=[R["gateT_r"]])

    def ffn(self, i, with_ctx=True):
        nc, S, M = self.nc, self.S, self.M
        xT, xTr = self.xT, self.xTr
        w_router = self.inp("moe_w_router", [4, D, NE])
        b_router = self.inp("moe_b_router", [4, NE])
        w_gu = self.inp("moe_w_gu", [4, KMOE_NE, D, 2 * D])
        w_dn = self.inp("moe_w_down", [4, KMOE_NE, D, D])
        b_gu = self.inp("moe_b_gu_fm", [4, 128, NE, 16])
        b_dn = self.inp("moe_b_down", [4, NE, D])
        blks = [b for b in BLKS if with_ctx or b[0] != 0]
        with ExitStack() as ps:
            hT, hT_r = M.sb(ps, [128, 8, T], BF16, "hT")
            gateT, gateT_r = M.sb(ps, [NE, T], F32, "gateT")
            bgu, bgu_r = M.sb(ps, [128, NE, 16], F32, "bgu")
            bdn, bdn_r = M.sb(ps, [NE, D], F32, "bdn")
            S.dma("sp", bgu[:], b_gu[i], writes=[bgu_r], key=bgu_r)
            S.op("dve", lambda e: e.tensor_scalar(out=bgu[:, :, 8:16], in0=bgu[:, :, 8:16], scalar1=1.0, scalar2=None, op0=ALU.add),
                 reads=[bgu_r], writes=[bgu_r])
            S.dma("sp", bdn[:], b_dn[i], writes=[bdn_r], key=bdn_r)
            with ExitStack() as ps2:
                wr, wr_r = M.sb(ps2, [128, 8, NE], F32, "wr")
                brb, brb_r = M.sb(ps2, [128, NE], F32, "brb")
                S.dma("sp", wr[:], w_router[i].rearrange("(k p) n -> p k n", p=128), writes=[wr_r], key=wr_r)
                S.dma("sp", brb[:], b_router[i].partition_broadcast(128), writes=[brb_r], key=brb_r)
                self.adanorm(ps2, hT, hT_r, 3, with_ctx=with_ctx,
                             router=dict(wr=wr, wr_r=wr_r, brb=brb, brb_r=brb_r, gateT=gateT, gateT_r=gateT_r))
                S.barrier()
                S.release([wr_r, brb_r])
            wg = Ring([M.sb(ps, [128, 8, 2, 512], BF16, "wg") for _ in range(2)])
            wd = Ring([M.sb(ps, [128, 4, D], BF16, "wd") for _ in range(2)])
            pgl = Ring([M.ps(ps, [128, 2, 512], F32, "pgl") for _ in range(2)])
            pout = Ring([M.ps(ps, [128, 512], F32, "pout") for _ in range(2)])
            pgb = Ring([M.ps(ps, [128, 512], F32, "pgb") for _ in range(2)])
            gB = Ring([M.sb(ps, [128, 512], F32, "gB") for _ in range(2)])
            mg = Ring([M.sb(ps, [NE, 512], F32, "mg") for _ in range(2)])
            aT = Ring([M.sb(ps, [128, 4, 512], BF16, "aT") for _ in range(2)])
            tg = Ring([M.sb(ps, [128, 512], F32, "tg") for _ in range(2)])
            tsg = Ring([M.sb(ps, [128, 512], F32, "tsg") for _ in range(2)])
            tl = Ring([M.sb(ps, [128, 512], F32, "tl") for _ in range(2)])
            for (s, n) in blks:
                c = 1 if s == 0 else 0
                for oc in range(8):
                    po, po_r = pout.next()
                    S.op("pe", lambda e, po=po, oc=oc, s=s, n=n: e.matmul(
                        po[:, :n], lhsT=bdn[:, oc * 128:(oc + 1) * 128], rhs=gateT[:, s:s + n], start=True, stop=True),
                        reads=[bdn_r, gateT_r], writes=[po_r], pe_acc=True)
                    S.op("dve", lambda e, po=po, oc=oc, s=s, n=n, c=c: e.scalar_tensor_tensor(
                        out=xT[:, oc, s:s + n], in0=po[:, :n], scalar=self.mv[:, c, 5, oc:oc + 1], in1=xT[:, oc, s:s + n],
                        op0=ALU.mult, op1=ALU.add),
                        reads=[po_r, self.mv_r, xTr], writes=[xTr])
            pieces = [(ex, half) for ex in range(KMOE_NE) for half in range(2)]
            loaded = {}

            def load_piece(idx):
                ex, half = pieces[idx]
                wgt, wg_r = wg.next()
                wdt, wd_r = wd.next()
                for gl in range(2):
                    S.dma("pool", wgt[:, :, gl, :],
                          w_gu[i, ex, :, gl * D + half * 512: gl * D + half * 512 + 512].rearrange("(k p) n -> p k n", p=128),
                          writes=[wg_r], key=wg_r)
                S.dma("pool", wdt[:], w_dn[i, ex, half * 512:(half + 1) * 512, :].rearrange("(j p) n -> p j n", p=128),
                      writes=[wd_r], key=wd_r)
                loaded[idx] = (wgt, wg_r, wdt, wd_r)

            load_piece(0)
            for idx in range(len(pieces)):
                if True:
                    ex, half = pieces[idx]
                    if idx + 1 < len(pieces):
                        load_piece(idx + 1)
                    wgt, wg_r, wdt, wd_r = loaded.pop(idx)
                    for (s, n) in blks:
                        c = 1 if s == 0 else 0
                        pb, pb_r = pgb.next()
                        mg_, mg_r = mg.next()
                        S.op("act", lambda e, mg_=mg_, ex=ex, s=s, n=n: e.activation(
                            out=mg_[:, :n], in_=gateT[:, s:s + n], func=AF.Copy, scale=self.identf[0:NE, ex:ex + 1]),
                            reads=[gateT_r, self.identf_r], writes=[mg_r])
                        S.op("pe", lambda e, pb=pb, mg_=mg_, n=n: e.matmul(
                            pb[:, :n], lhsT=self.onesf[0:NE, :], rhs=mg_[:, :n], start=True, stop=True),
                            reads=[mg_r, self.onesf_r], writes=[pb_r], pe_acc=True)
                        g_, g_r = gB.next()
                        S.op("act", lambda e, g_=g_, pb=pb, n=n: e.activation(out=g_[:, :n], in_=pb[:, :n], func=AF.Copy),
                             reads=[pb_r], writes=[g_r])
                        a_, a_r = aT.next()
                        for jj in range(4):
                            j = half * 4 + jj
                            pg, pg_r = pgl.next()
                            for gl in range(2):
                                for k in range(8):
                                    S.op("pe", lambda e, pg=pg, gl=gl, k=k, jj=jj, s=s, n=n, wgt=wgt: e.matmul(
                                        pg[:, gl, :n], lhsT=wgt[:, k, gl, jj * 128:(jj + 1) * 128], rhs=hT[:, k, s:s + n],
                                        start=(k == 0), stop=(k == 7)),
                                        reads=[wg_r, hT_r], writes=[pg_r], pe_acc=True)
                            t1, t1_r = tg.next()
                            S.op("dve", lambda e, t1=t1, pg=pg, n=n, ex=ex, j=j: e.tensor_scalar(
                                out=t1[:, :n], in0=pg[:, 0, :n], scalar1=bgu[:, ex, j:j + 1], scalar2=7.0, op0=ALU.add, op1=ALU.min),
                                reads=[pg_r, bgu_r], writes=[t1_r])
                            t2, t2_r = tsg.next()
                            S.op("act", lambda e, t2=t2, t1=t1, n=n: e.activation(out=t2[:, :n], in_=t1[:, :n], func=AF.Sigmoid, scale=1.702),
                                 reads=[t1_r], writes=[t2_r])
                            t3, t3_r = tl.next()
                            S.op("dve", lambda e, t3=t3, pg=pg, n=n, ex=ex, j=j: e.tensor_scalar(
                                out=t3[:, :n], in0=pg[:, 1, :n], scalar1=bgu[:, ex, 8 + j:9 + j], scalar2=-6.0, op0=ALU.add, op1=ALU.max),
                                reads=[pg_r, bgu_r], writes=[t3_r])
                            S.op("pool", lambda e, t1=t1, t2=t2, n=n: e.tensor_tensor(out=t1[:, :n], in0=t1[:, :n], in1=t2[:, :n], op=ALU.mult),
                                 reads=[t1_r, t2_r], writes=[t1_r])
                            S.op("dve", lambda e, t1=t1, t3=t3, n=n: e.scalar_tensor_tensor(
                                out=t3[:, :n], in0=t3[:, :n], scalar=8.0, in1=t1[:, :n], op0=ALU.min, op1=ALU.mult),
                                reads=[t1_r, t3_r], writes=[t3_r])
                            S.op("pool", lambda e, a_=a_, t3=t3, g_=g_, jj=jj, n=n: e.tensor_tensor(out=a_[:, jj, :n], in0=t3[:, :n], in1=g_[:, :n], op=ALU.mult),
                                 reads=[t3_r, g_r], writes=[a_r])
                        for oc in range(8):
                            po, po_r = pout.next()
                            for jj in range(4):
                                S.op("pe", lambda e, po=po, oc=oc, jj=jj, n=n, wdt=wdt, a_=a_: e.matmul(
                                    po[:, :n], lhsT=wdt[:, jj, oc * 128:(oc + 1) * 128], rhs=a_[:, jj, :n],
                                    start=(jj == 0), stop=(jj == 3)),
                                    reads=[wd_r, a_r], writes=[po_r], pe_acc=True)
                            S.op("dve", lambda e, po=po, oc=oc, s=s, n=n, c=c: e.scalar_tensor_tensor(
                                out=xT[:, oc, s:s + n], in0=po[:, :n], scalar=self.mv[:, c, 5, oc:oc + 1], in1=xT[:, oc, s:s + n],
                                op0=ALU.mult, op1=ALU.add),
                                reads=[po_r, self.mv_r, xTr], writes=[xTr])
            S.barrier()
            S.release([bgu_r, bdn_r] + [r for _, r in wg.items] + [r for _, r in wd.items])

    def resid(self, po, po_r, n, oc, s, gb=None, tring=None):
        S = self.S
        c = 1 if s < NCTX else 0
        xT, xTr = self.xT, self.xTr
        if gb is None:
            S.op("dve", lambda e: e.scalar_tensor_tensor(
                out=xT[:, oc, s:s + n], in0=po[:, :n], scalar=self.mv[:, c, 2, oc:oc + 1], in1=xT[:, oc, s:s + n],
                op0=ALU.mult, op1=ALU.add), reads=[po_r, self.mv_r, xTr], writes=[xTr])
        else:
            gbt, gbt_r = gb
            t_, t_r = tring.next()
            S.op("act", lambda e: e.activation(out=t_[:, :n], in_=po[:, :n], func=AF.Identity,
                                               scale=self.mv[:, c, 2, oc:oc + 1], bias=gbt[:, c, oc:oc + 1]),
                 reads=[po_r, self.mv_r, gbt_r], writes=[t_r])
            S.op("pool", lambda e: e.tensor_tensor(out=xT[:, oc, s:s + n], in0=xT[:, oc, s:s + n], in1=t_[:, :n], op=ALU.add),
                 reads=[t_r, xTr], writes=[xTr])

    def gate_bias(self, ps, bvec_ap):
        S, M = self.S, self.M
        gb, gb_r = M.sb(ps, [128, 2, 8], F32, "gb")
        for c in range(2):
            S.op("dve", lambda e, c=c: e.tensor_tensor(out=gb[:, c, :], in0=self.mv[:, c, 2, :], in1=bvec_ap, op=ALU.mult),
                 reads=[self.mv_r] + self._vec_deps, writes=[gb_r])
        return gb, gb_r

    def mixer0(self, i):
        nc, S, M = self.nc, self.S, self.M
        w1 = self.inp("conv_w_pw1", [1, D, 2 * D])
        w2 = self.inp("conv_w_pw2", [1, D, D])
        cvd = self.inp("conv_vec", [128, 296])
        UW = 2364

        def ucol(s):
            return 15 if s == 0 else s + 45
        with ExitStack() as pA:
            cv, cv_r = M.sb(pA, [128, 296], F32, "cv")
            S.dma("sp", cv[:], cvd, writes=[cv_r], key=cv_r)
            self._vec_deps = [cv_r]
            V, V_r = M.sb(pA, [128, 8, T], BF16, "V")
            with ExitStack() as pB:
                U, U_r = M.sb(pB, [128, 8, UW], BF16, "U")
                S.op("pool", lambda e: e.memset(U[:], 0.0), writes=[U_r])
                with ExitStack() as pC:
                    hT, hT_r = M.sb(pC, [128, 8, T], BF16, "hT")
                    with ExitStack() as pD:
                        self.adanorm(pD, hT, hT_r, 0)
                        S.barrier()
                    wp = Ring([M.sb(pC, [128, 8, 2, 128], BF16, "wp") for _ in range(2)])
                    pa = Ring([M.ps(pC, [128, 2, 512], F32, "pa") for _ in range(2)])
                    sg = Ring([M.sb(pC, [128, 512], F32, "sg") for _ in range(2)])
                    for oc in range(8):
                        wt, wt_r = wp.next()
                        for gl in range(2):
                            S.dma("pool", wt[:, :, gl, :],
                                  w1[0, :, gl * D + oc * 128: gl * D + (oc + 1) * 128].rearrange("(k p) n -> p k n", p=128),
                                  writes=[wt_r], key=wt_r)
                        for (s, n) in BLKS:
                            p_, p_r = pa.next()
                            for gl in range(2):
                                for k in range(8):
                                    S.op("pe", lambda e, p_=p_, gl=gl, k=k, s=s, n=n, wt=wt: e.matmul(
                                        p_[:, gl, :n], lhsT=wt[:, k, gl, :], rhs=hT[:, k, s:s + n], start=(k == 0), stop=(k == 7)),
                                        reads=[wt_r, hT_r], writes=[p_r], pe_acc=True)
                            g_, g_r = sg.next()
                            S.op("act", lambda e, g_=g_, p_=p_, n=n, oc=oc: e.activation(
                                out=g_[:, :n], in_=p_[:, 1, :n], func=AF.Sigmoid, bias=cv[:, 8 + oc:9 + oc], scale=1.0),
                                reads=[p_r, cv_r], writes=[g_r])
                            S.op("dve", lambda e, g_=g_, p_=p_, n=n, oc=oc, s=s: e.scalar_tensor_tensor(
                                out=U[:, oc, ucol(s):ucol(s) + n], in0=p_[:, 0, :n], scalar=cv[:, oc:oc + 1], in1=g_[:, :n],
                                op0=ALU.add, op1=ALU.mult),
                                reads=[p_r, cv_r, g_r], writes=[U_r])
                    S.barrier()
                    S.release([r for _, r in wp.items])
                dgr = Ring([M.sb(pB, [128, 31, 128], BF16, "dg") for _ in range(2)])
                pc = Ring([M.ps(pB, [128, 512], F32, "pc") for _ in range(2)])
                for oc in range(8):
                    dg, dg_r = dgr.next()
                    for w in range(31):
                        S.op("dve", lambda e, dg=dg, w=w, oc=oc: e.tensor_scalar(
                            out=dg[:, w, :], in0=self.identb[:], scalar1=cv[:, 16 + oc * 31 + w:17 + oc * 31 + w], scalar2=None,
                            op0=ALU.mult), reads=[self.identb_r, cv_r], writes=[dg_r])
                    for (s, n) in BLKS:
                        o0 = 0 if s == 0 else s + 30
                        p_, p_r = pc.next()
                        for w in range(31):
                            S.op("pe", lambda e, p_=p_, w=w, oc=oc, o0=o0, n=n, dg=dg: e.matmul(
                                p_[:, :n], lhsT=dg[:, w, :], rhs=U[:, oc, o0 + w:o0 + w + n], start=(w == 0), stop=(w == 30)),
                                reads=[dg_r, U_r], writes=[p_r], pe_acc=True)
                        S.op("act", lambda e, p_=p_, oc=oc, s=s, n=n: e.activation(
                            out=V[:, oc, s:s + n], in_=p_[:, :n], func=AF.Identity, bias=cv[:, 264 + oc:265 + oc], scale=1.0),
                            reads=[p_r, cv_r], writes=[V_r])
                S.barrier()
            h2, h2_r = M.sb(pA, [128, 8, T], BF16, "h2")
            w2t, w2_r = M.sb(pA, [128, 8, D], BF16, "w2t")
            S.dma("pool", w2t[:], w2[0].rearrange("(k p) n -> p k n", p=128), writes=[w2_r], key=w2_r)
            gb = self.gate_bias(pA, cv[:, 288:296])
            ps1 = Ring([M.ps(pA, [128, 512], F32, "ps1") for _ in range(1)])
            ps2 = Ring([M.ps(pA, [128, 512], F32, "ps2") for _ in range(1)])
            po = Ring([M.ps(pA, [128, 512], F32, "po") for _ in range(2)])
            vsq = Ring([M.sb(pA, [128, 512], BF16, "vsq") for _ in range(2)])
            mu = Ring([M.sb(pA, [128, 512], F32, "mu") for _ in range(1)])
            rs = Ring([M.sb(pA, [128, 512], F32, "rs") for _ in range(1)])
            tt = Ring([M.sb(pA, [128, 512], F32, "tt") for _ in range(2)])
            tr = Ring([M.sb(pA, [128, 512], F32, "tr") for _ in range(2)])
            for (s, n) in BLKS:
                a1, a1_r = ps1.next()
                a2, a2_r = ps2.next()
                for k in range(8):
                    q, q_r = vsq.next()
                    S.op("pool", lambda e, q=q, k=k, s=s, n=n: e.tensor_tensor(out=q[:, :n], in0=V[:, k, s:s + n], in1=V[:, k, s:s + n], op=ALU.mult),
                         reads=[V_r], writes=[q_r])
                    S.op("pe", lambda e, a1=a1, k=k, s=s, n=n: e.matmul(a1[:, :n], lhsT=self.onesb[:], rhs=V[:, k, s:s + n],
                                                                         start=(k == 0), stop=(k == 7)),
                         reads=[V_r, self.onesb_r], writes=[a1_r], pe_acc=True)
                    S.op("pe", lambda e, a2=a2, q=q, k=k, n=n: e.matmul(a2[:, :n], lhsT=self.onesb[:], rhs=q[:, :n],
                                                                        start=(k == 0), stop=(k == 7)),
                         reads=[q_r, self.onesb_r], writes=[a2_r], pe_acc=True)
                m_, m_r = mu.next()
                r_, r_r = rs.next()
                S.op("act", lambda e, m_=m_, a1=a1, n=n: e.activation(out=m_[:, :n], in_=a1[:, :n], func=AF.Copy, scale=1.0 / D),
                     reads=[a1_r], writes=[m_r])
                S.op("dve", lambda e, r_=r_, m_=m_, n=n: e.tensor_tensor(out=r_[:, :n], in0=m_[:, :n], in1=m_[:, :n], op=ALU.mult),
                     reads=[m_r], writes=[r_r])
                S.op("dve", lambda e, r_=r_, a2=a2, n=n: e.scalar_tensor_tensor(out=r_[:, :n], in0=a2[:, :n], scalar=1.0 / D, in1=r_[:, :n],
                                                                               op0=ALU.mult, op1=ALU.subtract),
                     reads=[a2_r, r_r], writes=[r_r])
                S.op("act", lambda e, r_=r_, n=n: e.activation(out=r_[:, :n], in_=r_[:, :n], func=AF.Sqrt, bias=self.epsb[:, 0:1], scale=1.0),
                     reads=[r_r, self.epsb_r], writes=[r_r])
                S.op("dve", lambda e, r_=r_, n=n: e.reciprocal(out=r_[:, :n], in_=r_[:, :n]), reads=[r_r], writes=[r_r])
                for k in range(8):
                    t_, t_r = tt.next()
                    S.op("dve", lambda e, t_=t_, k=k, s=s, n=n, m_=m_: e.tensor_tensor(out=t_[:, :n], in0=V[:, k, s:s + n], in1=m_[:, :n], op=ALU.subtract),
                         reads=[V_r, m_r], writes=[t_r])
                    S.op("pool", lambda e, t_=t_, r_=r_, n=n: e.tensor_tensor(out=t_[:, :n], in0=t_[:, :n], in1=r_[:, :n], op=ALU.mult),
                         reads=[t_r, r_r], writes=[t_r])
                    S.op("act", lambda e, t_=t_, k=k, s=s, n=n: e.activation(
                        out=h2[:, k, s:s + n], in_=t_[:, :n], func=AF.Silu, scale=cv[:, 272 + k:273 + k], bias=cv[:, 280 + k:281 + k]),
                        reads=[t_r, cv_r], writes=[h2_r])
            for (s, n) in BLKS:
                for oc in range(8):
                    p_, p_r = po.next()
                    for k in range(8):
                        S.op("pe", lambda e, p_=p_, k=k, oc=oc, s=s, n=n: e.matmul(
                            p_[:, :n], lhsT=w2t[:, k, oc * 128:(oc + 1) * 128], rhs=h2[:, k, s:s + n], start=(k == 0), stop=(k == 7)),
                            reads=[w2_r, h2_r], writes=[p_r], pe_acc=True)
                    self.resid(p_, p_r, n, oc, s, gb=gb, tring=tr)
            S.barrier()
            S.release([cv_r, w2_r])

    def load_w(self, ps, src2d, kch, n, name, npart=128):
        S, M = self.S, self.M
        t, t_r = M.sb(ps, [npart, kch, n], BF16, name)
        S.dma("pool", t[:], src2d.rearrange("(k p) n -> p k n", p=npart), writes=[t_r], key=t_r)
        return t, t_r

    def swap_halves(self, ps, w, w_r, kch, nh, name):
        S, M = self.S, self.M
        ws, ws_r = M.sb(ps, [128, kch, nh * 64], BF16, name)
        for h in range(nh):
            S.op("pool", lambda e, h=h: e.tensor_copy(out=ws[:, :, h * 64:h * 64 + 32], in_=w[:, :, h * 64 + 32:h * 64 + 64]),
                 reads=[w_r], writes=[ws_r])
            S.op("pool", lambda e, h=h: e.tensor_copy(out=ws[:, :, h * 64 + 32:h * 64 + 64], in_=w[:, :, h * 64:h * 64 + 32]),
                 reads=[w_r], writes=[ws_r])
        return ws, ws_r

    def proj_rope(self, pj, pj_r, w, w_r, ws, ws_r, c0, b, bs, b_r, hT, hT_r, out, out_r, blks, C, Sn, tab_r, t1r, t2r):
        S = self.S
        for (s, n) in blks:
            for a, (ww, ww_r) in enumerate(((w, w_r), (ws, ws_r))):
                for k in range(8):
                    S.op("pe", lambda e, a=a, ww=ww, k=k, s=s, n=n: e.matmul(
                        pj[0:64, a, :n], lhsT=ww[:, k, c0:c0 + 64], rhs=hT[:, k, s:s + n], start=(k == 0), stop=(k == 7)),
                        reads=[ww_r, hT_r], writes=[pj_r], pe_acc=True)
            t1, t1_r = t1r.next()
            t2, t2_r = t2r.next()
            S.op("dve", lambda e, t1=t1, s=s, n=n: e.scalar_tensor_tensor(
                out=t1[0:64, :n], in0=pj[0:64, 0, :n], scalar=b, in1=C[:, s:s + n], op0=ALU.add, op1=ALU.mult),
                reads=[pj_r, b_r, tab_r], writes=[t1_r])
            S.op("dve", lambda e, t2=t2, s=s, n=n: e.scalar_tensor_tensor(
                out=t2[0:64, :n], in0=pj[0:64, 1, :n], scalar=bs, in1=Sn[:, s:s + n], op0=ALU.add, op1=ALU.mult),
                reads=[pj_r, b_r, tab_r], writes=[t2_r])
            S.op("pool", lambda e, t1=t1, t2=t2, s=s, n=n: e.tensor_tensor(out=out[0:64, s:s + n], in0=t1[0:64, :n], in1=t2[0:64, :n], op=ALU.add),
                 reads=[t1_r, t2_r], writes=[out_r])

    def load_tables(self, ps):
        S, M = self.S, self.M
        Cd = self.inp("rope_c", [64, T])
        Sd = self.inp("rope_s", [64, T])
        C, C_r = M.sb(ps, [64, T], F32, "ropeC")
        Sn, _ = M.sb(ps, [64, T], F32, "ropeS")
        S.dma("sp", C[:], Cd, writes=[C_r], key=C_r)
        S.dma("sp", Sn[:], Sd, writes=[C_r], key=C_r)
        return C, Sn, C_r

    def mixer2(self, i):
        nc, S, M = self.nc, self.S, self.M
        wqkv = self.inp("swa_w_qkv", [1, D, 1536])
        bqkv = self.inp("swa_b_qkv", [1, 1536])
        wo = self.inp("swa_w_o", [1, D, D])
        bh = self.inp("swa_bh", [64, 44])
        sinks = self.inp("swa_sinks", [1, 16])
        bo = self.inp("swa_bo_fm", [128, 8])
        NEG = -30000.0
        with ExitStack() as pA:
            hT, hT_r = M.sb(pA, [128, 8, T], BF16, "hT")
            with ExitStack() as pD:
                self.adanorm(pD, hT, hT_r, 0)
                S.barrier()
            C, Sn, tab_r = self.load_tables(pA)
            bht, bht_r = M.sb(pA, [64, 44], F32, "bht")
            S.dma("sp", bht[:], bh, writes=[bht_r], key=bht_r)
            skb, skb_r = M.sb(pA, [128, 16], F32, "skb")
            S.dma("sp", skb[:], sinks[0].partition_broadcast(128), writes=[skb_r], key=skb_r)
            bot, bot_r = M.sb(pA, [128, 8], F32, "bot")
            S.dma("sp", bot[:], bo, writes=[bot_r], key=bot_r)
            self._vec_deps = [bot_r]
            gb = self.gate_bias(pA, bot[:])
            mW, mW_r = M.sb(pA, [128, 384], F32, "mW")
            S.op("pool", lambda e: e.memset(mW[:], 0.0), writes=[mW_r])
            S.op("pool", lambda e: e.affine_select(out=mW[:, 0:128], in_=mW[:, 0:128], pattern=[[1, 128]], compare_op=ALU.is_ge,
                                                   fill=NEG, base=0, channel_multiplier=-1), reads=[mW_r], writes=[mW_r])
            S.op("pool", lambda e: e.affine_select(out=mW[:, 256:384], in_=mW[:, 256:384], pattern=[[-1, 128]], compare_op=ALU.is_ge,
                                                   fill=NEG, base=0, channel_multiplier=1), reads=[mW_r], writes=[mW_r])
            kT, kT_r = M.sb(pA, [64, T], BF16, "kT")
            vs, vs_r = M.sb(pA, [128, NT, 64], BF16, "vs")
            qTr = Ring([M.sb(pA, [64, T], BF16, "qT") for _ in range(2)])
            oTg, oTg_r = M.sb(pA, [64, 4, T], BF16, "oTg")
            bvb, bvb_r = M.sb(pA, [128, 64], F32, "bvb")
            t1r = Ring([M.sb(pA, [64, 512], F32, "rp1") for _ in range(2)])
            t2r = Ring([M.sb(pA, [64, 512], F32, "rp2") for _ in range(2)])
            tr = Ring([M.sb(pA, [128, 512], F32, "tr") for _ in range(2)])
            swr = Ring([M.sb(pA, [128, 384], F32, "sw") for _ in range(2)])
            pwr = Ring([M.sb(pA, [128, 640], BF16, "pw") for _ in range(2)])
            pTsr = Ring([M.sb(pA, [128, 5, 128], BF16, "pTs") for _ in range(2)])
            osr = Ring([M.sb(pA, [128, 64], F32, "osb") for _ in range(2)])
            smr = Ring([M.sb(pA, [128, 8], F32, "sm") for _ in range(3)])
            pj, pj_r = M.ps(pA, [128, 2, 512], F32, "pj")
            pscr = Ring([M.ps(pA, [128, 2, 512], F32, "psc") for _ in range(2)])
            pT, pT_r = M.ps(pA, [128, 5, 128], BF16, "pT")
            pso, pso_r = M.ps(pA, [128, 512], F32, "pso")
            for g in range(4):
                if (self.dbg == 7 and g == 1) or (self.dbg == 17 and g == 2) or (self.dbg == 18 and g == 3):
                    return
                with ExitStack() as pG:
                    wq, wq_r = self.load_w(pG, wqkv[0, :, g * 256:(g + 1) * 256], 8, 256, "wq")
                    wk, wk_r = self.load_w(pG, wqkv[0, :, 1024 + g * 64:1024 + (g + 1) * 64], 8, 64, "wk")
                    wv, wv_r = self.load_w(pG, wqkv[0, :, 1280 + g * 64:1280 + (g + 1) * 64], 8, 64, "wv")
                    wqs, wqs_r = self.swap_halves(pG, wq, wq_r, 8, 4, "wqs")
                    wks, wks_r = self.swap_halves(pG, wk, wk_r, 8, 1, "wks")
                    wog, wog_r = self.load_w(pG, wo[0, g * 256:(g + 1) * 256, :], 4, D, "wog", npart=64)
                    S.dma("sp", bvb[:], bqkv[0, 1280 + g * 64:1280 + (g + 1) * 64].partition_broadcast(128),
                          reads=[], writes=[bvb_r], key=bvb_r)
                    self.proj_rope(pj, pj_r, wk, wk_r, wks, wks_r, 0, bht[:, 16 + g:17 + g], bht[:, 24 + 16 + g:25 + 16 + g], bht_r,
                                   hT, hT_r, kT, kT_r, BLKS, C, Sn, tab_r, t1r, t2r)
                    for t in range(NT):
                        for k in range(8):
                            S.op("pe", lambda e, t=t, k=k: e.matmul(pso[:, 0:64], lhsT=hT[:, k, t * 128:(t + 1) * 128], rhs=wv[:, k, :],
                                                                    start=(k == 0), stop=(k == 7)),
                                 reads=[hT_r, wv_r], writes=[pso_r], pe_acc=True)
                        S.op("dve", lambda e, t=t: e.tensor_tensor(out=vs[:, t, :], in0=pso[:, 0:64], in1=bvb[:], op=ALU.add),
                             reads=[pso_r, bvb_r], writes=[vs_r])
                    if self.dbg == 1 or (self.dbg == 11 and g == 1):
                        return
                    for hh in range(4):
                        h = g * 4 + hh
                        qT, qT_r = qTr.next()
                        self.proj_rope(pj, pj_r, wq, wq_r, wqs, wqs_r, hh * 64, bht[:, h:h + 1], bht[:, 24 + h:25 + h], bht_r,
                                       hT, hT_r, qT, qT_r, BLKS, C, Sn, tab_r, t1r, t2r)
                        if self.dbg == 2 or (self.dbg == 12 and g == 1):
                            return
                        for qt in range(NT):
                            if self.dbg == 3 and qt == 1:
                                return
                            if self.dbg == 4 and qt == 3:
                                return
                            if self.dbg == 5 and hh == 1:
                                return
                            psc, psc_r = pscr.next()
                            sm, sm_r = smr.next()
                            pw, pw_r = pwr.next()
                            lat = qt >= 2
                            S.op("pe", lambda e, psc=psc, qT=qT, qt=qt: e.matmul(
                                psc[:, 1, 0:256], lhsT=qT[:, qt * 128:(qt + 1) * 128], rhs=kT[:, 0:256], start=True, stop=True),
                                reads=[qT_r, kT_r], writes=[psc_r], pe_acc=True)
                            if lat:
                                qb = qt - 2
                                lo = max(0, qb - 1)
                                hi = min(15, qb + 1)
                                c0 = (lo - (qb - 1)) * 128
                                c1 = c0 + (hi - lo + 1) * 128
                                ktiles = list(range(lo + 2, hi + 3))
                                S.op("pe", lambda e, psc=psc, qT=qT, qt=qt, lo=lo, hi=hi, c0=c0, c1=c1: e.matmul(
                                    psc[:, 0, c0:c1], lhsT=qT[:, qt * 128:(qt + 1) * 128], rhs=kT[:, 256 + lo * 128:256 + (hi + 1) * 128],
                                    start=True, stop=True), reads=[qT_r, kT_r], writes=[psc_r], pe_acc=True)
                                sw, sw_r = swr.next()
                                S.op("dve", lambda e, sw=sw, psc=psc, c0=c0, c1=c1: e.tensor_tensor(
                                    out=sw[:, c0:c1], in0=psc[:, 0, c0:c1], in1=mW[:, c0:c1], op=ALU.add),
                                    reads=[psc_r, mW_r], writes=[sw_r])
                                S.op("dve", lambda e, sm=sm, sw=sw, c0=c0, c1=c1: e.reduce_max(out=sm[:, 0:1], in_=sw[:, c0:c1], axis=AX.X),
                                     reads=[sw_r], writes=[sm_r])
                            else:
                                ktiles = []
                            S.op("dve", lambda e, sm=sm, psc=psc: e.reduce_max(out=sm[:, 1:2], in_=psc[:, 1, 0:256], axis=AX.X),
                                 reads=[psc_r], writes=[sm_r])
                            if lat:
                                S.op("dve", lambda e, sm=sm: e.tensor_tensor(out=sm[:, 1:2], in0=sm[:, 0:1], in1=sm[:, 1:2], op=ALU.max),
                                     reads=[sm_r], writes=[sm_r])
                            S.op("dve", lambda e, sm=sm, h=h: e.scalar_tensor_tensor(out=sm[:, 2:3], in0=sm[:, 1:2], scalar=0.125, in1=skb[:, h:h + 1],
                                                                                   op0=ALU.mult, op1=ALU.max),
                                 reads=[sm_r, skb_r], writes=[sm_r])
                            S.op("dve", lambda e, sm=sm: e.tensor_scalar(out=sm[:, 3:4], in0=sm[:, 2:3], scalar1=-1.0, scalar2=None, op0=ALU.mult),
                                 reads=[sm_r], writes=[sm_r])
                            S.op("pool", lambda e, sm=sm: e.memset(sm[:, 4:7], 0.0), reads=[sm_r], writes=[sm_r])
                            if lat:
                                S.op("act", lambda e, pw=pw, sw=sw, sm=sm, c0=c0, c1=c1: e.activation(
                                    out=pw[:, c0:c1], in_=sw[:, c0:c1], func=AF.Exp, scale=0.125, bias=sm[:, 3:4], accum_out=sm[:, 4:5]),
                                    reads=[sw_r, sm_r], writes=[pw_r, sm_r])
                            S.op("act", lambda e, pw=pw, psc=psc, sm=sm: e.activation(
                                out=pw[:, 384:640], in_=psc[:, 1, 0:256], func=AF.Exp, scale=0.125, bias=sm[:, 3:4], accum_out=sm[:, 5:6]),
                                reads=[psc_r, sm_r], writes=[pw_r, sm_r])
                            S.op("act", lambda e, sm=sm, h=h: e.activation(out=sm[:, 6:7], in_=skb[:, h:h + 1], func=AF.Exp, scale=1.0, bias=sm[:, 3:4]),
                                 reads=[skb_r, sm_r], writes=[sm_r])
                            S.op("dve", lambda e, sm=sm: e.tensor_tensor(out=sm[:, 4:5], in0=sm[:, 4:5], in1=sm[:, 5:6], op=ALU.add),
                                 reads=[sm_r], writes=[sm_r])
                            S.op("dve", lambda e, sm=sm: e.tensor_tensor(out=sm[:, 4:5], in0=sm[:, 4:5], in1=sm[:, 6:7], op=ALU.add),
                                 reads=[sm_r], writes=[sm_r])
                            S.op("dve", lambda e, sm=sm: e.reciprocal(out=sm[:, 7:8], in_=sm[:, 4:5]), reads=[sm_r], writes=[sm_r])
                            srcs = []
                            if lat:
                                for j, kt_ in enumerate(ktiles):
                                    srcs.append((c0 + j * 128, kt_))
                            srcs += [(384, 0), (512, 1)]
                            for j, (col, kt_) in enumerate(srcs):
                                S.op("pe", lambda e, j=j, col=col, pw=pw: e.transpose(out=pT[:, j, :], in_=pw[:, col:col + 128], identity=self.identb[:]),
                                     reads=[pw_r, self.identb_r], writes=[pT_r], pe_acc=True)
                            pTs, pTs_r = pTsr.next()
                            nj = len(srcs)
                            S.op("act", lambda e, pTs=pTs, nj=nj: e.activation(out=pTs[:, 0:nj, :], in_=pT[:, 0:nj, :], func=AF.Copy),
                                 reads=[pT_r], writes=[pTs_r])
                            for j, (col, kt_) in enumerate(srcs):
                                S.op("pe", lambda e, j=j, kt_=kt_, pTs=pTs, nj=nj: e.matmul(
                                    pso[:, 64:128], lhsT=pTs[:, j, :], rhs=vs[:, kt_, :], start=(j == 0), stop=(j == nj - 1)),
                                    reads=[pTs_r, vs_r], writes=[pso_r], pe_acc=True)
                            osb, osb_r = osr.next()
                            S.op("dve", lambda e, osb=osb, sm=sm: e.tensor_scalar(out=osb[:], in0=pso[:, 64:128], scalar1=sm[:, 7:8], scalar2=None, op0=ALU.mult),
                                 reads=[pso_r, sm_r], writes=[osb_r])
                            S.op("pe", lambda e, osb=osb: e.transpose(out=pso[0:64, 128:256], in_=osb[:], identity=self.identf[:]),
                                 reads=[osb_r, self.identf_r], writes=[pso_r], pe_acc=True)
                            S.op("act", lambda e, hh=hh, qt=qt: e.activation(out=oTg[:, hh, qt * 128:(qt + 1) * 128], in_=pso[0:64, 128:256], func=AF.Copy),
                                 reads=[pso_r], writes=[oTg_r])
                    if self.dbg == 6 or (self.dbg == 16 and g == 1):
                        return
                    for (s, n) in BLKS:
                        for oc in range(8):
                            for hh in range(4):
                                S.op("pe", lambda e, hh=hh, oc=oc, s=s, n=n: e.matmul(
                                    pj[:, 0, :n], lhsT=wog[:, hh, oc * 128:(oc + 1) * 128], rhs=oTg[:, hh, s:s + n], start=(hh == 0), stop=(hh == 3)),
                                    reads=[wog_r, oTg_r], writes=[pj_r], pe_acc=True)
                            if g == 0:
                                self.resid(pj[:, 0, :], pj_r, n, oc, s, gb=gb, tring=tr)
                            else:
                                self.resid(pj[:, 0, :], pj_r, n, oc, s)
                    S.barrier()
                    S.release([wq_r, wk_r, wv_r, wog_r])

    def mixer3(self, i):
        nc, S, M = self.nc, self.S, self.M
        wqkv = self.inp("diff_w_qkv", [1, D, 3 * D])
        wo = self.inp("diff_w_o", [1, D, D])
        lvec = self.inp("diff_lam", [4, 64])
        sg = self.inp("diff_subln_g", [128, 1])
        lambda_init = 0.8 - 0.6 * math.exp(-0.3 * i)
        LBLK = BLKS[1:]
        with ExitStack() as pA:
            hT, hT_r = M.sb(pA, [128, 8, T], BF16, "hT")
            with ExitStack() as pD:
                self.adanorm(pD, hT, hT_r, 0)
                S.barrier()
            C, Sn, tab_r = self.load_tables(pA)
            zb, zb_r = M.sb(pA, [64, 1], F32, "zb")
            S.op("pool", lambda e: e.memset(zb[:], 0.0), writes=[zb_r])
            sgt, sgt_r = M.sb(pA, [128, 1], F32, "sgt")
            S.dma("sp", sgt[:], sg, writes=[sgt_r], key=sgt_r)
            lv, lv_r = M.sb(pA, [128, 4, 64], F32, "lv")
            for a in range(4):
                S.dma("sp", lv[:, a, :], lvec[a].partition_broadcast(128), writes=[lv_r], key=lv_r)
            lam, lam_r = M.sb(pA, [128, 4], F32, "lam")
            lp, lp_r = M.sb(pA, [128, 2, 64], F32, "lp")
            for a in range(2):
                S.op("dve", lambda e, a=a: e.tensor_tensor(out=lp[:, a, :], in0=lv[:, 2 * a, :], in1=lv[:, 2 * a + 1, :], op=ALU.mult),
                     reads=[lv_r], writes=[lp_r])
                S.op("dve", lambda e, a=a: e.reduce_sum(out=lam[:, a:a + 1], in_=lp[:, a, :], axis=AX.X), reads=[lp_r], writes=[lam_r])
            S.op("act", lambda e: e.activation(out=lam[:, 0:2], in_=lam[:, 0:2], func=AF.Exp), reads=[lam_r], writes=[lam_r])
            S.op("dve", lambda e: e.tensor_tensor(out=lam[:, 2:3], in0=lam[:, 0:1], in1=lam[:, 1:2], op=ALU.subtract), reads=[lam_r], writes=[lam_r])
            S.op("dve", lambda e: e.tensor_scalar(out=lam[:, 3:4], in0=lam[:, 2:3], scalar1=float(lambda_init), scalar2=None, op0=ALU.add),
                 reads=[lam_r], writes=[lam_r])
            kTs = [M.sb(pA, [64, T], BF16, "kT%d" % t) for t in range(2)]
            qTs = [M.sb(pA, [64, T], BF16, "qT%d" % t) for t in range(2)]
            vs, vs_r = M.sb(pA, [128, NT, 128], BF16, "vs")
            oT, oT_r = M.sb(pA, [128, NLAT], BF16, "oT")
            pbr = Ring([M.sb(pA, [128, T], BF16, "pb") for _ in range(2)])
            pTs, pTs_r = M.sb(pA, [128, NT, 128], BF16, "pTs")
            t1r = Ring([M.sb(pA, [64, 512], F32, "rp1") for _ in range(2)])
            t2r = Ring([M.sb(pA, [64, 512], F32, "rp2") for _ in range(2)])
            smr = Ring([M.sb(pA, [128, 16], F32, "sm") for _ in range(4)])
            o1r = Ring([M.sb(pA, [128, 128], F32, "o1") for _ in range(2)])
            o2r = Ring([M.sb(pA, [128, 128], F32, "o2") for _ in range(2)])
            jkr = Ring([M.sb(pA, [128, 128], F32, "jk") for _ in range(2)])
            psc, psc_r = M.ps(pA, [128, 5, 512], F32, "psc")
            pT, pT_r = M.ps(pA, [128, 6, 128], BF16, "pT")
            po, po_r = M.ps(pA, [128, 2, 128], F32, "po")
            pout, pout_r = M.ps(pA, [128, 512], F32, "pout")
            KB = [(j * 512, min(512, T - j * 512)) for j in range(5)]
            for c in range(8):
                with ExitStack() as pG:
                    wq, wq_r = self.load_w(pG, wqkv[0, :, c * 128:(c + 1) * 128], 8, 128, "wq")
                    wk, wk_r = self.load_w(pG, wqkv[0, :, D + c * 128:D + (c + 1) * 128], 8, 128, "wk")
                    wv, wv_r = self.load_w(pG, wqkv[0, :, 2 * D + c * 128:2 * D + (c + 1) * 128], 8, 128, "wv")
                    woc, woc_r = self.load_w(pG, wo[0, c * 128:(c + 1) * 128, :], 1, D, "woc")
                    wqs, wqs_r = self.swap_halves(pG, wq, wq_r, 8, 2, "wqs")
                    wks, wks_r = self.swap_halves(pG, wk, wk_r, 8, 2, "wks")
                    for t in range(2):
                        self.proj_rope(psc, psc_r, wk, wk_r, wks, wks_r, t * 64, zb[:, 0:1], zb[:, 0:1], zb_r,
                                       hT, hT_r, kTs[t][0], kTs[t][1], BLKS, C, Sn, tab_r, t1r, t2r)
                        self.proj_rope(psc, psc_r, wq, wq_r, wqs, wqs_r, t * 64, zb[:, 0:1], zb[:, 0:1], zb_r,
                                       hT, hT_r, qTs[t][0], qTs[t][1], LBLK, C, Sn, tab_r, t1r, t2r)
                    for tt in range(NT):
                        for k in range(8):
                            S.op("pe", lambda e, tt=tt, k=k: e.matmul(pout[:, 0:128], lhsT=hT[:, k, tt * 128:(tt + 1) * 128], rhs=wv[:, k, :],
                                                                      start=(k == 0), stop=(k == 7)),
                                 reads=[hT_r, wv_r], writes=[pout_r], pe_acc=True)
                        S.op("act", lambda e, tt=tt: e.activation(out=vs[:, tt, :], in_=pout[:, 0:128], func=AF.Copy),
                             reads=[pout_r], writes=[vs_r])
                    for qb in range(16):
                        q0 = NCTX + qb * 128
                        sms = []
                        for t in range(2):
                            qT, qT_r = qTs[t]
                            kT, kT_r = kTs[t]
                            sm, sm_r = smr.next()
                            sms.append((sm, sm_r))
                            for j, (k0, kn) in enumerate(KB):
                                S.op("pe", lambda e, j=j, k0=k0, kn=kn, qT=qT, kT=kT, q0=q0: e.matmul(
                                    psc[:, j, :kn], lhsT=qT[:, q0:q0 + 128], rhs=kT[:, k0:k0 + kn], start=True, stop=True),
                                    reads=[qT_r, kT_r], writes=[psc_r], pe_acc=True)
                            for j, (k0, kn) in enumerate(KB):
                                S.op("dve", lambda e, j=j, kn=kn, sm=sm: e.reduce_max(out=sm[:, j:j + 1], in_=psc[:, j, :kn], axis=AX.X),
                                     reads=[psc_r], writes=[sm_r])
                            S.op("dve", lambda e, sm=sm: e.reduce_max(out=sm[:, 5:6], in_=sm[:, 0:5], axis=AX.X), reads=[sm_r], writes=[sm_r])
                            S.op("dve", lambda e, sm=sm: e.tensor_scalar(out=sm[:, 6:7], in0=sm[:, 5:6], scalar1=-0.125, scalar2=None, op0=ALU.mult),
                                 reads=[sm_r], writes=[sm_r])
                            S.op("pool", lambda e, sm=sm: e.memset(sm[:, 8:13], 0.0), reads=[sm_r], writes=[sm_r])
                            pb, pb_r = pbr.next()
                            for j, (k0, kn) in enumerate(KB):
                                S.op("act", lambda e, j=j, k0=k0, kn=kn, sm=sm, pb=pb: e.activation(
                                    out=pb[:, k0:k0 + kn], in_=psc[:, j, :kn], func=AF.Exp, scale=0.125, bias=sm[:, 6:7], accum_out=sm[:, 8 + j:9 + j]),
                                    reads=[psc_r, sm_r], writes=[pb_r, sm_r])
                            S.op("dve", lambda e, sm=sm: e.reduce_sum(out=sm[:, 13:14], in_=sm[:, 8:13], axis=AX.X), reads=[sm_r], writes=[sm_r])
                            S.op("dve", lambda e, sm=sm: e.reciprocal(out=sm[:, 14:15], in_=sm[:, 13:14]), reads=[sm_r], writes=[sm_r])
                            for b3 in range(3):
                                for jj in range(6):
                                    j = b3 * 6 + jj
                                    S.op("pe", lambda e, j=j, jj=jj, pb=pb: e.transpose(out=pT[:, jj, :], in_=pb[:, j * 128:(j + 1) * 128], identity=self.identb[:]),
                                         reads=[pb_r, self.identb_r], writes=[pT_r], pe_acc=True)
                                if b3 % 2 == 0:
                                    S.op("act", lambda e, b3=b3: e.activation(out=pTs[:, b3 * 6:(b3 + 1) * 6, :], in_=pT[:], func=AF.Copy),
                                         reads=[pT_r], writes=[pTs_r])
                                else:
                                    S.op("dve", lambda e, b3=b3: e.tensor_copy(out=pTs[:, b3 * 6:(b3 + 1) * 6, :], in_=pT[:]),
                                         reads=[pT_r], writes=[pTs_r])
                            for j in range(NT):
                                S.op("pe", lambda e, j=j, t=t: e.matmul(po[:, t, :], lhsT=pTs[:, j, :], rhs=vs[:, j, :], start=(j == 0), stop=(j == NT - 1)),
                                     reads=[pTs_r, vs_r], writes=[po_r], pe_acc=True)
                        (sm0, sm0_r), (sm1, sm1_r) = sms
                        o1, o1_r = o1r.next()
                        o2, o2_r = o2r.next()
                        jk, jk_r = jkr.next()
                        S.op("dve", lambda e, sm1=sm1: e.tensor_tensor(out=sm1[:, 15:16], in0=sm1[:, 14:15], in1=lam[:, 3:4], op=ALU.mult),
                             reads=[sm1_r, lam_r], writes=[sm1_r])
                        S.op("dve", lambda e, o1=o1, sm1=sm1: e.tensor_scalar(out=o1[:], in0=po[:, 1, :], scalar1=sm1[:, 15:16], scalar2=None, op0=ALU.mult),
                             reads=[po_r, sm1_r], writes=[o1_r])
                        S.op("dve", lambda e, o1=o1, o2=o2, sm0=sm0: e.scalar_tensor_tensor(out=o2[:], in0=po[:, 0, :], scalar=sm0[:, 14:15], in1=o1[:],
                                                                                         op0=ALU.mult, op1=ALU.subtract),
                             reads=[po_r, sm0_r, o1_r], writes=[o2_r])
                        S.op("pool", lambda e, sm0=sm0: e.memset(sm0[:, 7:8], 0.0), reads=[sm0_r], writes=[sm0_r])
                        S.op("act", lambda e, jk=jk, o2=o2, sm0=sm0: e.activation(out=jk[:], in_=o2[:], func=AF.Square, accum_out=sm0[:, 7:8]),
                             reads=[o2_r, sm0_r], writes=[jk_r, sm0_r])
                        S.op("act", lambda e, sm0=sm0: e.activation(out=sm0[:, 7:8], in_=sm0[:, 7:8], func=AF.Sqrt, scale=1.0 / 128, bias=self.epsb[:, 0:1]),
                             reads=[sm0_r, self.epsb_r], writes=[sm0_r])
                        S.op("dve", lambda e, sm0=sm0: e.reciprocal(out=sm0[:, 7:8], in_=sm0[:, 7:8]), reads=[sm0_r], writes=[sm0_r])
                        S.op("dve", lambda e, o2=o2, sm0=sm0: e.tensor_scalar(out=o2[:], in0=o2[:], scalar1=sm0[:, 7:8], scalar2=float(1.0 - lambda_init),
                                                                            op0=ALU.mult, op1=ALU.mult),
                             reads=[o2_r, sm0_r], writes=[o2_r])
                        S.op("pe", lambda e, o2=o2: e.transpose(out=pout[:, 128:256], in_=o2[:], identity=self.identf[:]),
                             reads=[o2_r, self.identf_r], writes=[pout_r], pe_acc=True)
                        S.op("act", lambda e, qb=qb: e.activation(out=oT[:, qb * 128:(qb + 1) * 128], in_=pout[:, 128:256], func=AF.Identity,
                                                                  scale=sgt[:, 0:1]),
                             reads=[pout_r, sgt_r], writes=[oT_r])
                    for (s, n) in LBLK:
                        for oc in range(8):
                            S.op("pe", lambda e, oc=oc, s=s, n=n: e.matmul(
                                pout[:, :n], lhsT=woc[:, 0, oc * 128:(oc + 1) * 128], rhs=oT[:, s - NCTX:s - NCTX + n], start=True, stop=True),
                                reads=[woc_r, oT_r], writes=[pout_r], pe_acc=True)
                            self.resid(pout, pout_r, n, oc, s)
                    S.barrier()
                    S.release([wq_r, wk_r, wv_r, woc_r])

    def mixer1(self, i):
        nc, S, M = self.nc, self.S, self.M
        w_in = self.inp("ssm_w_in", [1, D, 5184])
        w_out = self.inp("ssm_w_out", [1, 2048, D])
        svec = self.inp("ssm_vec", [128, 160])
        a_log = self.inp("ssm_a_log", [1, 64])
        dt_bias = self.inp("ssm_dt_bias", [1, 64])
        d_skip = self.inp("ssm_d", [1, 32])
        dr = lambda name, shape: (nc.dram_tensor(name, list(shape), BF16, kind="Internal").ap(), Res(name))
        XS, XS_r = dr("scr_xs", [NT, 128, 2048])
        BTd, BTd_r = dr("scr_bt", [NT, 128, 4, 128])
        CTd, CTd_r = dr("scr_ct", [NT, 128, 4, 128])
        BMd, BMd_r = dr("scr_bm", [NT, 128, 512])
        ZS, ZS_r = dr("scr_zs", [NT, 128, 2048])
        HB, HB_r = dr("scr_hb", [NT, 128, 2048])
        PW = 2312
        OW = 2308

        def pcol(s):
            return 2 if s == 0 else s + 6
        with ExitStack() as pA:
            sv, sv_r = M.sb(pA, [128, 160], F32, "sv")
            S.dma("sp", sv[:], svec, writes=[sv_r], key=sv_r)
            msk, msk_r = M.sb(pA, [128, 4, 128], F32, "msk")
            S.op("pool", lambda e: e.memset(msk[:], 1.0), writes=[msk_r])
            for a, (pat, cm, op) in enumerate((([[1, 128]], -1, ALU.is_ge), ([[-1, 128]], 1, ALU.is_ge),
                                              ([[-1, 128]], 1, ALU.is_gt), ([[1, 128]], -1, ALU.is_gt))):
                S.op("pool", lambda e, a=a, pat=pat, cm=cm, op=op: e.affine_select(
                    out=msk[:, a, :], in_=msk[:, a, :], pattern=pat, compare_op=op, fill=0.0, base=0, channel_multiplier=cm),
                    reads=[msk_r], writes=[msk_r])
            triF, triB, mltF, mltB = (msk[:, a, :] for a in range(4))
            dt, dt_r = M.sb(pA, [128, NT, 64], F32, "dt")
            loga, loga_r = M.sb(pA, [128, NT, 64], F32, "loga")
            expA, expA_r = M.sb(pA, [128, NT, 64], F32, "expA")
            dec, dec_r = M.sb(pA, [128, NT, 64], F32, "dec")
            wst, wst_r = M.sb(pA, [128, NT, 64], F32, "wst")
            abc, abc_r = M.sb(pA, [128, 64], F32, "abc")
            dtb, dtb_r = M.sb(pA, [128, 64], F32, "dtb")
            dsk, dsk_r = M.sb(pA, [128, 32], F32, "dsk")
            S.dma("sp", abc[:], a_log[0].partition_broadcast(128), writes=[abc_r], key=abc_r)
            S.dma("sp", dtb[:], dt_bias[0].partition_broadcast(128), writes=[dtb_r], key=dtb_r)
            S.dma("sp", dsk[:], d_skip[0].partition_broadcast(128), writes=[dsk_r], key=dsk_r)
            S.op("act", lambda e: e.activation(out=abc[:], in_=abc[:], func=AF.Exp), reads=[abc_r], writes=[abc_r])
            S.op("dve", lambda e: e.tensor_scalar(out=abc[:], in0=abc[:], scalar1=-1.0, scalar2=None, op0=ALU.mult), reads=[abc_r], writes=[abc_r])
            with ExitStack() as pB:
                hT, hT_r = M.sb(pB, [128, 8, T], BF16, "hT")
                with ExitStack() as pD:
                    self.adanorm(pD, hT, hT_r, 0)
                    S.barrier()
                with ExitStack() as pC:
                    wdt, wdt_r = self.load_w(pC, w_in[0, :, 5120:5184], 8, 64, "wdt")
                    pdt = Ring([M.ps(pC, [128, 64], F32, "pdt") for _ in range(2)])
                    pcs = Ring([M.ps(pC, [128, 2, 64], F32, "pcs") for _ in range(2)])
                    tmr = Ring([[M.sb(pC, [128, 64], F32, "sp%d" % j) for j in range(4)] for _ in range(2)])
                    for t in range(NT):
                        p_, p_r = pdt.next()
                        for k in range(8):
                            S.op("pe", lambda e, p_=p_, t=t, k=k: e.matmul(p_[:], lhsT=hT[:, k, t * 128:(t + 1) * 128], rhs=wdt[:, k, :],
                                                                          start=(k == 0), stop=(k == 7)),
                                 reads=[hT_r, wdt_r], writes=[p_r], pe_acc=True)
                        (x_, x_r), (ax, ax_r), (ex, ex_r), (rl, rl_r) = tmr.next()
                        S.op("dve", lambda e, x_=x_, p_=p_: e.tensor_tensor(out=x_[:], in0=p_[:], in1=dtb[:], op=ALU.add),
                             reads=[p_r, dtb_r], writes=[x_r])
                        S.op("act", lambda e, ax=ax, x_=x_: e.activation(out=ax[:], in_=x_[:], func=AF.Abs),
                             reads=[x_r], writes=[ax_r])
                        S.op("act", lambda e, ex=ex, ax=ax: e.activation(out=ex[:], in_=ax[:], func=AF.Exp, scale=-1.0), reads=[ax_r], writes=[ex_r])
                        S.op("act", lambda e, ex=ex: e.activation(out=ex[:], in_=ex[:], func=AF.Ln, bias=self.onesf[:, 0:1], scale=1.0),
                             reads=[ex_r, self.onesf_r], writes=[ex_r])
                        S.op("dve", lambda e, rl=rl, x_=x_: e.tensor_scalar(out=rl[:], in0=x_[:], scalar1=0.0, scalar2=None, op0=ALU.max),
                             reads=[x_r], writes=[rl_r])
                        S.op("dve", lambda e, rl=rl, ex=ex, t=t: e.tensor_tensor(out=dt[:, t, :], in0=rl[:], in1=ex[:], op=ALU.add),
                             reads=[rl_r, ex_r], writes=[dt_r])
                        S.op("dve", lambda e, t=t: e.tensor_tensor(out=loga[:, t, :], in0=dt[:, t, :], in1=abc[:], op=ALU.mult),
                             reads=[dt_r, abc_r], writes=[loga_r])
                        c_, c_r = pcs.next()
                        S.op("pe", lambda e, c_=c_, t=t: e.matmul(c_[:, 0, 0:32], lhsT=triF, rhs=loga[:, t, 0:32], start=True, stop=True),
                             reads=[msk_r, loga_r], writes=[c_r], pe_acc=True)
                        S.op("pe", lambda e, c_=c_, t=t: e.matmul(c_[:, 0, 32:64], lhsT=triB, rhs=loga[:, t, 32:64], start=True, stop=True),
                             reads=[msk_r, loga_r], writes=[c_r], pe_acc=True)
                        S.op("pe", lambda e, c_=c_, t=t: e.matmul(c_[:, 1, :], lhsT=self.onesf[:], rhs=loga[:, t, :], start=True, stop=True),
                             reads=[self.onesf_r, loga_r], writes=[c_r], pe_acc=True)
                        S.op("act", lambda e, c_=c_, t=t: e.activation(out=expA[:, t, :], in_=c_[:, 0, :], func=AF.Exp), reads=[c_r], writes=[expA_r])
                        S.op("act", lambda e, c_=c_, t=t: e.activation(out=dec[:, t, :], in_=c_[:, 1, :], func=AF.Exp), reads=[c_r], writes=[dec_r])
                        S.op("act", lambda e, c_=c_, ax=ax: e.activation(out=ax[:], in_=c_[:, 0, :], func=AF.Copy), reads=[c_r, ax_r], writes=[ax_r])
                        S.op("dve", lambda e, c_=c_, ax=ax: e.tensor_tensor(out=ax[:], in0=c_[:, 1, :], in1=ax[:], op=ALU.subtract),
                             reads=[c_r, ax_r], writes=[ax_r])
                        S.op("act", lambda e, ax=ax: e.activation(out=ax[:], in_=ax[:], func=AF.Exp), reads=[ax_r], writes=[ax_r])
                        S.op("dve", lambda e, ax=ax, t=t: e.tensor_tensor(out=wst[:, t, :], in0=ax[:], in1=dt[:, t, :], op=ALU.mult),
                             reads=[ax_r, dt_r], writes=[wst_r])
                    S.barrier()
                    S.release([wdt_r])
                with ExitStack() as pC:
                    wz, wz_r = self.load_w(pC, w_in[0, :, 0:2048], 8, 2048, "wz")
                    pz, pz_r = M.ps(pC, [128, 4, 512], F32, "pz")
                    zr = Ring([M.sb(pC, [128, 2048], BF16, "zt") for _ in range(2)])
                    for t in range(NT):
                        for nb in range(4):
                            for k in range(8):
                                S.op("pe", lambda e, t=t, nb=nb, k=k: e.matmul(pz[:, nb, :], lhsT=hT[:, k, t * 128:(t + 1) * 128],
                                                                               rhs=wz[:, k, nb * 512:(nb + 1) * 512], start=(k == 0), stop=(k == 7)),
                                     reads=[hT_r, wz_r], writes=[pz_r], pe_acc=True)
                        z_, z_r = zr.next()
                        S.op("act", lambda e, z_=z_: e.activation(out=z_[:].rearrange("p (a b) -> p a b", b=512), in_=pz[:], func=AF.Silu),
                             reads=[pz_r], writes=[z_r])
                        S.dma("sp", ZS[t], z_[:], reads=[z_r], writes=[ZS_r], key=z_r)
                    S.barrier()
                    S.release([wz_r] + [r for _, r in zr.items])
                with ExitStack() as pC:
                    wpr = Ring([M.sb(pC, [128, 8, 128], BF16, "wxp") for _ in range(3)])
                    upr = Ring([M.sb(pC, [128, PW], F32, "upad") for _ in range(1)])
                    acr = Ring([M.sb(pC, [128, OW], F32, "cacc") for _ in range(1)])
                    scr = Ring([M.sb(pC, [128, T], BF16, "scc") for _ in range(2)])
                    xh, xh_r = M.sb(pC, [128, NT, 512], BF16, "xhalf")
                    btm, btm_r = M.sb(pC, [128, NT, 128], BF16, "btm")
                    pp = Ring([M.ps(pC, [128, 512], F32, "pxp") for _ in range(2)])
                    ptr = Ring([M.ps(pC, [128, 6, 128], BF16, "ptx") for _ in range(2)])
                    for u, u_r in upr.items:
                        S.op("pool", lambda e, u=u: e.memset(u[:], 0.0), writes=[u_r])
                    for cc in range(24):
                        wt, wt_r = wpr.next()
                        S.dma("pool", wt[:], w_in[0, :, 2048 + cc * 128:2048 + (cc + 1) * 128].rearrange("(k p) n -> p k n", p=128),
                              writes=[wt_r], key=wt_r)
                        u, u_r = upr.next()
                        for (s, n) in BLKS:
                            p_, p_r = pp.next()
                            for k in range(8):
                                S.op("pe", lambda e, p_=p_, wt=wt, k=k, s=s, n=n: e.matmul(p_[:, :n], lhsT=wt[:, k, :], rhs=hT[:, k, s:s + n],
                                                                                          start=(k == 0), stop=(k == 7)),
                                     reads=[wt_r, hT_r], writes=[p_r], pe_acc=True)
                            S.op("act", lambda e, p_=p_, u=u, s=s, n=n: e.activation(out=u[:, pcol(s):pcol(s) + n], in_=p_[:, :n], func=AF.Copy),
                                 reads=[p_r], writes=[u_r])
                        ac, ac_r = acr.next()
                        eng = "dve"
                        S.op(eng, lambda e, ac=ac, u=u, cc=cc: e.tensor_scalar(
                            out=ac[:], in0=u[:, 0:OW], scalar1=sv[:, cc * 5:cc * 5 + 1], scalar2=sv[:, 120 + cc:121 + cc], op0=ALU.mult, op1=ALU.add),
                            reads=[u_r, sv_r], writes=[ac_r])
                        for w in range(1, 5):
                            S.op(eng, lambda e, ac=ac, u=u, cc=cc, w=w: e.scalar_tensor_tensor(
                                out=ac[:], in0=u[:, w:w + OW], scalar=sv[:, cc * 5 + w:cc * 5 + w + 1], in1=ac[:], op0=ALU.mult, op1=ALU.add),
                                reads=[u_r, sv_r, ac_r], writes=[ac_r])
                        sc, sc_r = scr.next()
                        S.op("act", lambda e, sc=sc, ac=ac: e.activation(out=sc[:, 0:NCTX], in_=ac[:, 0:NCTX], func=AF.Silu), reads=[ac_r], writes=[sc_r])
                        S.op("act", lambda e, sc=sc, ac=ac: e.activation(out=sc[:, NCTX:T], in_=ac[:, 260:260 + NLAT], func=AF.Silu), reads=[ac_r], writes=[sc_r])
                        if cc < 20:
                            for b3 in range(3):
                                pt, pt_r = ptr.next()
                                for jj in range(6):
                                    t = b3 * 6 + jj
                                    S.op("pe", lambda e, pt=pt, jj=jj, t=t, sc=sc: e.transpose(out=pt[:, jj, :], in_=sc[:, t * 128:(t + 1) * 128], identity=self.identb[:]),
                                         reads=[sc_r, self.identb_r], writes=[pt_r], pe_acc=True)
                                if cc < 16:
                                    c8 = cc % 4
                                    S.op("dve", lambda e, pt=pt, b3=b3, c8=c8: e.tensor_copy(out=xh[:, b3 * 6:(b3 + 1) * 6, c8 * 128:(c8 + 1) * 128], in_=pt[:]),
                                         reads=[pt_r], writes=[xh_r])
                                else:
                                    S.op("dve", lambda e, pt=pt, b3=b3: e.tensor_copy(out=btm[:, b3 * 6:(b3 + 1) * 6, :], in_=pt[:]),
                                         reads=[pt_r], writes=[btm_r])
                        if cc < 16 and cc % 4 == 3:
                            half = cc // 4
                            S.dma("sp", XS[:, :, half * 512:(half + 1) * 512].rearrange("t l f -> l t f"), xh[:],
                                  reads=[xh_r], writes=[XS_r], key=xh_r)
                        if 16 <= cc < 20:
                            g = cc - 16
                            S.dma("sp", BTd[:, :, g, :].rearrange("t n l -> n t l"), sc[:].rearrange("p (t l) -> p t l", l=128),
                                  reads=[sc_r], writes=[BTd_r], key=sc_r)
                            S.dma("sp", BMd[:, :, g * 128:(g + 1) * 128].rearrange("t l n -> l t n"), btm[:],
                                  reads=[btm_r], writes=[BMd_r], key=btm_r)
                        if cc >= 20:
                            g = cc - 20
                            S.dma("sp", CTd[:, :, g, :].rearrange("t n l -> n t l"), sc[:].rearrange("p (t l) -> p t l", l=128),
                                  reads=[sc_r], writes=[CTd_r], key=sc_r)
                    S.barrier()
                    S.release([r for _, r in wpr.items] + [r for _, r in scr.items] + [xh_r, btm_r])
            order_b = [1, 0] + list(range(NT - 1, 1, -1))
            Hf, Hf_r = M.sb(pA, [128, 4, 512], F32, "Hst")
            Hb16, Hb16_r = M.sb(pA, [128, 4, 512], BF16, "Hst16")
            pst, pst_r = M.ps(pA, [128, 512], F32, "pst")
            xsr = Ring([M.sb(pA, [128, 2048], BF16, "xs") for _ in range(2)])
            bmr = Ring([M.sb(pA, [128, 512], BF16, "bm") for _ in range(2)])
            xwr = Ring([M.sb(pA, [128, 2048], BF16, "xw") for _ in range(1)])
            hbo = Ring([M.sb(pA, [128, 2048], BF16, "hbo") for _ in range(2)])

            def bc(ap2d):
                return ap2d.unsqueeze(2).to_broadcast([128, 8, 64])

            def v3(ap2d):
                return ap2d.rearrange("p (h d) -> p h d", d=64)

            def state_update(t, xs, xs_r, bm, bm_r, d0):
                xw, xw_r = xwr.next()
                for g in range(4):
                    S.op("pool" if g % 2 else "dve", lambda e, g=g, xw=xw, xs=xs, t=t: e.tensor_tensor(
                        out=v3(xw[:, g * 512:(g + 1) * 512]), in0=v3(xs[:, g * 512:(g + 1) * 512]),
                        in1=bc(wst[:, t, d0 + g * 8:d0 + (g + 1) * 8]), op=ALU.mult),
                        reads=[xs_r, wst_r], writes=[xw_r])
                for g in range(4):
                    S.op("pe", lambda e, g=g, bm=bm, xw=xw: e.matmul(pst[:], lhsT=bm[:, g * 128:(g + 1) * 128], rhs=xw[:, g * 512:(g + 1) * 512],
                                                                     start=True, stop=True),
                         reads=[bm_r, xw_r], writes=[pst_r], pe_acc=True)
                    S.op("dve", lambda e, g=g, t=t: e.tensor_tensor(out=v3(Hf[:, g, :]), in0=v3(Hf[:, g, :]),
                                                                    in1=bc(dec[:, t, d0 + g * 8:d0 + (g + 1) * 8]), op=ALU.mult),
                         reads=[Hf_r, dec_r], writes=[Hf_r])
                    S.op("dve", lambda e, g=g: e.tensor_tensor(out=Hf[:, g, :], in0=Hf[:, g, :], in1=pst[:], op=ALU.add),
                         reads=[Hf_r, pst_r], writes=[Hf_r])
            S.op("pool", lambda e: e.memset(Hf[:], 0.0), writes=[Hf_r])
            for t in order_b:
                xs, xs_r = xsr.next()
                bm, bm_r = bmr.next()
                S.dma("sp", xs[:], XS[t], reads=[XS_r], writes=[xs_r], key=xs_r)
                S.dma("sp", bm[:], BMd[t], reads=[BMd_r], writes=[bm_r], key=bm_r)
                ho, ho_r = hbo.next()
                S.op("act", lambda e, ho=ho: e.activation(out=ho[:].rearrange("p (g f) -> p g f", f=512), in_=Hf[:], func=AF.Copy),
                     reads=[Hf_r], writes=[ho_r])
                S.dma("sp", HB[t], ho[:], reads=[ho_r], writes=[HB_r], key=ho_r)
                state_update(t, xs, xs_r, bm, bm_r, 32)
            S.barrier()
            S.op("pool", lambda e: e.memset(Hf[:], 0.0), writes=[Hf_r])
            S.op("pool", lambda e: e.memset(Hb16[:], 0.0), writes=[Hb16_r])
            wo_t, wo_r = self.load_w(pA, w_out[0], 16, D, "wout")
            for kc in range(16):
                S.op("dve", lambda e, kc=kc: e.tensor_scalar(out=wo_t[:, kc, :], in0=wo_t[:, kc, :], scalar1=sv[:, 144 + kc:145 + kc], scalar2=None,
                                                            op0=ALU.mult), reads=[wo_r, sv_r], writes=[wo_r])
            btr = Ring([M.sb(pA, [128, 4, 128], BF16, "btc") for _ in range(2)])
            ctr = Ring([M.sb(pA, [128, 4, 128], BF16, "ctc") for _ in range(2)])
            zsr = Ring([M.sb(pA, [128, 2048], BF16, "zsc") for _ in range(2)])
            xdf, xdf_r = M.sb(pA, [128, 2048], BF16, "xdf")
            xdb, xdb_r = M.sb(pA, [128, 2048], BF16, "xdb")
            gm, gm_r = M.sb(pA, [128, 2, 128], F32, "gm")
            lhr = Ring([M.sb(pA, [128, 128], F32, "lh") for _ in range(3)])
            dhr = Ring([M.sb(pA, [128, 128], F32, "dh") for _ in range(3)])
            mtr = Ring([M.sb(pA, [128, 128], BF16, "mt") for _ in range(3)])
            yg, yg_r = M.sb(pA, [128, 512], F32, "yg")
            tq = Ring([M.sb(pA, [128, 512], F32, "tq") for _ in range(2)])
            ybf, ybf_r = M.sb(pA, [128, 2048], BF16, "ybf")
            ynT, ynT_r = M.sb(pA, [128, 16, 128], BF16, "ynT")
            ssq, ssq_r = M.sb(pA, [128, 8], F32, "ssq")
            psg = Ring([M.ps(pA, [128, 128], F32, "psg") for _ in range(2)])
            pd, pd_r = M.ps(pA, [128, 512], F32, "pd")
            pf, pf_r = M.ps(pA, [128, 2, 512], F32, "pf")
            ptT, ptT_r = M.ps(pA, [128, 8, 128], BF16, "ptT")
            pout, pout_r = M.ps(pA, [128, 128], F32, "pout")
            for t in range(NT):
                xs, xs_r = xsr.next()
                bm, bm_r = bmr.next()
                bt, bt_r = btr.next()
                ct, ct_r = ctr.next()
                zs, zs_r = zsr.next()
                hb, hb_r = hbo.next()
                S.dma("sp", xs[:], XS[t], reads=[XS_r], writes=[xs_r], key=xs_r)
                S.dma("sp", bm[:], BMd[t], reads=[BMd_r], writes=[bm_r], key=bm_r)
                S.dma("sp", bt[:], BTd[t], reads=[BTd_r], writes=[bt_r], key=bt_r)
                S.dma("sp", ct[:], CTd[t], reads=[CTd_r], writes=[ct_r], key=ct_r)
                S.dma("sp", zs[:], ZS[t], reads=[ZS_r], writes=[zs_r], key=zs_r)
                S.dma("sp", hb[:], HB[t], reads=[HB_r], writes=[hb_r], key=hb_r)
                for g in range(4):
                    S.op("dve", lambda e, g=g, xs=xs, t=t: e.tensor_tensor(out=v3(xdf[:, g * 512:(g + 1) * 512]), in0=v3(xs[:, g * 512:(g + 1) * 512]),
                                                                        in1=bc(dt[:, t, g * 8:(g + 1) * 8]), op=ALU.mult),
                         reads=[xs_r, dt_r], writes=[xdf_r])
                    S.op("pool", lambda e, g=g, xs=xs, t=t: e.tensor_tensor(out=v3(xdb[:, g * 512:(g + 1) * 512]), in0=v3(xs[:, g * 512:(g + 1) * 512]),
                                                                         in1=bc(dt[:, t, 32 + g * 8:32 + (g + 1) * 8]), op=ALU.mult),
                         reads=[xs_r, dt_r], writes=[xdb_r])
                S.op("pool", lambda e: e.memset(ssq[:], 0.0), reads=[ssq_r], writes=[ssq_r])
                for g in range(4):
                    S.op("pe", lambda e, g=g, bt=bt, ct=ct: e.matmul(pst[:, 0:128], lhsT=bt[:, g, :], rhs=ct[:, g, :], start=True, stop=True),
                         reads=[bt_r, ct_r], writes=[pst_r], pe_acc=True)
                    S.op("dve", lambda e: e.tensor_tensor(out=gm[:, 0, :], in0=pst[:, 0:128], in1=triF, op=ALU.mult), reads=[pst_r, msk_r], writes=[gm_r])
                    S.op("dve", lambda e: e.tensor_tensor(out=gm[:, 1, :], in0=pst[:, 0:128], in1=triB, op=ALU.mult), reads=[pst_r, msk_r], writes=[gm_r])
                    for r in range(8):
                        h = g * 8 + r
                        for d_, (mlt, tri, xd, xd_r) in enumerate(((mltF, triF, xdf, xdf_r), (mltB, triB, xdb, xdb_r))):
                            lh, lh_r = lhr.next()
                            S.op("act", lambda e, lh=lh, mlt=mlt, t=t, h=h, d_=d_: e.activation(
                                out=lh[:], in_=mlt, func=AF.Copy, scale=loga[:, t, d_ * 32 + h:d_ * 32 + h + 1]),
                                reads=[msk_r, loga_r], writes=[lh_r])
                            sg_, sg_r = psg.next()
                            S.op("pe", lambda e, sg_=sg_, lh=lh, tri=tri: e.matmul(sg_[:], lhsT=lh[:], rhs=tri, start=True, stop=True),
                                 reads=[lh_r, msk_r], writes=[sg_r], pe_acc=True)
                            dh, dh_r = dhr.next()
                            S.op("act", lambda e, dh=dh, sg_=sg_: e.activation(out=dh[:], in_=sg_[:], func=AF.Exp), reads=[sg_r], writes=[dh_r])
                            mt, mt_r = mtr.next()
                            S.op("dve", lambda e, mt=mt, dh=dh, d_=d_: e.tensor_tensor(out=mt[:], in0=gm[:, d_, :], in1=dh[:], op=ALU.mult),
                                 reads=[gm_r, dh_r], writes=[mt_r])
                            S.op("pe", lambda e, mt=mt, xd=xd, r=r, h=h, d_=d_: e.matmul(
                                pd[:, r * 64:(r + 1) * 64], lhsT=mt[:], rhs=xd[:, h * 64:(h + 1) * 64], start=(d_ == 0), stop=(d_ == 1)),
                                reads=[mt_r, xd_r], writes=[pd_r], pe_acc=True)
                    S.op("pe", lambda e, g=g, ct=ct: e.matmul(pf[:, 0, :], lhsT=ct[:, g, :], rhs=Hb16[:, g, :], start=True, stop=True),
                         reads=[ct_r, Hb16_r], writes=[pf_r], pe_acc=True)
                    S.op("pe", lambda e, g=g, ct=ct, hb=hb: e.matmul(pf[:, 1, :], lhsT=ct[:, g, :], rhs=hb[:, g * 512:(g + 1) * 512], start=True, stop=True),
                         reads=[ct_r, hb_r], writes=[pf_r], pe_acc=True)
                    S.op("act", lambda e: e.activation(out=yg[:], in_=pd[:], func=AF.Copy), reads=[pd_r], writes=[yg_r])
                    for d_ in range(2):
                        q_, q_r = tq.next()
                        S.op("dve", lambda e, q_=q_, d_=d_, t=t, g=g: e.tensor_tensor(out=v3(q_[:]), in0=v3(pf[:, d_, :]),
                                                                                  in1=bc(expA[:, t, d_ * 32 + g * 8:d_ * 32 + (g + 1) * 8]), op=ALU.mult),
                             reads=[pf_r, expA_r], writes=[q_r])
                        S.op("pool", lambda e, q_=q_: e.tensor_tensor(out=yg[:], in0=yg[:], in1=q_[:], op=ALU.add), reads=[yg_r, q_r], writes=[yg_r])
                    q_, q_r = tq.next()
                    S.op("dve", lambda e, q_=q_, g=g, xs=xs: e.tensor_tensor(out=v3(q_[:]), in0=v3(xs[:, g * 512:(g + 1) * 512]),
                                                                          in1=bc(dsk[:, g * 8:(g + 1) * 8]), op=ALU.mult),
                         reads=[xs_r, dsk_r], writes=[q_r])
                    S.op("pool", lambda e, q_=q_: e.tensor_tensor(out=yg[:], in0=yg[:], in1=q_[:], op=ALU.add), reads=[yg_r, q_r], writes=[yg_r])
                    S.op("dve", lambda e, g=g, zs=zs: e.tensor_tensor(out=yg[:], in0=yg[:], in1=zs[:, g * 512:(g + 1) * 512], op=ALU.mult),
                         reads=[yg_r, zs_r], writes=[yg_r])
                    jk, jk_r = tq.next()
                    S.op("act", lambda e, g=g, jk=jk: e.activation(out=jk[:], in_=yg[:], func=AF.Square, accum_out=ssq[:, g:g + 1]),
                         reads=[yg_r, ssq_r], writes=[jk_r, ssq_r])
                    S.op("pool", lambda e, g=g: e.tensor_copy(out=ybf[:, g * 512:(g + 1) * 512], in_=yg[:]), reads=[yg_r], writes=[ybf_r])
                S.op("dve", lambda e: e.reduce_sum(out=ssq[:, 4:5], in_=ssq[:, 0:4], axis=AX.X), reads=[ssq_r], writes=[ssq_r])
                S.op("act", lambda e: e.activation(out=ssq[:, 5:6], in_=ssq[:, 4:5], func=AF.Sqrt, scale=1.0 / 2048, bias=self.epsb[:, 0:1]),
                     reads=[ssq_r, self.epsb_r], writes=[ssq_r])
                S.op("dve", lambda e: e.reciprocal(out=ssq[:, 6:7], in_=ssq[:, 5:6]), reads=[ssq_r], writes=[ssq_r])
                S.op("dve", lambda e: e.tensor_scalar(out=ybf[:], in0=ybf[:], scalar1=ssq[:, 6:7], scalar2=None, op0=ALU.mult),
                     reads=[ybf_r, ssq_r], writes=[ybf_r])
                for b2 in range(2):
                    for jj in range(8):
                        kc = b2 * 8 + jj
                        S.op("pe", lambda e, jj=jj, kc=kc: e.transpose(out=ptT[:, jj, :], in_=ybf[:, kc * 128:(kc + 1) * 128], identity=self.identb[:]),
                             reads=[ybf_r, self.identb_r], writes=[ptT_r], pe_acc=True)
                    S.op("act", lambda e, b2=b2: e.activation(out=ynT[:, b2 * 8:(b2 + 1) * 8, :], in_=ptT[:], func=AF.Copy), reads=[ptT_r], writes=[ynT_r])
                for oc in range(8):
                    for kc in range(16):
                        S.op("pe", lambda e, oc=oc, kc=kc: e.matmul(pout[:], lhsT=wo_t[:, kc, oc * 128:(oc + 1) * 128], rhs=ynT[:, kc, :],
                                                                    start=(kc == 0), stop=(kc == 15)),
                             reads=[wo_r, ynT_r], writes=[pout_r], pe_acc=True)
                    self.resid(pout, pout_r, 128, oc, t * 128)
                state_update(t, xs, xs_r, bm, bm_r, 0)
                S.op("act", lambda e: e.activation(out=Hb16[:], in_=Hf[:], func=AF.Copy), reads=[Hf_r], writes=[Hb16_r])
            S.barrier()

    def out_raw(self):
        nc, S, M = self.nc, self.S, self.M
        y = nc.dram_tensor("y", [T, D], F32, kind="ExternalOutput").ap()
        y_r = Res("y")
        with ExitStack() as ps:
            pst = Ring([M.ps(ps, [128, 4, 128], F32, "otp") for _ in range(2)])
            stg = Ring([M.sb(ps, [128, D], F32, "ostg") for _ in range(2)])
            for t in range(NT):
                st, st_r = stg.next()
                for h in range(2):
                    pt, pt_r = pst.next()
                    for k in range(4):
                        S.op("pe", lambda e, pt=pt, k=k, h=h, t=t: e.transpose(
                            out=pt[:, k, :], in_=self.xT[:, h * 4 + k, t * 128:(t + 1) * 128], identity=self.identf[:]),
                            reads=[self.xTr, self.identf_r], writes=[pt_r], pe_acc=True)
                    S.op("dve", lambda e, pt=pt, st=st, h=h: e.tensor_copy(out=st[:, h * 512:(h + 1) * 512], in_=pt[:]),
                         reads=[pt_r], writes=[st_r])
                S.dma("sp", y[t * 128:(t + 1) * 128, :], st[:], reads=[st_r], writes=[y_r], key=st_r)
            S.barrier()

    def out_final(self):
        nc, S, M = self.nc, self.S, self.M
        y = nc.dram_tensor("y", [NLAT, D], F32, kind="ExternalOutput").ap()
        gfin = self.inp("g_final", [D])
        y_r = Res("y")
        with ExitStack() as ps:
            gb, gb_r = M.sb(ps, [128, D], F32, "gfin")
            S.dma("sp", gb[:], gfin.partition_broadcast(128), writes=[gb_r], key=gb_r)
            pst = Ring([M.ps(ps, [128, 4, 128], F32, "otp") for _ in range(2)])
            stg = Ring([M.sb(ps, [128, D], F32, "ostg") for _ in range(2)])
            jk = Ring([M.sb(ps, [128, D], F32, "ojk") for _ in range(2)])
            ssq = Ring([M.sb(ps, [128, 1], F32, "ossq") for _ in range(2)])
            for t in range(NCTX // 128, NT):
                st, st_r = stg.next()
                for h in range(2):
                    pt, pt_r = pst.next()
                    for k in range(4):
                        S.op("pe", lambda e, pt=pt, k=k, h=h, t=t: e.transpose(
                            out=pt[:, k, :], in_=self.xT[:, h * 4 + k, t * 128:(t + 1) * 128], identity=self.identf[:]),
                            reads=[self.xTr, self.identf_r], writes=[pt_r], pe_acc=True)
                    S.op("dve", lambda e, pt=pt, st=st, h=h: e.tensor_copy(out=st[:, h * 512:(h + 1) * 512], in_=pt[:]),
                         reads=[pt_r], writes=[st_r])
                j_, j_r = jk.next()
                q, q_r = ssq.next()
                S.op("act", lambda e, j_=j_, st=st, q=q: e.activation(out=j_[:], in_=st[:], func=AF.Square, accum_out=q[:]),
                     reads=[st_r], writes=[j_r, q_r])
                S.op("act", lambda e, q=q: e.activation(out=q[:], in_=q[:], func=AF.Sqrt, scale=1.0 / D, bias=self.epsb[:, 0:1]),
                     reads=[q_r, self.epsb_r], writes=[q_r])
                S.op("dve", lambda e, q=q: e.reciprocal(out=q[:], in_=q[:]), reads=[q_r], writes=[q_r])
                S.op("dve", lambda e, j_=j_, st=st, q=q: e.scalar_tensor_tensor(out=j_[:], in0=st[:], scalar=q[:, 0:1], in1=gb[:],
                                                                              op0=ALU.mult, op1=ALU.mult),
                     reads=[st_r, q_r, gb_r, j_r], writes=[j_r])
                r0 = (t - NCTX // 128) * 128
                S.dma("sp", y[r0:r0 + 128, :], j_[:], reads=[j_r], writes=[y_r], key=j_r)
            S.barrier()


FULL_STEPS = []
for _i in range(4):
    FULL_STEPS += [("mods", _i), ("mixer", _i), ("ffn", _i, _i < 3)]


def make_in_maps(inputs, ncores=8, xs=None, cs=None):
    f32 = np.float32
    shared = {}
    lvec = np.zeros((4, 128, 64), f32)
    for i in range(4):
        lvec[i, :, 0:48] = fm(inputs["ada_b"][i])
        lvec[i, :, 48:56] = fm(inputs["g_mix"][i])
        lvec[i, :, 56:64] = fm(inputs["g_ffn"][i])
    shared["lvec"] = lvec
    shared["ada_w"] = np.ascontiguousarray(inputs["ada_w"], f32)
    shared["g_final"] = np.ascontiguousarray(inputs["g_final"], f32)
    shared["moe_w_router"] = np.ascontiguousarray(inputs["moe_w_router"], f32)
    shared["moe_b_router"] = np.ascontiguousarray(inputs["moe_b_router"], f32)
    shared["moe_w_gu"] = np.ascontiguousarray(inputs["moe_w_gu"], f32)
    shared["moe_w_down"] = np.ascontiguousarray(inputs["moe_w_down"], f32)
    shared["moe_b_down"] = np.ascontiguousarray(inputs["moe_b_down"], f32)
    bgu = np.asarray(inputs["moe_b_gu"], f32)
    shared["moe_b_gu_fm"] = np.ascontiguousarray(bgu.reshape(4, NE, 16, 128).transpose(0, 3, 1, 2))
    if "conv_w_pw1" in inputs:
        shared["conv_w_pw1"] = np.ascontiguousarray(inputs["conv_w_pw1"], f32)
        shared["conv_w_pw2"] = np.ascontiguousarray(inputs["conv_w_pw2"], f32)
        cvec = np.zeros((128, 296), f32)
        cvec[:, 0:16] = fm(inputs["conv_b_pw1"][0])
        wdw = np.asarray(inputs["conv_w_dw"][0], f32)
        cvec[:, 16:264] = wdw.reshape(31, 8, 128).transpose(2, 1, 0).reshape(128, 248)
        cvec[:, 264:272] = fm(inputs["conv_b_dw"][0])
        cvec[:, 272:280] = fm(inputs["conv_ln_g"][0])
        cvec[:, 280:288] = fm(inputs["conv_ln_b"][0])
        cvec[:, 288:296] = fm(inputs["conv_b_pw2"][0])
        shared["conv_vec"] = cvec
    if "swa_w_qkv" in inputs:
        shared["swa_w_qkv"] = np.ascontiguousarray(inputs["swa_w_qkv"], f32)
        shared["swa_b_qkv"] = np.ascontiguousarray(inputs["swa_b_qkv"], f32)
        shared["swa_w_o"] = np.ascontiguousarray(inputs["swa_w_o"], f32)
        shared["swa_sinks"] = np.ascontiguousarray(inputs["swa_sinks"], f32)
        bq = np.asarray(inputs["swa_b_qkv"][0], f32).reshape(24, 64).T
        bh = np.zeros((64, 44), f32)
        bh[:, 0:24] = bq
        bh[:, 24:44] = np.roll(bq[:, 0:20], 32, axis=0)
        shared["swa_bh"] = bh
        shared["swa_bo_fm"] = fm(inputs["swa_b_o"][0])
        Ct, St = rope_tables()
        shared["rope_c"] = Ct
        shared["rope_s"] = St
    if "diff_w_qkv" in inputs:
        shared["diff_w_qkv"] = np.ascontiguousarray(inputs["diff_w_qkv"], f32)
        shared["diff_w_o"] = np.ascontiguousarray(inputs["diff_w_o"], f32)
        shared["diff_lam"] = np.ascontiguousarray(np.stack([inputs["diff_lambda_q1"][0], inputs["diff_lambda_k1"][0],
                                                            inputs["diff_lambda_q2"][0], inputs["diff_lambda_k2"][0]], 0), f32)
        shared["diff_subln_g"] = np.ascontiguousarray(np.asarray(inputs["diff_subln_g"][0], f32).reshape(128, 1))
        if "rope_c" not in shared:
            Ct, St = rope_tables()
            shared["rope_c"] = Ct
            shared["rope_s"] = St
    if "ssm_w_in" in inputs:
        shared["ssm_w_in"] = np.ascontiguousarray(inputs["ssm_w_in"], f32)
        shared["ssm_w_out"] = np.ascontiguousarray(inputs["ssm_w_out"], f32)
        svec = np.zeros((128, 160), f32)
        wc = np.asarray(inputs["ssm_w_conv"][0], f32)
        svec[:, 0:120] = wc.reshape(5, 24, 128).transpose(2, 1, 0).reshape(128, 120)
        svec[:, 120:144] = fm(inputs["ssm_b_conv"][0])
        svec[:, 144:160] = fm(inputs["ssm_norm_g"][0])
        shared["ssm_vec"] = svec
        shared["ssm_a_log"] = np.ascontiguousarray(np.asarray(inputs["ssm_a_log"], f32).reshape(1, 64))
        shared["ssm_dt_bias"] = np.ascontiguousarray(np.asarray(inputs["ssm_dt_bias"], f32).reshape(1, 64))
        shared["ssm_d"] = np.ascontiguousarray(np.asarray(inputs["ssm_d"], f32).reshape(1, 32))
    maps = []
    for b in range(ncores):
        m = dict(shared)
        m["x_in"] = np.ascontiguousarray(np.concatenate([inputs["ctx"][b], inputs["x"][b]], axis=0), f32)
        cond = np.stack([fm(inputs["c"][b]), fm(inputs["c_ctx"])], axis=-1)
        m["cond"] = np.ascontiguousarray(cond, f32)
        maps.append(m)
    return maps


def kernel(**inputs):
    prog = Prog(FULL_STEPS)
    nc = prog.build()
    maps = make_in_maps(inputs)
    maps = [{k: v for k, v in m.items() if k in prog.din} for m in maps]
    res = run_bass_kernel_spmd(nc, maps, core_ids=list(range(8)))
    return np.stack([r["y"] for r in res.results], axis=0).astype(np.float32)
```

```python
import math
import os
from contextlib import ExitStack

import numpy as np
import concourse.bass as bass
import concourse.mybir as mybir
from concourse.bass_utils import run_bass_kernel_spmd

F32 = mybir.dt.float32
BF16 = mybir.dt.bfloat16
I32 = mybir.dt.int32
AF = mybir.ActivationFunctionType
ALU = mybir.AluOpType
AX = mybir.AxisListType

D = 1024
NCTX = 256
NLAT = 2048
T = NCTX + NLAT
NT = T // 128
BLKS = [(0, 256), (256, 512), (768, 512), (1280, 512), (1792, 512)]
EPS = 1e-6
NE = 32
KMOE_NE = int(os.environ.get("KMOE_NE", NE))


class Res:
    __slots__ = ("name", "w", "r", "dsem", "dcnt")

    def __init__(self, name):
        self.name = name
        self.w = None
        self.r = []
        self.dsem = None
        self.dcnt = 0


class Sched:
    CE = ("pe", "act", "dve", "pool")
    ALLE = ("pe", "act", "dve", "pool", "sp")

    def __init__(self, nc, es):
        self.nc = nc
        self.es = es
        self.ops = {e: [] for e in self.ALLE}
        self.sems = {}
        for e in self.CE:
            self.sems["c_" + e] = es.enter_context(nc.semaphore("c_" + e))
        self.cnt = {e: 0 for e in self.CE}
        self.waited = {e: {} for e in self.ALLE}
        self.dtot = {}
        self.free_dsems = []
        self.ndsem = 0

    def _dsem(self, res):
        if res.dsem is None:
            if self.free_dsems:
                k = self.free_dsems.pop()
            else:
                k = "d%d" % self.ndsem
                self.ndsem += 1
                self.sems[k] = self.es.enter_context(self.nc.semaphore(k))
                self.dtot[k] = 0
            res.dsem = k
        return res.dsem

    def release(self, ress):
        for r in ress:
            if r.dsem is not None:
                self.free_dsems.append(r.dsem)
                r.dsem = None

    def _collect(self, e, reads, writes, pe_acc=False, skip_key=None):
        deps = {}

        def add(ev):
            if ev is None:
                return
            k, v = ev
            if deps.get(k, 0) < v:
                deps[k] = v
        for r in reads:
            add(r.w)
        for w in writes:
            if not (pe_acc and w.w is not None and w.w[0] == "c_pe") and not (
                    skip_key is not None and w.w is not None and w.w[0] == skip_key):
                add(w.w)
            for ev in w.r:
                add(ev)
        out = []
        wd = self.waited[e]
        for k, v in deps.items():
            if wd.get(k, 0) >= v:
                continue
            wd[k] = v
            out.append((k, v))
        return out

    def op(self, e, fn, reads=(), writes=(), pe_acc=False):
        waits = self._collect(e, reads, writes, pe_acc)
        self.cnt[e] += 1
        ev = ("c_" + e, self.cnt[e])
        for r in reads:
            r.r.append(ev)
        for w in writes:
            w.w = ev
            w.r = []
        self.ops[e].append((waits, fn, ev[0], 1))
        return ev

    def dma(self, q, out, in_, reads=(), writes=(), key=None, **kw):
        k = self._dsem(key)
        waits = self._collect(q, reads, writes, skip_key=k)
        self.dtot[k] += 16
        ev = (k, self.dtot[k])
        for r in reads:
            r.r.append(ev)
        for w in writes:
            w.w = ev
            w.r = []
        self.ops[q].append((waits, lambda eng: eng.dma_start(out=out, in_=in_, **kw), k, 16))
        return ev

    def idma(self, out, out_off, in_, in_off, bound, reads=(), writes=(), key=None):
        k = self._dsem(key)
        waits = self._collect("pool", reads, writes, skip_key=k)
        self.dtot[k] += 16
        ev = (k, self.dtot[k])
        for r in reads:
            r.r.append(ev)
        for w in writes:
            w.w = ev
            w.r = []
        oo = None if out_off is None else bass.IndirectOffsetOnAxis(ap=out_off, axis=0)
        io = None if in_off is None else bass.IndirectOffsetOnAxis(ap=in_off, axis=0)
        self.ops["pool"].append((waits, lambda eng: eng.indirect_dma_start(
            out=out, out_offset=oo, in_=in_, in_offset=io), k, 16))
        return ev

    def barrier(self):
        allev = [("c_" + e, self.cnt[e]) for e in self.CE if self.cnt[e] > 0]
        allev += [(k, v) for k, v in self.dtot.items() if v > 0]
        for e in self.ALLE:
            wd = self.waited[e]
            waits = []
            for k, v in allev:
                if wd.get(k, 0) < v:
                    wd[k] = v
                    waits.append((k, v))
            if waits:
                self.ops[e].append((waits, None, None, 0))

    def replay(self):
        nc = self.nc
        sems = self.sems
        ops = self.ops

        def run(eng, lst):
            for waits, fn, sk, n in lst:
                for k, v in waits:
                    eng.wait_ge(sems[k], v)
                if fn is not None:
                    fn(eng).then_inc(sems[sk], n)
        with nc.Block() as block:
            @block.sync
            def _(e):
                run(e, ops["sp"])

            @block.tensor
            def _(e):
                run(e, ops["pe"])

            @block.scalar
            def _(e):
                run(e, ops["act"])

            @block.vector
            def _(e):
                run(e, ops["dve"])

            @block.gpsimd
            def _(e):
                run(e, ops["pool"])


class Mem:
    def __init__(self, nc):
        self.nc = nc
        self.n = 0

    def sb(self, es, shape, dt, name=None):
        self.n += 1
        name = (name or "sb") + "_%d" % self.n
        t = es.enter_context(self.nc.sbuf_tensor(name, list(shape), dt))
        return t, Res(name)

    def ps(self, es, shape, dt, name=None):
        self.n += 1
        name = (name or "ps") + "_%d" % self.n
        t = es.enter_context(self.nc.psum_tensor(name, list(shape), dt))
        return t, Res(name)


class Ring:
    def __init__(self, items):
        self.items = items
        self.i = 0

    def next(self):
        it = self.items[self.i % len(self.items)]
        self.i += 1
        return it


def fm(v):
    v = np.asarray(v, np.float32)
    return np.ascontiguousarray(v.reshape(-1, 128).T)


def rope_tables():
    t = np.arange(NLAT)
    row = (t // 64).astype(np.float32)
    col = (t % 64).astype(np.float32)
    quarter = 16
    inv = (10000.0 ** (-np.arange(quarter, dtype=np.float32) / quarter)).astype(np.float32)
    ang = np.concatenate([row[:, None] * inv, col[:, None] * inv], axis=-1).astype(np.float32)
    cos = np.cos(ang).T.astype(np.float32)
    sin = np.sin(ang).T.astype(np.float32)
    C = np.ones((64, T), np.float32)
    S = np.zeros((64, T), np.float32)
    C[0:32, NCTX:] = cos
    C[32:64, NCTX:] = cos
    S[0:32, NCTX:] = -sin
    S[32:64, NCTX:] = sin
    return C, S


class Prog:
    def __init__(self, steps, raw_out=False):
        self.steps = steps
        self.raw_out = raw_out
        self.dbg = int(os.environ.get("KDBG", "0"))
        self.nc = bass.Bass("TRN2", target_bir_lowering=False)
        self.din = {}

    def inp(self, name, shape, dt=F32):
        if name not in self.din:
            self.din[name] = self.nc.dram_tensor(name, list(shape), dt, kind="ExternalInput").ap()
        return self.din[name]

    def build(self):
        nc = self.nc
        with ExitStack() as es:
            self.S = S = Sched(nc, es)
            self.M = M = Mem(nc)
            self.es = es
            self.xT, self.xTr = M.sb(es, [128, 8, T], F32, "xT")
            self.identf, self.identf_r = M.sb(es, [128, 128], F32, "identf")
            self.identb, self.identb_r = M.sb(es, [128, 128], BF16, "identb")
            self.onesf, self.onesf_r = M.sb(es, [128, 128], F32, "onesf")
            self.condT, self.condT_r = M.sb(es, [128, 8, 2], F32, "condT")
            self.mv, self.mv_r = M.sb(es, [128, 2, 6, 8], F32, "mv")
            self.epsb, self.epsb_r = M.sb(es, [128, 1], F32, "epsb")
            self.onesb, self.onesb_r = M.sb(es, [128, 128], BF16, "onesb")
            self.setup()
            for st in self.steps:
                kind = st[0]
                if kind == "mods":
                    self.mods(st[1])
                elif kind == "ffn":
                    self.ffn(st[1], with_ctx=st[2])
                elif kind == "mixer":
                    getattr(self, "mixer%d" % st[1])(st[1])
                S.barrier()
            if self.raw_out:
                self.out_raw()
            else:
                self.out_final()
            S.barrier()
            S.replay()
        return nc

    def setup(self):
        nc, S, M = self.nc, self.S, self.M
        x_in = self.inp("x_in", [T, D])
        cond = self.inp("cond", [128, 8, 2])
        identf, ifr = self.identf, self.identf_r
        S.op("pool", lambda e: e.memset(identf[:], 1.0), writes=[ifr])
        S.op("pool", lambda e: e.affine_select(out=identf[:], in_=identf[:], pattern=[[-1, 128]],
                                               compare_op=ALU.is_equal, fill=0.0, base=0, channel_multiplier=1),
             reads=[ifr], writes=[ifr])
        S.op("dve", lambda e: e.tensor_copy(out=self.identb[:], in_=identf[:]), reads=[ifr], writes=[self.identb_r])
        S.op("pool", lambda e: e.memset(self.onesf[:], 1.0), writes=[self.onesf_r])
        S.op("pool", lambda e: e.memset(self.epsb[:], EPS), writes=[self.epsb_r])
        S.op("pool", lambda e: e.memset(self.onesb[:], 1.0), writes=[self.onesb_r])
        with ExitStack() as ps:
            craw, craw_r = M.sb(ps, [128, 8, 2], F32, "craw")
            S.dma("sp", craw[:], cond, writes=[craw_r], key=craw_r)
            S.op("act", lambda e: e.activation(out=self.condT[:], in_=craw[:], func=AF.Silu),
                 reads=[craw_r], writes=[self.condT_r])
            stg = Ring([M.sb(ps, [128, D], F32, "xstg") for _ in range(2)])
            pst = Ring([M.ps(ps, [128, 4, 128], F32, "xtp") for _ in range(2)])
            for t in range(NT):
                st, st_r = stg.next()
                S.dma("sp", st[:], x_in[t * 128:(t + 1) * 128, :], writes=[st_r], key=st_r)
                for h in range(2):
                    pt, pt_r = pst.next()
                    for k in range(4):
                        S.op("pe", lambda e, pt=pt, st=st, k=k, h=h: e.transpose(
                            out=pt[:, k, :], in_=st[:, (h * 4 + k) * 128:(h * 4 + k + 1) * 128], identity=identf[:]),
                            reads=[st_r, ifr], writes=[pt_r], pe_acc=True)
                    eng = "dve" if h == 0 else "act"
                    if eng == "dve":
                        S.op("dve", lambda e, pt=pt, h=h, t=t: e.tensor_copy(
                            out=self.xT[:, h * 4:(h + 1) * 4, t * 128:(t + 1) * 128], in_=pt[:]),
                            reads=[pt_r], writes=[self.xTr])
                    else:
                        S.op("act", lambda e, pt=pt, h=h, t=t: e.activation(
                            out=self.xT[:, h * 4:(h + 1) * 4, t * 128:(t + 1) * 128], in_=pt[:], func=AF.Copy),
                            reads=[pt_r], writes=[self.xTr])
            S.barrier()
            S.release([craw_r] + [r for _, r in stg.items])

    def mods(self, i):
        nc, S, M = self.nc, self.S, self.M
        ada_w = self.inp("ada_w", [4, D, 6 * D])
        lv = self.inp("lvec", [4, 128, 64])
        with ExitStack() as ps:
            lvt, lvt_r = M.sb(ps, [128, 64], F32, "lvt")
            S.dma("sp", lvt[:], lv[i], writes=[lvt_r], key=lvt_r)
            wring = Ring([M.sb(ps, [128, 8, 512], F32, "adaw") for _ in range(2)])
            pm, pm_r = M.ps(ps, [128, 48, 2], F32, "pm")
            md, md_r = M.sb(ps, [128, 2, 48], F32, "md")
            for pi in range(12):
                wt, wt_r = wring.next()
                S.dma("sp", wt[:], ada_w[i, :, pi * 512:(pi + 1) * 512].rearrange("(k p) n -> p k n", p=128),
                      writes=[wt_r], key=wt_r)
                for o4 in range(4):
                    ob = pi * 4 + o4
                    for k in range(8):
                        S.op("pe", lambda e, wt=wt, k=k, o4=o4, ob=ob: e.matmul(
                            pm[:, ob, :], lhsT=wt[:, k, o4 * 128:(o4 + 1) * 128], rhs=self.condT[:, k, :],
                            start=(k == 0), stop=(k == 7)),
                            reads=[wt_r, self.condT_r], writes=[pm_r], pe_acc=True)
            for c in range(2):
                S.op("dve", lambda e, c=c: e.tensor_tensor(out=md[:, c, :], in0=pm[:, :, c], in1=lvt[:, 0:48], op=ALU.add),
                     reads=[pm_r, lvt_r], writes=[md_r])
            mv, mv_r = self.mv, self.mv_r
            for c in range(2):
                S.op("dve", lambda e, c=c: e.scalar_tensor_tensor(out=mv[:, c, 0, :], in0=md[:, c, 8:16], scalar=1.0,
                                                                  in1=lvt[:, 48:56], op0=ALU.add, op1=ALU.mult),
                     reads=[md_r, lvt_r], writes=[mv_r])
                S.op("dve", lambda e, c=c: e.tensor_copy(out=mv[:, c, 1, :], in_=md[:, c, 0:8]), reads=[md_r], writes=[mv_r])
                S.op("dve", lambda e, c=c: e.tensor_copy(out=mv[:, c, 2, :], in_=md[:, c, 16:24]), reads=[md_r], writes=[mv_r])
                S.op("dve", lambda e, c=c: e.scalar_tensor_tensor(out=mv[:, c, 3, :], in0=md[:, c, 32:40], scalar=1.0,
                                                                  in1=lvt[:, 56:64], op0=ALU.add, op1=ALU.mult),
                     reads=[md_r, lvt_r], writes=[mv_r])
                S.op("dve", lambda e, c=c: e.tensor_copy(out=mv[:, c, 4, :], in_=md[:, c, 24:32]), reads=[md_r], writes=[mv_r])
                S.op("dve", lambda e, c=c: e.tensor_copy(out=mv[:, c, 5, :], in_=md[:, c, 40:48]), reads=[md_r], writes=[mv_r])
            S.barrier()
            S.release([lvt_r] + [r for _, r in wring.items])

    def adanorm(self, ps, hT, hT_r, slotA, with_ctx=True, router=None):
        nc, S, M = self.nc, self.S, self.M
        xT, xTr = self.xT, self.xTr
        sq = Ring([M.sb(ps, [128, 512], F32, "nsq") for _ in range(2)])
        if router is not None:
            h32 = Ring([M.sb(ps, [128, 8, 512], F32, "nh32") for _ in range(1)])
        pss = Ring([M.ps(ps, [128, 512], F32, "nss") for _ in range(2)])
        rst = Ring([M.sb(ps, [128, 512], F32, "nrstd") for _ in range(2)])
        tmp = Ring([M.sb(ps, [128, 512], F32, "ntmp") for _ in range(2)])
        if router is not None:
            plg = Ring([M.ps(ps, [128, 32], F32, "rlg") for _ in range(2)])
            pgt = Ring([M.ps(ps, [32, 128], F32, "rgt") for _ in range(2)])
            rt = Ring([[M.sb(ps, [128, 32], F32, "rt%d" % j) for j in range(4)] for _ in range(2)])
            rs = Ring([[M.sb(ps, [128, 8], F32, "rs%d" % j) for j in range(4)] for _ in range(2)])
        for (s, n) in BLKS:
            if s == 0 and not with_ctx:
                continue
            c = 1 if s == 0 else 0
            A = self.mv[:, c, slotA, :]
            B = self.mv[:, c, slotA + 1, :]
            pp, pp_r = pss.next()
            for k in range(8):
                q, q_r = sq.next()
                S.op("act", lambda e, q=q, s=s, n=n, k=k: e.activation(out=q[:, :n], in_=xT[:, k, s:s + n], func=AF.Square),
                     reads=[xTr], writes=[q_r])
                S.op("pe", lambda e, pp=pp, q=q, k=k, n=n: e.matmul(pp[:, :n], lhsT=self.onesf[:], rhs=q[:, :n],
                                                                    start=(k == 0), stop=(k == 7)),
                     reads=[q_r, self.onesf_r], writes=[pp_r], pe_acc=True)
            r, r_r = rst.next()
            S.op("act", lambda e, r=r, pp=pp, n=n: e.activation(out=r[:, :n], in_=pp[:, :n], func=AF.Sqrt, scale=1.0 / D,
                                                                bias=self.epsb[:, 0:1]),
                 reads=[pp_r, self.epsb_r], writes=[r_r])
            S.op("dve", lambda e, r=r, n=n: e.reciprocal(out=r[:, :n], in_=r[:, :n]), reads=[r_r], writes=[r_r])
            if router is not None:
                hh, hh_r = h32.next()
            for k in range(8):
                t_, t_r = tmp.next()
                S.op("dve", lambda e, t_=t_, k=k, s=s, n=n, r=r: e.tensor_tensor(out=t_[:, :n], in0=xT[:, k, s:s + n], in1=r[:, :n],
                                                                                 op=ALU.mult),
                     reads=[xTr, r_r], writes=[t_r])
                if router is not None:
                    S.op("act", lambda e, t_=t_, hh=hh, k=k, n=n, A=A, B=B: e.activation(
                        out=hh[:, k, :n], in_=t_[:, :n], func=AF.Identity, scale=A[:, k:k + 1], bias=B[:, k:k + 1]),
                        reads=[t_r, self.mv_r], writes=[hh_r])
                    S.op("pool", lambda e, hh=hh, k=k, s=s, n=n: e.tensor_copy(out=hT[:, k, s:s + n], in_=hh[:, k, :n]),
                         reads=[hh_r], writes=[hT_r])
                else:
                    S.op("act", lambda e, t_=t_, k=k, s=s, n=n, A=A, B=B: e.activation(
                        out=hT[:, k, s:s + n], in_=t_[:, :n], func=AF.Identity, scale=A[:, k:k + 1], bias=B[:, k:k + 1]),
                        reads=[t_r, self.mv_r], writes=[hT_r])
            if router is not None:
                R = router
                for tt in range(n // 128):
                    lg, lg_r = plg.next()
                    for k in range(8):
                        S.op("pe", lambda e, lg=lg, hh=hh, k=k, tt=tt: e.matmul(
                            lg[:], lhsT=hh[:, k, tt * 128:(tt + 1) * 128], rhs=R["wr"][:, k, :], start=(k == 0), stop=(k == 7)),
                            reads=[hh_r, R["wr_r"]], writes=[lg_r], pe_acc=True)
                    (l, l_r), (ex, ex_r), (mk, mk_r), (gt, gt_r) = rt.next()
                    (m8, m8_r), (ng, ng_r), (sm, sm_r), (rc, rc_r) = rs.next()
                    S.op("dve", lambda e, l=l, lg=lg: e.tensor_tensor(out=l[:], in0=lg[:], in1=R["brb"][:], op=ALU.add),
                         reads=[lg_r, R["brb_r"]], writes=[l_r])
                    S.op("dve", lambda e, m8=m8, l=l: e.max(out=m8[:], in_=l[:]), reads=[l_r], writes=[m8_r])
                    S.op("dve", lambda e, ng=ng, m8=m8: e.tensor_scalar(out=ng[:, 0:1], in0=m8[:, 0:1], scalar1=-1.0, scalar2=None,
                                                                        op0=ALU.mult),
                         reads=[m8_r], writes=[ng_r])
                    S.op("act", lambda e, ex=ex, l=l, ng=ng: e.activation(out=ex[:], in_=l[:], func=AF.Exp, bias=ng[:, 0:1], scale=1.0),
                         reads=[l_r, ng_r], writes=[ex_r])
                    S.op("dve", lambda e, mk=mk, l=l, m8=m8: e.tensor_scalar(out=mk[:], in0=l[:], scalar1=m8[:, 3:4], scalar2=None,
                                                                            op0=ALU.is_ge),
                         reads=[l_r, m8_r], writes=[mk_r])
                    S.op("dve", lambda e, ex=ex, mk=mk: e.tensor_tensor(out=ex[:], in0=ex[:], in1=mk[:], op=ALU.mult),
                         reads=[ex_r, mk_r], writes=[ex_r])
                    S.op("dve", lambda e, sm=sm, ex=ex: e.reduce_sum(out=sm[:, 0:1], in_=ex[:], axis=AX.X), reads=[ex_r], writes=[sm_r])
                    S.op("dve", lambda e, rc=rc, sm=sm: e.reciprocal(out=rc[:, 0:1], in_=sm[:, 0:1]), reads=[sm_r], writes=[rc_r])
                    S.op("dve", lambda e, gt=gt, ex=ex, rc=rc: e.tensor_scalar(out=gt[:], in0=ex[:], scalar1=rc[:, 0:1], scalar2=None,
                                                                              op0=ALU.mult),
                         reads=[ex_r, rc_r], writes=[gt_r])
                    pg, pg_r = pgt.next()
                    S.op("pe", lambda e, pg=pg, gt=gt: e.transpose(out=pg[:], in_=gt[:], identity=self.identf[:]),
                         reads=[gt_r, self.identf_r], writes=[pg_r], pe_acc=True)
                    S.op("act", lambda e, pg=pg, s=s, tt=tt: e.activation(
                        out=R["gateT"][:, s + tt * 128:s + (tt + 1) * 128], in_=pg[:], func=AF.Copy),
                        reads=[pg_r], writes=[R["gateT_r"]])

    def ffn_dense(self, i, with_ctx=True):
        nc, S, M = self.nc, self.S, self.M
        xT, xTr = self.xT, self.xTr
        w_router = self.inp("moe_w_router", [4, D, NE])
        b_router = self.inp("moe_b_router", [4, NE])
        w_gu = self.inp("moe_w_gu", [4, KMOE_NE, D, 2 * D])
        w_dn = self.inp("moe_w_down", [4, KMOE_NE, D, D])
        b_gu = self.inp("moe_b_gu_fm", [4, 128, NE, 16])
        b_dn = self.inp("moe_b_down", [4, NE, D])
        blks = [b for b in BLKS if with_ctx or b[0] != 0]
        with ExitStack() as ps:
            hT, hT_r = M.sb(ps, [128, 8, T], BF16, "hT")
            gateT, gateT_r = M.sb(ps, [NE, T], F32, "gateT")
            bgu, bgu_r = M.sb(ps, [128, NE, 16], F32, "bgu")
            bdn, bdn_r = M.sb(ps, [NE, D], F32, "bdn")
            S.dma("sp", bgu[:], b_gu[i], writes=[bgu_r], key=bgu_r)
            S.op("dve", lambda e: e.tensor_scalar(out=bgu[:, :, 8:16], in0=bgu[:, :, 8:16], scalar1=1.0, scalar2=None, op0=ALU.add),
                 reads=[bgu_r], writes=[bgu_r])
            S.dma("sp", bdn[:], b_dn[i], writes=[bdn_r], key=bdn_r)
            with ExitStack() as ps2:
                wr, wr_r = M.sb(ps2, [128, 8, NE], F32, "wr")
                brb, brb_r = M.sb(ps2, [128, NE], F32, "brb")
                S.dma("sp", wr[:], w_router[i].rearrange("(k p) n -> p k n", p=128), writes=[wr_r], key=wr_r)
                S.dma("sp", brb[:], b_router[i].partition_broadcast(128), writes=[brb_r], key=brb_r)
                self.adanorm(ps2, hT, hT_r, 3, with_ctx=with_ctx,
                             router=dict(wr=wr, wr_r=wr_r, brb=brb, brb_r=brb_r, gateT=gateT, gateT_r=gateT_r))
                S.barrier()
                S.release([wr_r, brb_r])
            wg = Ring([M.sb(ps, [128, 8, 2, 512], BF16, "wg") for _ in range(2)])
            wd = Ring([M.sb(ps, [128, 4, D], BF16, "wd") for _ in range(2)])
            pgl = Ring([M.ps(ps, [128, 2, 512], F32, "pgl") for _ in range(2)])
            pout = Ring([M.ps(ps, [128, 512], F32, "pout") for _ in range(2)])
            pgb = Ring([M.ps(ps, [128, 512], F32, "pgb") for _ in range(2)])
            gB = Ring([M.sb(ps, [128, 512], F32, "gB") for _ in range(2)])
            mg = Ring([M.sb(ps, [NE, 512], F32, "mg") for _ in range(2)])
            aT = Ring([M.sb(ps, [128, 4, 512], BF16, "aT") for _ in range(2)])
            tg = Ring([M.sb(ps, [128, 512], F32, "tg") for _ in range(2)])
            tsg = Ring([M.sb(ps, [128, 512], F32, "tsg") for _ in range(2)])
            tl = Ring([M.sb(ps, [128, 512], F32, "tl") for _ in range(2)])
            for (s, n) in blks:
                c = 1 if s == 0 else 0
                for oc in range(8):
                    po, po_r = pout.next()
                    S.op("pe", lambda e, po=po, oc=oc, s=s, n=n: e.matmul(
                        po[:, :n], lhsT=bdn[:, oc * 128:(oc + 1) * 128], rhs=gateT[:, s:s + n], start=True, stop=True),
                        reads=[bdn_r, gateT_r], writes=[po_r], pe_acc=True)
                    S.op("dve", lambda e, po=po, oc=oc, s=s, n=n, c=c: e.scalar_tensor_tensor(
                        out=xT[:, oc, s:s + n], in0=po[:, :n], scalar=self.mv[:, c, 5, oc:oc + 1], in1=xT[:, oc, s:s + n],
                        op0=ALU.mult, op1=ALU.add),
                        reads=[po_r, self.mv_r, xTr], writes=[xTr])
            pieces = [(ex, half) for ex in range(KMOE_NE) for half in range(2)]
            loaded = {}

            def load_piece(idx):
                ex, half = pieces[idx]
                wgt, wg_r = wg.next()
                wdt, wd_r = wd.next()
                for gl in range(2):
                    S.dma("pool", wgt[:, :, gl, :],
                          w_gu[i, ex, :, gl * D + half * 512: gl * D + half * 512 + 512].rearrange("(k p) n -> p k n", p=128),
                          writes=[wg_r], key=wg_r)
                S.dma("pool", wdt[:], w_dn[i, ex, half * 512:(half + 1) * 512, :].rearrange("(j p) n -> p j n", p=128),
                      writes=[wd_r], key=wd_r)
                loaded[idx] = (wgt, wg_r, wdt, wd_r)

            load_piece(0)
            for idx in range(len(pieces)):
                if True:
                    ex, half = pieces[idx]
                    if idx + 1 < len(pieces):
                        load_piece(idx + 1)
                    wgt, wg_r, wdt, wd_r = loaded.pop(idx)
                    for (s, n) in blks:
                        c = 1 if s == 0 else 0
                        pb, pb_r = pgb.next()
                        mg_, mg_r = mg.next()
                        S.op("act", lambda e, mg_=mg_, ex=ex, s=s, n=n: e.activation(
                            out=mg_[:, :n], in_=gateT[:, s:s + n], func=AF.Copy, scale=self.identf[0:NE, ex:ex + 1]),
                            reads=[gateT_r, self.identf_r], writes=[mg_r])
                        S.op("pe", lambda e, pb=pb, mg_=mg_, n=n: e.matmul(
                            pb[:, :n], lhsT=self.onesf[0:NE, :], rhs=mg_[:, :n], start=True, stop=True),
                            reads=[mg_r, self.onesf_r], writes=[pb_r], pe_acc=True)
                        g_, g_r = gB.next()
                        S.op("act", lambda e, g_=g_, pb=pb, n=n: e.activation(out=g_[:, :n], in_=pb[:, :n], func=AF.Copy),
                             reads=[pb_r], writes=[g_r])
                        a_, a_r = aT.next()
                        for jj in range(4):
                            j = half * 4 + jj
                            pg, pg_r = pgl.next()
                            for gl in range(2):
                                for k in range(8):
                                    S.op("pe", lambda e, pg=pg, gl=gl, k=k, jj=jj, s=s, n=n, wgt=wgt: e.matmul(
                                        pg[:, gl, :n], lhsT=wgt[:, k, gl, jj * 128:(jj + 1) * 128], rhs=hT[:, k, s:s + n],
                                        start=(k == 0), stop=(k == 7)),
                                        reads=[wg_r, hT_r], writes=[pg_r], pe_acc=True)
                            t1, t1_r = tg.next()
                            S.op("dve", lambda e, t1=t1, pg=pg, n=n, ex=ex, j=j: e.tensor_scalar(
                                out=t1[:, :n], in0=pg[:, 0, :n], scalar1=bgu[:, ex, j:j + 1], scalar2=7.0, op0=ALU.add, op1=ALU.min),
                                reads=[pg_r, bgu_r], writes=[t1_r])
                            t2, t2_r = tsg.next()
                            S.op("act", lambda e, t2=t2, t1=t1, n=n: e.activation(out=t2[:, :n], in_=t1[:, :n], func=AF.Sigmoid, scale=1.702),
                                 reads=[t1_r], writes=[t2_r])
                            t3, t3_r = tl.next()
                            S.op("dve", lambda e, t3=t3, pg=pg, n=n, ex=ex, j=j: e.tensor_scalar(
                                out=t3[:, :n], in0=pg[:, 1, :n], scalar1=bgu[:, ex, 8 + j:9 + j], scalar2=-6.0, op0=ALU.add, op1=ALU.max),
                                reads=[pg_r, bgu_r], writes=[t3_r])
                            S.op("pool", lambda e, t1=t1, t2=t2, n=n: e.tensor_tensor(out=t1[:, :n], in0=t1[:, :n], in1=t2[:, :n], op=ALU.mult),
                                 reads=[t1_r, t2_r], writes=[t1_r])
                            S.op("dve", lambda e, t1=t1, t3=t3, n=n: e.scalar_tensor_tensor(
                                out=t3[:, :n], in0=t3[:, :n], scalar=8.0, in1=t1[:, :n], op0=ALU.min, op1=ALU.mult),
                                reads=[t1_r, t3_r], writes=[t3_r])
                            S.op("pool", lambda e, a_=a_, t3=t3, g_=g_, jj=jj, n=n: e.tensor_tensor(out=a_[:, jj, :n], in0=t3[:, :n], in1=g_[:, :n], op=ALU.mult),
                                 reads=[t3_r, g_r], writes=[a_r])
                        for oc in range(8):
                            po, po_r = pout.next()
                            for jj in range(4):
                                S.op("pe", lambda e, po=po, oc=oc, jj=jj, n=n, wdt=wdt, a_=a_: e.matmul(
                                    po[:, :n], lhsT=wdt[:, jj, oc * 128:(oc + 1) * 128], rhs=a_[:, jj, :n],
                                    start=(jj == 0), stop=(jj == 3)),
                                    reads=[wd_r, a_r], writes=[po_r], pe_acc=True)
                            S.op("dve", lambda e, po=po, oc=oc, s=s, n=n, c=c: e.scalar_tensor_tensor(
                                out=xT[:, oc, s:s + n], in0=po[:, :n], scalar=self.mv[:, c, 5, oc:oc + 1], in1=xT[:, oc, s:s + n],
                                op0=ALU.mult, op1=ALU.add),
                                reads=[po_r, self.mv_r, xTr], writes=[xTr])
            S.barrier()
            S.release([bgu_r, bdn_r] + [r for _, r in wg.items] + [r for _, r in wd.items])

    def ffn(self, i, with_ctx=True):
        nc, S, M = self.nc, self.S, self.M
        xT, xTr = self.xT, self.xTr
        w_router = self.inp("moe_w_router", [4, D, NE])
        b_router = self.inp("moe_b_router", [4, NE])
        w_gu = self.inp("moe_w_gu2d", [4 * NE * D, 2 * D])
        w_dn = self.inp("moe_w_down2d", [4 * NE * D, D])
        b_gu = self.inp("moe_b_gu_rows", [4 * NE * 128, 16])
        b_dn = self.inp("moe_b_down2d", [4 * NE, D])
        tiles = list(range(NT)) if with_ctx else list(range(2, NT))
        t0 = tiles[0]
        ntl = len(tiles)
        NB = ntl + NE
        NBMAX = NT + NE
        blks = [b for b in BLKS if with_ctx or b[0] != 0]
        if not hasattr(self, "XE"):
            self.XE = nc.dram_tensor("scr_xe", [NBMAX * 512, D], BF16, kind="Internal").ap()
            self.YE = nc.dram_tensor("scr_ye", [NBMAX * 512, D], F32, kind="Internal").ap()
            self.XE_r, self.YE_r = Res("XE"), Res("YE")
            first = True
        else:
            first = False
        XE, YE, XE_r, YE_r = self.XE, self.YE, self.XE_r, self.YE_r
        with ExitStack() as pA:
            g4, g4_r = M.sb(pA, [128, NT, 4], F32, "g4")
            IDXi, IDXi_r = M.sb(pA, [128, NT * 4], I32, "IDXi")
            idxw, idxw_r = M.sb(pA, [128, NBMAX, 8], I32, "idxw")
            idxb, idxb_r = M.sb(pA, [128, NBMAX], I32, "idxb")
            idxd, idxd_r = M.sb(pA, [128, NBMAX], I32, "idxd")
            if first:
                with ExitStack() as pz:
                    z, z_r = M.sb(pz, [128, 4, D], BF16, "zero")
                    S.op("pool", lambda e: e.memset(z[:], 0.0), writes=[z_r])
                    for b in range(NBMAX):
                        S.dma("sp", XE[b * 512:(b + 1) * 512, :].rearrange("(s p) f -> p s f", p=128), z[:],
                              reads=[z_r], writes=[XE_r], key=z_r)
                    S.barrier()
                    S.release([z_r])
            with ExitStack() as pH:
                hTM, hTM_r = M.sb(pH, [128, NT, D], BF16, "hTM")
                lgs, lgs_r = M.sb(pH, [128, NT, NE], F32, "lgs")
                m8s, m8s_r = M.sb(pH, [128, NT, 8], F32, "m8s")
                MK, MK_r = M.sb(pH, [128, NT, NE], F32, "MK")
                with ExitStack() as pR:
                    wr, wr_r = M.sb(pR, [128, 8, NE], F32, "wr")
                    brb, brb_r = M.sb(pR, [128, NE], F32, "brb")
                    S.dma("sp", wr[:], w_router[i].rearrange("(k p) n -> p k n", p=128), writes=[wr_r], key=wr_r)
                    S.dma("sp", brb[:], b_router[i].partition_broadcast(128), writes=[brb_r], key=brb_r)
                    sq = Ring([M.sb(pR, [128, 512], F32, "nsq") for _ in range(2)])
                    h32, h32_r = M.sb(pR, [128, 8, 512], F32, "nh32")
                    pss = Ring([M.ps(pR, [128, 512], F32, "nss") for _ in range(2)])
                    rst = Ring([M.sb(pR, [128, 512], F32, "nrstd") for _ in range(2)])
                    tmp = Ring([M.sb(pR, [128, 512], F32, "ntmp") for _ in range(2)])
                    plg = Ring([M.ps(pR, [128, NE], F32, "rlg") for _ in range(2)])
                    ptk = Ring([M.ps(pR, [128, 4, 128], F32, "ptk") for _ in range(2)])
                    rsm = Ring([M.sb(pR, [128, 8], F32, "rsm") for _ in range(2)])
                    for (s, n) in blks:
                        c = 1 if s == 0 else 0
                        A = self.mv[:, c, 3, :]
                        B = self.mv[:, c, 4, :]
                        pp, pp_r = pss.next()
                        for k in range(8):
                            q, q_r = sq.next()
                            S.op("act", lambda e, q=q, s=s, n=n, k=k: e.activation(out=q[:, :n], in_=xT[:, k, s:s + n], func=AF.Square),
                                 reads=[xTr], writes=[q_r])
                            S.op("pe", lambda e, pp=pp, q=q, k=k, n=n: e.matmul(pp[:, :n], lhsT=self.onesf[:], rhs=q[:, :n],
                                                                                start=(k == 0), stop=(k == 7)),
                                 reads=[q_r, self.onesf_r], writes=[pp_r], pe_acc=True)
                        r, r_r = rst.next()
                        S.op("act", lambda e, r=r, pp=pp, n=n: e.activation(out=r[:, :n], in_=pp[:, :n], func=AF.Sqrt, scale=1.0 / D,
                                                                            bias=self.epsb[:, 0:1]),
                             reads=[pp_r, self.epsb_r], writes=[r_r])
                        S.op("dve", lambda e, r=r, n=n: e.reciprocal(out=r[:, :n], in_=r[:, :n]), reads=[r_r], writes=[r_r])
                        for k in range(8):
                            t_, t_r = tmp.next()
                            S.op("dve", lambda e, t_=t_, k=k, s=s, n=n, r=r: e.tensor_tensor(out=t_[:, :n], in0=xT[:, k, s:s + n], in1=r[:, :n],
                                                                                             op=ALU.mult),
                                 reads=[xTr, r_r], writes=[t_r])
                            S.op("act", lambda e, t_=t_, k=k, n=n, A=A, B=B: e.activation(
                                out=h32[:, k, :n], in_=t_[:, :n], func=AF.Identity, scale=A[:, k:k + 1], bias=B[:, k:k + 1]),
                                reads=[t_r, self.mv_r], writes=[h32_r])
                        for tt in range(n // 128):
                            t = s // 128 + tt
                            lg, lg_r = plg.next()
                            for k in range(8):
                                S.op("pe", lambda e, lg=lg, k=k, tt=tt: e.matmul(
                                    lg[:], lhsT=h32[:, k, tt * 128:(tt + 1) * 128], rhs=wr[:, k, :], start=(k == 0), stop=(k == 7)),
                                    reads=[h32_r, wr_r], writes=[lg_r], pe_acc=True)
                            sm, sm_r = rsm.next()
                            S.op("dve", lambda e, lg=lg, t=t: e.tensor_tensor(out=lgs[:, t, :], in0=lg[:], in1=brb[:], op=ALU.add),
                                 reads=[lg_r, brb_r], writes=[lgs_r])
                            S.op("dve", lambda e, t=t: e.max(out=m8s[:, t, :], in_=lgs[:, t, :]), reads=[lgs_r], writes=[m8s_r])
                            S.op("dve", lambda e, t=t: e.tensor_scalar(out=MK[:, t, :], in0=lgs[:, t, :], scalar1=m8s[:, t, 3:4], scalar2=None,
                                                                       op0=ALU.is_ge), reads=[lgs_r, m8s_r], writes=[MK_r])
                            S.op("dve", lambda e, sm=sm, t=t: e.tensor_scalar(out=sm[:, 0:1], in0=m8s[:, t, 0:1], scalar1=-1.0, scalar2=None,
                                                                            op0=ALU.mult), reads=[m8s_r], writes=[sm_r])
                            S.op("act", lambda e, sm=sm, t=t: e.activation(out=sm[:, 4:8], in_=m8s[:, t, 0:4], func=AF.Exp, bias=sm[:, 0:1], scale=1.0),
                                 reads=[m8s_r, sm_r], writes=[sm_r])
                            S.op("dve", lambda e, sm=sm: e.reduce_sum(out=sm[:, 1:2], in_=sm[:, 4:8], axis=AX.X), reads=[sm_r], writes=[sm_r])
                            S.op("dve", lambda e, sm=sm: e.reciprocal(out=sm[:, 2:3], in_=sm[:, 1:2]), reads=[sm_r], writes=[sm_r])
                            S.op("dve", lambda e, sm=sm, t=t: e.tensor_scalar(out=g4[:, t, :], in0=sm[:, 4:8], scalar1=sm[:, 2:3], scalar2=None,
                                                                            op0=ALU.mult), reads=[sm_r], writes=[g4_r])
                            for hf in range(2):
                                pk, pk_r = ptk.next()
                                for kk in range(4):
                                    k = hf * 4 + kk
                                    S.op("pe", lambda e, pk=pk, kk=kk, k=k, tt=tt: e.transpose(
                                        out=pk[:, kk, :], in_=h32[:, k, tt * 128:(tt + 1) * 128], identity=self.identf[:]),
                                        reads=[h32_r, self.identf_r], writes=[pk_r], pe_acc=True)
                                if hf == 0:
                                    S.op("act", lambda e, pk=pk, t=t: e.activation(out=hTM[:, t, 0:512], in_=pk[:].rearrange("p a b -> p (a b)"), func=AF.Copy),
                                         reads=[pk_r], writes=[hTM_r])
                                else:
                                    S.op("dve", lambda e, pk=pk, t=t: e.tensor_copy(out=hTM[:, t, 512:1024], in_=pk[:].rearrange("p a b -> p (a b)")),
                                         reads=[pk_r], writes=[hTM_r])
                    S.barrier()
                    S.release([wr_r, brb_r])
                with ExitStack() as pI:
                    f = lambda name, shape: M.sb(pI, shape, F32, name)
                    msum, msum_r = f("msum", [128, NE])
                    cnt, cnt_r = f("cnt", [128, NE])
                    nb, nb_r = f("nb", [128, NE])
                    sz, sz_r = f("sz", [128, NE])
                    sa, sa_r = f("sa", [128, NE])
                    sbb, sbb_r = f("sbb", [128, NE])
                    offX, offX_r = f("offX", [128, NE])
                    run, run_r = f("run", [128, NE])
                    mlt, mlt_r = f("mlt", [128, 128])
                    IDXf, IDXf_r = f("IDXf", [128, NT * 4])
                    ebf, ebf_r = f("ebf", [128, NBMAX])
                    eb2, eb2_r = f("eb2", [128, NBMAX])
                    kp, kp_r = f("kp", [128, 8])
                    pc, pc_r = f("pc", [128, 1])
                    iwf, iwf_r = f("iwf", [128, NBMAX, 8])
                    slr = Ring([f("slot", [128, NE]) for _ in range(2)])
                    ohr = Ring([f("oh", [128, NE]) for _ in range(2)])
                    cmr = Ring([f("cmp", [128, NE]) for _ in range(2)])
                    pcn, pcn_r = M.ps(pI, [128, NE], F32, "pcn")
                    ppr = Ring([M.ps(pI, [128, NE], F32, "ppos") for _ in range(2)])
                    S.op("pool", lambda e: e.memset(mlt[:], 1.0), writes=[mlt_r])
                    S.op("pool", lambda e: e.affine_select(out=mlt[:], in_=mlt[:], pattern=[[1, 128]], compare_op=ALU.is_gt, fill=0.0,
                                                           base=0, channel_multiplier=-1), reads=[mlt_r], writes=[mlt_r])
                    S.op("pool", lambda e: e.iota(kp[:], pattern=[[128, 8]], base=i * NE * D, channel_multiplier=1, allow_small_or_imprecise_dtypes=True),
                         writes=[kp_r])
                    S.op("pool", lambda e: e.iota(pc[:], pattern=[[0, 1]], base=i * NE * 128, channel_multiplier=1, allow_small_or_imprecise_dtypes=True),
                         writes=[pc_r])
                    S.op("pool", lambda e: e.memset(IDXf[:], 0.0), writes=[IDXf_r])
                    S.op("pool", lambda e: e.memset(ebf[:], 31.0), writes=[ebf_r])
                    S.op("dve", lambda e: e.reduce_sum(out=msum[:], in_=MK[:, t0:NT, :].rearrange("p t e -> p e t"), axis=AX.X),
                         reads=[MK_r], writes=[msum_r])
                    S.op("pe", lambda e: e.matmul(pcn[:], lhsT=self.onesf[:], rhs=msum[:], start=True, stop=True),
                         reads=[self.onesf_r, msum_r], writes=[pcn_r], pe_acc=True)
                    S.op("act", lambda e: e.activation(out=cnt[:], in_=pcn[:], func=AF.Copy), reads=[pcn_r], writes=[cnt_r])
                    S.op("dve", lambda e: e.tensor_scalar(out=nb[:], in0=cnt[:], scalar1=0.0, scalar2=None, op0=ALU.is_gt), reads=[cnt_r], writes=[nb_r])
                    for m in range(1, 5):
                        S.op("dve", lambda e, m=m: e.scalar_tensor_tensor(out=nb[:], in0=cnt[:], scalar=512.0 * m, in1=nb[:], op0=ALU.is_gt, op1=ALU.add),
                             reads=[cnt_r, nb_r], writes=[nb_r])
                    S.op("dve", lambda e: e.tensor_scalar(out=sz[:], in0=nb[:], scalar1=512.0, scalar2=None, op0=ALU.mult), reads=[nb_r], writes=[sz_r])
                    S.op("dve", lambda e: e.tensor_copy(out=sa[:], in_=sz[:]), reads=[sz_r], writes=[sa_r])
                    cur, cur_r, oth, oth_r = sa, sa_r, sbb, sbb_r
                    for dd in (1, 2, 4, 8, 16):
                        S.op("dve", lambda e, cur=cur, oth=oth, dd=dd: e.tensor_copy(out=oth[:, 0:dd], in_=cur[:, 0:dd]), reads=[cur_r], writes=[oth_r])
                        S.op("dve", lambda e, cur=cur, oth=oth, dd=dd: e.tensor_tensor(out=oth[:, dd:NE], in0=cur[:, dd:NE], in1=cur[:, 0:NE - dd], op=ALU.add),
                             reads=[cur_r, oth_r], writes=[oth_r])
                        cur, cur_r, oth, oth_r = oth, oth_r, cur, cur_r
                    offE, offE_r = cur, cur_r
                    S.op("dve", lambda e: e.tensor_tensor(out=offX[:], in0=offE[:], in1=sz[:], op=ALU.subtract), reads=[offE_r, sz_r], writes=[offX_r])
                    S.op("pool", lambda e: e.memset(run[:], 0.0), writes=[run_r])
                    for t in tiles:
                        pp, pp_r = ppr.next()
                        S.op("pe", lambda e, pp=pp, t=t: e.matmul(pp[:], lhsT=mlt[:], rhs=MK[:, t, :], start=True, stop=False),
                             reads=[mlt_r, MK_r], writes=[pp_r], pe_acc=True)
                        S.op("pe", lambda e, pp=pp: e.matmul(pp[:], lhsT=self.onesf[:], rhs=run[:], start=False, stop=True),
                             reads=[self.onesf_r, run_r], writes=[pp_r], pe_acc=True)
                        sl, sl_r = slr.next()
                        S.op("dve", lambda e, sl=sl, pp=pp: e.tensor_tensor(out=sl[:], in0=pp[:], in1=offX[:], op=ALU.add),
                             reads=[pp_r, offX_r], writes=[sl_r])
                        S.op("dve", lambda e, t=t: e.tensor_tensor(out=run[:], in0=run[:], in1=MK[:, t, :], op=ALU.add),
                             reads=[run_r, MK_r], writes=[run_r])
                        for j in range(4):
                            oh, oh_r = ohr.next()
                            S.op("dve", lambda e, oh=oh, t=t, j=j: e.tensor_scalar(out=oh[:], in0=lgs[:, t, :], scalar1=m8s[:, t, j:j + 1], scalar2=None,
                                                                                 op0=ALU.is_equal), reads=[lgs_r, m8s_r], writes=[oh_r])
                            S.op("dve", lambda e, oh=oh, sl=sl: e.tensor_tensor(out=oh[:], in0=oh[:], in1=sl[:], op=ALU.mult),
                                 reads=[oh_r, sl_r], writes=[oh_r])
                            S.op("dve", lambda e, oh=oh, t=t, j=j: e.reduce_sum(out=IDXf[:, t * 4 + j:t * 4 + j + 1], in_=oh[:], axis=AX.X),
                                 reads=[oh_r], writes=[IDXf_r])
                    S.op("dve", lambda e: e.tensor_copy(out=IDXi[:], in_=IDXf[:]), reads=[IDXf_r], writes=[IDXi_r])
                    for b in range(NB):
                        cm, cm_r = cmr.next()
                        S.op("dve", lambda e, cm=cm, b=b: e.tensor_scalar(out=cm[:], in0=offE[:], scalar1=512.0 * b, scalar2=None, op0=ALU.is_le),
                             reads=[offE_r], writes=[cm_r])
                        S.op("dve", lambda e, cm=cm, b=b: e.reduce_sum(out=ebf[:, b:b + 1], in_=cm[:], axis=AX.X), reads=[cm_r], writes=[ebf_r])
                    S.op("dve", lambda e: e.tensor_scalar(out=ebf[:], in0=ebf[:], scalar1=31.0, scalar2=None, op0=ALU.min), reads=[ebf_r], writes=[ebf_r])
                    S.op("dve", lambda e: e.tensor_scalar(out=eb2[:], in0=ebf[:], scalar1=float(i * NE), scalar2=None, op0=ALU.add),
                         reads=[ebf_r], writes=[eb2_r])
                    S.op("dve", lambda e: e.tensor_copy(out=idxd[:], in_=eb2[:]), reads=[eb2_r], writes=[idxd_r])
                    S.op("dve", lambda e: e.tensor_scalar(out=eb2[:], in0=ebf[:], scalar1=128.0, scalar2=pc[:, 0:1], op0=ALU.mult, op1=ALU.add),
                         reads=[ebf_r, pc_r, idxd_r], writes=[eb2_r])
                    S.op("dve", lambda e: e.tensor_copy(out=idxb[:], in_=eb2[:]), reads=[eb2_r], writes=[idxb_r])
                    S.op("dve", lambda e: e.tensor_scalar(out=eb2[:], in0=ebf[:], scalar1=1024.0, scalar2=None, op0=ALU.mult),
                         reads=[ebf_r, idxb_r], writes=[eb2_r])
                    S.op("dve", lambda e: e.tensor_tensor(out=iwf[:], in0=eb2[:].unsqueeze(2).to_broadcast([128, NBMAX, 8]),
                                                          in1=kp[:].unsqueeze(1).to_broadcast([128, NBMAX, 8]), op=ALU.add),
                         reads=[eb2_r, kp_r], writes=[iwf_r])
                    S.op("dve", lambda e: e.tensor_copy(out=idxw[:], in_=iwf[:]), reads=[iwf_r], writes=[idxw_r])
                    S.barrier()
                for t in tiles:
                    for j in range(4):
                        S.idma(XE, IDXi[:, t * 4 + j:t * 4 + j + 1], hTM[:, t, :], None, NBMAX * 512 - 1,
                               reads=[hTM_r, IDXi_r], writes=[XE_r], key=hTM_r)
                S.barrier()
                S.release([hTM_r])
            with ExitStack() as pE:
                wgr = Ring([M.sb(pE, [128, 8, 2 * D], BF16, "wgb") for _ in range(2)])
                wd, wd_r = M.sb(pE, [128, 8, D], BF16, "wdb")
                bgr = Ring([M.sb(pE, [128, 16], F32, "bgb") for _ in range(2)])
                bdr = Ring([M.sb(pE, [128, D], F32, "bdb") for _ in range(1)])
                xer = Ring([M.sb(pE, [128, 4, D], BF16, "xe") for _ in range(1)])
                xeT, xeT_r = M.sb(pE, [128, 8, 512], BF16, "xeT")
                aT, aT_r = M.sb(pE, [128, 8, 512], BF16, "aT")
                tg = Ring([M.sb(pE, [128, 512], F32, "tg") for _ in range(2)])
                tsg = Ring([M.sb(pE, [128, 512], F32, "tsg") for _ in range(2)])
                tl = Ring([M.sb(pE, [128, 512], F32, "tl") for _ in range(2)])
                yer = Ring([M.sb(pE, [128, D], F32, "ye") for _ in range(2)])
                ptx, ptx_r = M.ps(pE, [128, 8, 128], BF16, "ptx")
                pgl = Ring([M.ps(pE, [128, 2, 512], F32, "pgl") for _ in range(2)])
                pout = Ring([M.ps(pE, [128, 512], F32, "pout") for _ in range(2)])
                loaded = {}

                def load_block(b):
                    wg, wg_r = wgr.next()
                    bg, bg_r = bgr.next()
                    for k in range(8):
                        S.idma(wg[:, k, :], None, w_gu, idxw[:, b, k:k + 1], 4 * NE * D - 1, reads=[idxw_r], writes=[wg_r], key=wg_r)
                    S.idma(bg[:], None, b_gu, idxb[:, b:b + 1], 4 * NE * 128 - 1, reads=[idxb_r], writes=[bg_r], key=bg_r)
                    S.op("dve", lambda e, bg=bg: e.tensor_scalar(out=bg[:, 8:16], in0=bg[:, 8:16], scalar1=1.0, scalar2=None, op0=ALU.add),
                         reads=[bg_r], writes=[bg_r])
                    loaded[b] = (wg, wg_r, bg, bg_r)

                load_block(0)
                for b in range(NB):
                    wg, wg_r, bg, bg_r = loaded.pop(b)
                    bd, bd_r = bdr.next()
                    xe, xe_r = xer.next()
                    S.dma("sp", xe[:], XE[b * 512:(b + 1) * 512, :].rearrange("(s p) f -> p s f", p=128), reads=[XE_r], writes=[xe_r], key=xe_r)
                    S.idma(bd[:], None, b_dn, idxd[:, b:b + 1], 4 * NE - 1, reads=[idxd_r], writes=[bd_r], key=bd_r)
                    for k in range(8):
                        S.idma(wd[:, k, :], None, w_dn, idxw[:, b, k:k + 1], 4 * NE * D - 1, reads=[idxw_r], writes=[wd_r], key=wd_r)
                    if b + 1 < NB:
                        load_block(b + 1)
                    for st in range(4):
                        for k in range(8):
                            S.op("pe", lambda e, st=st, k=k, xe=xe: e.transpose(out=ptx[:, k, :], in_=xe[:, st, k * 128:(k + 1) * 128], identity=self.identb[:]),
                                 reads=[xe_r, self.identb_r], writes=[ptx_r], pe_acc=True)
                        S.op("act", lambda e, st=st: e.activation(out=xeT[:, :, st * 128:(st + 1) * 128], in_=ptx[:], func=AF.Copy),
                             reads=[ptx_r], writes=[xeT_r])
                    for j in range(8):
                        pg, pg_r = pgl.next()
                        for gl in range(2):
                            for k in range(8):
                                S.op("pe", lambda e, pg=pg, gl=gl, k=k, j=j, wg=wg: e.matmul(
                                    pg[:, gl, :], lhsT=wg[:, k, gl * D + j * 128:gl * D + (j + 1) * 128], rhs=xeT[:, k, :],
                                    start=(k == 0), stop=(k == 7)), reads=[wg_r, xeT_r], writes=[pg_r], pe_acc=True)
                        t1, t1_r = tg.next()
                        t2, t2_r = tsg.next()
                        t3, t3_r = tl.next()
                        S.op("dve", lambda e, t1=t1, pg=pg, j=j, bg=bg: e.tensor_scalar(
                            out=t1[:], in0=pg[:, 0, :], scalar1=bg[:, j:j + 1], scalar2=7.0, op0=ALU.add, op1=ALU.min),
                            reads=[pg_r, bg_r], writes=[t1_r])
                        S.op("act", lambda e, t2=t2, t1=t1: e.activation(out=t2[:], in_=t1[:], func=AF.Sigmoid, scale=1.702), reads=[t1_r], writes=[t2_r])
                        S.op("dve", lambda e, t3=t3, pg=pg, j=j, bg=bg: e.tensor_scalar(
                            out=t3[:], in0=pg[:, 1, :], scalar1=bg[:, 8 + j:9 + j], scalar2=-6.0, op0=ALU.add, op1=ALU.max),
                            reads=[pg_r, bg_r], writes=[t3_r])
                        S.op("pool", lambda e, t1=t1, t2=t2: e.tensor_tensor(out=t1[:], in0=t1[:], in1=t2[:], op=ALU.mult), reads=[t1_r, t2_r], writes=[t1_r])
                        S.op("dve", lambda e, t1=t1, t3=t3, j=j: e.scalar_tensor_tensor(
                            out=aT[:, j, :], in0=t3[:], scalar=8.0, in1=t1[:], op0=ALU.min, op1=ALU.mult), reads=[t1_r, t3_r], writes=[aT_r])
                    for st in range(4):
                        ye, ye_r = yer.next()
                        for hf in range(2):
                            po, po_r = pout.next()
                            for j in range(8):
                                S.op("pe", lambda e, po=po, j=j, st=st, hf=hf: e.matmul(
                                    po[:], lhsT=aT[:, j, st * 128:(st + 1) * 128], rhs=wd[:, j, hf * 512:(hf + 1) * 512],
                                    start=(j == 0), stop=(j == 7)), reads=[aT_r, wd_r], writes=[po_r], pe_acc=True)
                            S.op("dve", lambda e, po=po, ye=ye, hf=hf, bd=bd: e.tensor_tensor(
                                out=ye[:, hf * 512:(hf + 1) * 512], in0=po[:], in1=bd[:, hf * 512:(hf + 1) * 512], op=ALU.add),
                                reads=[po_r, bd_r], writes=[ye_r])
                        S.dma("sp", YE[b * 512 + st * 128:b * 512 + (st + 1) * 128, :], ye[:], reads=[ye_r], writes=[YE_r], key=ye_r)
                S.barrier()
                S.release([r for _, r in wgr.items] + [wd_r] + [r for _, r in bgr.items] + [r for _, r in bdr.items]
                          + [r for _, r in xer.items] + [r for _, r in yer.items])
            with ExitStack() as pC:
                yjr = Ring([M.sb(pC, [128, D], F32, "yj") for _ in range(3)])
                acr = Ring([M.sb(pC, [128, D], F32, "acc") for _ in range(2)])
                pct = Ring([M.ps(pC, [128, 4, 128], F32, "pct") for _ in range(2)])
                for t in tiles:
                    c = 1 if t < 2 else 0
                    ac, ac_r = acr.next()
                    for j in range(4):
                        yj, yj_r = yjr.next()
                        S.idma(yj[:], None, YE, IDXi[:, t * 4 + j:t * 4 + j + 1], NBMAX * 512 - 1, reads=[YE_r, IDXi_r], writes=[yj_r], key=yj_r)
                        if j == 0:
                            S.op("dve", lambda e, ac=ac, yj=yj, t=t: e.tensor_scalar(out=ac[:], in0=yj[:], scalar1=g4[:, t, 0:1], scalar2=None, op0=ALU.mult),
                                 reads=[yj_r, g4_r], writes=[ac_r])
                        else:
                            S.op("dve", lambda e, ac=ac, yj=yj, t=t, j=j: e.scalar_tensor_tensor(
                                out=ac[:], in0=yj[:], scalar=g4[:, t, j:j + 1], in1=ac[:], op0=ALU.mult, op1=ALU.add),
                                reads=[yj_r, g4_r, ac_r], writes=[ac_r])
                    for hf in range(2):
                        pk, pk_r = pct.next()
                        for kk in range(4):
                            k = hf * 4 + kk
                            S.op("pe", lambda e, pk=pk, kk=kk, k=k, ac=ac: e.transpose(out=pk[:, kk, :], in_=ac[:, k * 128:(k + 1) * 128], identity=self.identf[:]),
                                 reads=[ac_r, self.identf_r], writes=[pk_r], pe_acc=True)
                        for kk in range(4):
                            k = hf * 4 + kk
                            S.op("dve", lambda e, pk=pk, kk=kk, k=k, t=t, c=c: e.scalar_tensor_tensor(
                                out=xT[:, k, t * 128:(t + 1) * 128], in0=pk[:, kk, :], scalar=self.mv[:, c, 5, k:k + 1], in1=xT[:, k, t * 128:(t + 1) * 128],
                                op0=ALU.mult, op1=ALU.add), reads=[pk_r, self.mv_r, xTr], writes=[xTr])
                S.barrier()
                S.release([r for _, r in yjr.items])

    def resid(self, po, po_r, n, oc, s, gb=None, tring=None):
        S = self.S
        c = 1 if s < NCTX else 0
        xT, xTr = self.xT, self.xTr
        if gb is None:
            S.op("dve", lambda e: e.scalar_tensor_tensor(
                out=xT[:, oc, s:s + n], in0=po[:, :n], scalar=self.mv[:, c, 2, oc:oc + 1], in1=xT[:, oc, s:s + n],
                op0=ALU.mult, op1=ALU.add), reads=[po_r, self.mv_r, xTr], writes=[xTr])
        else:
            gbt, gbt_r = gb
            t_, t_r = tring.next()
            S.op("act", lambda e: e.activation(out=t_[:, :n], in_=po[:, :n], func=AF.Identity,
                                               scale=self.mv[:, c, 2, oc:oc + 1], bias=gbt[:, c, oc:oc + 1]),
                 reads=[po_r, self.mv_r, gbt_r], writes=[t_r])
            S.op("pool", lambda e: e.tensor_tensor(out=xT[:, oc, s:s + n], in0=xT[:, oc, s:s + n], in1=t_[:, :n], op=ALU.add),
                 reads=[t_r, xTr], writes=[xTr])

    def gate_bias(self, ps, bvec_ap):
        S, M = self.S, self.M
        gb, gb_r = M.sb(ps, [128, 2, 8], F32, "gb")
        for c in range(2):
            S.op("dve", lambda e, c=c: e.tensor_tensor(out=gb[:, c, :], in0=self.mv[:, c, 2, :], in1=bvec_ap, op=ALU.mult),
                 reads=[self.mv_r] + self._vec_deps, writes=[gb_r])
        return gb, gb_r

    def mixer0(self, i):
        nc, S, M = self.nc, self.S, self.M
        w1 = self.inp("conv_w_pw1", [1, D, 2 * D])
        w2 = self.inp("conv_w_pw2", [1, D, D])
        cvd = self.inp("conv_vec", [128, 296])
        UW = 2364

        def ucol(s):
            return 15 if s == 0 else s + 45
        with ExitStack() as pA:
            cv, cv_r = M.sb(pA, [128, 296], F32, "cv")
            S.dma("sp", cv[:], cvd, writes=[cv_r], key=cv_r)
            self._vec_deps = [cv_r]
            V, V_r = M.sb(pA, [128, 8, T], BF16, "V")
            with ExitStack() as pB:
                U, U_r = M.sb(pB, [128, 8, UW], BF16, "U")
                S.op("pool", lambda e: e.memset(U[:], 0.0), writes=[U_r])
                with ExitStack() as pC:
                    hT, hT_r = M.sb(pC, [128, 8, T], BF16, "hT")
                    with ExitStack() as pD:
                        self.adanorm(pD, hT, hT_r, 0)
                        S.barrier()
                    wp = Ring([M.sb(pC, [128, 8, 2, 128], BF16, "wp") for _ in range(2)])
                    pa = Ring([M.ps(pC, [128, 2, 512], F32, "pa") for _ in range(2)])
                    sg = Ring([M.sb(pC, [128, 512], F32, "sg") for _ in range(2)])
                    for oc in range(8):
                        wt, wt_r = wp.next()
                        for gl in range(2):
                            S.dma("pool", wt[:, :, gl, :],
                                  w1[0, :, gl * D + oc * 128: gl * D + (oc + 1) * 128].rearrange("(k p) n -> p k n", p=128),
                                  writes=[wt_r], key=wt_r)
                        for (s, n) in BLKS:
                            p_, p_r = pa.next()
                            for gl in range(2):
                                for k in range(8):
                                    S.op("pe", lambda e, p_=p_, gl=gl, k=k, s=s, n=n, wt=wt: e.matmul(
                                        p_[:, gl, :n], lhsT=wt[:, k, gl, :], rhs=hT[:, k, s:s + n], start=(k == 0), stop=(k == 7)),
                                        reads=[wt_r, hT_r], writes=[p_r], pe_acc=True)
                            g_, g_r = sg.next()
                            S.op("act", lambda e, g_=g_, p_=p_, n=n, oc=oc: e.activation(
                                out=g_[:, :n], in_=p_[:, 1, :n], func=AF.Sigmoid, bias=cv[:, 8 + oc:9 + oc], scale=1.0),
                                reads=[p_r, cv_r], writes=[g_r])
                            S.op("dve", lambda e, g_=g_, p_=p_, n=n, oc=oc, s=s: e.scalar_tensor_tensor(
                                out=U[:, oc, ucol(s):ucol(s) + n], in0=p_[:, 0, :n], scalar=cv[:, oc:oc + 1], in1=g_[:, :n],
                                op0=ALU.add, op1=ALU.mult),
                                reads=[p_r, cv_r, g_r], writes=[U_r])
                    S.barrier()
                    S.release([r for _, r in wp.items])
                dgr = Ring([M.sb(pB, [128, 31, 128], BF16, "dg") for _ in range(2)])
                pc = Ring([M.ps(pB, [128, 512], F32, "pc") for _ in range(2)])
                for oc in range(8):
                    dg, dg_r = dgr.next()
                    for w in range(31):
                        S.op("dve", lambda e, dg=dg, w=w, oc=oc: e.tensor_scalar(
                            out=dg[:, w, :], in0=self.identb[:], scalar1=cv[:, 16 + oc * 31 + w:17 + oc * 31 + w], scalar2=None,
                            op0=ALU.mult), reads=[self.identb_r, cv_r], writes=[dg_r])
                    for (s, n) in BLKS:
                        o0 = 0 if s == 0 else s + 30
                        p_, p_r = pc.next()
                        for w in range(31):
                            S.op("pe", lambda e, p_=p_, w=w, oc=oc, o0=o0, n=n, dg=dg: e.matmul(
                                p_[:, :n], lhsT=dg[:, w, :], rhs=U[:, oc, o0 + w:o0 + w + n], start=(w == 0), stop=(w == 30)),
                                reads=[dg_r, U_r], writes=[p_r], pe_acc=True)
                        S.op("act", lambda e, p_=p_, oc=oc, s=s, n=n: e.activation(
                            out=V[:, oc, s:s + n], in_=p_[:, :n], func=AF.Identity, bias=cv[:, 264 + oc:265 + oc], scale=1.0),
                            reads=[p_r, cv_r], writes=[V_r])
                S.barrier()
            h2, h2_r = M.sb(pA, [128, 8, T], BF16, "h2")
            w2t, w2_r = M.sb(pA, [128, 8, D], BF16, "w2t")
            S.dma("pool", w2t[:], w2[0].rearrange("(k p) n -> p k n", p=128), writes=[w2_r], key=w2_r)
            gb = self.gate_bias(pA, cv[:, 288:296])
            ps1 = Ring([M.ps(pA, [128, 512], F32, "ps1") for _ in range(1)])
            ps2 = Ring([M.ps(pA, [128, 512], F32, "ps2") for _ in range(1)])
            po = Ring([M.ps(pA, [128, 512], F32, "po") for _ in range(2)])
            vsq = Ring([M.sb(pA, [128, 512], BF16, "vsq") for _ in range(2)])
            mu = Ring([M.sb(pA, [128, 512], F32, "mu") for _ in range(1)])
            rs = Ring([M.sb(pA, [128, 512], F32, "rs") for _ in range(1)])
            tt = Ring([M.sb(pA, [128, 512], F32, "tt") for _ in range(2)])
            tr = Ring([M.sb(pA, [128, 512], F32, "tr") for _ in range(2)])
            for (s, n) in BLKS:
                a1, a1_r = ps1.next()
                a2, a2_r = ps2.next()
                for k in range(8):
                    q, q_r = vsq.next()
                    S.op("pool", lambda e, q=q, k=k, s=s, n=n: e.tensor_tensor(out=q[:, :n], in0=V[:, k, s:s + n], in1=V[:, k, s:s + n], op=ALU.mult),
                         reads=[V_r], writes=[q_r])
                    S.op("pe", lambda e, a1=a1, k=k, s=s, n=n: e.matmul(a1[:, :n], lhsT=self.onesb[:], rhs=V[:, k, s:s + n],
                                                                         start=(k == 0), stop=(k == 7)),
                         reads=[V_r, self.onesb_r], writes=[a1_r], pe_acc=True)
                    S.op("pe", lambda e, a2=a2, q=q, k=k, n=n: e.matmul(a2[:, :n], lhsT=self.onesb[:], rhs=q[:, :n],
                                                                        start=(k == 0), stop=(k == 7)),
                         reads=[q_r, self.onesb_r], writes=[a2_r], pe_acc=True)
                m_, m_r = mu.next()
                r_, r_r = rs.next()
                S.op("act", lambda e, m_=m_, a1=a1, n=n: e.activation(out=m_[:, :n], in_=a1[:, :n], func=AF.Copy, scale=1.0 / D),
                     reads=[a1_r], writes=[m_r])
                S.op("dve", lambda e, r_=r_, m_=m_, n=n: e.tensor_tensor(out=r_[:, :n], in0=m_[:, :n], in1=m_[:, :n], op=ALU.mult),
                     reads=[m_r], writes=[r_r])
                S.op("dve", lambda e, r_=r_, a2=a2, n=n: e.scalar_tensor_tensor(out=r_[:, :n], in0=a2[:, :n], scalar=1.0 / D, in1=r_[:, :n],
                                                                               op0=ALU.mult, op1=ALU.subtract),
                     reads=[a2_r, r_r], writes=[r_r])
                S.op("act", lambda e, r_=r_, n=n: e.activation(out=r_[:, :n], in_=r_[:, :n], func=AF.Sqrt, bias=self.epsb[:, 0:1], scale=1.0),
                     reads=[r_r, self.epsb_r], writes=[r_r])
                S.op("dve", lambda e, r_=r_, n=n: e.reciprocal(out=r_[:, :n], in_=r_[:, :n]), reads=[r_r], writes=[r_r])
                for k in range(8):
                    t_, t_r = tt.next()
                    S.op("dve", lambda e, t_=t_, k=k, s=s, n=n, m_=m_: e.tensor_tensor(out=t_[:, :n], in0=V[:, k, s:s + n], in1=m_[:, :n], op=ALU.subtract),
                         reads=[V_r, m_r], writes=[t_r])
                    S.op("pool", lambda e, t_=t_, r_=r_, n=n: e.tensor_tensor(out=t_[:, :n], in0=t_[:, :n], in1=r_[:, :n], op=ALU.mult),
                         reads=[t_r, r_r], writes=[t_r])
                    S.op("act", lambda e, t_=t_, k=k, s=s, n=n: e.activation(
                        out=h2[:, k, s:s + n], in_=t_[:, :n], func=AF.Silu, scale=cv[:, 272 + k:273 + k], bias=cv[:, 280 + k:281 + k]),
                        reads=[t_r, cv_r], writes=[h2_r])
            for (s, n) in BLKS:
                for oc in range(8):
                    p_, p_r = po.next()
                    for k in range(8):
                        S.op("pe", lambda e, p_=p_, k=k, oc=oc, s=s, n=n: e.matmul(
                            p_[:, :n], lhsT=w2t[:, k, oc * 128:(oc + 1) * 128], rhs=h2[:, k, s:s + n], start=(k == 0), stop=(k == 7)),
                            reads=[w2_r, h2_r], writes=[p_r], pe_acc=True)
                    self.resid(p_, p_r, n, oc, s, gb=gb, tring=tr)
            S.barrier()
            S.release([cv_r, w2_r])

    def load_w(self, ps, src2d, kch, n, name, npart=128):
        S, M = self.S, self.M
        t, t_r = M.sb(ps, [npart, kch, n], BF16, name)
        S.dma("pool", t[:], src2d.rearrange("(k p) n -> p k n", p=npart), writes=[t_r], key=t_r)
        return t, t_r

    def swap_halves(self, ps, w, w_r, kch, nh, name):
        S, M = self.S, self.M
        ws, ws_r = M.sb(ps, [128, kch, nh * 64], BF16, name)
        for h in range(nh):
            S.op("pool", lambda e, h=h: e.tensor_copy(out=ws[:, :, h * 64:h * 64 + 32], in_=w[:, :, h * 64 + 32:h * 64 + 64]),
                 reads=[w_r], writes=[ws_r])
            S.op("pool", lambda e, h=h: e.tensor_copy(out=ws[:, :, h * 64 + 32:h * 64 + 64], in_=w[:, :, h * 64:h * 64 + 32]),
                 reads=[w_r], writes=[ws_r])
        return ws, ws_r

    def proj_rope(self, pj, pj_r, w, w_r, ws, ws_r, c0, b, bs, b_r, hT, hT_r, out, out_r, blks, C, Sn, tab_r, t1r, t2r):
        S = self.S
        for (s, n) in blks:
            for a, (ww, ww_r) in enumerate(((w, w_r), (ws, ws_r))):
                for k in range(8):
                    S.op("pe", lambda e, a=a, ww=ww, k=k, s=s, n=n: e.matmul(
                        pj[0:64, a, :n], lhsT=ww[:, k, c0:c0 + 64], rhs=hT[:, k, s:s + n], start=(k == 0), stop=(k == 7)),
                        reads=[ww_r, hT_r], writes=[pj_r], pe_acc=True)
            t1, t1_r = t1r.next()
            t2, t2_r = t2r.next()
            S.op("dve", lambda e, t1=t1, s=s, n=n: e.scalar_tensor_tensor(
                out=t1[0:64, :n], in0=pj[0:64, 0, :n], scalar=b, in1=C[:, s:s + n], op0=ALU.add, op1=ALU.mult),
                reads=[pj_r, b_r, tab_r], writes=[t1_r])
            S.op("dve", lambda e, t2=t2, s=s, n=n: e.scalar_tensor_tensor(
                out=t2[0:64, :n], in0=pj[0:64, 1, :n], scalar=bs, in1=Sn[:, s:s + n], op0=ALU.add, op1=ALU.mult),
                reads=[pj_r, b_r, tab_r], writes=[t2_r])
            S.op("pool", lambda e, t1=t1, t2=t2, s=s, n=n: e.tensor_tensor(out=out[0:64, s:s + n], in0=t1[0:64, :n], in1=t2[0:64, :n], op=ALU.add),
                 reads=[t1_r, t2_r], writes=[out_r])

    def load_tables(self, ps):
        S, M = self.S, self.M
        Cd = self.inp("rope_c", [64, T])
        Sd = self.inp("rope_s", [64, T])
        C, C_r = M.sb(ps, [64, T], F32, "ropeC")
        Sn, _ = M.sb(ps, [64, T], F32, "ropeS")
        S.dma("sp", C[:], Cd, writes=[C_r], key=C_r)
        S.dma("sp", Sn[:], Sd, writes=[C_r], key=C_r)
        return C, Sn, C_r

    def mixer2(self, i):
        nc, S, M = self.nc, self.S, self.M
        wqkv = self.inp("swa_w_qkv", [1, D, 1536])
        bqkv = self.inp("swa_b_qkv", [1, 1536])
        wo = self.inp("swa_w_o", [1, D, D])
        bh = self.inp("swa_bh", [64, 44])
        sinks = self.inp("swa_sinks", [1, 16])
        bo = self.inp("swa_bo_fm", [128, 8])
        NEG = -30000.0
        with ExitStack() as pA:
            hT, hT_r = M.sb(pA, [128, 8, T], BF16, "hT")
            with ExitStack() as pD:
                self.adanorm(pD, hT, hT_r, 0)
                S.barrier()
            C, Sn, tab_r = self.load_tables(pA)
            bht, bht_r = M.sb(pA, [64, 44], F32, "bht")
            S.dma("sp", bht[:], bh, writes=[bht_r], key=bht_r)
            skb, skb_r = M.sb(pA, [128, 16], F32, "skb")
            S.dma("sp", skb[:], sinks[0].partition_broadcast(128), writes=[skb_r], key=skb_r)
            bot, bot_r = M.sb(pA, [128, 8], F32, "bot")
            S.dma("sp", bot[:], bo, writes=[bot_r], key=bot_r)
            self._vec_deps = [bot_r]
            gb = self.gate_bias(pA, bot[:])
            mW, mW_r = M.sb(pA, [128, 384], F32, "mW")
            S.op("pool", lambda e: e.memset(mW[:], 0.0), writes=[mW_r])
            S.op("pool", lambda e: e.affine_select(out=mW[:, 0:128], in_=mW[:, 0:128], pattern=[[1, 128]], compare_op=ALU.is_ge,
                                                   fill=NEG, base=0, channel_multiplier=-1), reads=[mW_r], writes=[mW_r])
            S.op("pool", lambda e: e.affine_select(out=mW[:, 256:384], in_=mW[:, 256:384], pattern=[[-1, 128]], compare_op=ALU.is_ge,
                                                   fill=NEG, base=0, channel_multiplier=1), reads=[mW_r], writes=[mW_r])
            kT, kT_r = M.sb(pA, [64, T], BF16, "kT")
            vs, vs_r = M.sb(pA, [128, NT, 64], BF16, "vs")
            qTr = Ring([M.sb(pA, [64, T], BF16, "qT") for _ in range(2)])
            oTg, oTg_r = M.sb(pA, [64, 4, T], BF16, "oTg")
            bvb, bvb_r = M.sb(pA, [128, 64], F32, "bvb")
            t1r = Ring([M.sb(pA, [64, 512], F32, "rp1") for _ in range(2)])
            t2r = Ring([M.sb(pA, [64, 512], F32, "rp2") for _ in range(2)])
            tr = Ring([M.sb(pA, [128, 512], F32, "tr") for _ in range(2)])
            swr = Ring([M.sb(pA, [128, 384], F32, "sw") for _ in range(2)])
            pwr = Ring([M.sb(pA, [128, 640], BF16, "pw") for _ in range(2)])
            pTsr = Ring([M.sb(pA, [128, 5, 128], BF16, "pTs") for _ in range(2)])
            osr = Ring([M.sb(pA, [128, 64], F32, "osb") for _ in range(2)])
            smr = Ring([M.sb(pA, [128, 8], F32, "sm") for _ in range(3)])
            pj, pj_r = M.ps(pA, [128, 2, 512], F32, "pj")
            pscr = Ring([M.ps(pA, [128, 2, 512], F32, "psc") for _ in range(2)])
            pT, pT_r = M.ps(pA, [128, 5, 128], BF16, "pT")
            pso, pso_r = M.ps(pA, [128, 512], F32, "pso")
            for g in range(4):
                if (self.dbg == 7 and g == 1) or (self.dbg == 17 and g == 2) or (self.dbg == 18 and g == 3):
                    return
                with ExitStack() as pG:
                    wq, wq_r = self.load_w(pG, wqkv[0, :, g * 256:(g + 1) * 256], 8, 256, "wq")
                    wk, wk_r = self.load_w(pG, wqkv[0, :, 1024 + g * 64:1024 + (g + 1) * 64], 8, 64, "wk")
                    wv, wv_r = self.load_w(pG, wqkv[0, :, 1280 + g * 64:1280 + (g + 1) * 64], 8, 64, "wv")
                    wqs, wqs_r = self.swap_halves(pG, wq, wq_r, 8, 4, "wqs")
                    wks, wks_r = self.swap_halves(pG, wk, wk_r, 8, 1, "wks")
                    wog, wog_r = self.load_w(pG, wo[0, g * 256:(g + 1) * 256, :], 4, D, "wog", npart=64)
                    S.dma("sp", bvb[:], bqkv[0, 1280 + g * 64:1280 + (g + 1) * 64].partition_broadcast(128),
                          reads=[], writes=[bvb_r], key=bvb_r)
                    self.proj_rope(pj, pj_r, wk, wk_r, wks, wks_r, 0, bht[:, 16 + g:17 + g], bht[:, 24 + 16 + g:25 + 16 + g], bht_r,
                                   hT, hT_r, kT, kT_r, BLKS, C, Sn, tab_r, t1r, t2r)
                    for t in range(NT):
                        for k in range(8):
                            S.op("pe", lambda e, t=t, k=k: e.matmul(pso[:, 0:64], lhsT=hT[:, k, t * 128:(t + 1) * 128], rhs=wv[:, k, :],
                                                                    start=(k == 0), stop=(k == 7)),
                                 reads=[hT_r, wv_r], writes=[pso_r], pe_acc=True)
                        S.op("dve", lambda e, t=t: e.tensor_tensor(out=vs[:, t, :], in0=pso[:, 0:64], in1=bvb[:], op=ALU.add),
                             reads=[pso_r, bvb_r], writes=[vs_r])
                    if self.dbg == 1 or (self.dbg == 11 and g == 1):
                        return
                    for hh in range(4):
                        h = g * 4 + hh
                        qT, qT_r = qTr.next()
                        self.proj_rope(pj, pj_r, wq, wq_r, wqs, wqs_r, hh * 64, bht[:, h:h + 1], bht[:, 24 + h:25 + h], bht_r,
                                       hT, hT_r, qT, qT_r, BLKS, C, Sn, tab_r, t1r, t2r)
                        if self.dbg == 2 or (self.dbg == 12 and g == 1):
                            return
                        for qt in range(NT):
                            if self.dbg == 3 and qt == 1:
                                return
                            if self.dbg == 4 and qt == 3:
                                return
                            if self.dbg == 5 and hh == 1:
                                return
                            psc, psc_r = pscr.next()
                            sm, sm_r = smr.next()
                            pw, pw_r = pwr.next()
                            lat = qt >= 2
                            S.op("pe", lambda e, psc=psc, qT=qT, qt=qt: e.matmul(
                                psc[:, 1, 0:256], lhsT=qT[:, qt * 128:(qt + 1) * 128], rhs=kT[:, 0:256], start=True, stop=True),
                                reads=[qT_r, kT_r], writes=[psc_r], pe_acc=True)
                            if lat:
                                qb = qt - 2
                                lo = max(0, qb - 1)
                                hi = min(15, qb + 1)
                                c0 = (lo - (qb - 1)) * 128
                                c1 = c0 + (hi - lo + 1) * 128
                                ktiles = list(range(lo + 2, hi + 3))
                                S.op("pe", lambda e, psc=psc, qT=qT, qt=qt, lo=lo, hi=hi, c0=c0, c1=c1: e.matmul(
                                    psc[:, 0, c0:c1], lhsT=qT[:, qt * 128:(qt + 1) * 128], rhs=kT[:, 256 + lo * 128:256 + (hi + 1) * 128],
                                    start=True, stop=True), reads=[qT_r, kT_r], writes=[psc_r], pe_acc=True)
                                sw, sw_r = swr.next()
                                S.op("dve", lambda e, sw=sw, psc=psc, c0=c0, c1=c1: e.tensor_tensor(
                                    out=sw[:, c0:c1], in0=psc[:, 0, c0:c1], in1=mW[:, c0:c1], op=ALU.add),
                                    reads=[psc_r, mW_r], writes=[sw_r])
                                S.op("dve", lambda e, sm=sm, sw=sw, c0=c0, c1=c1: e.reduce_max(out=sm[:, 0:1], in_=sw[:, c0:c1], axis=AX.X),
                                     reads=[sw_r], writes=[sm_r])
                            else:
                                ktiles = []
                            S.op("dve", lambda e, sm=sm, psc=psc: e.reduce_max(out=sm[:, 1:2], in_=psc[:, 1, 0:256], axis=AX.X),
                                 reads=[psc_r], writes=[sm_r])
                            if lat:
                                S.op("dve", lambda e, sm=sm: e.tensor_tensor(out=sm[:, 1:2], in0=sm[:, 0:1], in1=sm[:, 1:2], op=ALU.max),
                                     reads=[sm_r], writes=[sm_r])
                            S.op("dve", lambda e, sm=sm, h=h: e.scalar_tensor_tensor(out=sm[:, 2:3], in0=sm[:, 1:2], scalar=0.125, in1=skb[:, h:h + 1],
                                                                                   op0=ALU.mult, op1=ALU.max),
                                 reads=[sm_r, skb_r], writes=[sm_r])
                            S.op("dve", lambda e, sm=sm: e.tensor_scalar(out=sm[:, 3:4], in0=sm[:, 2:3], scalar1=-1.0, scalar2=None, op0=ALU.mult),
                                 reads=[sm_r], writes=[sm_r])
                            S.op("pool", lambda e, sm=sm: e.memset(sm[:, 4:7], 0.0), reads=[sm_r], writes=[sm_r])
                            if lat:
                                S.op("act", lambda e, pw=pw, sw=sw, sm=sm, c0=c0, c1=c1: e.activation(
                                    out=pw[:, c0:c1], in_=sw[:, c0:c1], func=AF.Exp, scale=0.125, bias=sm[:, 3:4], accum_out=sm[:, 4:5]),
                                    reads=[sw_r, sm_r], writes=[pw_r, sm_r])
                            S.op("act", lambda e, pw=pw, psc=psc, sm=sm: e.activation(
                                out=pw[:, 384:640], in_=psc[:, 1, 0:256], func=AF.Exp, scale=0.125, bias=sm[:, 3:4], accum_out=sm[:, 5:6]),
                                reads=[psc_r, sm_r], writes=[pw_r, sm_r])
                            S.op("act", lambda e, sm=sm, h=h: e.activation(out=sm[:, 6:7], in_=skb[:, h:h + 1], func=AF.Exp, scale=1.0, bias=sm[:, 3:4]),
                                 reads=[skb_r, sm_r], writes=[sm_r])
                            S.op("dve", lambda e, sm=sm: e.tensor_tensor(out=sm[:, 4:5], in0=sm[:, 4:5], in1=sm[:, 5:6], op=ALU.add),
                                 reads=[sm_r], writes=[sm_r])
                            S.op("dve", lambda e, sm=sm: e.tensor_tensor(out=sm[:, 4:5], in0=sm[:, 4:5], in1=sm[:, 6:7], op=ALU.add),
                                 reads=[sm_r], writes=[sm_r])
                            S.op("dve", lambda e, sm=sm: e.reciprocal(out=sm[:, 7:8], in_=sm[:, 4:5]), reads=[sm_r], writes=[sm_r])
                            srcs = []
                            if lat:
                                for j, kt_ in enumerate(ktiles):
                                    srcs.append((c0 + j * 128, kt_))
                            srcs += [(384, 0), (512, 1)]
                            for j, (col, kt_) in enumerate(srcs):
                                S.op("pe", lambda e, j=j, col=col, pw=pw: e.transpose(out=pT[:, j, :], in_=pw[:, col:col + 128], identity=self.identb[:]),
                                     reads=[pw_r, self.identb_r], writes=[pT_r], pe_acc=True)
                            pTs, pTs_r = pTsr.next()
                            nj = len(srcs)
                            S.op("act", lambda e, pTs=pTs, nj=nj: e.activation(out=pTs[:, 0:nj, :], in_=pT[:, 0:nj, :], func=AF.Copy),
                                 reads=[pT_r], writes=[pTs_r])
                            for j, (col, kt_) in enumerate(srcs):
                                S.op("pe", lambda e, j=j, kt_=kt_, pTs=pTs, nj=nj: e.matmul(
                                    pso[:, 64:128], lhsT=pTs[:, j, :], rhs=vs[:, kt_, :], start=(j == 0), stop=(j == nj - 1)),
                                    reads=[pTs_r, vs_r], writes=[pso_r], pe_acc=True)
                            osb, osb_r = osr.next()
                            S.op("dve", lambda e, osb=osb, sm=sm: e.tensor_scalar(out=osb[:], in0=pso[:, 64:128], scalar1=sm[:, 7:8], scalar2=None, op0=ALU.mult),
                                 reads=[pso_r, sm_r], writes=[osb_r])
                            S.op("pe", lambda e, osb=osb: e.transpose(out=pso[0:64, 128:256], in_=osb[:], identity=self.identf[:]),
                                 reads=[osb_r, self.identf_r], writes=[pso_r], pe_acc=True)
                            S.op("act", lambda e, hh=hh, qt=qt: e.activation(out=oTg[:, hh, qt * 128:(qt + 1) * 128], in_=pso[0:64, 128:256], func=AF.Copy),
                                 reads=[pso_r], writes=[oTg_r])
                    if self.dbg == 6 or (self.dbg == 16 and g == 1):
                        return
                    for (s, n) in BLKS:
                        for oc in range(8):
                            for hh in range(4):
                                S.op("pe", lambda e, hh=hh, oc=oc, s=s, n=n: e.matmul(
                                    pj[:, 0, :n], lhsT=wog[:, hh, oc * 128:(oc + 1) * 128], rhs=oTg[:, hh, s:s + n], start=(hh == 0), stop=(hh == 3)),
                                    reads=[wog_r, oTg_r], writes=[pj_r], pe_acc=True)
                            if g == 0:
                                self.resid(pj[:, 0, :], pj_r, n, oc, s, gb=gb, tring=tr)
                            else:
                                self.resid(pj[:, 0, :], pj_r, n, oc, s)
                    S.barrier()
                    S.release([wq_r, wk_r, wv_r, wog_r])

    def mixer3(self, i):
        nc, S, M = self.nc, self.S, self.M
        wqkv = self.inp("diff_w_qkv", [1, D, 3 * D])
        wo = self.inp("diff_w_o", [1, D, D])
        lvec = self.inp("diff_lam", [4, 64])
        sg = self.inp("diff_subln_g", [128, 1])
        lambda_init = 0.8 - 0.6 * math.exp(-0.3 * i)
        LBLK = BLKS[1:]
        with ExitStack() as pA:
            hT, hT_r = M.sb(pA, [128, 8, T], BF16, "hT")
            with ExitStack() as pD:
                self.adanorm(pD, hT, hT_r, 0)
                S.barrier()
            C, Sn, tab_r = self.load_tables(pA)
            zb, zb_r = M.sb(pA, [64, 1], F32, "zb")
            S.op("pool", lambda e: e.memset(zb[:], 0.0), writes=[zb_r])
            sgt, sgt_r = M.sb(pA, [128, 1], F32, "sgt")
            S.dma("sp", sgt[:], sg, writes=[sgt_r], key=sgt_r)
            lv, lv_r = M.sb(pA, [128, 4, 64], F32, "lv")
            for a in range(4):
                S.dma("sp", lv[:, a, :], lvec[a].partition_broadcast(128), writes=[lv_r], key=lv_r)
            lam, lam_r = M.sb(pA, [128, 4], F32, "lam")
            lp, lp_r = M.sb(pA, [128, 2, 64], F32, "lp")
            for a in range(2):
                S.op("dve", lambda e, a=a: e.tensor_tensor(out=lp[:, a, :], in0=lv[:, 2 * a, :], in1=lv[:, 2 * a + 1, :], op=ALU.mult),
                     reads=[lv_r], writes=[lp_r])
                S.op("dve", lambda e, a=a: e.reduce_sum(out=lam[:, a:a + 1], in_=lp[:, a, :], axis=AX.X), reads=[lp_r], writes=[lam_r])
            S.op("act", lambda e: e.activation(out=lam[:, 0:2], in_=lam[:, 0:2], func=AF.Exp), reads=[lam_r], writes=[lam_r])
            S.op("dve", lambda e: e.tensor_tensor(out=lam[:, 2:3], in0=lam[:, 0:1], in1=lam[:, 1:2], op=ALU.subtract), reads=[lam_r], writes=[lam_r])
            S.op("dve", lambda e: e.tensor_scalar(out=lam[:, 3:4], in0=lam[:, 2:3], scalar1=float(lambda_init), scalar2=None, op0=ALU.add),
                 reads=[lam_r], writes=[lam_r])
            kTs = [M.sb(pA, [64, T], BF16, "kT%d" % t) for t in range(2)]
            qTs = [M.sb(pA, [64, T], BF16, "qT%d" % t) for t in range(2)]
            vs, vs_r = M.sb(pA, [128, NT, 128], BF16, "vs")
            oT, oT_r = M.sb(pA, [128, NLAT], BF16, "oT")
            pbr = Ring([M.sb(pA, [128, T], BF16, "pb") for _ in range(2)])
            pTs, pTs_r = M.sb(pA, [128, NT, 128], BF16, "pTs")
            t1r = Ring([M.sb(pA, [64, 512], F32, "rp1") for _ in range(2)])
            t2r = Ring([M.sb(pA, [64, 512], F32, "rp2") for _ in range(2)])
            smr = Ring([M.sb(pA, [128, 16], F32, "sm") for _ in range(4)])
            o1r = Ring([M.sb(pA, [128, 128], F32, "o1") for _ in range(2)])
            o2r = Ring([M.sb(pA, [128, 128], F32, "o2") for _ in range(2)])
            jkr = Ring([M.sb(pA, [128, 128], F32, "jk") for _ in range(2)])
            psc, psc_r = M.ps(pA, [128, 5, 512], F32, "psc")
            pT, pT_r = M.ps(pA, [128, 6, 128], BF16, "pT")
            po, po_r = M.ps(pA, [128, 2, 128], F32, "po")
            pout, pout_r = M.ps(pA, [128, 512], F32, "pout")
            KB = [(j * 512, min(512, T - j * 512)) for j in range(5)]
            for c in range(8):
                with ExitStack() as pG:
                    wq, wq_r = self.load_w(pG, wqkv[0, :, c * 128:(c + 1) * 128], 8, 128, "wq")
                    wk, wk_r = self.load_w(pG, wqkv[0, :, D + c * 128:D + (c + 1) * 128], 8, 128, "wk")
                    wv, wv_r = self.load_w(pG, wqkv[0, :, 2 * D + c * 128:2 * D + (c + 1) * 128], 8, 128, "wv")
                    woc, woc_r = self.load_w(pG, wo[0, c * 128:(c + 1) * 128, :], 1, D, "woc")
                    wqs, wqs_r = self.swap_halves(pG, wq, wq_r, 8, 2, "wqs")
                    wks, wks_r = self.swap_halves(pG, wk, wk_r, 8, 2, "wks")
                    for t in range(2):
                        self.proj_rope(psc, psc_r, wk, wk_r, wks, wks_r, t * 64, zb[:, 0:1], zb[:, 0:1], zb_r,
                                       hT, hT_r, kTs[t][0], kTs[t][1], BLKS, C, Sn, tab_r, t1r, t2r)
                        self.proj_rope(psc, psc_r, wq, wq_r, wqs, wqs_r, t * 64, zb[:, 0:1], zb[:, 0:1], zb_r,
                                       hT, hT_r, qTs[t][0], qTs[t][1], LBLK, C, Sn, tab_r, t1r, t2r)
                    for tt in range(NT):
                        for k in range(8):
                            S.op("pe", lambda e, tt=tt, k=k: e.matmul(pout[:, 0:128], lhsT=hT[:, k, tt * 128:(tt + 1) * 128], rhs=wv[:, k, :],
                                                                      start=(k == 0), stop=(k == 7)),
                                 reads=[hT_r, wv_r], writes=[pout_r], pe_acc=True)
                        S.op("act", lambda e, tt=tt: e.activation(out=vs[:, tt, :], in_=pout[:, 0:128], func=AF.Copy),
                             reads=[pout_r], writes=[vs_r])
                    for qb in range(16):
                        q0 = NCTX + qb * 128
                        sms = []
                        for t in range(2):
                            qT, qT_r = qTs[t]
                            kT, kT_r = kTs[t]
                            sm, sm_r = smr.next()
                            sms.append((sm, sm_r))
                            for j, (k0, kn) in enumerate(KB):
                                S.op("pe", lambda e, j=j, k0=k0, kn=kn, qT=qT, kT=kT, q0=q0: e.matmul(
                                    psc[:, j, :kn], lhsT=qT[:, q0:q0 + 128], rhs=kT[:, k0:k0 + kn], start=True, stop=True),
                                    reads=[qT_r, kT_r], writes=[psc_r], pe_acc=True)
                            for j, (k0, kn) in enumerate(KB):
                                S.op("dve", lambda e, j=j, kn=kn, sm=sm: e.reduce_max(out=sm[:, j:j + 1], in_=psc[:, j, :kn], axis=AX.X),
                                     reads=[psc_r], writes=[sm_r])
                            S.op("dve", lambda e, sm=sm: e.reduce_max(out=sm[:, 5:6], in_=sm[:, 0:5], axis=AX.X), reads=[sm_r], writes=[sm_r])
                            S.op("dve", lambda e, sm=sm: e.tensor_scalar(out=sm[:, 6:7], in0=sm[:, 5:6], scalar1=-0.125, scalar2=None, op0=ALU.mult),
                                 reads=[sm_r], writes=[sm_r])
                            S.op("pool", lambda e, sm=sm: e.memset(sm[:, 8:13], 0.0), reads=[sm_r], writes=[sm_r])
                            pb, pb_r = pbr.next()
                            for j, (k0, kn) in enumerate(KB):
                                S.op("act", lambda e, j=j, k0=k0, kn=kn, sm=sm, pb=pb: e.activation(
                                    out=pb[:, k0:k0 + kn], in_=psc[:, j, :kn], func=AF.Exp, scale=0.125, bias=sm[:, 6:7], accum_out=sm[:, 8 + j:9 + j]),
                                    reads=[psc_r, sm_r], writes=[pb_r, sm_r])
                            S.op("dve", lambda e, sm=sm: e.reduce_sum(out=sm[:, 13:14], in_=sm[:, 8:13], axis=AX.X), reads=[sm_r], writes=[sm_r])
                            S.op("dve", lambda e, sm=sm: e.reciprocal(out=sm[:, 14:15], in_=sm[:, 13:14]), reads=[sm_r], writes=[sm_r])
                            for b3 in range(3):
                                for jj in range(6):
                                    j = b3 * 6 + jj
                                    S.op("pe", lambda e, j=j, jj=jj, pb=pb: e.transpose(out=pT[:, jj, :], in_=pb[:, j * 128:(j + 1) * 128], identity=self.identb[:]),
                                         reads=[pb_r, self.identb_r], writes=[pT_r], pe_acc=True)
                                if b3 % 2 == 0:
                                    S.op("act", lambda e, b3=b3: e.activation(out=pTs[:, b3 * 6:(b3 + 1) * 6, :], in_=pT[:], func=AF.Copy),
                                         reads=[pT_r], writes=[pTs_r])
                                else:
                                    S.op("dve", lambda e, b3=b3: e.tensor_copy(out=pTs[:, b3 * 6:(b3 + 1) * 6, :], in_=pT[:]),
                                         reads=[pT_r], writes=[pTs_r])
                            for j in range(NT):
                                S.op("pe", lambda e, j=j, t=t: e.matmul(po[:, t, :], lhsT=pTs[:, j, :], rhs=vs[:, j, :], start=(j == 0), stop=(j == NT - 1)),
                                     reads=[pTs_r, vs_r], writes=[po_r], pe_acc=True)
                        (sm0, sm0_r), (sm1, sm1_r) = sms
                        o1, o1_r = o1r.next()
                        o2, o2_r = o2r.next()
                        jk, jk_r = jkr.next()
                        S.op("dve", lambda e, sm1=sm1: e.tensor_tensor(out=sm1[:, 15:16], in0=sm1[:, 14:15], in1=lam[:, 3:4], op=ALU.mult),
                             reads=[sm1_r, lam_r], writes=[sm1_r])
                        S.op("dve", lambda e, o1=o1, sm1=sm1: e.tensor_scalar(out=o1[:], in0=po[:, 1, :], scalar1=sm1[:, 15:16], scalar2=None, op0=ALU.mult),
                             reads=[po_r, sm1_r], writes=[o1_r])
                        S.op("dve", lambda e, o1=o1, o2=o2, sm0=sm0: e.scalar_tensor_tensor(out=o2[:], in0=po[:, 0, :], scalar=sm0[:, 14:15], in1=o1[:],
                                                                                         op0=ALU.mult, op1=ALU.subtract),
                             reads=[po_r, sm0_r, o1_r], writes=[o2_r])
                        S.op("pool", lambda e, sm0=sm0: e.memset(sm0[:, 7:8], 0.0), reads=[sm0_r], writes=[sm0_r])
                        S.op("act", lambda e, jk=jk, o2=o2, sm0=sm0: e.activation(out=jk[:], in_=o2[:], func=AF.Square, accum_out=sm0[:, 7:8]),
                             reads=[o2_r, sm0_r], writes=[jk_r, sm0_r])
                        S.op("act", lambda e, sm0=sm0: e.activation(out=sm0[:, 7:8], in_=sm0[:, 7:8], func=AF.Sqrt, scale=1.0 / 128, bias=self.epsb[:, 0:1]),
                             reads=[sm0_r, self.epsb_r], writes=[sm0_r])
                        S.op("dve", lambda e, sm0=sm0: e.reciprocal(out=sm0[:, 7:8], in_=sm0[:, 7:8]), reads=[sm0_r], writes=[sm0_r])
                        S.op("dve", lambda e, o2=o2, sm0=sm0: e.tensor_scalar(out=o2[:], in0=o2[:], scalar1=sm0[:, 7:8], scalar2=float(1.0 - lambda_init),
                                                                            op0=ALU.mult, op1=ALU.mult),
                             reads=[o2_r, sm0_r], writes=[o2_r])
                        S.op("pe", lambda e, o2=o2: e.transpose(out=pout[:, 128:256], in_=o2[:], identity=self.identf[:]),
                             reads=[o2_r, self.identf_r], writes=[pout_r], pe_acc=True)
                        S.op("act", lambda e, qb=qb: e.activation(out=oT[:, qb * 128:(qb + 1) * 128], in_=pout[:, 128:256], func=AF.Identity,
                                                                  scale=sgt[:, 0:1]),
                             reads=[pout_r, sgt_r], writes=[oT_r])
                    for (s, n) in LBLK:
                        for oc in range(8):
                            S.op("pe", lambda e, oc=oc, s=s, n=n: e.matmul(
                                pout[:, :n], lhsT=woc[:, 0, oc * 128:(oc + 1) * 128], rhs=oT[:, s - NCTX:s - NCTX + n], start=True, stop=True),
                                reads=[woc_r, oT_r], writes=[pout_r], pe_acc=True)
                            self.resid(pout, pout_r, n, oc, s)
                    S.barrier()
                    S.release([wq_r, wk_r, wv_r, woc_r])

    def mixer1(self, i):
        nc, S, M = self.nc, self.S, self.M
        w_in = self.inp("ssm_w_in", [1, D, 5184])
        w_out = self.inp("ssm_w_out", [1, 2048, D])
        svec = self.inp("ssm_vec", [128, 160])
        a_log = self.inp("ssm_a_log", [1, 64])
        dt_bias = self.inp("ssm_dt_bias", [1, 64])
        d_skip = self.inp("ssm_d", [1, 32])
        dr = lambda name, shape: (nc.dram_tensor(name, list(shape), BF16, kind="Internal").ap(), Res(name))
        XS, XS_r = dr("scr_xs", [NT, 128, 2048])
        BTd, BTd_r = dr("scr_bt", [NT, 128, 4, 128])
        CTd, CTd_r = dr("scr_ct", [NT, 128, 4, 128])
        BMd, BMd_r = dr("scr_bm", [NT, 128, 512])
        ZS, ZS_r = dr("scr_zs", [NT, 128, 2048])
        HB, HB_r = dr("scr_hb", [NT, 128, 2048])
        PW = 2312
        OW = 2308

        def pcol(s):
            return 2 if s == 0 else s + 6
        with ExitStack() as pA:
            sv, sv_r = M.sb(pA, [128, 160], F32, "sv")
            S.dma("sp", sv[:], svec, writes=[sv_r], key=sv_r)
            msk, msk_r = M.sb(pA, [128, 4, 128], F32, "msk")
            S.op("pool", lambda e: e.memset(msk[:], 1.0), writes=[msk_r])
            for a, (pat, cm, op) in enumerate((([[1, 128]], -1, ALU.is_ge), ([[-1, 128]], 1, ALU.is_ge),
                                              ([[-1, 128]], 1, ALU.is_gt), ([[1, 128]], -1, ALU.is_gt))):
                S.op("pool", lambda e, a=a, pat=pat, cm=cm, op=op: e.affine_select(
                    out=msk[:, a, :], in_=msk[:, a, :], pattern=pat, compare_op=op, fill=0.0, base=0, channel_multiplier=cm),
                    reads=[msk_r], writes=[msk_r])
            triF, triB, mltF, mltB = (msk[:, a, :] for a in range(4))
            dt, dt_r = M.sb(pA, [128, NT, 64], F32, "dt")
            loga, loga_r = M.sb(pA, [128, NT, 64], F32, "loga")
            expA, expA_r = M.sb(pA, [128, NT, 64], F32, "expA")
            dec, dec_r = M.sb(pA, [128, NT, 64], F32, "dec")
            wst, wst_r = M.sb(pA, [128, NT, 64], F32, "wst")
            abc, abc_r = M.sb(pA, [128, 64], F32, "abc")
            dtb, dtb_r = M.sb(pA, [128, 64], F32, "dtb")
            dsk, dsk_r = M.sb(pA, [128, 32], F32, "dsk")
            S.dma("sp", abc[:], a_log[0].partition_broadcast(128), writes=[abc_r], key=abc_r)
            S.dma("sp", dtb[:], dt_bias[0].partition_broadcast(128), writes=[dtb_r], key=dtb_r)
            S.dma("sp", dsk[:], d_skip[0].partition_broadcast(128), writes=[dsk_r], key=dsk_r)
            S.op("act", lambda e: e.activation(out=abc[:], in_=abc[:], func=AF.Exp), reads=[abc_r], writes=[abc_r])
            S.op("dve", lambda e: e.tensor_scalar(out=abc[:], in0=abc[:], scalar1=-1.0, scalar2=None, op0=ALU.mult), reads=[abc_r], writes=[abc_r])
            with ExitStack() as pB:
                hT, hT_r = M.sb(pB, [128, 8, T], BF16, "hT")
                with ExitStack() as pD:
                    self.adanorm(pD, hT, hT_r, 0)
                    S.barrier()
                with ExitStack() as pC:
                    wdt, wdt_r = self.load_w(pC, w_in[0, :, 5120:5184], 8, 64, "wdt")
                    pdt = Ring([M.ps(pC, [128, 64], F32, "pdt") for _ in range(2)])
                    pcs = Ring([M.ps(pC, [128, 2, 64], F32, "pcs") for _ in range(2)])
                    tmr = Ring([[M.sb(pC, [128, 64], F32, "sp%d" % j) for j in range(4)] for _ in range(2)])
                    for t in range(NT):
                        p_, p_r = pdt.next()
                        for k in range(8):
                            S.op("pe", lambda e, p_=p_, t=t, k=k: e.matmul(p_[:], lhsT=hT[:, k, t * 128:(t + 1) * 128], rhs=wdt[:, k, :],
                                                                          start=(k == 0), stop=(k == 7)),
                                 reads=[hT_r, wdt_r], writes=[p_r], pe_acc=True)
                        (x_, x_r), (ax, ax_r), (ex, ex_r), (rl, rl_r) = tmr.next()
                        S.op("dve", lambda e, x_=x_, p_=p_: e.tensor_tensor(out=x_[:], in0=p_[:], in1=dtb[:], op=ALU.add),
                             reads=[p_r, dtb_r], writes=[x_r])
                        S.op("act", lambda e, ax=ax, x_=x_: e.activation(out=ax[:], in_=x_[:], func=AF.Abs),
                             reads=[x_r], writes=[ax_r])
                        S.op("act", lambda e, ex=ex, ax=ax: e.activation(out=ex[:], in_=ax[:], func=AF.Exp, scale=-1.0), reads=[ax_r], writes=[ex_r])
                        S.op("act", lambda e, ex=ex: e.activation(out=ex[:], in_=ex[:], func=AF.Ln, bias=self.onesf[:, 0:1], scale=1.0),
                             reads=[ex_r, self.onesf_r], writes=[ex_r])
                        S.op("dve", lambda e, rl=rl, x_=x_: e.tensor_scalar(out=rl[:], in0=x_[:], scalar1=0.0, scalar2=None, op0=ALU.max),
                             reads=[x_r], writes=[rl_r])
                        S.op("dve", lambda e, rl=rl, ex=ex, t=t: e.tensor_tensor(out=dt[:, t, :], in0=rl[:], in1=ex[:], op=ALU.add),
                             reads=[rl_r, ex_r], writes=[dt_r])
                        S.op("dve", lambda e, t=t: e.tensor_tensor(out=loga[:, t, :], in0=dt[:, t, :], in1=abc[:], op=ALU.mult),
                             reads=[dt_r, abc_r], writes=[loga_r])
                        c_, c_r = pcs.next()
                        S.op("pe", lambda e, c_=c_, t=t: e.matmul(c_[:, 0, 0:32], lhsT=triF, rhs=loga[:, t, 0:32], start=True, stop=True),
                             reads=[msk_r, loga_r], writes=[c_r], pe_acc=True)
                        S.op("pe", lambda e, c_=c_, t=t: e.matmul(c_[:, 0, 32:64], lhsT=triB, rhs=loga[:, t, 32:64], start=True, stop=True),
                             reads=[msk_r, loga_r], writes=[c_r], pe_acc=True)
                        S.op("pe", lambda e, c_=c_, t=t: e.matmul(c_[:, 1, :], lhsT=self.onesf[:], rhs=loga[:, t, :], start=True, stop=True),
                             reads=[self.onesf_r, loga_r], writes=[c_r], pe_acc=True)
                        S.op("act", lambda e, c_=c_, t=t: e.activation(out=expA[:, t, :], in_=c_[:, 0, :], func=AF.Exp), reads=[c_r], writes=[expA_r])
                        S.op("act", lambda e, c_=c_, t=t: e.activation(out=dec[:, t, :], in_=c_[:, 1, :], func=AF.Exp), reads=[c_r], writes=[dec_r])
                        S.op("act", lambda e, c_=c_, ax=ax: e.activation(out=ax[:], in_=c_[:, 0, :], func=AF.Copy), reads=[c_r, ax_r], writes=[ax_r])
                        S.op("dve", lambda e, c_=c_, ax=ax: e.tensor_tensor(out=ax[:], in0=c_[:, 1, :], in1=ax[:], op=ALU.subtract),
                             reads=[c_r, ax_r], writes=[ax_r])
                        S.op("act", lambda e, ax=ax: e.activation(out=ax[:], in_=ax[:], func=AF.Exp), reads=[ax_r], writes=[ax_r])
                        S.op("dve", lambda e, ax=ax, t=t: e.tensor_tensor(out=wst[:, t, :], in0=ax[:], in1=dt[:, t, :], op=ALU.mult),
                             reads=[ax_r, dt_r], writes=[wst_r])
                    S.barrier()
                    S.release([wdt_r])
                with ExitStack() as pC:
                    wz, wz_r = self.load_w(pC, w_in[0, :, 0:2048], 8, 2048, "wz")
                    pz, pz_r = M.ps(pC, [128, 4, 512], F32, "pz")
                    zr = Ring([M.sb(pC, [128, 2048], BF16, "zt") for _ in range(2)])
                    for t in range(NT):
                        for nb in range(4):
                            for k in range(8):
                                S.op("pe", lambda e, t=t, nb=nb, k=k: e.matmul(pz[:, nb, :], lhsT=hT[:, k, t * 128:(t + 1) * 128],
                                                                               rhs=wz[:, k, nb * 512:(nb + 1) * 512], start=(k == 0), stop=(k == 7)),
                                     reads=[hT_r, wz_r], writes=[pz_r], pe_acc=True)
                        z_, z_r = zr.next()
                        S.op("act", lambda e, z_=z_: e.activation(out=z_[:].rearrange("p (a b) -> p a b", b=512), in_=pz[:], func=AF.Silu),
                             reads=[pz_r], writes=[z_r])
                        S.dma("sp", ZS[t], z_[:], reads=[z_r], writes=[ZS_r], key=z_r)
                    S.barrier()
                    S.release([wz_r] + [r for _, r in zr.items])
                with ExitStack() as pC:
                    wpr = Ring([M.sb(pC, [128, 8, 128], BF16, "wxp") for _ in range(3)])
                    upr = Ring([M.sb(pC, [128, PW], F32, "upad") for _ in range(1)])
                    acr = Ring([M.sb(pC, [128, OW], F32, "cacc") for _ in range(1)])
                    scr = Ring([M.sb(pC, [128, T], BF16, "scc") for _ in range(2)])
                    xh, xh_r = M.sb(pC, [128, NT, 512], BF16, "xhalf")
                    btm, btm_r = M.sb(pC, [128, NT, 128], BF16, "btm")
                    pp = Ring([M.ps(pC, [128, 512], F32, "pxp") for _ in range(2)])
                    ptr = Ring([M.ps(pC, [128, 6, 128], BF16, "ptx") for _ in range(2)])
                    for u, u_r in upr.items:
                        S.op("pool", lambda e, u=u: e.memset(u[:], 0.0), writes=[u_r])
                    for cc in range(24):
                        wt, wt_r = wpr.next()
                        S.dma("pool", wt[:], w_in[0, :, 2048 + cc * 128:2048 + (cc + 1) * 128].rearrange("(k p) n -> p k n", p=128),
                              writes=[wt_r], key=wt_r)
                        u, u_r = upr.next()
                        for (s, n) in BLKS:
                            p_, p_r = pp.next()
                            for k in range(8):
                                S.op("pe", lambda e, p_=p_, wt=wt, k=k, s=s, n=n: e.matmul(p_[:, :n], lhsT=wt[:, k, :], rhs=hT[:, k, s:s + n],
                                                                                          start=(k == 0), stop=(k == 7)),
                                     reads=[wt_r, hT_r], writes=[p_r], pe_acc=True)
                            S.op("act", lambda e, p_=p_, u=u, s=s, n=n: e.activation(out=u[:, pcol(s):pcol(s) + n], in_=p_[:, :n], func=AF.Copy),
                                 reads=[p_r], writes=[u_r])
                        ac, ac_r = acr.next()
                        eng = "dve"
                        S.op(eng, lambda e, ac=ac, u=u, cc=cc: e.tensor_scalar(
                            out=ac[:], in0=u[:, 0:OW], scalar1=sv[:, cc * 5:cc * 5 + 1], scalar2=sv[:, 120 + cc:121 + cc], op0=ALU.mult, op1=ALU.add),
                            reads=[u_r, sv_r], writes=[ac_r])
                        for w in range(1, 5):
                            S.op(eng, lambda e, ac=ac, u=u, cc=cc, w=w: e.scalar_tensor_tensor(
                                out=ac[:], in0=u[:, w:w + OW], scalar=sv[:, cc * 5 + w:cc * 5 + w + 1], in1=ac[:], op0=ALU.mult, op1=ALU.add),
                                reads=[u_r, sv_r, ac_r], writes=[ac_r])
                        sc, sc_r = scr.next()
                        S.op("act", lambda e, sc=sc, ac=ac: e.activation(out=sc[:, 0:NCTX], in_=ac[:, 0:NCTX], func=AF.Silu), reads=[ac_r], writes=[sc_r])
                        S.op("act", lambda e, sc=sc, ac=ac: e.activation(out=sc[:, NCTX:T], in_=ac[:, 260:260 + NLAT], func=AF.Silu), reads=[ac_r], writes=[sc_r])
                        if cc < 20:
                            for b3 in range(3):
                                pt, pt_r = ptr.next()
                                for jj in range(6):
                                    t = b3 * 6 + jj
                                    S.op("pe", lambda e, pt=pt, jj=jj, t=t, sc=sc: e.transpose(out=pt[:, jj, :], in_=sc[:, t * 128:(t + 1) * 128], identity=self.identb[:]),
                                         reads=[sc_r, self.identb_r], writes=[pt_r], pe_acc=True)
                                if cc < 16:
                                    c8 = cc % 4
                                    S.op("dve", lambda e, pt=pt, b3=b3, c8=c8: e.tensor_copy(out=xh[:, b3 * 6:(b3 + 1) * 6, c8 * 128:(c8 + 1) * 128], in_=pt[:]),
                                         reads=[pt_r], writes=[xh_r])
                                else:
                                    S.op("dve", lambda e, pt=pt, b3=b3: e.tensor_copy(out=btm[:, b3 * 6:(b3 + 1) * 6, :], in_=pt[:]),
                                         reads=[pt_r], writes=[btm_r])
                        if cc < 16 and cc % 4 == 3:
                            half = cc // 4
                            S.dma("sp", XS[:, :, half * 512:(half + 1) * 512].rearrange("t l f -> l t f"), xh[:],
                                  reads=[xh_r], writes=[XS_r], key=xh_r)
                        if 16 <= cc < 20:
                            g = cc - 16
                            S.dma("sp", BTd[:, :, g, :].rearrange("t n l -> n t l"), sc[:].rearrange("p (t l) -> p t l", l=128),
                                  reads=[sc_r], writes=[BTd_r], key=sc_r)
                            S.dma("sp", BMd[:, :, g * 128:(g + 1) * 128].rearrange("t l n -> l t n"), btm[:],
                                  reads=[btm_r], writes=[BMd_r], key=btm_r)
                        if cc >= 20:
                            g = cc - 20
                            S.dma("sp", CTd[:, :, g, :].rearrange("t n l -> n t l"), sc[:].rearrange("p (t l) -> p t l", l=128),
                                  reads=[sc_r], writes=[CTd_r], key=sc_r)
                    S.barrier()
                    S.release([r for _, r in wpr.items] + [r for _, r in scr.items] + [xh_r, btm_r])
            order_b = [1, 0] + list(range(NT - 1, 1, -1))
            Hf, Hf_r = M.sb(pA, [128, 4, 512], F32, "Hst")
            Hb16, Hb16_r = M.sb(pA, [128, 4, 512], BF16, "Hst16")
            pst, pst_r = M.ps(pA, [128, 512], F32, "pst")
            xsr = Ring([M.sb(pA, [128, 2048], BF16, "xs") for _ in range(2)])
            bmr = Ring([M.sb(pA, [128, 512], BF16, "bm") for _ in range(2)])
            xwr = Ring([M.sb(pA, [128, 2048], BF16, "xw") for _ in range(1)])
            hbo = Ring([M.sb(pA, [128, 2048], BF16, "hbo") for _ in range(2)])

            def bc(ap2d):
                return ap2d.unsqueeze(2).to_broadcast([128, 8, 64])

            def v3(ap2d):
                return ap2d.rearrange("p (h d) -> p h d", d=64)

            def state_update(t, xs, xs_r, bm, bm_r, d0):
                xw, xw_r = xwr.next()
                for g in range(4):
                    S.op("pool" if g % 2 else "dve", lambda e, g=g, xw=xw, xs=xs, t=t: e.tensor_tensor(
                        out=v3(xw[:, g * 512:(g + 1) * 512]), in0=v3(xs[:, g * 512:(g + 1) * 512]),
                        in1=bc(wst[:, t, d0 + g * 8:d0 + (g + 1) * 8]), op=ALU.mult),
                        reads=[xs_r, wst_r], writes=[xw_r])
                for g in range(4):
                    S.op("pe", lambda e, g=g, bm=bm, xw=xw: e.matmul(pst[:], lhsT=bm[:, g * 128:(g + 1) * 128], rhs=xw[:, g * 512:(g + 1) * 512],
                                                                     start=True, stop=True),
                         reads=[bm_r, xw_r], writes=[pst_r], pe_acc=True)
                    S.op("dve", lambda e, g=g, t=t: e.tensor_tensor(out=v3(Hf[:, g, :]), in0=v3(Hf[:, g, :]),
                                                                    in1=bc(dec[:, t, d0 + g * 8:d0 + (g + 1) * 8]), op=ALU.mult),
                         reads=[Hf_r, dec_r], writes=[Hf_r])
                    S.op("dve", lambda e, g=g: e.tensor_tensor(out=Hf[:, g, :], in0=Hf[:, g, :], in1=pst[:], op=ALU.add),
                         reads=[Hf_r, pst_r], writes=[Hf_r])
            S.op("pool", lambda e: e.memset(Hf[:], 0.0), writes=[Hf_r])
            for t in order_b:
                xs, xs_r = xsr.next()
                bm, bm_r = bmr.next()
                S.dma("sp", xs[:], XS[t], reads=[XS_r], writes=[xs_r], key=xs_r)
                S.dma("sp", bm[:], BMd[t], reads=[BMd_r], writes=[bm_r], key=bm_r)
                ho, ho_r = hbo.next()
                S.op("act", lambda e, ho=ho: e.activation(out=ho[:].rearrange("p (g f) -> p g f", f=512), in_=Hf[:], func=AF.Copy),
                     reads=[Hf_r], writes=[ho_r])
                S.dma("sp", HB[t], ho[:], reads=[ho_r], writes=[HB_r], key=ho_r)
                state_update(t, xs, xs_r, bm, bm_r, 32)
            S.barrier()
            S.op("pool", lambda e: e.memset(Hf[:], 0.0), writes=[Hf_r])
            S.op("pool", lambda e: e.memset(Hb16[:], 0.0), writes=[Hb16_r])
            wo_t, wo_r = self.load_w(pA, w_out[0], 16, D, "wout")
            for kc in range(16):
                S.op("dve", lambda e, kc=kc: e.tensor_scalar(out=wo_t[:, kc, :], in0=wo_t[:, kc, :], scalar1=sv[:, 144 + kc:145 + kc], scalar2=None,
                                                            op0=ALU.mult), reads=[wo_r, sv_r], writes=[wo_r])
            btr = Ring([M.sb(pA, [128, 4, 128], BF16, "btc") for _ in range(2)])
            ctr = Ring([M.sb(pA, [128, 4, 128], BF16, "ctc") for _ in range(2)])
            zsr = Ring([M.sb(pA, [128, 2048], BF16, "zsc") for _ in range(2)])
            xdf, xdf_r = M.sb(pA, [128, 2048], BF16, "xdf")
            xdb, xdb_r = M.sb(pA, [128, 2048], BF16, "xdb")
            gm, gm_r = M.sb(pA, [128, 2, 128], F32, "gm")
            lhr = Ring([M.sb(pA, [128, 128], F32, "lh") for _ in range(3)])
            dhr = Ring([M.sb(pA, [128, 128], F32, "dh") for _ in range(3)])
            mtr = Ring([M.sb(pA, [128, 128], BF16, "mt") for _ in range(3)])
            yg, yg_r = M.sb(pA, [128, 512], F32, "yg")
            tq = Ring([M.sb(pA, [128, 512], F32, "tq") for _ in range(2)])
            ybf, ybf_r = M.sb(pA, [128, 2048], BF16, "ybf")
            ynT, ynT_r = M.sb(pA, [128, 16, 128], BF16, "ynT")
            ssq, ssq_r = M.sb(pA, [128, 8], F32, "ssq")
            psg = Ring([M.ps(pA, [128, 128], F32, "psg") for _ in range(2)])
            pd, pd_r = M.ps(pA, [128, 512], F32, "pd")
            pf, pf_r = M.ps(pA, [128, 2, 512], F32, "pf")
            ptT, ptT_r = M.ps(pA, [128, 8, 128], BF16, "ptT")
            pout, pout_r = M.ps(pA, [128, 128], F32, "pout")
            for t in range(NT):
                xs, xs_r = xsr.next()
                bm, bm_r = bmr.next()
                bt, bt_r = btr.next()
                ct, ct_r = ctr.next()
                zs, zs_r = zsr.next()
                hb, hb_r = hbo.next()
                S.dma("sp", xs[:], XS[t], reads=[XS_r], writes=[xs_r], key=xs_r)
                S.dma("sp", bm[:], BMd[t], reads=[BMd_r], writes=[bm_r], key=bm_r)
                S.dma("sp", bt[:], BTd[t], reads=[BTd_r], writes=[bt_r], key=bt_r)
                S.dma("sp", ct[:], CTd[t], reads=[CTd_r], writes=[ct_r], key=ct_r)
                S.dma("sp", zs[:], ZS[t], reads=[ZS_r], writes=[zs_r], key=zs_r)
                S.dma("sp", hb[:], HB[t], reads=[HB_r], writes=[hb_r], key=hb_r)
                for g in range(4):
                    S.op("dve", lambda e, g=g, xs=xs, t=t: e.tensor_tensor(out=v3(xdf[:, g * 512:(g + 1) * 512]), in0=v3(xs[:, g * 512:(g + 1) * 512]),
                                                                        in1=bc(dt[:, t, g * 8:(g + 1) * 8]), op=ALU.mult),
                         reads=[xs_r, dt_r], writes=[xdf_r])
                    S.op("pool", lambda e, g=g, xs=xs, t=t: e.tensor_tensor(out=v3(xdb[:, g * 512:(g + 1) * 512]), in0=v3(xs[:, g * 512:(g + 1) * 512]),
                                                                         in1=bc(dt[:, t, 32 + g * 8:32 + (g + 1) * 8]), op=ALU.mult),
                         reads=[xs_r, dt_r], writes=[xdb_r])
                S.op("pool", lambda e: e.memset(ssq[:], 0.0), reads=[ssq_r], writes=[ssq_r])
                for g in range(4):
                    S.op("pe", lambda e, g=g, bt=bt, ct=ct: e.matmul(pst[:, 0:128], lhsT=bt[:, g, :], rhs=ct[:, g, :], start=True, stop=True),
                         reads=[bt_r, ct_r], writes=[pst_r], pe_acc=True)
                    S.op("dve", lambda e: e.tensor_tensor(out=gm[:, 0, :], in0=pst[:, 0:128], in1=triF, op=ALU.mult), reads=[pst_r, msk_r], writes=[gm_r])
                    S.op("dve", lambda e: e.tensor_tensor(out=gm[:, 1, :], in0=pst[:, 0:128], in1=triB, op=ALU.mult), reads=[pst_r, msk_r], writes=[gm_r])
                    for r in range(8):
                        h = g * 8 + r
                        for d_, (mlt, tri, xd, xd_r) in enumerate(((mltF, triF, xdf, xdf_r), (mltB, triB, xdb, xdb_r))):
                            lh, lh_r = lhr.next()
                            S.op("act", lambda e, lh=lh, mlt=mlt, t=t, h=h, d_=d_: e.activation(
                                out=lh[:], in_=mlt, func=AF.Copy, scale=loga[:, t, d_ * 32 + h:d_ * 32 + h + 1]),
                                reads=[msk_r, loga_r], writes=[lh_r])
                            sg_, sg_r = psg.next()
                            S.op("pe", lambda e, sg_=sg_, lh=lh, tri=tri: e.matmul(sg_[:], lhsT=lh[:], rhs=tri, start=True, stop=True),
                                 reads=[lh_r, msk_r], writes=[sg_r], pe_acc=True)
                            dh, dh_r = dhr.next()
                            S.op("act", lambda e, dh=dh, sg_=sg_: e.activation(out=dh[:], in_=sg_[:], func=AF.Exp), reads=[sg_r], writes=[dh_r])
                            mt, mt_r = mtr.next()
                            S.op("dve", lambda e, mt=mt, dh=dh, d_=d_: e.tensor_tensor(out=mt[:], in0=gm[:, d_, :], in1=dh[:], op=ALU.mult),
                                 reads=[gm_r, dh_r], writes=[mt_r])
                            S.op("pe", lambda e, mt=mt, xd=xd, r=r, h=h, d_=d_: e.matmul(
                                pd[:, r * 64:(r + 1) * 64], lhsT=mt[:], rhs=xd[:, h * 64:(h + 1) * 64], start=(d_ == 0), stop=(d_ == 1)),
                                reads=[mt_r, xd_r], writes=[pd_r], pe_acc=True)
                    S.op("pe", lambda e, g=g, ct=ct: e.matmul(pf[:, 0, :], lhsT=ct[:, g, :], rhs=Hb16[:, g, :], start=True, stop=True),
                         reads=[ct_r, Hb16_r], writes=[pf_r], pe_acc=True)
                    S.op("pe", lambda e, g=g, ct=ct, hb=hb: e.matmul(pf[:, 1, :], lhsT=ct[:, g, :], rhs=hb[:, g * 512:(g + 1) * 512], start=True, stop=True),
                         reads=[ct_r, hb_r], writes=[pf_r], pe_acc=True)
                    S.op("act", lambda e: e.activation(out=yg[:], in_=pd[:], func=AF.Copy), reads=[pd_r], writes=[yg_r])
                    for d_ in range(2):
                        q_, q_r = tq.next()
                        S.op("dve", lambda e, q_=q_, d_=d_, t=t, g=g: e.tensor_tensor(out=v3(q_[:]), in0=v3(pf[:, d_, :]),
                                                                                  in1=bc(expA[:, t, d_ * 32 + g * 8:d_ * 32 + (g + 1) * 8]), op=ALU.mult),
                             reads=[pf_r, expA_r], writes=[q_r])
                        S.op("pool", lambda e, q_=q_: e.tensor_tensor(out=yg[:], in0=yg[:], in1=q_[:], op=ALU.add), reads=[yg_r, q_r], writes=[yg_r])
                    q_, q_r = tq.next()
                    S.op("dve", lambda e, q_=q_, g=g, xs=xs: e.tensor_tensor(out=v3(q_[:]), in0=v3(xs[:, g * 512:(g + 1) * 512]),
                                                                          in1=bc(dsk[:, g * 8:(g + 1) * 8]), op=ALU.mult),
                         reads=[xs_r, dsk_r], writes=[q_r])
                    S.op("pool", lambda e, q_=q_: e.tensor_tensor(out=yg[:], in0=yg[:], in1=q_[:], op=ALU.add), reads=[yg_r, q_r], writes=[yg_r])
                    S.op("dve", lambda e, g=g, zs=zs: e.tensor_tensor(out=yg[:], in0=yg[:], in1=zs[:, g * 512:(g + 1) * 512], op=ALU.mult),
                         reads=[yg_r, zs_r], writes=[yg_r])
                    jk, jk_r = tq.next()
                    S.op("act", lambda e, g=g, jk=jk: e.activation(out=jk[:], in_=yg[:], func=AF.Square, accum_out=ssq[:, g:g + 1]),
                         reads=[yg_r, ssq_r], writes=[jk_r, ssq_r])
                    S.op("pool", lambda e, g=g: e.tensor_copy(out=ybf[:, g * 512:(g + 1) * 512], in_=yg[:]), reads=[yg_r], writes=[ybf_r])
                S.op("dve", lambda e: e.reduce_sum(out=ssq[:, 4:5], in_=ssq[:, 0:4], axis=AX.X), reads=[ssq_r], writes=[ssq_r])
                S.op("act", lambda e: e.activation(out=ssq[:, 5:6], in_=ssq[:, 4:5], func=AF.Sqrt, scale=1.0 / 2048, bias=self.epsb[:, 0:1]),
                     reads=[ssq_r, self.epsb_r], writes=[ssq_r])
                S.op("dve", lambda e: e.reciprocal(out=ssq[:, 6:7], in_=ssq[:, 5:6]), reads=[ssq_r], writes=[ssq_r])
                S.op("dve", lambda e: e.tensor_scalar(out=ybf[:], in0=ybf[:], scalar1=ssq[:, 6:7], scalar2=None, op0=ALU.mult),
                     reads=[ybf_r, ssq_r], writes=[ybf_r])
                for b2 in range(2):
                    for jj in range(8):
                        kc = b2 * 8 + jj
                        S.op("pe", lambda e, jj=jj, kc=kc: e.transpose(out=ptT[:, jj, :], in_=ybf[:, kc * 128:(kc + 1) * 128], identity=self.identb[:]),
                             reads=[ybf_r, self.identb_r], writes=[ptT_r], pe_acc=True)
                    S.op("act", lambda e, b2=b2: e.activation(out=ynT[:, b2 * 8:(b2 + 1) * 8, :], in_=ptT[:], func=AF.Copy), reads=[ptT_r], writes=[ynT_r])
                for oc in range(8):
                    for kc in range(16):
                        S.op("pe", lambda e, oc=oc, kc=kc: e.matmul(pout[:], lhsT=wo_t[:, kc, oc * 128:(oc + 1) * 128], rhs=ynT[:, kc, :],
                                                                    start=(kc == 0), stop=(kc == 15)),
                             reads=[wo_r, ynT_r], writes=[pout_r], pe_acc=True)
                    self.resid(pout, pout_r, 128, oc, t * 128)
                state_update(t, xs, xs_r, bm, bm_r, 0)
                S.op("act", lambda e: e.activation(out=Hb16[:], in_=Hf[:], func=AF.Copy), reads=[Hf_r], writes=[Hb16_r])
            S.barrier()

    def out_raw(self):
        nc, S, M = self.nc, self.S, self.M
        y = nc.dram_tensor("y", [T, D], F32, kind="ExternalOutput").ap()
        y_r = Res("y")
        with ExitStack() as ps:
            pst = Ring([M.ps(ps, [128, 4, 128], F32, "otp") for _ in range(2)])
            stg = Ring([M.sb(ps, [128, D], F32, "ostg") for _ in range(2)])
            for t in range(NT):
                st, st_r = stg.next()
                for h in range(2):
                    pt, pt_r = pst.next()
                    for k in range(4):
                        S.op("pe", lambda e, pt=pt, k=k, h=h, t=t: e.transpose(
                            out=pt[:, k, :], in_=self.xT[:, h * 4 + k, t * 128:(t + 1) * 128], identity=self.identf[:]),
                            reads=[self.xTr, self.identf_r], writes=[pt_r], pe_acc=True)
                    S.op("dve", lambda e, pt=pt, st=st, h=h: e.tensor_copy(out=st[:, h * 512:(h + 1) * 512], in_=pt[:]),
                         reads=[pt_r], writes=[st_r])
                S.dma("sp", y[t * 128:(t + 1) * 128, :], st[:], reads=[st_r], writes=[y_r], key=st_r)
            S.barrier()

    def out_final(self):
        nc, S, M = self.nc, self.S, self.M
        y = nc.dram_tensor("y", [NLAT, D], F32, kind="ExternalOutput").ap()
        gfin = self.inp("g_final", [D])
        y_r = Res("y")
        with ExitStack() as ps:
            gb, gb_r = M.sb(ps, [128, D], F32, "gfin")
            S.dma("sp", gb[:], gfin.partition_broadcast(128), writes=[gb_r], key=gb_r)
            pst = Ring([M.ps(ps, [128, 4, 128], F32, "otp") for _ in range(2)])
            stg = Ring([M.sb(ps, [128, D], F32, "ostg") for _ in range(2)])
            jk = Ring([M.sb(ps, [128, D], F32, "ojk") for _ in range(2)])
            ssq = Ring([M.sb(ps, [128, 1], F32, "ossq") for _ in range(2)])
            for t in range(NCTX // 128, NT):
                st, st_r = stg.next()
                for h in range(2):
                    pt, pt_r = pst.next()
                    for k in range(4):
                        S.op("pe", lambda e, pt=pt, k=k, h=h, t=t: e.transpose(
                            out=pt[:, k, :], in_=self.xT[:, h * 4 + k, t * 128:(t + 1) * 128], identity=self.identf[:]),
                            reads=[self.xTr, self.identf_r], writes=[pt_r], pe_acc=True)
                    S.op("dve", lambda e, pt=pt, st=st, h=h: e.tensor_copy(out=st[:, h * 512:(h + 1) * 512], in_=pt[:]),
                         reads=[pt_r], writes=[st_r])
                j_, j_r = jk.next()
                q, q_r = ssq.next()
                S.op("act", lambda e, j_=j_, st=st, q=q: e.activation(out=j_[:], in_=st[:], func=AF.Square, accum_out=q[:]),
                     reads=[st_r], writes=[j_r, q_r])
                S.op("act", lambda e, q=q: e.activation(out=q[:], in_=q[:], func=AF.Sqrt, scale=1.0 / D, bias=self.epsb[:, 0:1]),
                     reads=[q_r, self.epsb_r], writes=[q_r])
                S.op("dve", lambda e, q=q: e.reciprocal(out=q[:], in_=q[:]), reads=[q_r], writes=[q_r])
                S.op("dve", lambda e, j_=j_, st=st, q=q: e.scalar_tensor_tensor(out=j_[:], in0=st[:], scalar=q[:, 0:1], in1=gb[:],
                                                                              op0=ALU.mult, op1=ALU.mult),
                     reads=[st_r, q_r, gb_r, j_r], writes=[j_r])
                r0 = (t - NCTX // 128) * 128
                S.dma("sp", y[r0:r0 + 128, :], j_[:], reads=[j_r], writes=[y_r], key=j_r)
            S.barrier()


FULL_STEPS = []
for _i in range(4):
    FULL_STEPS += [("mods", _i), ("mixer", _i), ("ffn", _i, _i < 3)]


def make_in_maps(inputs, ncores=8, xs=None, cs=None):
    f32 = np.float32
    shared = {}
    lvec = np.zeros((4, 128, 64), f32)
    for i in range(4):
        lvec[i, :, 0:48] = fm(inputs["ada_b"][i])
        lvec[i, :, 48:56] = fm(inputs["g_mix"][i])
        lvec[i, :, 56:64] = fm(inputs["g_ffn"][i])
    shared["lvec"] = lvec
    shared["ada_w"] = np.ascontiguousarray(inputs["ada_w"], f32)
    shared["g_final"] = np.ascontiguousarray(inputs["g_final"], f32)
    shared["moe_w_router"] = np.ascontiguousarray(inputs["moe_w_router"], f32)
    shared["moe_b_router"] = np.ascontiguousarray(inputs["moe_b_router"], f32)
    wgu = np.ascontiguousarray(inputs["moe_w_gu"], f32)
    wdn = np.ascontiguousarray(inputs["moe_w_down"], f32)
    shared["moe_w_gu"] = wgu
    shared["moe_w_down"] = wdn
    shared["moe_w_gu2d"] = wgu.reshape(-1, 2 * D)
    shared["moe_w_down2d"] = wdn.reshape(-1, D)
    shared["moe_b_down2d"] = np.ascontiguousarray(inputs["moe_b_down"], f32).reshape(-1, D)
    shared["moe_b_down"] = np.ascontiguousarray(inputs["moe_b_down"], f32)
    bgu = np.asarray(inputs["moe_b_gu"], f32)
    shared["moe_b_gu_fm"] = np.ascontiguousarray(bgu.reshape(4, NE, 16, 128).transpose(0, 3, 1, 2))
    shared["moe_b_gu_rows"] = np.ascontiguousarray(bgu.reshape(4, NE, 16, 128).transpose(0, 1, 3, 2)).reshape(4 * NE * 128, 16)
    if "conv_w_pw1" in inputs:
        shared["conv_w_pw1"] = np.ascontiguousarray(inputs["conv_w_pw1"], f32)
        shared["conv_w_pw2"] = np.ascontiguousarray(inputs["conv_w_pw2"], f32)
        cvec = np.zeros((128, 296), f32)
        cvec[:, 0:16] = fm(inputs["conv_b_pw1"][0])
        wdw = np.asarray(inputs["conv_w_dw"][0], f32)
        cvec[:, 16:264] = wdw.reshape(31, 8, 128).transpose(2, 1, 0).reshape(128, 248)
        cvec[:, 264:272] = fm(inputs["conv_b_dw"][0])
        cvec[:, 272:280] = fm(inputs["conv_ln_g"][0])
        cvec[:, 280:288] = fm(inputs["conv_ln_b"][0])
        cvec[:, 288:296] = fm(inputs["conv_b_pw2"][0])
        shared["conv_vec"] = cvec
    if "swa_w_qkv" in inputs:
        shared["swa_w_qkv"] = np.ascontiguousarray(inputs["swa_w_qkv"], f32)
        shared["swa_b_qkv"] = np.ascontiguousarray(inputs["swa_b_qkv"], f32)
        shared["swa_w_o"] = np.ascontiguousarray(inputs["swa_w_o"], f32)
        shared["swa_sinks"] = np.ascontiguousarray(inputs["swa_sinks"], f32)
        bq = np.asarray(inputs["swa_b_qkv"][0], f32).reshape(24, 64).T
        bh = np.zeros((64, 44), f32)
        bh[:, 0:24] = bq
        bh[:, 24:44] = np.roll(bq[:, 0:20], 32, axis=0)
        shared["swa_bh"] = bh
        shared["swa_bo_fm"] = fm(inputs["swa_b_o"][0])
        Ct, St = rope_tables()
        shared["rope_c"] = Ct
        shared["rope_s"] = St
    if "diff_w_qkv" in inputs:
        shared["diff_w_qkv"] = np.ascontiguousarray(inputs["diff_w_qkv"], f32)
        shared["diff_w_o"] = np.ascontiguousarray(inputs["diff_w_o"], f32)
        shared["diff_lam"] = np.ascontiguousarray(np.stack([inputs["diff_lambda_q1"][0], inputs["diff_lambda_k1"][0],
                                                            inputs["diff_lambda_q2"][0], inputs["diff_lambda_k2"][0]], 0), f32)
        shared["diff_subln_g"] = np.ascontiguousarray(np.asarray(inputs["diff_subln_g"][0], f32).reshape(128, 1))
        if "rope_c" not in shared:
            Ct, St = rope_tables()
            shared["rope_c"] = Ct
            shared["rope_s"] = St
    if "ssm_w_in" in inputs:
        shared["ssm_w_in"] = np.ascontiguousarray(inputs["ssm_w_in"], f32)
        shared["ssm_w_out"] = np.ascontiguousarray(inputs["ssm_w_out"], f32)
        svec = np.zeros((128, 160), f32)
        wc = np.asarray(inputs["ssm_w_conv"][0], f32)
        svec[:, 0:120] = wc.reshape(5, 24, 128).transpose(2, 1, 0).reshape(128, 120)
        svec[:, 120:144] = fm(inputs["ssm_b_conv"][0])
        svec[:, 144:160] = fm(inputs["ssm_norm_g"][0])
        shared["ssm_vec"] = svec
        shared["ssm_a_log"] = np.ascontiguousarray(np.asarray(inputs["ssm_a_log"], f32).reshape(1, 64))
        shared["ssm_dt_bias"] = np.ascontiguousarray(np.asarray(inputs["ssm_dt_bias"], f32).reshape(1, 64))
        shared["ssm_d"] = np.ascontiguousarray(np.asarray(inputs["ssm_d"], f32).reshape(1, 32))
    maps = []
    for b in range(ncores):
        m = dict(shared)
        m["x_in"] = np.ascontiguousarray(np.concatenate([inputs["ctx"][b], inputs["x"][b]], axis=0), f32)
        cond = np.stack([fm(inputs["c"][b]), fm(inputs["c_ctx"])], axis=-1)
        m["cond"] = np.ascontiguousarray(cond, f32)
        maps.append(m)
    return maps


def kernel(**inputs):
    prog = Prog(FULL_STEPS)
    nc = prog.build()
    maps = make_in_maps(inputs)
    maps = [{k: v for k, v in m.items() if k in prog.din} for m in maps]
    res = run_bass_kernel_spmd(nc, maps, core_ids=list(range(8)))
    return np.stack([r["y"] for r in res.results], axis=0).astype(np.float32)
```

```python
import math
import os
from contextlib import ExitStack

import numpy as np
import concourse.bass as bass
import concourse.mybir as mybir
from concourse.bass_utils import run_bass_kernel_spmd

F32 = mybir.dt.float32
BF16 = mybir.dt.bfloat16
I32 = mybir.dt.int32
AF = mybir.ActivationFunctionType
ALU = mybir.AluOpType
AX = mybir.AxisListType

D = 1024
NCTX = 256
NLAT = 2048
T = NCTX + NLAT
NT = T // 128
BLKS = [(0, 256), (256, 512), (768, 512), (1280, 512), (1792, 512)]
EPS = 1e-6
NE = 32
KMOE_NE = int(os.environ.get("KMOE_NE", NE))


class Res:
    __slots__ = ("name", "w", "r", "dsem", "dcnt")

    def __init__(self, name):
        self.name = name
        self.w = None
        self.r = []
        self.dsem = None
        self.dcnt = 0


class Sched:
    CE = ("pe", "act", "dve", "pool")
    ALLE = ("pe", "act", "dve", "pool", "sp")

    def __init__(self, nc, es):
        self.nc = nc
        self.es = es
        self.ops = {e: [] for e in self.ALLE}
        self.sems = {}
        for e in self.CE:
            self.sems["c_" + e] = es.enter_context(nc.semaphore("c_" + e))
        self.cnt = {e: 0 for e in self.CE}
        self.waited = {e: {} for e in self.ALLE}
        self.dtot = {}
        self.free_dsems = []
        self.ndsem = 0

    def _dsem(self, res):
        if res.dsem is None:
            if self.free_dsems:
                k = self.free_dsems.pop()
            else:
                k = "d%d" % self.ndsem
                self.ndsem += 1
                self.sems[k] = self.es.enter_context(self.nc.semaphore(k))
                self.dtot[k] = 0
            res.dsem = k
        return res.dsem

    def release(self, ress):
        for r in ress:
            if r.dsem is not None:
                self.free_dsems.append(r.dsem)
                r.dsem = None

    def _collect(self, e, reads, writes, pe_acc=False, skip_key=None):
        deps = {}

        def add(ev):
            if ev is None:
                return
            k, v = ev
            if deps.get(k, 0) < v:
                deps[k] = v
        for r in reads:
            add(r.w)
        for w in writes:
            if not (pe_acc and w.w is not None and w.w[0] == "c_pe") and not (
                    skip_key is not None and w.w is not None and w.w[0] == skip_key):
                add(w.w)
            for ev in w.r:
                add(ev)
        out = []
        wd = self.waited[e]
        for k, v in deps.items():
            if wd.get(k, 0) >= v:
                continue
            wd[k] = v
            out.append((k, v))
        return out

    def op(self, e, fn, reads=(), writes=(), pe_acc=False):
        waits = self._collect(e, reads, writes, pe_acc)
        self.cnt[e] += 1
        ev = ("c_" + e, self.cnt[e])
        for r in reads:
            r.r.append(ev)
        for w in writes:
            w.w = ev
            w.r = []
        self.ops[e].append((waits, fn, ev[0], 1))
        return ev

    def dma(self, q, out, in_, reads=(), writes=(), key=None, **kw):
        k = self._dsem(key)
        waits = self._collect(q, reads, writes, skip_key=k)
        self.dtot[k] += 16
        ev = (k, self.dtot[k])
        for r in reads:
            r.r.append(ev)
        for w in writes:
            w.w = ev
            w.r = []
        self.ops[q].append((waits, lambda eng: eng.dma_start(out=out, in_=in_, **kw), k, 16))
        return ev

    def idma(self, out, out_off, in_, in_off, bound, reads=(), writes=(), key=None):
        k = self._dsem(key)
        waits = self._collect("pool", reads, writes, skip_key=k)
        self.dtot[k] += 16
        ev = (k, self.dtot[k])
        for r in reads:
            r.r.append(ev)
        for w in writes:
            w.w = ev
            w.r = []
        oo = None if out_off is None else bass.IndirectOffsetOnAxis(ap=out_off, axis=0)
        io = None if in_off is None else bass.IndirectOffsetOnAxis(ap=in_off, axis=0)
        self.ops["pool"].append((waits, lambda eng: eng.indirect_dma_start(
            out=out, out_offset=oo, in_=in_, in_offset=io), k, 16))
        return ev

    def barrier(self):
        allev = [("c_" + e, self.cnt[e]) for e in self.CE if self.cnt[e] > 0]
        allev += [(k, v) for k, v in self.dtot.items() if v > 0]
        for e in self.ALLE:
            wd = self.waited[e]
            waits = []
            for k, v in allev:
                if wd.get(k, 0) < v:
                    wd[k] = v
                    waits.append((k, v))
            if waits:
                self.ops[e].append((waits, None, None, 0))

    def replay(self):
        nc = self.nc
        sems = self.sems
        ops = self.ops

        def run(eng, lst):
            for waits, fn, sk, n in lst:
                for k, v in waits:
                    eng.wait_ge(sems[k], v)
                if fn is not None:
                    fn(eng).then_inc(sems[sk], n)
        with nc.Block() as block:
            @block.sync
            def _(e):
                run(e, ops["sp"])

            @block.tensor
            def _(e):
                run(e, ops["pe"])

            @block.scalar
            def _(e):
                run(e, ops["act"])

            @block.vector
            def _(e):
                run(e, ops["dve"])

            @block.gpsimd
            def _(e):
                run(e, ops["pool"])


class Mem:
    def __init__(self, nc):
        self.nc = nc
        self.n = 0

    def sb(self, es, shape, dt, name=None):
        self.n += 1
        name = (name or "sb") + "_%d" % self.n
        t = es.enter_context(self.nc.sbuf_tensor(name, list(shape), dt))
        return t, Res(name)

    def ps(self, es, shape, dt, name=None):
        self.n += 1
        name = (name or "ps") + "_%d" % self.n
        t = es.enter_context(self.nc.psum_tensor(name, list(shape), dt))
        return t, Res(name)


class Ring:
    def __init__(self, items):
        self.items = items
        self.i = 0

    def next(self):
        it = self.items[self.i % len(self.items)]
        self.i += 1
        return it


def fm(v):
    v = np.asarray(v, np.float32)
    return np.ascontiguousarray(v.reshape(-1, 128).T)


def rope_tables():
    t = np.arange(NLAT)
    row = (t // 64).astype(np.float32)
    col = (t % 64).astype(np.float32)
    quarter = 16
    inv = (10000.0 ** (-np.arange(quarter, dtype=np.float32) / quarter)).astype(np.float32)
    ang = np.concatenate([row[:, None] * inv, col[:, None] * inv], axis=-1).astype(np.float32)
    cos = np.cos(ang).T.astype(np.float32)
    sin = np.sin(ang).T.astype(np.float32)
    C = np.ones((64, T), np.float32)
    S = np.zeros((64, T), np.float32)
    C[0:32, NCTX:] = cos
    C[32:64, NCTX:] = cos
    S[0:32, NCTX:] = -sin
    S[32:64, NCTX:] = sin
    return C, S


class Prog:
    def __init__(self, steps, raw_out=False):
        self.steps = steps
        self.raw_out = raw_out
        self.dbg = int(os.environ.get("KDBG", "0"))
        self.nc = bass.Bass("TRN2", target_bir_lowering=False)
        self.din = {}

    def inp(self, name, shape, dt=F32):
        if name not in self.din:
            self.din[name] = self.nc.dram_tensor(name, list(shape), dt, kind="ExternalInput").ap()
        return self.din[name]

    def build(self):
        nc = self.nc
        with ExitStack() as es:
            self.S = S = Sched(nc, es)
            self.M = M = Mem(nc)
            self.es = es
            self.xT, self.xTr = M.sb(es, [128, 8, T], F32, "xT")
            self.identf, self.identf_r = M.sb(es, [128, 128], F32, "identf")
            self.identb, self.identb_r = M.sb(es, [128, 128], BF16, "identb")
            self.onesf, self.onesf_r = M.sb(es, [128, 128], F32, "onesf")
            self.condT, self.condT_r = M.sb(es, [128, 8, 2], F32, "condT")
            self.mv, self.mv_r = M.sb(es, [128, 2, 6, 8], F32, "mv")
            self.epsb, self.epsb_r = M.sb(es, [128, 1], F32, "epsb")
            self.onesb, self.onesb_r = M.sb(es, [128, 128], BF16, "onesb")
            self.setup()
            for st in self.steps:
                kind = st[0]
                if kind == "mods":
                    self.mods(st[1])
                elif kind == "ffn":
                    self.ffn(st[1], with_ctx=st[2])
                elif kind == "mixer":
                    getattr(self, "mixer%d" % st[1])(st[1])
                S.barrier()
            if self.raw_out:
                self.out_raw()
            else:
                self.out_final()
            S.barrier()
            S.replay()
        return nc

    def setup(self):
        nc, S, M = self.nc, self.S, self.M
        x_in = self.inp("x_in", [T, D])
        cond = self.inp("cond", [128, 8, 2])
        identf, ifr = self.identf, self.identf_r
        S.op("pool", lambda e: e.memset(identf[:], 1.0), writes=[ifr])
        S.op("pool", lambda e: e.affine_select(out=identf[:], in_=identf[:], pattern=[[-1, 128]],
                                               compare_op=ALU.is_equal, fill=0.0, base=0, channel_multiplier=1),
             reads=[ifr], writes=[ifr])
        S.op("dve", lambda e: e.tensor_copy(out=self.identb[:], in_=identf[:]), reads=[ifr], writes=[self.identb_r])
        S.op("pool", lambda e: e.memset(self.onesf[:], 1.0), writes=[self.onesf_r])
        S.op("pool", lambda e: e.memset(self.epsb[:], EPS), writes=[self.epsb_r])
        S.op("pool", lambda e: e.memset(self.onesb[:], 1.0), writes=[self.onesb_r])
        with ExitStack() as ps:
            craw, craw_r = M.sb(ps, [128, 8, 2], F32, "craw")
            S.dma("sp", craw[:], cond, writes=[craw_r], key=craw_r)
            S.op("act", lambda e: e.activation(out=self.condT[:], in_=craw[:], func=AF.Silu),
                 reads=[craw_r], writes=[self.condT_r])
            stg = Ring([M.sb(ps, [128, D], F32, "xstg") for _ in range(2)])
            pst = Ring([M.ps(ps, [128, 4, 128], F32, "xtp") for _ in range(2)])
            for t in range(NT):
                st, st_r = stg.next()
                S.dma("sp", st[:], x_in[t * 128:(t + 1) * 128, :], writes=[st_r], key=st_r)
                for h in range(2):
                    pt, pt_r = pst.next()
                    for k in range(4):
                        S.op("pe", lambda e, pt=pt, st=st, k=k, h=h: e.transpose(
                            out=pt[:, k, :], in_=st[:, (h * 4 + k) * 128:(h * 4 + k + 1) * 128], identity=identf[:]),
                            reads=[st_r, ifr], writes=[pt_r], pe_acc=True)
                    eng = "dve" if h == 0 else "act"
                    if eng == "dve":
                        S.op("dve", lambda e, pt=pt, h=h, t=t: e.tensor_copy(
                            out=self.xT[:, h * 4:(h + 1) * 4, t * 128:(t + 1) * 128], in_=pt[:]),
                            reads=[pt_r], writes=[self.xTr])
                    else:
                        S.op("act", lambda e, pt=pt, h=h, t=t: e.activation(
                            out=self.xT[:, h * 4:(h + 1) * 4, t * 128:(t + 1) * 128], in_=pt[:], func=AF.Copy),
                            reads=[pt_r], writes=[self.xTr])
            S.barrier()
            S.release([craw_r] + [r for _, r in stg.items])

    def mods(self, i):
        nc, S, M = self.nc, self.S, self.M
        ada_w = self.inp("ada_w", [4, D, 6 * D])
        lv = self.inp("lvec", [4, 128, 64])
        with ExitStack() as ps:
            lvt, lvt_r = M.sb(ps, [128, 64], F32, "lvt")
            S.dma("sp", lvt[:], lv[i], writes=[lvt_r], key=lvt_r)
            wring = Ring([M.sb(ps, [128, 8, 512], F32, "adaw") for _ in range(2)])
            pm, pm_r = M.ps(ps, [128, 48, 2], F32, "pm")
            md, md_r = M.sb(ps, [128, 2, 48], F32, "md")
            for pi in range(12):
                wt, wt_r = wring.next()
                S.dma("sp", wt[:], ada_w[i, :, pi * 512:(pi + 1) * 512].rearrange("(k p) n -> p k n", p=128),
                      writes=[wt_r], key=wt_r)
                for o4 in range(4):
                    ob = pi * 4 + o4
                    for k in range(8):
                        S.op("pe", lambda e, wt=wt, k=k, o4=o4, ob=ob: e.matmul(
                            pm[:, ob, :], lhsT=wt[:, k, o4 * 128:(o4 + 1) * 128], rhs=self.condT[:, k, :],
                            start=(k == 0), stop=(k == 7)),
                            reads=[wt_r, self.condT_r], writes=[pm_r], pe_acc=True)
            for c in range(2):
                S.op("dve", lambda e, c=c: e.tensor_tensor(out=md[:, c, :], in0=pm[:, :, c], in1=lvt[:, 0:48], op=ALU.add),
                     reads=[pm_r, lvt_r], writes=[md_r])
            mv, mv_r = self.mv, self.mv_r
            for c in range(2):
                S.op("dve", lambda e, c=c: e.scalar_tensor_tensor(out=mv[:, c, 0, :], in0=md[:, c, 8:16], scalar=1.0,
                                                                  in1=lvt[:, 48:56], op0=ALU.add, op1=ALU.mult),
                     reads=[md_r, lvt_r], writes=[mv_r])
                S.op("dve", lambda e, c=c: e.tensor_copy(out=mv[:, c, 1, :], in_=md[:, c, 0:8]), reads=[md_r], writes=[mv_r])
                S.op("dve", lambda e, c=c: e.tensor_copy(out=mv[:, c, 2, :], in_=md[:, c, 16:24]), reads=[md_r], writes=[mv_r])
                S.op("dve", lambda e, c=c: e.scalar_tensor_tensor(out=mv[:, c, 3, :], in0=md[:, c, 32:40], scalar=1.0,
                                                                  in1=lvt[:, 56:64], op0=ALU.add, op1=ALU.mult),
                     reads=[md_r, lvt_r], writes=[mv_r])
                S.op("dve", lambda e, c=c: e.tensor_copy(out=mv[:, c, 4, :], in_=md[:, c, 24:32]), reads=[md_r], writes=[mv_r])
                S.op("dve", lambda e, c=c: e.tensor_copy(out=mv[:, c, 5, :], in_=md[:, c, 40:48]), reads=[md_r], writes=[mv_r])
            S.barrier()
            S.release([lvt_r] + [r for _, r in wring.items])

    def adanorm(self, ps, hT, hT_r, slotA, with_ctx=True, router=None):
        nc, S, M = self.nc, self.S, self.M
        xT, xTr = self.xT, self.xTr
        sq = Ring([M.sb(ps, [128, 512], F32, "nsq") for _ in range(2)])
        if router is not None:
            h32 = Ring([M.sb(ps, [128, 8, 512], F32, "nh32") for _ in range(1)])
        pss = Ring([M.ps(ps, [128, 512], F32, "nss") for _ in range(2)])
        rst = Ring([M.sb(ps, [128, 512], F32, "nrstd") for _ in range(2)])
        tmp = Ring([M.sb(ps, [128, 512], F32, "ntmp") for _ in range(2)])
        if router is not None:
            plg = Ring([M.ps(ps, [128, 32], F32, "rlg") for _ in range(2)])
            pgt = Ring([M.ps(ps, [32, 128], F32, "rgt") for _ in range(2)])
            rt = Ring([[M.sb(ps, [128, 32], F32, "rt%d" % j) for j in range(4)] for _ in range(2)])
            rs = Ring([[M.sb(ps, [128, 8], F32, "rs%d" % j) for j in range(4)] for _ in range(2)])
        for (s, n) in BLKS:
            if s == 0 and not with_ctx:
                continue
            c = 1 if s == 0 else 0
            A = self.mv[:, c, slotA, :]
            B = self.mv[:, c, slotA + 1, :]
            pp, pp_r = pss.next()
            for k in range(8):
                q, q_r = sq.next()
                S.op("act", lambda e, q=q, s=s, n=n, k=k: e.activation(out=q[:, :n], in_=xT[:, k, s:s + n], func=AF.Square),
                     reads=[xTr], writes=[q_r])
                S.op("pe", lambda e, pp=pp, q=q, k=k, n=n: e.matmul(pp[:, :n], lhsT=self.onesf[:], rhs=q[:, :n],
                                                                    start=(k == 0), stop=(k == 7)),
                     reads=[q_r, self.onesf_r], writes=[pp_r], pe_acc=True)
            r, r_r = rst.next()
            S.op("act", lambda e, r=r, pp=pp, n=n: e.activation(out=r[:, :n], in_=pp[:, :n], func=AF.Sqrt, scale=1.0 / D,
                                                                bias=self.epsb[:, 0:1]),
                 reads=[pp_r, self.epsb_r], writes=[r_r])
            S.op("dve", lambda e, r=r, n=n: e.reciprocal(out=r[:, :n], in_=r[:, :n]), reads=[r_r], writes=[r_r])
            if router is not None:
                hh, hh_r = h32.next()
            for k in range(8):
                t_, t_r = tmp.next()
                S.op("dve", lambda e, t_=t_, k=k, s=s, n=n, r=r: e.tensor_tensor(out=t_[:, :n], in0=xT[:, k, s:s + n], in1=r[:, :n],
                                                                                 op=ALU.mult),
                     reads=[xTr, r_r], writes=[t_r])
                if router is not None:
                    S.op("act", lambda e, t_=t_, hh=hh, k=k, n=n, A=A, B=B: e.activation(
                        out=hh[:, k, :n], in_=t_[:, :n], func=AF.Identity, scale=A[:, k:k + 1], bias=B[:, k:k + 1]),
                        reads=[t_r, self.mv_r], writes=[hh_r])
                    S.op("pool", lambda e, hh=hh, k=k, s=s, n=n: e.tensor_copy(out=hT[:, k, s:s + n], in_=hh[:, k, :n]),
                         reads=[hh_r], writes=[hT_r])
                else:
                    S.op("act", lambda e, t_=t_, k=k, s=s, n=n, A=A, B=B: e.activation(
                        out=hT[:, k, s:s + n], in_=t_[:, :n], func=AF.Identity, scale=A[:, k:k + 1], bias=B[:, k:k + 1]),
                        reads=[t_r, self.mv_r], writes=[hT_r])
            if router is not None:
                R = router
                for tt in range(n // 128):
                    lg, lg_r = plg.next()
                    for k in range(8):
                        S.op("pe", lambda e, lg=lg, hh=hh, k=k, tt=tt: e.matmul(
                            lg[:], lhsT=hh[:, k, tt * 128:(tt + 1) * 128], rhs=R["wr"][:, k, :], start=(k == 0), stop=(k == 7)),
                            reads=[hh_r, R["wr_r"]], writes=[lg_r], pe_acc=True)
                    (l, l_r), (ex, ex_r), (mk, mk_r), (gt, gt_r) = rt.next()
                    (m8, m8_r), (ng, ng_r), (sm, sm_r), (rc, rc_r) = rs.next()
                    S.op("dve", lambda e, l=l, lg=lg: e.tensor_tensor(out=l[:], in0=lg[:], in1=R["brb"][:], op=ALU.add),
                         reads=[lg_r, R["brb_r"]], writes=[l_r])
                    S.op("dve", lambda e, m8=m8, l=l: e.max(out=m8[:], in_=l[:]), reads=[l_r], writes=[m8_r])
                    S.op("dve", lambda e, ng=ng, m8=m8: e.tensor_scalar(out=ng[:, 0:1], in0=m8[:, 0:1], scalar1=-1.0, scalar2=None,
                                                                        op0=ALU.mult),
                         reads=[m8_r], writes=[ng_r])
                    S.op("act", lambda e, ex=ex, l=l, ng=ng: e.activation(out=ex[:], in_=l[:], func=AF.Exp, bias=ng[:, 0:1], scale=1.0),
                         reads=[l_r, ng_r], writes=[ex_r])
                    S.op("dve", lambda e, mk=mk, l=l, m8=m8: e.tensor_scalar(out=mk[:], in0=l[:], scalar1=m8[:, 3:4], scalar2=None,
                                                                            op0=ALU.is_ge),
                         reads=[l_r, m8_r], writes=[mk_r])
                    S.op("dve", lambda e, ex=ex, mk=mk: e.tensor_tensor(out=ex[:], in0=ex[:], in1=mk[:], op=ALU.mult),
                         reads=[ex_r, mk_r], writes=[ex_r])
                    S.op("dve", lambda e, sm=sm, ex=ex: e.reduce_sum(out=sm[:, 0:1], in_=ex[:], axis=AX.X), reads=[ex_r], writes=[sm_r])
                    S.op("dve", lambda e, rc=rc, sm=sm: e.reciprocal(out=rc[:, 0:1], in_=sm[:, 0:1]), reads=[sm_r], writes=[rc_r])
                    S.op("dve", lambda e, gt=gt, ex=ex, rc=rc: e.tensor_scalar(out=gt[:], in0=ex[:], scalar1=rc[:, 0:1], scalar2=None,
                                                                              op0=ALU.mult),
                         reads=[ex_r, rc_r], writes=[gt_r])
                    pg, pg_r = pgt.next()
                    S.op("pe", lambda e, pg=pg, gt=gt: e.transpose(out=pg[:], in_=gt[:], identity=self.identf[:]),
                         reads=[gt_r, self.identf_r], writes=[pg_r], pe_acc=True)
                    S.op("act", lambda e, pg=pg, s=s, tt=tt: e.activation(
                        out=R["gateT"][:, s + tt * 128:s + (tt + 1) * 128], in_=pg[:], func=AF.Copy),
                        reads=[pg_r], writes=[R["gateT_r"]])

    def ffn_dense(self, i, with_ctx=True):
        nc, S, M = self.nc, self.S, self.M
        xT, xTr = self.xT, self.xTr
        w_router = self.inp("moe_w_router", [4, D, NE])
        b_router = self.inp("moe_b_router", [4, NE])
        w_gu = self.inp("moe_w_gu", [4, KMOE_NE, D, 2 * D])
        w_dn = self.inp("moe_w_down", [4, KMOE_NE, D, D])
        b_gu = self.inp("moe_b_gu_fm", [4, 128, NE, 16])
        b_dn = self.inp("moe_b_down", [4, NE, D])
        blks = [b for b in BLKS if with_ctx or b[0] != 0]
        with ExitStack() as ps:
            hT, hT_r = M.sb(ps, [128, 8, T], BF16, "hT")
            gateT, gateT_r = M.sb(ps, [NE, T], F32, "gateT")
            bgu, bgu_r = M.sb(ps, [128, NE, 16], F32, "bgu")
            bdn, bdn_r = M.sb(ps, [NE, D], F32, "bdn")
            S.dma("sp", bgu[:], b_gu[i], writes=[bgu_r], key=bgu_r)
            S.op("dve", lambda e: e.tensor_scalar(out=bgu[:, :, 8:16], in0=bgu[:, :, 8:16], scalar1=1.0, scalar2=None, op0=ALU.add),
                 reads=[bgu_r], writes=[bgu_r])
            S.dma("sp", bdn[:], b_dn[i], writes=[bdn_r], key=bdn_r)
            with ExitStack() as ps2:
                wr, wr_r = M.sb(ps2, [128, 8, NE], F32, "wr")
                brb, brb_r = M.sb(ps2, [128, NE], F32, "brb")
                S.dma("sp", wr[:], w_router[i].rearrange("(k p) n -> p k n", p=128), writes=[wr_r], key=wr_r)
                S.dma("sp", brb[:], b_router[i].partition_broadcast(128), writes=[brb_r], key=brb_r)
                self.adanorm(ps2, hT, hT_r, 3, with_ctx=with_ctx,
                             router=dict(wr=wr, wr_r=wr_r, brb=brb, brb_r=brb_r, gateT=gateT, gateT_r=gateT_r))
                S.barrier()
                S.release([wr_r, brb_r])
            wg = Ring([M.sb(ps, [128, 8, 2, 512], BF16, "wg") for _ in range(2)])
            wd = Ring([M.sb(ps, [128, 4, D], BF16, "wd") for _ in range(2)])
            pgl = Ring([M.ps(ps, [128, 2, 512], F32, "pgl") for _ in range(2)])
            pout = Ring([M.ps(ps, [128, 512], F32, "pout") for _ in range(2)])
            pgb = Ring([M.ps(ps, [128, 512], F32, "pgb") for _ in range(2)])
            gB = Ring([M.sb(ps, [128, 512], F32, "gB") for _ in range(2)])
            mg = Ring([M.sb(ps, [NE, 512], F32, "mg") for _ in range(2)])
            aT = Ring([M.sb(ps, [128, 4, 512], BF16, "aT") for _ in range(2)])
            tg = Ring([M.sb(ps, [128, 512], F32, "tg") for _ in range(2)])
            tsg = Ring([M.sb(ps, [128, 512], F32, "tsg") for _ in range(2)])
            tl = Ring([M.sb(ps, [128, 512], F32, "tl") for _ in range(2)])
            for (s, n) in blks:
                c = 1 if s == 0 else 0
                for oc in range(8):
                    po, po_r = pout.next()
                    S.op("pe", lambda e, po=po, oc=oc, s=s, n=n: e.matmul(
                        po[:, :n], lhsT=bdn[:, oc * 128:(oc + 1) * 128], rhs=gateT[:, s:s + n], start=True, stop=True),
                        reads=[bdn_r, gateT_r], writes=[po_r], pe_acc=True)
                    S.op("dve", lambda e, po=po, oc=oc, s=s, n=n, c=c: e.scalar_tensor_tensor(
                        out=xT[:, oc, s:s + n], in0=po[:, :n], scalar=self.mv[:, c, 5, oc:oc + 1], in1=xT[:, oc, s:s + n],
                        op0=ALU.mult, op1=ALU.add),
                        reads=[po_r, self.mv_r, xTr], writes=[xTr])
            pieces = [(ex, half) for ex in range(KMOE_NE) for half in range(2)]
            loaded = {}

            def load_piece(idx):
                ex, half = pieces[idx]
                wgt, wg_r = wg.next()
                wdt, wd_r = wd.next()
                for gl in range(2):
                    S.dma("pool", wgt[:, :, gl, :],
                          w_gu[i, ex, :, gl * D + half * 512: gl * D + half * 512 + 512].rearrange("(k p) n -> p k n", p=128),
                          writes=[wg_r], key=wg_r)
                S.dma("pool", wdt[:], w_dn[i, ex, half * 512:(half + 1) * 512, :].rearrange("(j p) n -> p j n", p=128),
                      writes=[wd_r], key=wd_r)
                loaded[idx] = (wgt, wg_r, wdt, wd_r)

            load_piece(0)
            for idx in range(len(pieces)):
                if True:
                    ex, half = pieces[idx]
                    if idx + 1 < len(pieces):
                        load_piece(idx + 1)
                    wgt, wg_r, wdt, wd_r = loaded.pop(idx)
                    for (s, n) in blks:
                        c = 1 if s == 0 else 0
                        pb, pb_r = pgb.next()
                        mg_, mg_r = mg.next()
                        S.op("act", lambda e, mg_=mg_, ex=ex, s=s, n=n: e.activation(
                            out=mg_[:, :n], in_=gateT[:, s:s + n], func=AF.Copy, scale=self.identf[0:NE, ex:ex + 1]),
                            reads=[gateT_r, self.identf_r], writes=[mg_r])
                        S.op("pe", lambda e, pb=pb, mg_=mg_, n=n: e.matmul(
                            pb[:, :n], lhsT=self.onesf[0:NE, :], rhs=mg_[:, :n], start=True, stop=True),
                            reads=[mg_r, self.onesf_r], writes=[pb_r], pe_acc=True)
                        g_, g_r = gB.next()
                        S.op("act", lambda e, g_=g_, pb=pb, n=n: e.activation(out=g_[:, :n], in_=pb[:, :n], func=AF.Copy),
                             reads=[pb_r], writes=[g_r])
                        a_, a_r = aT.next()
                        for jj in range(4):
                            j = half * 4 + jj
                            pg, pg_r = pgl.next()
                            for gl in range(2):
                                for k in range(8):
                                    S.op("pe", lambda e, pg=pg, gl=gl, k=k, jj=jj, s=s, n=n, wgt=wgt: e.matmul(
                                        pg[:, gl, :n], lhsT=wgt[:, k, gl, jj * 128:(jj + 1) * 128], rhs=hT[:, k, s:s + n],
                                        start=(k == 0), stop=(k == 7)),
                                        reads=[wg_r, hT_r], writes=[pg_r], pe_acc=True)
                            t1, t1_r = tg.next()
                            S.op("dve", lambda e, t1=t1, pg=pg, n=n, ex=ex, j=j: e.tensor_scalar(
                                out=t1[:, :n], in0=pg[:, 0, :n], scalar1=bgu[:, ex, j:j + 1], scalar2=7.0, op0=ALU.add, op1=ALU.min),
                                reads=[pg_r, bgu_r], writes=[t1_r])
                            t2, t2_r = tsg.next()
                            S.op("act", lambda e, t2=t2, t1=t1, n=n: e.activation(out=t2[:, :n], in_=t1[:, :n], func=AF.Sigmoid, scale=1.702),
                                 reads=[t1_r], writes=[t2_r])
                            t3, t3_r = tl.next()
                            S.op("dve", lambda e, t3=t3, pg=pg, n=n, ex=ex, j=j: e.tensor_scalar(
                                out=t3[:, :n], in0=pg[:, 1, :n], scalar1=bgu[:, ex, 8 + j:9 + j], scalar2=-6.0, op0=ALU.add, op1=ALU.max),
                                reads=[pg_r, bgu_r], writes=[t3_r])
                            S.op("pool", lambda e, t1=t1, t2=t2, n=n: e.tensor_tensor(out=t1[:, :n], in0=t1[:, :n], in1=t2[:, :n], op=ALU.mult),
                                 reads=[t1_r, t2_r], writes=[t1_r])
                            S.op("dve", lambda e, t1=t1, t3=t3, n=n: e.scalar_tensor_tensor(
                                out=t3[:, :n], in0=t3[:, :n], scalar=8.0, in1=t1[:, :n], op0=ALU.min, op1=ALU.mult),
                                reads=[t1_r, t3_r], writes=[t3_r])
                            S.op("pool", lambda e, a_=a_, t3=t3, g_=g_, jj=jj, n=n: e.tensor_tensor(out=a_[:, jj, :n], in0=t3[:, :n], in1=g_[:, :n], op=ALU.mult),
                                 reads=[t3_r, g_r], writes=[a_r])
                        for oc in range(8):
                            po, po_r = pout.next()
                            for jj in range(4):
                                S.op("pe", lambda e, po=po, oc=oc, jj=jj, n=n, wdt=wdt, a_=a_: e.matmul(
                                    po[:, :n], lhsT=wdt[:, jj, oc * 128:(oc + 1) * 128], rhs=a_[:, jj, :n],
                                    start=(jj == 0), stop=(jj == 3)),
                                    reads=[wd_r, a_r], writes=[po_r], pe_acc=True)
                            S.op("dve", lambda e, po=po, oc=oc, s=s, n=n, c=c: e.scalar_tensor_tensor(
                                out=xT[:, oc, s:s + n], in0=po[:, :n], scalar=self.mv[:, c, 5, oc:oc + 1], in1=xT[:, oc, s:s + n],
                                op0=ALU.mult, op1=ALU.add),
                                reads=[po_r, self.mv_r, xTr], writes=[xTr])
            S.barrier()
            S.release([bgu_r, bdn_r] + [r for _, r in wg.items] + [r for _, r in wd.items])

    def ffn(self, i, with_ctx=True):
        nc, S, M = self.nc, self.S, self.M
        xT, xTr = self.xT, self.xTr
        w_router = self.inp("moe_w_router", [4, D, NE])
        b_router = self.inp("moe_b_router", [4, NE])
        w_gu = self.inp("moe_w_gu2d", [4 * NE * D, 2 * D])
        w_dn = self.inp("moe_w_down2d", [4 * NE * D, D])
        b_gu = self.inp("moe_b_gu_rows", [4 * NE * 128, 16])
        b_dn = self.inp("moe_b_down2d", [4 * NE, D])
        tiles = list(range(NT)) if with_ctx else list(range(2, NT))
        t0 = tiles[0]
        ntl = len(tiles)
        NB = ntl + NE
        NBMAX = NT + NE
        blks = [b for b in BLKS if with_ctx or b[0] != 0]
        if not hasattr(self, "XE"):
            self.XE = nc.dram_tensor("scr_xe", [NBMAX * 512, D], BF16, kind="Internal").ap()
            self.YE = nc.dram_tensor("scr_ye", [NBMAX * 512, D], F32, kind="Internal").ap()
            self.XE_r, self.YE_r = Res("XE"), Res("YE")
            first = True
        else:
            first = False
        XE, YE, XE_r, YE_r = self.XE, self.YE, self.XE_r, self.YE_r
        with ExitStack() as pA:
            g4, g4_r = M.sb(pA, [128, NT, 4], F32, "g4")
            IDXi, IDXi_r = M.sb(pA, [128, NT * 4], I32, "IDXi")
            idxw, idxw_r = M.sb(pA, [128, NBMAX, 8], I32, "idxw")
            idxb, idxb_r = M.sb(pA, [128, NBMAX], I32, "idxb")
            idxd, idxd_r = M.sb(pA, [128, NBMAX], I32, "idxd")
            if first:
                with ExitStack() as pz:
                    z, z_r = M.sb(pz, [128, 4, D], BF16, "zero")
                    S.op("pool", lambda e: e.memset(z[:], 0.0), writes=[z_r])
                    for b in range(NBMAX):
                        S.dma("sp", XE[b * 512:(b + 1) * 512, :].rearrange("(s p) f -> p s f", p=128), z[:],
                              reads=[z_r], writes=[XE_r], key=z_r)
                    S.barrier()
                    S.release([z_r])
            with ExitStack() as pH:
                hTM, hTM_r = M.sb(pH, [128, NT, D], BF16, "hTM")
                lgs, lgs_r = M.sb(pH, [128, NT, NE], F32, "lgs")
                m8s, m8s_r = M.sb(pH, [128, NT, 8], F32, "m8s")
                MK, MK_r = M.sb(pH, [128, NT, NE], F32, "MK")
                with ExitStack() as pR:
                    wr, wr_r = M.sb(pR, [128, 8, NE], F32, "wr")
                    brb, brb_r = M.sb(pR, [128, NE], F32, "brb")
                    S.dma("sp", wr[:], w_router[i].rearrange("(k p) n -> p k n", p=128), writes=[wr_r], key=wr_r)
                    S.dma("sp", brb[:], b_router[i].partition_broadcast(128), writes=[brb_r], key=brb_r)
                    sq = Ring([M.sb(pR, [128, 512], F32, "nsq") for _ in range(2)])
                    h32, h32_r = M.sb(pR, [128, 8, 512], F32, "nh32")
                    pss = Ring([M.ps(pR, [128, 512], F32, "nss") for _ in range(2)])
                    rst = Ring([M.sb(pR, [128, 512], F32, "nrstd") for _ in range(2)])
                    tmp = Ring([M.sb(pR, [128, 512], F32, "ntmp") for _ in range(2)])
                    plg = Ring([M.ps(pR, [128, NE], F32, "rlg") for _ in range(2)])
                    ptk = Ring([M.ps(pR, [128, 4, 128], F32, "ptk") for _ in range(2)])
                    rsm = Ring([M.sb(pR, [128, 8], F32, "rsm") for _ in range(2)])
                    for (s, n) in blks:
                        c = 1 if s == 0 else 0
                        A = self.mv[:, c, 3, :]
                        B = self.mv[:, c, 4, :]
                        pp, pp_r = pss.next()
                        for k in range(8):
                            q, q_r = sq.next()
                            S.op("act", lambda e, q=q, s=s, n=n, k=k: e.activation(out=q[:, :n], in_=xT[:, k, s:s + n], func=AF.Square),
                                 reads=[xTr], writes=[q_r])
                            S.op("pe", lambda e, pp=pp, q=q, k=k, n=n: e.matmul(pp[:, :n], lhsT=self.onesf[:], rhs=q[:, :n],
                                                                                start=(k == 0), stop=(k == 7)),
                                 reads=[q_r, self.onesf_r], writes=[pp_r], pe_acc=True)
                        r, r_r = rst.next()
                        S.op("act", lambda e, r=r, pp=pp, n=n: e.activation(out=r[:, :n], in_=pp[:, :n], func=AF.Sqrt, scale=1.0 / D,
                                                                            bias=self.epsb[:, 0:1]),
                             reads=[pp_r, self.epsb_r], writes=[r_r])
                        S.op("dve", lambda e, r=r, n=n: e.reciprocal(out=r[:, :n], in_=r[:, :n]), reads=[r_r], writes=[r_r])
                        for k in range(8):
                            t_, t_r = tmp.next()
                            S.op("dve", lambda e, t_=t_, k=k, s=s, n=n, r=r: e.tensor_tensor(out=t_[:, :n], in0=xT[:, k, s:s + n], in1=r[:, :n],
                                                                                             op=ALU.mult),
                                 reads=[xTr, r_r], writes=[t_r])
                            S.op("act", lambda e, t_=t_, k=k, n=n, A=A, B=B: e.activation(
                                out=h32[:, k, :n], in_=t_[:, :n], func=AF.Identity, scale=A[:, k:k + 1], bias=B[:, k:k + 1]),
                                reads=[t_r, self.mv_r], writes=[h32_r])
                        for tt in range(n // 128):
                            t = s // 128 + tt
                            lg, lg_r = plg.next()
                            for k in range(8):
                                S.op("pe", lambda e, lg=lg, k=k, tt=tt: e.matmul(
                                    lg[:], lhsT=h32[:, k, tt * 128:(tt + 1) * 128], rhs=wr[:, k, :], start=(k == 0), stop=(k == 7)),
                                    reads=[h32_r, wr_r], writes=[lg_r], pe_acc=True)
                            sm, sm_r = rsm.next()
                            S.op("dve", lambda e, lg=lg, t=t: e.tensor_tensor(out=lgs[:, t, :], in0=lg[:], in1=brb[:], op=ALU.add),
                                 reads=[lg_r, brb_r], writes=[lgs_r])
                            S.op("dve", lambda e, t=t: e.max(out=m8s[:, t, :], in_=lgs[:, t, :]), reads=[lgs_r], writes=[m8s_r])
                            S.op("dve", lambda e, t=t: e.tensor_scalar(out=MK[:, t, :], in0=lgs[:, t, :], scalar1=m8s[:, t, 3:4], scalar2=None,
                                                                       op0=ALU.is_ge), reads=[lgs_r, m8s_r], writes=[MK_r])
                            S.op("dve", lambda e, sm=sm, t=t: e.tensor_scalar(out=sm[:, 0:1], in0=m8s[:, t, 0:1], scalar1=-1.0, scalar2=None,
                                                                            op0=ALU.mult), reads=[m8s_r], writes=[sm_r])
                            S.op("act", lambda e, sm=sm, t=t: e.activation(out=sm[:, 4:8], in_=m8s[:, t, 0:4], func=AF.Exp, bias=sm[:, 0:1], scale=1.0),
                                 reads=[m8s_r, sm_r], writes=[sm_r])
                            S.op("dve", lambda e, sm=sm: e.reduce_sum(out=sm[:, 1:2], in_=sm[:, 4:8], axis=AX.X), reads=[sm_r], writes=[sm_r])
                            S.op("dve", lambda e, sm=sm: e.reciprocal(out=sm[:, 2:3], in_=sm[:, 1:2]), reads=[sm_r], writes=[sm_r])
                            S.op("dve", lambda e, sm=sm, t=t: e.tensor_scalar(out=g4[:, t, :], in0=sm[:, 4:8], scalar1=sm[:, 2:3], scalar2=None,
                                                                            op0=ALU.mult), reads=[sm_r], writes=[g4_r])
                            for hf in range(2):
                                pk, pk_r = ptk.next()
                                for kk in range(4):
                                    k = hf * 4 + kk
                                    S.op("pe", lambda e, pk=pk, kk=kk, k=k, tt=tt: e.transpose(
                                        out=pk[:, kk, :], in_=h32[:, k, tt * 128:(tt + 1) * 128], identity=self.identf[:]),
                                        reads=[h32_r, self.identf_r], writes=[pk_r], pe_acc=True)
                                if hf == 0:
                                    S.op("act", lambda e, pk=pk, t=t: e.activation(out=hTM[:, t, 0:512], in_=pk[:].rearrange("p a b -> p (a b)"), func=AF.Copy),
                                         reads=[pk_r], writes=[hTM_r])
                                else:
                                    S.op("dve", lambda e, pk=pk, t=t: e.tensor_copy(out=hTM[:, t, 512:1024], in_=pk[:].rearrange("p a b -> p (a b)")),
                                         reads=[pk_r], writes=[hTM_r])
                    S.barrier()
                    S.release([wr_r, brb_r])
                with ExitStack() as pI:
                    f = lambda name, shape: M.sb(pI, shape, F32, name)
                    msum, msum_r = f("msum", [128, NE])
                    cnt, cnt_r = f("cnt", [128, NE])
                    nb, nb_r = f("nb", [128, NE])
                    sz, sz_r = f("sz", [128, NE])
                    sa, sa_r = f("sa", [128, NE])
                    sbb, sbb_r = f("sbb", [128, NE])
                    offX, offX_r = f("offX", [128, NE])
                    run, run_r = f("run", [128, NE])
                    mlt, mlt_r = f("mlt", [128, 128])
                    IDXf, IDXf_r = f("IDXf", [128, NT * 4])
                    ebf, ebf_r = f("ebf", [128, NBMAX])
                    eb2, eb2_r = f("eb2", [128, NBMAX])
                    kp, kp_r = f("kp", [128, 8])
                    pc, pc_r = f("pc", [128, 1])
                    iwf, iwf_r = f("iwf", [128, NBMAX, 8])
                    slr = Ring([f("slot", [128, NE]) for _ in range(2)])
                    ohr = Ring([f("oh", [128, NE]) for _ in range(2)])
                    cmr = Ring([f("cmp", [128, NE]) for _ in range(2)])
                    pcn, pcn_r = M.ps(pI, [128, NE], F32, "pcn")
                    ppr = Ring([M.ps(pI, [128, NE], F32, "ppos") for _ in range(2)])
                    S.op("pool", lambda e: e.memset(mlt[:], 1.0), writes=[mlt_r])
                    S.op("pool", lambda e: e.affine_select(out=mlt[:], in_=mlt[:], pattern=[[1, 128]], compare_op=ALU.is_gt, fill=0.0,
                                                           base=0, channel_multiplier=-1), reads=[mlt_r], writes=[mlt_r])
                    S.op("pool", lambda e: e.iota(kp[:], pattern=[[128, 8]], base=i * NE * D, channel_multiplier=1, allow_small_or_imprecise_dtypes=True),
                         writes=[kp_r])
                    S.op("pool", lambda e: e.iota(pc[:], pattern=[[0, 1]], base=i * NE * 128, channel_multiplier=1, allow_small_or_imprecise_dtypes=True),
                         writes=[pc_r])
                    S.op("pool", lambda e: e.memset(IDXf[:], 0.0), writes=[IDXf_r])
                    S.op("pool", lambda e: e.memset(ebf[:], 31.0), writes=[ebf_r])
                    S.op("dve", lambda e: e.reduce_sum(out=msum[:], in_=MK[:, t0:NT, :].rearrange("p t e -> p e t"), axis=AX.X),
                         reads=[MK_r], writes=[msum_r])
                    S.op("pe", lambda e: e.matmul(pcn[:], lhsT=self.onesf[:], rhs=msum[:], start=True, stop=True),
                         reads=[self.onesf_r, msum_r], writes=[pcn_r], pe_acc=True)
                    S.op("act", lambda e: e.activation(out=cnt[:], in_=pcn[:], func=AF.Copy), reads=[pcn_r], writes=[cnt_r])
                    S.op("dve", lambda e: e.tensor_scalar(out=nb[:], in0=cnt[:], scalar1=0.0, scalar2=None, op0=ALU.is_gt), reads=[cnt_r], writes=[nb_r])
                    for m in range(1, 5):
                        S.op("dve", lambda e, m=m: e.scalar_tensor_tensor(out=nb[:], in0=cnt[:], scalar=512.0 * m, in1=nb[:], op0=ALU.is_gt, op1=ALU.add),
                             reads=[cnt_r, nb_r], writes=[nb_r])
                    S.op("dve", lambda e: e.tensor_scalar(out=sz[:], in0=nb[:], scalar1=512.0, scalar2=None, op0=ALU.mult), reads=[nb_r], writes=[sz_r])
                    S.op("dve", lambda e: e.tensor_copy(out=sa[:], in_=sz[:]), reads=[sz_r], writes=[sa_r])
                    cur, cur_r, oth, oth_r = sa, sa_r, sbb, sbb_r
                    for dd in (1, 2, 4, 8, 16):
                        S.op("dve", lambda e, cur=cur, oth=oth, dd=dd: e.tensor_copy(out=oth[:, 0:dd], in_=cur[:, 0:dd]), reads=[cur_r], writes=[oth_r])
                        S.op("dve", lambda e, cur=cur, oth=oth, dd=dd: e.tensor_tensor(out=oth[:, dd:NE], in0=cur[:, dd:NE], in1=cur[:, 0:NE - dd], op=ALU.add),
                             reads=[cur_r, oth_r], writes=[oth_r])
                        cur, cur_r, oth, oth_r = oth, oth_r, cur, cur_r
                    offE, offE_r = cur, cur_r
                    S.op("dve", lambda e: e.tensor_tensor(out=offX[:], in0=offE[:], in1=sz[:], op=ALU.subtract), reads=[offE_r, sz_r], writes=[offX_r])
                    S.op("pool", lambda e: e.memset(run[:], 0.0), writes=[run_r])
                    for t in tiles:
                        pp, pp_r = ppr.next()
                        S.op("pe", lambda e, pp=pp, t=t: e.matmul(pp[:], lhsT=mlt[:], rhs=MK[:, t, :], start=True, stop=False),
                             reads=[mlt_r, MK_r], writes=[pp_r], pe_acc=True)
                        S.op("pe", lambda e, pp=pp: e.matmul(pp[:], lhsT=self.onesf[:], rhs=run[:], start=False, stop=True),
                             reads=[self.onesf_r, run_r], writes=[pp_r], pe_acc=True)
                        sl, sl_r = slr.next()
                        S.op("dve", lambda e, sl=sl, pp=pp: e.tensor_tensor(out=sl[:], in0=pp[:], in1=offX[:], op=ALU.add),
                             reads=[pp_r, offX_r], writes=[sl_r])
                        S.op("dve", lambda e, t=t: e.tensor_tensor(out=run[:], in0=run[:], in1=MK[:, t, :], op=ALU.add),
                             reads=[run_r, MK_r], writes=[run_r])
                        for j in range(4):
                            oh, oh_r = ohr.next()
                            S.op("dve", lambda e, oh=oh, t=t, j=j: e.tensor_scalar(out=oh[:], in0=lgs[:, t, :], scalar1=m8s[:, t, j:j + 1], scalar2=None,
                                                                                 op0=ALU.is_equal), reads=[lgs_r, m8s_r], writes=[oh_r])
                            S.op("dve", lambda e, oh=oh, sl=sl: e.tensor_tensor(out=oh[:], in0=oh[:], in1=sl[:], op=ALU.mult),
                                 reads=[oh_r, sl_r], writes=[oh_r])
                            S.op("dve", lambda e, oh=oh, t=t, j=j: e.reduce_sum(out=IDXf[:, t * 4 + j:t * 4 + j + 1], in_=oh[:], axis=AX.X),
                                 reads=[oh_r], writes=[IDXf_r])
                    S.op("dve", lambda e: e.tensor_copy(out=IDXi[:], in_=IDXf[:]), reads=[IDXf_r], writes=[IDXi_r])
                    for b in range(NB):
                        cm, cm_r = cmr.next()
                        S.op("dve", lambda e, cm=cm, b=b: e.tensor_scalar(out=cm[:], in0=offE[:], scalar1=512.0 * b, scalar2=None, op0=ALU.is_le),
                             reads=[offE_r], writes=[cm_r])
                        S.op("dve", lambda e, cm=cm, b=b: e.reduce_sum(out=ebf[:, b:b + 1], in_=cm[:], axis=AX.X), reads=[cm_r], writes=[ebf_r])
                    S.op("dve", lambda e: e.tensor_scalar(out=ebf[:], in0=ebf[:], scalar1=31.0, scalar2=None, op0=ALU.min), reads=[ebf_r], writes=[ebf_r])
                    S.op("dve", lambda e: e.tensor_scalar(out=eb2[:], in0=ebf[:], scalar1=float(i * NE), scalar2=None, op0=ALU.add),
                         reads=[ebf_r], writes=[eb2_r])
                    S.op("dve", lambda e: e.tensor_copy(out=idxd[:], in_=eb2[:]), reads=[eb2_r], writes=[idxd_r])
                    S.op("dve", lambda e: e.tensor_scalar(out=eb2[:], in0=ebf[:], scalar1=128.0, scalar2=pc[:, 0:1], op0=ALU.mult, op1=ALU.add),
                         reads=[ebf_r, pc_r, idxd_r], writes=[eb2_r])
                    S.op("dve", lambda e: e.tensor_copy(out=idxb[:], in_=eb2[:]), reads=[eb2_r], writes=[idxb_r])
                    S.op("dve", lambda e: e.tensor_scalar(out=eb2[:], in0=ebf[:], scalar1=1024.0, scalar2=None, op0=ALU.mult),
                         reads=[ebf_r, idxb_r], writes=[eb2_r])
                    S.op("dve", lambda e: e.tensor_tensor(out=iwf[:], in0=eb2[:].unsqueeze(2).to_broadcast([128, NBMAX, 8]),
                                                          in1=kp[:].unsqueeze(1).to_broadcast([128, NBMAX, 8]), op=ALU.add),
                         reads=[eb2_r, kp_r], writes=[iwf_r])
                    S.op("dve", lambda e: e.tensor_copy(out=idxw[:], in_=iwf[:]), reads=[iwf_r], writes=[idxw_r])
                    S.barrier()
                for t in tiles:
                    for j in range(4):
                        S.idma(XE, IDXi[:, t * 4 + j:t * 4 + j + 1], hTM[:, t, :], None, NBMAX * 512 - 1,
                               reads=[hTM_r, IDXi_r], writes=[XE_r], key=hTM_r)
                S.barrier()
                S.release([hTM_r])
            with ExitStack() as pE:
                wgr = Ring([M.sb(pE, [128, 8, 2 * D], BF16, "wgb") for _ in range(2)])
                wd, wd_r = M.sb(pE, [128, 8, D], BF16, "wdb")
                bgr = Ring([M.sb(pE, [128, 16], F32, "bgb") for _ in range(2)])
                bdr = Ring([M.sb(pE, [128, D], F32, "bdb") for _ in range(1)])
                xet = [M.sb(pE, [128, D], BF16, "xe%d" % st) for st in range(4)]
                xeT, xeT_r = M.sb(pE, [128, 8, 512], BF16, "xeT")
                aT, aT_r = M.sb(pE, [128, 8, 512], BF16, "aT")
                aT_rs = [Res("aT%d" % j) for j in range(8)]
                tg = Ring([M.sb(pE, [128, 512], F32, "tg") for _ in range(2)])
                tsg = Ring([M.sb(pE, [128, 512], F32, "tsg") for _ in range(2)])
                tl = Ring([M.sb(pE, [128, 512], F32, "tl") for _ in range(2)])
                yer = Ring([M.sb(pE, [128, D], F32, "ye") for _ in range(2)])
                ptx, ptx_r = M.ps(pE, [128, 8, 128], BF16, "ptx")
                pgl = Ring([M.ps(pE, [128, 2, 512], F32, "pgl") for _ in range(2)])
                pout = Ring([M.ps(pE, [128, 512], F32, "pout") for _ in range(2)])
                loaded = {}

                def load_block(b):
                    wg, wg_r = wgr.next()
                    bg, bg_r = bgr.next()
                    for k in range(8):
                        S.idma(wg[:, k, :], None, w_gu, idxw[:, b, k:k + 1], 4 * NE * D - 1, reads=[idxw_r], writes=[wg_r], key=wg_r)
                    S.idma(bg[:], None, b_gu, idxb[:, b:b + 1], 4 * NE * 128 - 1, reads=[idxb_r], writes=[bg_r], key=bg_r)
                    loaded[b] = (wg, wg_r, bg, bg_r)

                def load_xe(b):
                    for st in range(4):
                        xe, xe_r = xet[st]
                        S.dma("act", xe[:], XE[b * 512 + st * 128:b * 512 + (st + 1) * 128, :], reads=[XE_r], writes=[xe_r], key=xe_r)

                load_block(0)
                load_xe(0)
                for b in range(NB):
                    wg, wg_r, bg, bg_r = loaded.pop(b)
                    bd, bd_r = bdr.next()
                    S.idma(bd[:], None, b_dn, idxd[:, b:b + 1], 4 * NE - 1, reads=[idxd_r], writes=[bd_r], key=bd_r)
                    for k in range(8):
                        S.idma(wd[:, k, :], None, w_dn, idxw[:, b, k:k + 1], 4 * NE * D - 1, reads=[idxw_r], writes=[wd_r], key=wd_r)
                    if b + 1 < NB:
                        load_block(b + 1)
                    for st in range(4):
                        xe, xe_r = xet[st]
                        for k in range(8):
                            S.op("pe", lambda e, st=st, k=k, xe=xe: e.transpose(out=ptx[:, k, :], in_=xe[:, k * 128:(k + 1) * 128], identity=self.identb[:]),
                                 reads=[xe_r, self.identb_r], writes=[ptx_r], pe_acc=True)
                        S.op("act", lambda e, st=st: e.activation(out=xeT[:, :, st * 128:(st + 1) * 128], in_=ptx[:], func=AF.Copy),
                             reads=[ptx_r], writes=[xeT_r])
                    if b + 1 < NB:
                        load_xe(b + 1)
                    S.op("dve", lambda e, bg=bg: e.tensor_scalar(out=bg[:, 8:16], in0=bg[:, 8:16], scalar1=1.0, scalar2=None, op0=ALU.add),
                         reads=[bg_r], writes=[bg_r])
                    pend = None
                    for j in range(8):
                        pg, pg_r = pgl.next()
                        for gl in range(2):
                            for k in range(8):
                                S.op("pe", lambda e, pg=pg, gl=gl, k=k, j=j, wg=wg: e.matmul(
                                    pg[:, gl, :], lhsT=wg[:, k, gl * D + j * 128:gl * D + (j + 1) * 128], rhs=xeT[:, k, :],
                                    start=(k == 0), stop=(k == 7)), reads=[wg_r, xeT_r], writes=[pg_r], pe_acc=True)
                        t1, t1_r = tg.next()
                        t2, t2_r = tsg.next()
                        t3, t3_r = tl.next()
                        S.op("dve", lambda e, t1=t1, pg=pg, j=j, bg=bg: e.tensor_scalar(
                            out=t1[:], in0=pg[:, 0, :], scalar1=bg[:, j:j + 1], scalar2=7.0, op0=ALU.add, op1=ALU.min),
                            reads=[pg_r, bg_r], writes=[t1_r])
                        S.op("act", lambda e, t2=t2, t1=t1: e.activation(out=t2[:], in_=t1[:], func=AF.Sigmoid, scale=1.702), reads=[t1_r], writes=[t2_r])
                        S.op("dve", lambda e, t3=t3, pg=pg, j=j, bg=bg: e.tensor_scalar(
                            out=t3[:], in0=pg[:, 1, :], scalar1=bg[:, 8 + j:9 + j], scalar2=-6.0, op0=ALU.add, op1=ALU.max),
                            reads=[pg_r, bg_r], writes=[t3_r])
                        if pend is not None:
                            pend()

                        def tail(t1=t1, t1_r=t1_r, t2=t2, t2_r=t2_r, t3=t3, t3_r=t3_r, j=j):
                            S.op("dve", lambda e: e.tensor_tensor(out=t1[:], in0=t1[:], in1=t2[:], op=ALU.mult), reads=[t1_r, t2_r], writes=[t1_r])
                            S.op("dve", lambda e: e.scalar_tensor_tensor(
                                out=aT[:, j, :], in0=t3[:], scalar=8.0, in1=t1[:], op0=ALU.min, op1=ALU.mult), reads=[t1_r, t3_r], writes=[aT_rs[j]])
                        pend = tail
                    pend()
                    for st in range(4):
                        ye, ye_r = yer.next()
                        for hf in range(2):
                            po, po_r = pout.next()
                            for j in range(8):
                                S.op("pe", lambda e, po=po, j=j, st=st, hf=hf: e.matmul(
                                    po[:], lhsT=aT[:, j, st * 128:(st + 1) * 128], rhs=wd[:, j, hf * 512:(hf + 1) * 512],
                                    start=(j == 0), stop=(j == 7)), reads=[aT_rs[j], wd_r], writes=[po_r], pe_acc=True)
                            S.op("dve", lambda e, po=po, ye=ye, hf=hf, bd=bd: e.tensor_tensor(
                                out=ye[:, hf * 512:(hf + 1) * 512], in0=po[:], in1=bd[:, hf * 512:(hf + 1) * 512], op=ALU.add),
                                reads=[po_r, bd_r], writes=[ye_r])
                        S.dma("sp", YE[b * 512 + st * 128:b * 512 + (st + 1) * 128, :], ye[:], reads=[ye_r], writes=[YE_r], key=ye_r)
                S.barrier()
                S.release([r for _, r in wgr.items] + [wd_r] + [r for _, r in bgr.items] + [r for _, r in bdr.items]
                          + [r for _, r in xet] + [r for _, r in yer.items])
            with ExitStack() as pC:
                yjr = Ring([M.sb(pC, [128, D], F32, "yj") for _ in range(3)])
                acr = Ring([M.sb(pC, [128, D], F32, "acc") for _ in range(2)])
                pct = Ring([M.ps(pC, [128, 4, 128], F32, "pct") for _ in range(2)])
                for t in tiles:
                    c = 1 if t < 2 else 0
                    ac, ac_r = acr.next()
                    for j in range(4):
                        yj, yj_r = yjr.next()
                        S.idma(yj[:], None, YE, IDXi[:, t * 4 + j:t * 4 + j + 1], NBMAX * 512 - 1, reads=[YE_r, IDXi_r], writes=[yj_r], key=yj_r)
                        if j == 0:
                            S.op("dve", lambda e, ac=ac, yj=yj, t=t: e.tensor_scalar(out=ac[:], in0=yj[:], scalar1=g4[:, t, 0:1], scalar2=None, op0=ALU.mult),
                                 reads=[yj_r, g4_r], writes=[ac_r])
                        else:
                            S.op("dve", lambda e, ac=ac, yj=yj, t=t, j=j: e.scalar_tensor_tensor(
                                out=ac[:], in0=yj[:], scalar=g4[:, t, j:j + 1], in1=ac[:], op0=ALU.mult, op1=ALU.add),
                                reads=[yj_r, g4_r, ac_r], writes=[ac_r])
                    for hf in range(2):
                        pk, pk_r = pct.next()
                        for kk in range(4):
                            k = hf * 4 + kk
                            S.op("pe", lambda e, pk=pk, kk=kk, k=k, ac=ac: e.transpose(out=pk[:, kk, :], in_=ac[:, k * 128:(k + 1) * 128], identity=self.identf[:]),
                                 reads=[ac_r, self.identf_r], writes=[pk_r], pe_acc=True)
                        for kk in range(4):
                            k = hf * 4 + kk
                            S.op("dve", lambda e, pk=pk, kk=kk, k=k, t=t, c=c: e.scalar_tensor_tensor(
                                out=xT[:, k, t * 128:(t + 1) * 128], in0=pk[:, kk, :], scalar=self.mv[:, c, 5, k:k + 1], in1=xT[:, k, t * 128:(t + 1) * 128],
                                op0=ALU.mult, op1=ALU.add), reads=[pk_r, self.mv_r, xTr], writes=[xTr])
                S.barrier()
                S.release([r for _, r in yjr.items])

    def resid(self, po, po_r, n, oc, s, gb=None, tring=None):
        S = self.S
        c = 1 if s < NCTX else 0
        xT, xTr = self.xT, self.xTr
        if gb is None:
            S.op("dve", lambda e: e.scalar_tensor_tensor(
                out=xT[:, oc, s:s + n], in0=po[:, :n], scalar=self.mv[:, c, 2, oc:oc + 1], in1=xT[:, oc, s:s + n],
                op0=ALU.mult, op1=ALU.add), reads=[po_r, self.mv_r, xTr], writes=[xTr])
        else:
            gbt, gbt_r = gb
            t_, t_r = tring.next()
            S.op("act", lambda e: e.activation(out=t_[:, :n], in_=po[:, :n], func=AF.Identity,
                                               scale=self.mv[:, c, 2, oc:oc + 1], bias=gbt[:, c, oc:oc + 1]),
                 reads=[po_r, self.mv_r, gbt_r], writes=[t_r])
            S.op("pool", lambda e: e.tensor_tensor(out=xT[:, oc, s:s + n], in0=xT[:, oc, s:s + n], in1=t_[:, :n], op=ALU.add),
                 reads=[t_r, xTr], writes=[xTr])

    def gate_bias(self, ps, bvec_ap):
        S, M = self.S, self.M
        gb, gb_r = M.sb(ps, [128, 2, 8], F32, "gb")
        for c in range(2):
            S.op("dve", lambda e, c=c: e.tensor_tensor(out=gb[:, c, :], in0=self.mv[:, c, 2, :], in1=bvec_ap, op=ALU.mult),
                 reads=[self.mv_r] + self._vec_deps, writes=[gb_r])
        return gb, gb_r

    def mixer0(self, i):
        nc, S, M = self.nc, self.S, self.M
        w1 = self.inp("conv_w_pw1", [1, D, 2 * D])
        w2 = self.inp("conv_w_pw2", [1, D, D])
        cvd = self.inp("conv_vec", [128, 296])
        UW = 2364

        def ucol(s):
            return 15 if s == 0 else s + 45
        with ExitStack() as pA:
            cv, cv_r = M.sb(pA, [128, 296], F32, "cv")
            S.dma("sp", cv[:], cvd, writes=[cv_r], key=cv_r)
            self._vec_deps = [cv_r]
            V, V_r = M.sb(pA, [128, 8, T], BF16, "V")
            with ExitStack() as pB:
                U, U_r = M.sb(pB, [128, 8, UW], BF16, "U")
                S.op("pool", lambda e: e.memset(U[:], 0.0), writes=[U_r])
                with ExitStack() as pC:
                    hT, hT_r = M.sb(pC, [128, 8, T], BF16, "hT")
                    with ExitStack() as pD:
                        self.adanorm(pD, hT, hT_r, 0)
                        S.barrier()
                    wp = Ring([M.sb(pC, [128, 8, 2, 128], BF16, "wp") for _ in range(2)])
                    pa = Ring([M.ps(pC, [128, 2, 512], F32, "pa") for _ in range(2)])
                    sg = Ring([M.sb(pC, [128, 512], F32, "sg") for _ in range(2)])
                    for oc in range(8):
                        wt, wt_r = wp.next()
                        for gl in range(2):
                            S.dma("pool", wt[:, :, gl, :],
                                  w1[0, :, gl * D + oc * 128: gl * D + (oc + 1) * 128].rearrange("(k p) n -> p k n", p=128),
                                  writes=[wt_r], key=wt_r)
                        for (s, n) in BLKS:
                            p_, p_r = pa.next()
                            for gl in range(2):
                                for k in range(8):
                                    S.op("pe", lambda e, p_=p_, gl=gl, k=k, s=s, n=n, wt=wt: e.matmul(
                                        p_[:, gl, :n], lhsT=wt[:, k, gl, :], rhs=hT[:, k, s:s + n], start=(k == 0), stop=(k == 7)),
                                        reads=[wt_r, hT_r], writes=[p_r], pe_acc=True)
                            g_, g_r = sg.next()
                            S.op("act", lambda e, g_=g_, p_=p_, n=n, oc=oc: e.activation(
                                out=g_[:, :n], in_=p_[:, 1, :n], func=AF.Sigmoid, bias=cv[:, 8 + oc:9 + oc], scale=1.0),
                                reads=[p_r, cv_r], writes=[g_r])
                            S.op("dve", lambda e, g_=g_, p_=p_, n=n, oc=oc, s=s: e.scalar_tensor_tensor(
                                out=U[:, oc, ucol(s):ucol(s) + n], in0=p_[:, 0, :n], scalar=cv[:, oc:oc + 1], in1=g_[:, :n],
                                op0=ALU.add, op1=ALU.mult),
                                reads=[p_r, cv_r, g_r], writes=[U_r])
                    S.barrier()
                    S.release([r for _, r in wp.items])
                dgr = Ring([M.sb(pB, [128, 31, 128], BF16, "dg") for _ in range(2)])
                pc = Ring([M.ps(pB, [128, 512], F32, "pc") for _ in range(2)])
                for oc in range(8):
                    dg, dg_r = dgr.next()
                    for w in range(31):
                        S.op("dve", lambda e, dg=dg, w=w, oc=oc: e.tensor_scalar(
                            out=dg[:, w, :], in0=self.identb[:], scalar1=cv[:, 16 + oc * 31 + w:17 + oc * 31 + w], scalar2=None,
                            op0=ALU.mult), reads=[self.identb_r, cv_r], writes=[dg_r])
                    for (s, n) in BLKS:
                        o0 = 0 if s == 0 else s + 30
                        p_, p_r = pc.next()
                        for w in range(31):
                            S.op("pe", lambda e, p_=p_, w=w, oc=oc, o0=o0, n=n, dg=dg: e.matmul(
                                p_[:, :n], lhsT=dg[:, w, :], rhs=U[:, oc, o0 + w:o0 + w + n], start=(w == 0), stop=(w == 30)),
                                reads=[dg_r, U_r], writes=[p_r], pe_acc=True)
                        S.op("act", lambda e, p_=p_, oc=oc, s=s, n=n: e.activation(
                            out=V[:, oc, s:s + n], in_=p_[:, :n], func=AF.Identity, bias=cv[:, 264 + oc:265 + oc], scale=1.0),
                            reads=[p_r, cv_r], writes=[V_r])
                S.barrier()
            h2, h2_r = M.sb(pA, [128, 8, T], BF16, "h2")
            w2t, w2_r = M.sb(pA, [128, 8, D], BF16, "w2t")
            S.dma("pool", w2t[:], w2[0].rearrange("(k p) n -> p k n", p=128), writes=[w2_r], key=w2_r)
            gb = self.gate_bias(pA, cv[:, 288:296])
            ps1 = Ring([M.ps(pA, [128, 512], F32, "ps1") for _ in range(1)])
            ps2 = Ring([M.ps(pA, [128, 512], F32, "ps2") for _ in range(1)])
            po = Ring([M.ps(pA, [128, 512], F32, "po") for _ in range(2)])
            vsq = Ring([M.sb(pA, [128, 512], BF16, "vsq") for _ in range(2)])
            mu = Ring([M.sb(pA, [128, 512], F32, "mu") for _ in range(1)])
            rs = Ring([M.sb(pA, [128, 512], F32, "rs") for _ in range(1)])
            tt = Ring([M.sb(pA, [128, 512], F32, "tt") for _ in range(2)])
            tr = Ring([M.sb(pA, [128, 512], F32, "tr") for _ in range(2)])
            for (s, n) in BLKS:
                a1, a1_r = ps1.next()
                a2, a2_r = ps2.next()
                for k in range(8):
                    q, q_r = vsq.next()
                    S.op("pool", lambda e, q=q, k=k, s=s, n=n: e.tensor_tensor(out=q[:, :n], in0=V[:, k, s:s + n], in1=V[:, k, s:s + n], op=ALU.mult),
                         reads=[V_r], writes=[q_r])
                    S.op("pe", lambda e, a1=a1, k=k, s=s, n=n: e.matmul(a1[:, :n], lhsT=self.onesb[:], rhs=V[:, k, s:s + n],
                                                                         start=(k == 0), stop=(k == 7)),
                         reads=[V_r, self.onesb_r], writes=[a1_r], pe_acc=True)
                    S.op("pe", lambda e, a2=a2, q=q, k=k, n=n: e.matmul(a2[:, :n], lhsT=self.onesb[:], rhs=q[:, :n],
                                                                        start=(k == 0), stop=(k == 7)),
                         reads=[q_r, self.onesb_r], writes=[a2_r], pe_acc=True)
                m_, m_r = mu.next()
                r_, r_r = rs.next()
                S.op("act", lambda e, m_=m_, a1=a1, n=n: e.activation(out=m_[:, :n], in_=a1[:, :n], func=AF.Copy, scale=1.0 / D),
                     reads=[a1_r], writes=[m_r])
                S.op("dve", lambda e, r_=r_, m_=m_, n=n: e.tensor_tensor(out=r_[:, :n], in0=m_[:, :n], in1=m_[:, :n], op=ALU.mult),
                     reads=[m_r], writes=[r_r])
                S.op("dve", lambda e, r_=r_, a2=a2, n=n: e.scalar_tensor_tensor(out=r_[:, :n], in0=a2[:, :n], scalar=1.0 / D, in1=r_[:, :n],
                                                                               op0=ALU.mult, op1=ALU.subtract),
                     reads=[a2_r, r_r], writes=[r_r])
                S.op("act", lambda e, r_=r_, n=n: e.activation(out=r_[:, :n], in_=r_[:, :n], func=AF.Sqrt, bias=self.epsb[:, 0:1], scale=1.0),
                     reads=[r_r, self.epsb_r], writes=[r_r])
                S.op("dve", lambda e, r_=r_, n=n: e.reciprocal(out=r_[:, :n], in_=r_[:, :n]), reads=[r_r], writes=[r_r])
                for k in range(8):
                    t_, t_r = tt.next()
                    S.op("dve", lambda e, t_=t_, k=k, s=s, n=n, m_=m_: e.tensor_tensor(out=t_[:, :n], in0=V[:, k, s:s + n], in1=m_[:, :n], op=ALU.subtract),
                         reads=[V_r, m_r], writes=[t_r])
                    S.op("pool", lambda e, t_=t_, r_=r_, n=n: e.tensor_tensor(out=t_[:, :n], in0=t_[:, :n], in1=r_[:, :n], op=ALU.mult),
                         reads=[t_r, r_r], writes=[t_r])
                    S.op("act", lambda e, t_=t_, k=k, s=s, n=n: e.activation(
                        out=h2[:, k, s:s + n], in_=t_[:, :n], func=AF.Silu, scale=cv[:, 272 + k:273 + k], bias=cv[:, 280 + k:281 + k]),
                        reads=[t_r, cv_r], writes=[h2_r])
            for (s, n) in BLKS:
                for oc in range(8):
                    p_, p_r = po.next()
                    for k in range(8):
                        S.op("pe", lambda e, p_=p_, k=k, oc=oc, s=s, n=n: e.matmul(
                            p_[:, :n], lhsT=w2t[:, k, oc * 128:(oc + 1) * 128], rhs=h2[:, k, s:s + n], start=(k == 0), stop=(k == 7)),
                            reads=[w2_r, h2_r], writes=[p_r], pe_acc=True)
                    self.resid(p_, p_r, n, oc, s, gb=gb, tring=tr)
            S.barrier()
            S.release([cv_r, w2_r])

    def load_w(self, ps, src2d, kch, n, name, npart=128):
        S, M = self.S, self.M
        t, t_r = M.sb(ps, [npart, kch, n], BF16, name)
        S.dma("pool", t[:], src2d.rearrange("(k p) n -> p k n", p=npart), writes=[t_r], key=t_r)
        return t, t_r

    def swap_halves(self, ps, w, w_r, kch, nh, name):
        S, M = self.S, self.M
        ws, ws_r = M.sb(ps, [128, kch, nh * 64], BF16, name)
        for h in range(nh):
            S.op("pool", lambda e, h=h: e.tensor_copy(out=ws[:, :, h * 64:h * 64 + 32], in_=w[:, :, h * 64 + 32:h * 64 + 64]),
                 reads=[w_r], writes=[ws_r])
            S.op("pool", lambda e, h=h: e.tensor_copy(out=ws[:, :, h * 64 + 32:h * 64 + 64], in_=w[:, :, h * 64:h * 64 + 32]),
                 reads=[w_r], writes=[ws_r])
        return ws, ws_r

    def proj_rope(self, pj, pj_r, w, w_r, ws, ws_r, c0, b, bs, b_r, hT, hT_r, out, out_r, blks, C, Sn, tab_r, t1r, t2r):
        S = self.S
        for (s, n) in blks:
            for a, (ww, ww_r) in enumerate(((w, w_r), (ws, ws_r))):
                for k in range(8):
                    S.op("pe", lambda e, a=a, ww=ww, k=k, s=s, n=n: e.matmul(
                        pj[0:64, a, :n], lhsT=ww[:, k, c0:c0 + 64], rhs=hT[:, k, s:s + n], start=(k == 0), stop=(k == 7)),
                        reads=[ww_r, hT_r], writes=[pj_r], pe_acc=True)
            t1, t1_r = t1r.next()
            t2, t2_r = t2r.next()
            S.op("dve", lambda e, t1=t1, s=s, n=n: e.scalar_tensor_tensor(
                out=t1[0:64, :n], in0=pj[0:64, 0, :n], scalar=b, in1=C[:, s:s + n], op0=ALU.add, op1=ALU.mult),
                reads=[pj_r, b_r, tab_r], writes=[t1_r])
            S.op("dve", lambda e, t2=t2, s=s, n=n: e.scalar_tensor_tensor(
                out=t2[0:64, :n], in0=pj[0:64, 1, :n], scalar=bs, in1=Sn[:, s:s + n], op0=ALU.add, op1=ALU.mult),
                reads=[pj_r, b_r, tab_r], writes=[t2_r])
            S.op("pool", lambda e, t1=t1, t2=t2, s=s, n=n: e.tensor_tensor(out=out[0:64, s:s + n], in0=t1[0:64, :n], in1=t2[0:64, :n], op=ALU.add),
                 reads=[t1_r, t2_r], writes=[out_r])

    def load_tables(self, ps):
        S, M = self.S, self.M
        Cd = self.inp("rope_c", [64, T])
        Sd = self.inp("rope_s", [64, T])
        C, C_r = M.sb(ps, [64, T], F32, "ropeC")
        Sn, _ = M.sb(ps, [64, T], F32, "ropeS")
        S.dma("sp", C[:], Cd, writes=[C_r], key=C_r)
        S.dma("sp", Sn[:], Sd, writes=[C_r], key=C_r)
        return C, Sn, C_r

    def mixer2(self, i):
        nc, S, M = self.nc, self.S, self.M
        wqkv = self.inp("swa_w_qkv", [1, D, 1536])
        bqkv = self.inp("swa_b_qkv", [1, 1536])
        wo = self.inp("swa_w_o", [1, D, D])
        bh = self.inp("swa_bh", [64, 44])
        sinks = self.inp("swa_sinks", [1, 16])
        bo = self.inp("swa_bo_fm", [128, 8])
        NEG = -30000.0
        with ExitStack() as pA:
            hT, hT_r = M.sb(pA, [128, 8, T], BF16, "hT")
            with ExitStack() as pD:
                self.adanorm(pD, hT, hT_r, 0)
                S.barrier()
            C, Sn, tab_r = self.load_tables(pA)
            bht, bht_r = M.sb(pA, [64, 44], F32, "bht")
            S.dma("sp", bht[:], bh, writes=[bht_r], key=bht_r)
            skb, skb_r = M.sb(pA, [128, 16], F32, "skb")
            S.dma("sp", skb[:], sinks[0].partition_broadcast(128), writes=[skb_r], key=skb_r)
            bot, bot_r = M.sb(pA, [128, 8], F32, "bot")
            S.dma("sp", bot[:], bo, writes=[bot_r], key=bot_r)
            self._vec_deps = [bot_r]
            gb = self.gate_bias(pA, bot[:])
            mW, mW_r = M.sb(pA, [128, 384], F32, "mW")
            S.op("pool", lambda e: e.memset(mW[:], 0.0), writes=[mW_r])
            S.op("pool", lambda e: e.affine_select(out=mW[:, 0:128], in_=mW[:, 0:128], pattern=[[1, 128]], compare_op=ALU.is_ge,
                                                   fill=NEG, base=0, channel_multiplier=-1), reads=[mW_r], writes=[mW_r])
            S.op("pool", lambda e: e.affine_select(out=mW[:, 256:384], in_=mW[:, 256:384], pattern=[[-1, 128]], compare_op=ALU.is_ge,
                                                   fill=NEG, base=0, channel_multiplier=1), reads=[mW_r], writes=[mW_r])
            kT, kT_r = M.sb(pA, [64, T], BF16, "kT")
            vs, vs_r = M.sb(pA, [128, NT, 64], BF16, "vs")
            qTr = Ring([M.sb(pA, [64, T], BF16, "qT") for _ in range(2)])
            oTg, oTg_r = M.sb(pA, [64, 4, T], BF16, "oTg")
            bvb, bvb_r = M.sb(pA, [128, 64], F32, "bvb")
            t1r = Ring([M.sb(pA, [64, 512], F32, "rp1") for _ in range(2)])
            t2r = Ring([M.sb(pA, [64, 512], F32, "rp2") for _ in range(2)])
            tr = Ring([M.sb(pA, [128, 512], F32, "tr") for _ in range(2)])
            swr = Ring([M.sb(pA, [128, 384], F32, "sw") for _ in range(2)])
            pwr = Ring([M.sb(pA, [128, 640], BF16, "pw") for _ in range(2)])
            pTsr = Ring([M.sb(pA, [128, 5, 128], BF16, "pTs") for _ in range(2)])
            osr = Ring([M.sb(pA, [128, 64], F32, "osb") for _ in range(2)])
            smr = Ring([M.sb(pA, [128, 8], F32, "sm") for _ in range(3)])
            pj, pj_r = M.ps(pA, [128, 2, 512], F32, "pj")
            pscr = Ring([M.ps(pA, [128, 2, 512], F32, "psc") for _ in range(2)])
            pT, pT_r = M.ps(pA, [128, 5, 128], BF16, "pT")
            pso, pso_r = M.ps(pA, [128, 512], F32, "pso")
            for g in range(4):
                if (self.dbg == 7 and g == 1) or (self.dbg == 17 and g == 2) or (self.dbg == 18 and g == 3):
                    return
                with ExitStack() as pG:
                    wq, wq_r = self.load_w(pG, wqkv[0, :, g * 256:(g + 1) * 256], 8, 256, "wq")
                    wk, wk_r = self.load_w(pG, wqkv[0, :, 1024 + g * 64:1024 + (g + 1) * 64], 8, 64, "wk")
                    wv, wv_r = self.load_w(pG, wqkv[0, :, 1280 + g * 64:1280 + (g + 1) * 64], 8, 64, "wv")
                    wqs, wqs_r = self.swap_halves(pG, wq, wq_r, 8, 4, "wqs")
                    wks, wks_r = self.swap_halves(pG, wk, wk_r, 8, 1, "wks")
                    wog, wog_r = self.load_w(pG, wo[0, g * 256:(g + 1) * 256, :], 4, D, "wog", npart=64)
                    S.dma("sp", bvb[:], bqkv[0, 1280 + g * 64:1280 + (g + 1) * 64].partition_broadcast(128),
                          reads=[], writes=[bvb_r], key=bvb_r)
                    self.proj_rope(pj, pj_r, wk, wk_r, wks, wks_r, 0, bht[:, 16 + g:17 + g], bht[:, 24 + 16 + g:25 + 16 + g], bht_r,
                                   hT, hT_r, kT, kT_r, BLKS, C, Sn, tab_r, t1r, t2r)
                    for t in range(NT):
                        for k in range(8):
                            S.op("pe", lambda e, t=t, k=k: e.matmul(pso[:, 0:64], lhsT=hT[:, k, t * 128:(t + 1) * 128], rhs=wv[:, k, :],
                                                                    start=(k == 0), stop=(k == 7)),
                                 reads=[hT_r, wv_r], writes=[pso_r], pe_acc=True)
                        S.op("dve", lambda e, t=t: e.tensor_tensor(out=vs[:, t, :], in0=pso[:, 0:64], in1=bvb[:], op=ALU.add),
                             reads=[pso_r, bvb_r], writes=[vs_r])
                    if self.dbg == 1 or (self.dbg == 11 and g == 1):
                        return
                    for hh in range(4):
                        h = g * 4 + hh
                        qT, qT_r = qTr.next()
                        self.proj_rope(pj, pj_r, wq, wq_r, wqs, wqs_r, hh * 64, bht[:, h:h + 1], bht[:, 24 + h:25 + h], bht_r,
                                       hT, hT_r, qT, qT_r, BLKS, C, Sn, tab_r, t1r, t2r)
                        if self.dbg == 2 or (self.dbg == 12 and g == 1):
                            return
                        for qt in range(NT):
                            if self.dbg == 3 and qt == 1:
                                return
                            if self.dbg == 4 and qt == 3:
                                return
                            if self.dbg == 5 and hh == 1:
                                return
                            psc, psc_r = pscr.next()
                            sm, sm_r = smr.next()
                            pw, pw_r = pwr.next()
                            lat = qt >= 2
                            S.op("pe", lambda e, psc=psc, qT=qT, qt=qt: e.matmul(
                                psc[:, 1, 0:256], lhsT=qT[:, qt * 128:(qt + 1) * 128], rhs=kT[:, 0:256], start=True, stop=True),
                                reads=[qT_r, kT_r], writes=[psc_r], pe_acc=True)
                            if lat:
                                qb = qt - 2
                                lo = max(0, qb - 1)
                                hi = min(15, qb + 1)
                                c0 = (lo - (qb - 1)) * 128
                                c1 = c0 + (hi - lo + 1) * 128
                                ktiles = list(range(lo + 2, hi + 3))
                                S.op("pe", lambda e, psc=psc, qT=qT, qt=qt, lo=lo, hi=hi, c0=c0, c1=c1: e.matmul(
                                    psc[:, 0, c0:c1], lhsT=qT[:, qt * 128:(qt + 1) * 128], rhs=kT[:, 256 + lo * 128:256 + (hi + 1) * 128],
                                    start=True, stop=True), reads=[qT_r, kT_r], writes=[psc_r], pe_acc=True)
                                sw, sw_r = swr.next()
                                S.op("dve", lambda e, sw=sw, psc=psc, c0=c0, c1=c1: e.tensor_tensor(
                                    out=sw[:, c0:c1], in0=psc[:, 0, c0:c1], in1=mW[:, c0:c1], op=ALU.add),
                                    reads=[psc_r, mW_r], writes=[sw_r])
                                S.op("dve", lambda e, sm=sm, sw=sw, c0=c0, c1=c1: e.reduce_max(out=sm[:, 0:1], in_=sw[:, c0:c1], axis=AX.X),
                                     reads=[sw_r], writes=[sm_r])
                            else:
                                ktiles = []
                            S.op("dve", lambda e, sm=sm, psc=psc: e.reduce_max(out=sm[:, 1:2], in_=psc[:, 1, 0:256], axis=AX.X),
                                 reads=[psc_r], writes=[sm_r])
                            if lat:
                                S.op("dve", lambda e, sm=sm: e.tensor_tensor(out=sm[:, 1:2], in0=sm[:, 0:1], in1=sm[:, 1:2], op=ALU.max),
                                     reads=[sm_r], writes=[sm_r])
                            S.op("dve", lambda e, sm=sm, h=h: e.scalar_tensor_tensor(out=sm[:, 2:3], in0=sm[:, 1:2], scalar=0.125, in1=skb[:, h:h + 1],
                                                                                   op0=ALU.mult, op1=ALU.max),
                                 reads=[sm_r, skb_r], writes=[sm_r])
                            S.op("dve", lambda e, sm=sm: e.tensor_scalar(out=sm[:, 3:4], in0=sm[:, 2:3], scalar1=-1.0, scalar2=None, op0=ALU.mult),
                                 reads=[sm_r], writes=[sm_r])
                            S.op("pool", lambda e, sm=sm: e.memset(sm[:, 4:7], 0.0), reads=[sm_r], writes=[sm_r])
                            if lat:
                                S.op("act", lambda e, pw=pw, sw=sw, sm=sm, c0=c0, c1=c1: e.activation(
                                    out=pw[:, c0:c1], in_=sw[:, c0:c1], func=AF.Exp, scale=0.125, bias=sm[:, 3:4], accum_out=sm[:, 4:5]),
                                    reads=[sw_r, sm_r], writes=[pw_r, sm_r])
                            S.op("act", lambda e, pw=pw, psc=psc, sm=sm: e.activation(
                                out=pw[:, 384:640], in_=psc[:, 1, 0:256], func=AF.Exp, scale=0.125, bias=sm[:, 3:4], accum_out=sm[:, 5:6]),
                                reads=[psc_r, sm_r], writes=[pw_r, sm_r])
                            S.op("act", lambda e, sm=sm, h=h: e.activation(out=sm[:, 6:7], in_=skb[:, h:h + 1], func=AF.Exp, scale=1.0, bias=sm[:, 3:4]),
                                 reads=[skb_r, sm_r], writes=[sm_r])
                            S.op("dve", lambda e, sm=sm: e.tensor_tensor(out=sm[:, 4:5], in0=sm[:, 4:5], in1=sm[:, 5:6], op=ALU.add),
                                 reads=[sm_r], writes=[sm_r])
                            S.op("dve", lambda e, sm=sm: e.tensor_tensor(out=sm[:, 4:5], in0=sm[:, 4:5], in1=sm[:, 6:7], op=ALU.add),
                                 reads=[sm_r], writes=[sm_r])
                            S.op("dve", lambda e, sm=sm: e.reciprocal(out=sm[:, 7:8], in_=sm[:, 4:5]), reads=[sm_r], writes=[sm_r])
                            srcs = []
                            if lat:
                                for j, kt_ in enumerate(ktiles):
                                    srcs.append((c0 + j * 128, kt_))
                            srcs += [(384, 0), (512, 1)]
                            for j, (col, kt_) in enumerate(srcs):
                                S.op("pe", lambda e, j=j, col=col, pw=pw: e.transpose(out=pT[:, j, :], in_=pw[:, col:col + 128], identity=self.identb[:]),
                                     reads=[pw_r, self.identb_r], writes=[pT_r], pe_acc=True)
                            pTs, pTs_r = pTsr.next()
                            nj = len(srcs)
                            S.op("act", lambda e, pTs=pTs, nj=nj: e.activation(out=pTs[:, 0:nj, :], in_=pT[:, 0:nj, :], func=AF.Copy),
                                 reads=[pT_r], writes=[pTs_r])
                            for j, (col, kt_) in enumerate(srcs):
                                S.op("pe", lambda e, j=j, kt_=kt_, pTs=pTs, nj=nj: e.matmul(
                                    pso[:, 64:128], lhsT=pTs[:, j, :], rhs=vs[:, kt_, :], start=(j == 0), stop=(j == nj - 1)),
                                    reads=[pTs_r, vs_r], writes=[pso_r], pe_acc=True)
                            osb, osb_r = osr.next()
                            S.op("dve", lambda e, osb=osb, sm=sm: e.tensor_scalar(out=osb[:], in0=pso[:, 64:128], scalar1=sm[:, 7:8], scalar2=None, op0=ALU.mult),
                                 reads=[pso_r, sm_r], writes=[osb_r])
                            S.op("pe", lambda e, osb=osb: e.transpose(out=pso[0:64, 128:256], in_=osb[:], identity=self.identf[:]),
                                 reads=[osb_r, self.identf_r], writes=[pso_r], pe_acc=True)
                            S.op("act", lambda e, hh=hh, qt=qt: e.activation(out=oTg[:, hh, qt * 128:(qt + 1) * 128], in_=pso[0:64, 128:256], func=AF.Copy),
                                 reads=[pso_r], writes=[oTg_r])
                    if self.dbg == 6 or (self.dbg == 16 and g == 1):
                        return
                    for (s, n) in BLKS:
                        for oc in range(8):
                            for hh in range(4):
                                S.op("pe", lambda e, hh=hh, oc=oc, s=s, n=n: e.matmul(
                                    pj[:, 0, :n], lhsT=wog[:, hh, oc * 128:(oc + 1) * 128], rhs=oTg[:, hh, s:s + n], start=(hh == 0), stop=(hh == 3)),
                                    reads=[wog_r, oTg_r], writes=[pj_r], pe_acc=True)
                            if g == 0:
                                self.resid(pj[:, 0, :], pj_r, n, oc, s, gb=gb, tring=tr)
                            else:
                                self.resid(pj[:, 0, :], pj_r, n, oc, s)
                    S.barrier()
                    S.release([wq_r, wk_r, wv_r, wog_r])

    def mixer3(self, i):
        nc, S, M = self.nc, self.S, self.M
        wqkv = self.inp("diff_w_qkv", [1, D, 3 * D])
        wo = self.inp("diff_w_o", [1, D, D])
        lvec = self.inp("diff_lam", [4, 64])
        sg = self.inp("diff_subln_g", [128, 1])
        lambda_init = 0.8 - 0.6 * math.exp(-0.3 * i)
        LBLK = BLKS[1:]
        with ExitStack() as pA:
            hT, hT_r = M.sb(pA, [128, 8, T], BF16, "hT")
            with ExitStack() as pD:
                self.adanorm(pD, hT, hT_r, 0)
                S.barrier()
            C, Sn, tab_r = self.load_tables(pA)
            zb, zb_r = M.sb(pA, [64, 1], F32, "zb")
            S.op("pool", lambda e: e.memset(zb[:], 0.0), writes=[zb_r])
            sgt, sgt_r = M.sb(pA, [128, 1], F32, "sgt")
            S.dma("sp", sgt[:], sg, writes=[sgt_r], key=sgt_r)
            lv, lv_r = M.sb(pA, [128, 4, 64], F32, "lv")
            for a in range(4):
                S.dma("sp", lv[:, a, :], lvec[a].partition_broadcast(128), writes=[lv_r], key=lv_r)
            lam, lam_r = M.sb(pA, [128, 4], F32, "lam")
            lp, lp_r = M.sb(pA, [128, 2, 64], F32, "lp")
            for a in range(2):
                S.op("dve", lambda e, a=a: e.tensor_tensor(out=lp[:, a, :], in0=lv[:, 2 * a, :], in1=lv[:, 2 * a + 1, :], op=ALU.mult),
                     reads=[lv_r], writes=[lp_r])
                S.op("dve", lambda e, a=a: e.reduce_sum(out=lam[:, a:a + 1], in_=lp[:, a, :], axis=AX.X), reads=[lp_r], writes=[lam_r])
            S.op("act", lambda e: e.activation(out=lam[:, 0:2], in_=lam[:, 0:2], func=AF.Exp), reads=[lam_r], writes=[lam_r])
            S.op("dve", lambda e: e.tensor_tensor(out=lam[:, 2:3], in0=lam[:, 0:1], in1=lam[:, 1:2], op=ALU.subtract), reads=[lam_r], writes=[lam_r])
            S.op("dve", lambda e: e.tensor_scalar(out=lam[:, 3:4], in0=lam[:, 2:3], scalar1=float(lambda_init), scalar2=None, op0=ALU.add),
                 reads=[lam_r], writes=[lam_r])
            kTs = [M.sb(pA, [64, T], BF16, "kT%d" % t) for t in range(2)]
            qTs = [M.sb(pA, [64, T], BF16, "qT%d" % t) for t in range(2)]
            vs, vs_r = M.sb(pA, [128, NT, 128], BF16, "vs")
            oT, oT_r = M.sb(pA, [128, NLAT], BF16, "oT")
            pbr = Ring([M.sb(pA, [128, T], BF16, "pb") for _ in range(2)])
            pTs, pTs_r = M.sb(pA, [128, NT, 128], BF16, "pTs")
            t1r = Ring([M.sb(pA, [64, 512], F32, "rp1") for _ in range(2)])
            t2r = Ring([M.sb(pA, [64, 512], F32, "rp2") for _ in range(2)])
            smr = Ring([M.sb(pA, [128, 16], F32, "sm") for _ in range(4)])
            o1r = Ring([M.sb(pA, [128, 128], F32, "o1") for _ in range(2)])
            o2r = Ring([M.sb(pA, [128, 128], F32, "o2") for _ in range(2)])
            jkr = Ring([M.sb(pA, [128, 128], F32, "jk") for _ in range(2)])
            psc, psc_r = M.ps(pA, [128, 5, 512], F32, "psc")
            pbank = [Res("pscb%d" % j) for j in range(5)]
            pT, pT_r = M.ps(pA, [128, 6, 128], BF16, "pT")
            po, po_r = M.ps(pA, [128, 2, 128], F32, "po")
            pout, pout_r = M.ps(pA, [128, 512], F32, "pout")
            KB = [(j * 512, min(512, T - j * 512)) for j in range(5)]
            for c in range(8):
                with ExitStack() as pG:
                    wq, wq_r = self.load_w(pG, wqkv[0, :, c * 128:(c + 1) * 128], 8, 128, "wq")
                    wk, wk_r = self.load_w(pG, wqkv[0, :, D + c * 128:D + (c + 1) * 128], 8, 128, "wk")
                    wv, wv_r = self.load_w(pG, wqkv[0, :, 2 * D + c * 128:2 * D + (c + 1) * 128], 8, 128, "wv")
                    woc, woc_r = self.load_w(pG, wo[0, c * 128:(c + 1) * 128, :], 1, D, "woc")
                    wqs, wqs_r = self.swap_halves(pG, wq, wq_r, 8, 2, "wqs")
                    wks, wks_r = self.swap_halves(pG, wk, wk_r, 8, 2, "wks")
                    for t in range(2):
                        self.proj_rope(psc, psc_r, wk, wk_r, wks, wks_r, t * 64, zb[:, 0:1], zb[:, 0:1], zb_r,
                                       hT, hT_r, kTs[t][0], kTs[t][1], BLKS, C, Sn, tab_r, t1r, t2r)
                        self.proj_rope(psc, psc_r, wq, wq_r, wqs, wqs_r, t * 64, zb[:, 0:1], zb[:, 0:1], zb_r,
                                       hT, hT_r, qTs[t][0], qTs[t][1], LBLK, C, Sn, tab_r, t1r, t2r)
                    for tt in range(NT):
                        for k in range(8):
                            S.op("pe", lambda e, tt=tt, k=k: e.matmul(pout[:, 0:128], lhsT=hT[:, k, tt * 128:(tt + 1) * 128], rhs=wv[:, k, :],
                                                                      start=(k == 0), stop=(k == 7)),
                                 reads=[hT_r, wv_r], writes=[pout_r], pe_acc=True)
                        S.op("act", lambda e, tt=tt: e.activation(out=vs[:, tt, :], in_=pout[:, 0:128], func=AF.Copy),
                             reads=[pout_r], writes=[vs_r])
                    S.barrier()
                    for qb in range(16):
                        q0 = NCTX + qb * 128
                        sms = []
                        for t in range(2):
                            qT, qT_r = qTs[t]
                            kT, kT_r = kTs[t]
                            sm, sm_r = smr.next()
                            sms.append((sm, sm_r))
                            for j, (k0, kn) in enumerate(KB):
                                S.op("pe", lambda e, j=j, k0=k0, kn=kn, qT=qT, kT=kT, q0=q0: e.matmul(
                                    psc[:, j, :kn], lhsT=qT[:, q0:q0 + 128], rhs=kT[:, k0:k0 + kn], start=True, stop=True),
                                    reads=[qT_r, kT_r, psc_r], writes=[pbank[j]], pe_acc=True)
                            for j, (k0, kn) in enumerate(KB):
                                S.op("dve", lambda e, j=j, kn=kn, sm=sm: e.reduce_max(out=sm[:, j:j + 1], in_=psc[:, j, :kn], axis=AX.X),
                                     reads=[pbank[j]], writes=[sm_r])
                            S.op("dve", lambda e, sm=sm: e.reduce_max(out=sm[:, 5:6], in_=sm[:, 0:5], axis=AX.X), reads=[sm_r], writes=[sm_r])
                            S.op("dve", lambda e, sm=sm: e.tensor_scalar(out=sm[:, 6:7], in0=sm[:, 5:6], scalar1=-0.125, scalar2=None, op0=ALU.mult),
                                 reads=[sm_r], writes=[sm_r])
                            S.op("pool", lambda e, sm=sm: e.memset(sm[:, 8:13], 0.0), reads=[sm_r], writes=[sm_r])
                            pb, pb_r = pbr.next()
                            for j, (k0, kn) in enumerate(KB):
                                S.op("act", lambda e, j=j, k0=k0, kn=kn, sm=sm, pb=pb: e.activation(
                                    out=pb[:, k0:k0 + kn], in_=psc[:, j, :kn], func=AF.Exp, scale=0.125, bias=sm[:, 6:7], accum_out=sm[:, 8 + j:9 + j]),
                                    reads=[pbank[j], sm_r], writes=[pb_r, sm_r])
                            S.op("dve", lambda e, sm=sm: e.reduce_sum(out=sm[:, 13:14], in_=sm[:, 8:13], axis=AX.X), reads=[sm_r], writes=[sm_r])
                            S.op("dve", lambda e, sm=sm: e.reciprocal(out=sm[:, 14:15], in_=sm[:, 13:14]), reads=[sm_r], writes=[sm_r])
                            for b3 in range(3):
                                for jj in range(6):
                                    j = b3 * 6 + jj
                                    S.op("pe", lambda e, j=j, jj=jj, pb=pb: e.transpose(out=pT[:, jj, :], in_=pb[:, j * 128:(j + 1) * 128], identity=self.identb[:]),
                                         reads=[pb_r, self.identb_r], writes=[pT_r], pe_acc=True)
                                if b3 % 2 == 0:
                                    S.op("act", lambda e, b3=b3: e.activation(out=pTs[:, b3 * 6:(b3 + 1) * 6, :], in_=pT[:], func=AF.Copy),
                                         reads=[pT_r], writes=[pTs_r])
                                else:
                                    S.op("dve", lambda e, b3=b3: e.tensor_copy(out=pTs[:, b3 * 6:(b3 + 1) * 6, :], in_=pT[:]),
                                         reads=[pT_r], writes=[pTs_r])
                            for j in range(NT):
                                S.op("pe", lambda e, j=j, t=t: e.matmul(po[:, t, :], lhsT=pTs[:, j, :], rhs=vs[:, j, :], start=(j == 0), stop=(j == NT - 1)),
                                     reads=[pTs_r, vs_r], writes=[po_r], pe_acc=True)
                        (sm0, sm0_r), (sm1, sm1_r) = sms
                        o1, o1_r = o1r.next()
                        o2, o2_r = o2r.next()
                        jk, jk_r = jkr.next()
                        S.op("dve", lambda e, sm1=sm1: e.tensor_tensor(out=sm1[:, 15:16], in0=sm1[:, 14:15], in1=lam[:, 3:4], op=ALU.mult),
                             reads=[sm1_r, lam_r], writes=[sm1_r])
                        S.op("dve", lambda e, o1=o1, sm1=sm1: e.tensor_scalar(out=o1[:], in0=po[:, 1, :], scalar1=sm1[:, 15:16], scalar2=None, op0=ALU.mult),
                             reads=[po_r, sm1_r], writes=[o1_r])
                        S.op("dve", lambda e, o1=o1, o2=o2, sm0=sm0: e.scalar_tensor_tensor(out=o2[:], in0=po[:, 0, :], scalar=sm0[:, 14:15], in1=o1[:],
                                                                                         op0=ALU.mult, op1=ALU.subtract),
                             reads=[po_r, sm0_r, o1_r], writes=[o2_r])
                        S.op("pool", lambda e, sm0=sm0: e.memset(sm0[:, 7:8], 0.0), reads=[sm0_r], writes=[sm0_r])
                        S.op("act", lambda e, jk=jk, o2=o2, sm0=sm0: e.activation(out=jk[:], in_=o2[:], func=AF.Square, accum_out=sm0[:, 7:8]),
                             reads=[o2_r, sm0_r], writes=[jk_r, sm0_r])
                        S.op("act", lambda e, sm0=sm0: e.activation(out=sm0[:, 7:8], in_=sm0[:, 7:8], func=AF.Sqrt, scale=1.0 / 128, bias=self.epsb[:, 0:1]),
                             reads=[sm0_r, self.epsb_r], writes=[sm0_r])
                        S.op("dve", lambda e, sm0=sm0: e.reciprocal(out=sm0[:, 7:8], in_=sm0[:, 7:8]), reads=[sm0_r], writes=[sm0_r])
                        S.op("dve", lambda e, o2=o2, sm0=sm0: e.tensor_scalar(out=o2[:], in0=o2[:], scalar1=sm0[:, 7:8], scalar2=float(1.0 - lambda_init),
                                                                            op0=ALU.mult, op1=ALU.mult),
                             reads=[o2_r, sm0_r], writes=[o2_r])
                        S.op("pe", lambda e, o2=o2: e.transpose(out=pout[:, 128:256], in_=o2[:], identity=self.identf[:]),
                             reads=[o2_r, self.identf_r], writes=[pout_r], pe_acc=True)
                        S.op("act", lambda e, qb=qb: e.activation(out=oT[:, qb * 128:(qb + 1) * 128], in_=pout[:, 128:256], func=AF.Identity,
                                                                  scale=sgt[:, 0:1]),
                             reads=[pout_r, sgt_r], writes=[oT_r])
                    for (s, n) in LBLK:
                        for oc in range(8):
                            S.op("pe", lambda e, oc=oc, s=s, n=n: e.matmul(
                                pout[:, :n], lhsT=woc[:, 0, oc * 128:(oc + 1) * 128], rhs=oT[:, s - NCTX:s - NCTX + n], start=True, stop=True),
                                reads=[woc_r, oT_r], writes=[pout_r], pe_acc=True)
                            self.resid(pout, pout_r, n, oc, s)
                    S.barrier()
                    S.release([wq_r, wk_r, wv_r, woc_r])

    def mixer1(self, i):
        nc, S, M = self.nc, self.S, self.M
        w_in = self.inp("ssm_w_in", [1, D, 5184])
        w_out = self.inp("ssm_w_out", [1, 2048, D])
        svec = self.inp("ssm_vec", [128, 160])
        a_log = self.inp("ssm_a_log", [1, 64])
        dt_bias = self.inp("ssm_dt_bias", [1, 64])
        d_skip = self.inp("ssm_d", [1, 32])
        dr = lambda name, shape: (nc.dram_tensor(name, list(shape), BF16, kind="Internal").ap(), Res(name))
        XS, XS_r = dr("scr_xs", [NT, 128, 2048])
        BTd, BTd_r = dr("scr_bt", [NT, 128, 4, 128])
        CTd, CTd_r = dr("scr_ct", [NT, 128, 4, 128])
        BMd, BMd_r = dr("scr_bm", [NT, 128, 512])
        ZS, ZS_r = dr("scr_zs", [NT, 128, 2048])
        HB, HB_r = dr("scr_hb", [NT, 128, 2048])
        PW = 2312
        OW = 2308

        def pcol(s):
            return 2 if s == 0 else s + 6
        with ExitStack() as pA:
            sv, sv_r = M.sb(pA, [128, 160], F32, "sv")
            S.dma("sp", sv[:], svec, writes=[sv_r], key=sv_r)
            msk, msk_r = M.sb(pA, [128, 4, 128], F32, "msk")
            S.op("pool", lambda e: e.memset(msk[:], 1.0), writes=[msk_r])
            for a, (pat, cm, op) in enumerate((([[1, 128]], -1, ALU.is_ge), ([[-1, 128]], 1, ALU.is_ge),
                                              ([[-1, 128]], 1, ALU.is_gt), ([[1, 128]], -1, ALU.is_gt))):
                S.op("pool", lambda e, a=a, pat=pat, cm=cm, op=op: e.affine_select(
                    out=msk[:, a, :], in_=msk[:, a, :], pattern=pat, compare_op=op, fill=0.0, base=0, channel_multiplier=cm),
                    reads=[msk_r], writes=[msk_r])
            triF, triB, mltF, mltB = (msk[:, a, :] for a in range(4))
            dt, dt_r = M.sb(pA, [128, NT, 64], F32, "dt")
            loga, loga_r = M.sb(pA, [128, NT, 64], F32, "loga")
            expA, expA_r = M.sb(pA, [128, NT, 64], F32, "expA")
            dec, dec_r = M.sb(pA, [128, NT, 64], F32, "dec")
            wst, wst_r = M.sb(pA, [128, NT, 64], F32, "wst")
            abc, abc_r = M.sb(pA, [128, 64], F32, "abc")
            dtb, dtb_r = M.sb(pA, [128, 64], F32, "dtb")
            dsk, dsk_r = M.sb(pA, [128, 32], F32, "dsk")
            S.dma("sp", abc[:], a_log[0].partition_broadcast(128), writes=[abc_r], key=abc_r)
            S.dma("sp", dtb[:], dt_bias[0].partition_broadcast(128), writes=[dtb_r], key=dtb_r)
            S.dma("sp", dsk[:], d_skip[0].partition_broadcast(128), writes=[dsk_r], key=dsk_r)
            S.op("act", lambda e: e.activation(out=abc[:], in_=abc[:], func=AF.Exp), reads=[abc_r], writes=[abc_r])
            S.op("dve", lambda e: e.tensor_scalar(out=abc[:], in0=abc[:], scalar1=-1.0, scalar2=None, op0=ALU.mult), reads=[abc_r], writes=[abc_r])
            with ExitStack() as pB:
                hT, hT_r = M.sb(pB, [128, 8, T], BF16, "hT")
                with ExitStack() as pD:
                    self.adanorm(pD, hT, hT_r, 0)
                    S.barrier()
                with ExitStack() as pC:
                    wdt, wdt_r = self.load_w(pC, w_in[0, :, 5120:5184], 8, 64, "wdt")
                    pdt = Ring([M.ps(pC, [128, 64], F32, "pdt") for _ in range(2)])
                    pcs = Ring([M.ps(pC, [128, 2, 64], F32, "pcs") for _ in range(2)])
                    tmr = Ring([[M.sb(pC, [128, 64], F32, "sp%d" % j) for j in range(4)] for _ in range(2)])
                    for t in range(NT):
                        p_, p_r = pdt.next()
                        for k in range(8):
                            S.op("pe", lambda e, p_=p_, t=t, k=k: e.matmul(p_[:], lhsT=hT[:, k, t * 128:(t + 1) * 128], rhs=wdt[:, k, :],
                                                                          start=(k == 0), stop=(k == 7)),
                                 reads=[hT_r, wdt_r], writes=[p_r], pe_acc=True)
                        (x_, x_r), (ax, ax_r), (ex, ex_r), (rl, rl_r) = tmr.next()
                        S.op("dve", lambda e, x_=x_, p_=p_: e.tensor_tensor(out=x_[:], in0=p_[:], in1=dtb[:], op=ALU.add),
                             reads=[p_r, dtb_r], writes=[x_r])
                        S.op("act", lambda e, ax=ax, x_=x_: e.activation(out=ax[:], in_=x_[:], func=AF.Abs),
                             reads=[x_r], writes=[ax_r])
                        S.op("act", lambda e, ex=ex, ax=ax: e.activation(out=ex[:], in_=ax[:], func=AF.Exp, scale=-1.0), reads=[ax_r], writes=[ex_r])
                        S.op("act", lambda e, ex=ex: e.activation(out=ex[:], in_=ex[:], func=AF.Ln, bias=self.onesf[:, 0:1], scale=1.0),
                             reads=[ex_r, self.onesf_r], writes=[ex_r])
                        S.op("dve", lambda e, rl=rl, x_=x_: e.tensor_scalar(out=rl[:], in0=x_[:], scalar1=0.0, scalar2=None, op0=ALU.max),
                             reads=[x_r], writes=[rl_r])
                        S.op("dve", lambda e, rl=rl, ex=ex, t=t: e.tensor_tensor(out=dt[:, t, :], in0=rl[:], in1=ex[:], op=ALU.add),
                             reads=[rl_r, ex_r], writes=[dt_r])
                        S.op("dve", lambda e, t=t: e.tensor_tensor(out=loga[:, t, :], in0=dt[:, t, :], in1=abc[:], op=ALU.mult),
                             reads=[dt_r, abc_r], writes=[loga_r])
                        c_, c_r = pcs.next()
                        S.op("pe", lambda e, c_=c_, t=t: e.matmul(c_[:, 0, 0:32], lhsT=triF, rhs=loga[:, t, 0:32], start=True, stop=True),
                             reads=[msk_r, loga_r], writes=[c_r], pe_acc=True)
                        S.op("pe", lambda e, c_=c_, t=t: e.matmul(c_[:, 0, 32:64], lhsT=triB, rhs=loga[:, t, 32:64], start=True, stop=True),
                             reads=[msk_r, loga_r], writes=[c_r], pe_acc=True)
                        S.op("pe", lambda e, c_=c_, t=t: e.matmul(c_[:, 1, :], lhsT=self.onesf[:], rhs=loga[:, t, :], start=True, stop=True),
                             reads=[self.onesf_r, loga_r], writes=[c_r], pe_acc=True)
                        S.op("act", lambda e, c_=c_, t=t: e.activation(out=expA[:, t, :], in_=c_[:, 0, :], func=AF.Exp), reads=[c_r], writes=[expA_r])
                        S.op("act", lambda e, c_=c_, t=t: e.activation(out=dec[:, t, :], in_=c_[:, 1, :], func=AF.Exp), reads=[c_r], writes=[dec_r])
                        S.op("act", lambda e, c_=c_, ax=ax: e.activation(out=ax[:], in_=c_[:, 0, :], func=AF.Copy), reads=[c_r, ax_r], writes=[ax_r])
                        S.op("dve", lambda e, c_=c_, ax=ax: e.tensor_tensor(out=ax[:], in0=c_[:, 1, :], in1=ax[:], op=ALU.subtract),
                             reads=[c_r, ax_r], writes=[ax_r])
                        S.op("act", lambda e, ax=ax: e.activation(out=ax[:], in_=ax[:], func=AF.Exp), reads=[ax_r], writes=[ax_r])
                        S.op("dve", lambda e, ax=ax, t=t: e.tensor_tensor(out=wst[:, t, :], in0=ax[:], in1=dt[:, t, :], op=ALU.mult),
                             reads=[ax_r, dt_r], writes=[wst_r])
                    S.barrier()
                    S.release([wdt_r])
                with ExitStack() as pC:
                    wz, wz_r = self.load_w(pC, w_in[0, :, 0:2048], 8, 2048, "wz")
                    pz, pz_r = M.ps(pC, [128, 4, 512], F32, "pz")
                    zr = Ring([M.sb(pC, [128, 2048], BF16, "zt") for _ in range(2)])
                    for t in range(NT):
                        for nb in range(4):
                            for k in range(8):
                                S.op("pe", lambda e, t=t, nb=nb, k=k: e.matmul(pz[:, nb, :], lhsT=hT[:, k, t * 128:(t + 1) * 128],
                                                                               rhs=wz[:, k, nb * 512:(nb + 1) * 512], start=(k == 0), stop=(k == 7)),
                                     reads=[hT_r, wz_r], writes=[pz_r], pe_acc=True)
                        z_, z_r = zr.next()
                        S.op("act", lambda e, z_=z_: e.activation(out=z_[:].rearrange("p (a b) -> p a b", b=512), in_=pz[:], func=AF.Silu),
                             reads=[pz_r], writes=[z_r])
                        S.dma("sp", ZS[t], z_[:], reads=[z_r], writes=[ZS_r], key=z_r)
                    S.barrier()
                    S.release([wz_r] + [r for _, r in zr.items])
                with ExitStack() as pC:
                    wpr = Ring([M.sb(pC, [128, 8, 128], BF16, "wxp") for _ in range(3)])
                    upr = Ring([M.sb(pC, [128, PW], F32, "upad") for _ in range(1)])
                    acr = Ring([M.sb(pC, [128, OW], F32, "cacc") for _ in range(1)])
                    scr = Ring([M.sb(pC, [128, T], BF16, "scc") for _ in range(2)])
                    xh, xh_r = M.sb(pC, [128, NT, 512], BF16, "xhalf")
                    btm, btm_r = M.sb(pC, [128, NT, 128], BF16, "btm")
                    pp = Ring([M.ps(pC, [128, 512], F32, "pxp") for _ in range(2)])
                    ptr = Ring([M.ps(pC, [128, 6, 128], BF16, "ptx") for _ in range(2)])
                    for u, u_r in upr.items:
                        S.op("pool", lambda e, u=u: e.memset(u[:], 0.0), writes=[u_r])
                    for cc in range(24):
                        wt, wt_r = wpr.next()
                        S.dma("pool", wt[:], w_in[0, :, 2048 + cc * 128:2048 + (cc + 1) * 128].rearrange("(k p) n -> p k n", p=128),
                              writes=[wt_r], key=wt_r)
                        u, u_r = upr.next()
                        for (s, n) in BLKS:
                            p_, p_r = pp.next()
                            for k in range(8):
                                S.op("pe", lambda e, p_=p_, wt=wt, k=k, s=s, n=n: e.matmul(p_[:, :n], lhsT=wt[:, k, :], rhs=hT[:, k, s:s + n],
                                                                                          start=(k == 0), stop=(k == 7)),
                                     reads=[wt_r, hT_r], writes=[p_r], pe_acc=True)
                            S.op("act", lambda e, p_=p_, u=u, s=s, n=n: e.activation(out=u[:, pcol(s):pcol(s) + n], in_=p_[:, :n], func=AF.Copy),
                                 reads=[p_r], writes=[u_r])
                        ac, ac_r = acr.next()
                        eng = "dve"
                        S.op(eng, lambda e, ac=ac, u=u, cc=cc: e.tensor_scalar(
                            out=ac[:], in0=u[:, 0:OW], scalar1=sv[:, cc * 5:cc * 5 + 1], scalar2=sv[:, 120 + cc:121 + cc], op0=ALU.mult, op1=ALU.add),
                            reads=[u_r, sv_r], writes=[ac_r])
                        for w in range(1, 5):
                            S.op(eng, lambda e, ac=ac, u=u, cc=cc, w=w: e.scalar_tensor_tensor(
                                out=ac[:], in0=u[:, w:w + OW], scalar=sv[:, cc * 5 + w:cc * 5 + w + 1], in1=ac[:], op0=ALU.mult, op1=ALU.add),
                                reads=[u_r, sv_r, ac_r], writes=[ac_r])
                        sc, sc_r = scr.next()
                        S.op("act", lambda e, sc=sc, ac=ac: e.activation(out=sc[:, 0:NCTX], in_=ac[:, 0:NCTX], func=AF.Silu), reads=[ac_r], writes=[sc_r])
                        S.op("act", lambda e, sc=sc, ac=ac: e.activation(out=sc[:, NCTX:T], in_=ac[:, 260:260 + NLAT], func=AF.Silu), reads=[ac_r], writes=[sc_r])
                        if cc < 20:
                            for b3 in range(3):
                                pt, pt_r = ptr.next()
                                for jj in range(6):
                                    t = b3 * 6 + jj
                                    S.op("pe", lambda e, pt=pt, jj=jj, t=t, sc=sc: e.transpose(out=pt[:, jj, :], in_=sc[:, t * 128:(t + 1) * 128], identity=self.identb[:]),
                                         reads=[sc_r, self.identb_r], writes=[pt_r], pe_acc=True)
                                if cc < 16:
                                    c8 = cc % 4
                                    S.op("dve", lambda e, pt=pt, b3=b3, c8=c8: e.tensor_copy(out=xh[:, b3 * 6:(b3 + 1) * 6, c8 * 128:(c8 + 1) * 128], in_=pt[:]),
                                         reads=[pt_r], writes=[xh_r])
                                else:
                                    S.op("dve", lambda e, pt=pt, b3=b3: e.tensor_copy(out=btm[:, b3 * 6:(b3 + 1) * 6, :], in_=pt[:]),
                                         reads=[pt_r], writes=[btm_r])
                        if cc < 16 and cc % 4 == 3:
                            half = cc // 4
                            S.dma("sp", XS[:, :, half * 512:(half + 1) * 512].rearrange("t l f -> l t f"), xh[:],
                                  reads=[xh_r], writes=[XS_r], key=xh_r)
                        if 16 <= cc < 20:
                            g = cc - 16
                            S.dma("sp", BTd[:, :, g, :].rearrange("t n l -> n t l"), sc[:].rearrange("p (t l) -> p t l", l=128),
                                  reads=[sc_r], writes=[BTd_r], key=sc_r)
                            S.dma("sp", BMd[:, :, g * 128:(g + 1) * 128].rearrange("t l n -> l t n"), btm[:],
                                  reads=[btm_r], writes=[BMd_r], key=btm_r)
                        if cc >= 20:
                            g = cc - 20
                            S.dma("sp", CTd[:, :, g, :].rearrange("t n l -> n t l"), sc[:].rearrange("p (t l) -> p t l", l=128),
                                  reads=[sc_r], writes=[CTd_r], key=sc_r)
                    S.barrier()
                    S.release([r for _, r in wpr.items] + [r for _, r in scr.items] + [xh_r, btm_r])
            order_b = [1, 0] + list(range(NT - 1, 1, -1))
            Hf, Hf_r = M.sb(pA, [128, 4, 512], F32, "Hst")
            Hb16, Hb16_r = M.sb(pA, [128, 4, 512], BF16, "Hst16")
            pst, pst_r = M.ps(pA, [128, 512], F32, "pst")
            xsr = Ring([M.sb(pA, [128, 2048], BF16, "xs") for _ in range(2)])
            bmr = Ring([M.sb(pA, [128, 512], BF16, "bm") for _ in range(2)])
            xwr = Ring([M.sb(pA, [128, 2048], BF16, "xw") for _ in range(1)])
            hbo = Ring([M.sb(pA, [128, 2048], BF16, "hbo") for _ in range(2)])

            def bc(ap2d):
                return ap2d.unsqueeze(2).to_broadcast([128, 8, 64])

            def v3(ap2d):
                return ap2d.rearrange("p (h d) -> p h d", d=64)

            def state_update(t, xs, xs_r, bm, bm_r, d0):
                xw, xw_r = xwr.next()
                for g in range(4):
                    S.op("pool" if g % 2 else "dve", lambda e, g=g, xw=xw, xs=xs, t=t: e.tensor_tensor(
                        out=v3(xw[:, g * 512:(g + 1) * 512]), in0=v3(xs[:, g * 512:(g + 1) * 512]),
                        in1=bc(wst[:, t, d0 + g * 8:d0 + (g + 1) * 8]), op=ALU.mult),
                        reads=[xs_r, wst_r], writes=[xw_r])
                for g in range(4):
                    S.op("pe", lambda e, g=g, bm=bm, xw=xw: e.matmul(pst[:], lhsT=bm[:, g * 128:(g + 1) * 128], rhs=xw[:, g * 512:(g + 1) * 512],
                                                                     start=True, stop=True),
                         reads=[bm_r, xw_r], writes=[pst_r], pe_acc=True)
                    S.op("dve", lambda e, g=g, t=t: e.tensor_tensor(out=v3(Hf[:, g, :]), in0=v3(Hf[:, g, :]),
                                                                    in1=bc(dec[:, t, d0 + g * 8:d0 + (g + 1) * 8]), op=ALU.mult),
                         reads=[Hf_r, dec_r], writes=[Hf_r])
                    S.op("dve", lambda e, g=g: e.tensor_tensor(out=Hf[:, g, :], in0=Hf[:, g, :], in1=pst[:], op=ALU.add),
                         reads=[Hf_r, pst_r], writes=[Hf_r])
            S.op("pool", lambda e: e.memset(Hf[:], 0.0), writes=[Hf_r])
            for t in order_b:
                xs, xs_r = xsr.next()
                bm, bm_r = bmr.next()
                S.dma("sp", xs[:], XS[t], reads=[XS_r], writes=[xs_r], key=xs_r)
                S.dma("sp", bm[:], BMd[t], reads=[BMd_r], writes=[bm_r], key=bm_r)
                ho, ho_r = hbo.next()
                S.op("act", lambda e, ho=ho: e.activation(out=ho[:].rearrange("p (g f) -> p g f", f=512), in_=Hf[:], func=AF.Copy),
                     reads=[Hf_r], writes=[ho_r])
                S.dma("sp", HB[t], ho[:], reads=[ho_r], writes=[HB_r], key=ho_r)
                state_update(t, xs, xs_r, bm, bm_r, 32)
            S.barrier()
            S.op("pool", lambda e: e.memset(Hf[:], 0.0), writes=[Hf_r])
            S.op("pool", lambda e: e.memset(Hb16[:], 0.0), writes=[Hb16_r])
            wo_t, wo_r = self.load_w(pA, w_out[0], 16, D, "wout")
            for kc in range(16):
                S.op("dve", lambda e, kc=kc: e.tensor_scalar(out=wo_t[:, kc, :], in0=wo_t[:, kc, :], scalar1=sv[:, 144 + kc:145 + kc], scalar2=None,
                                                            op0=ALU.mult), reads=[wo_r, sv_r], writes=[wo_r])
            btr = Ring([M.sb(pA, [128, 4, 128], BF16, "btc") for _ in range(2)])
            ctr = Ring([M.sb(pA, [128, 4, 128], BF16, "ctc") for _ in range(2)])
            zsr = Ring([M.sb(pA, [128, 2048], BF16, "zsc") for _ in range(2)])
            xdf, xdf_r = M.sb(pA, [128, 2048], BF16, "xdf")
            xdb, xdb_r = M.sb(pA, [128, 2048], BF16, "xdb")
            gm, gm_r = M.sb(pA, [128, 2, 128], F32, "gm")
            lhr = Ring([M.sb(pA, [128, 128], F32, "lh") for _ in range(3)])
            dhr = Ring([M.sb(pA, [128, 128], F32, "dh") for _ in range(3)])
            mtr = Ring([M.sb(pA, [128, 128], BF16, "mt") for _ in range(3)])
            yg, yg_r = M.sb(pA, [128, 512], F32, "yg")
            tq = Ring([M.sb(pA, [128, 512], F32, "tq") for _ in range(2)])
            ybf, ybf_r = M.sb(pA, [128, 2048], BF16, "ybf")
            ynT, ynT_r = M.sb(pA, [128, 16, 128], BF16, "ynT")
            ssq, ssq_r = M.sb(pA, [128, 8], F32, "ssq")
            psg = Ring([M.ps(pA, [128, 128], F32, "psg") for _ in range(2)])
            pd, pd_r = M.ps(pA, [128, 512], F32, "pd")
            pf, pf_r = M.ps(pA, [128, 2, 512], F32, "pf")
            ptT, ptT_r = M.ps(pA, [128, 8, 128], BF16, "ptT")
            pout, pout_r = M.ps(pA, [128, 128], F32, "pout")
            for t in range(NT):
                xs, xs_r = xsr.next()
                bm, bm_r = bmr.next()
                bt, bt_r = btr.next()
                ct, ct_r = ctr.next()
                zs, zs_r = zsr.next()
                hb, hb_r = hbo.next()
                S.dma("sp", xs[:], XS[t], reads=[XS_r], writes=[xs_r], key=xs_r)
                S.dma("sp", bm[:], BMd[t], reads=[BMd_r], writes=[bm_r], key=bm_r)
                S.dma("sp", bt[:], BTd[t], reads=[BTd_r], writes=[bt_r], key=bt_r)
                S.dma("sp", ct[:], CTd[t], reads=[CTd_r], writes=[ct_r], key=ct_r)
                S.dma("sp", zs[:], ZS[t], reads=[ZS_r], writes=[zs_r], key=zs_r)
                S.dma("sp", hb[:], HB[t], reads=[HB_r], writes=[hb_r], key=hb_r)
                for g in range(4):
                    S.op("dve", lambda e, g=g, xs=xs, t=t: e.tensor_tensor(out=v3(xdf[:, g * 512:(g + 1) * 512]), in0=v3(xs[:, g * 512:(g + 1) * 512]),
                                                                        in1=bc(dt[:, t, g * 8:(g + 1) * 8]), op=ALU.mult),
                         reads=[xs_r, dt_r], writes=[xdf_r])
                    S.op("pool", lambda e, g=g, xs=xs, t=t: e.tensor_tensor(out=v3(xdb[:, g * 512:(g + 1) * 512]), in0=v3(xs[:, g * 512:(g + 1) * 512]),
                                                                         in1=bc(dt[:, t, 32 + g * 8:32 + (g + 1) * 8]), op=ALU.mult),
                         reads=[xs_r, dt_r], writes=[xdb_r])
                S.op("pool", lambda e: e.memset(ssq[:], 0.0), reads=[ssq_r], writes=[ssq_r])
                for g in range(4):
                    S.op("pe", lambda e, g=g, bt=bt, ct=ct: e.matmul(pst[:, 0:128], lhsT=bt[:, g, :], rhs=ct[:, g, :], start=True, stop=True),
                         reads=[bt_r, ct_r], writes=[pst_r], pe_acc=True)
                    S.op("dve", lambda e: e.tensor_tensor(out=gm[:, 0, :], in0=pst[:, 0:128], in1=triF, op=ALU.mult), reads=[pst_r, msk_r], writes=[gm_r])
                    S.op("dve", lambda e: e.tensor_tensor(out=gm[:, 1, :], in0=pst[:, 0:128], in1=triB, op=ALU.mult), reads=[pst_r, msk_r], writes=[gm_r])
                    for r in range(8):
                        h = g * 8 + r
                        for d_, (mlt, tri, xd, xd_r) in enumerate(((mltF, triF, xdf, xdf_r), (mltB, triB, xdb, xdb_r))):
                            lh, lh_r = lhr.next()
                            S.op("act", lambda e, lh=lh, mlt=mlt, t=t, h=h, d_=d_: e.activation(
                                out=lh[:], in_=mlt, func=AF.Copy, scale=loga[:, t, d_ * 32 + h:d_ * 32 + h + 1]),
                                reads=[msk_r, loga_r], writes=[lh_r])
                            sg_, sg_r = psg.next()
                            S.op("pe", lambda e, sg_=sg_, lh=lh, tri=tri: e.matmul(sg_[:], lhsT=lh[:], rhs=tri, start=True, stop=True),
                                 reads=[lh_r, msk_r], writes=[sg_r], pe_acc=True)
                            dh, dh_r = dhr.next()
                            S.op("act", lambda e, dh=dh, sg_=sg_: e.activation(out=dh[:], in_=sg_[:], func=AF.Exp), reads=[sg_r], writes=[dh_r])
                            mt, mt_r = mtr.next()
                            S.op("dve", lambda e, mt=mt, dh=dh, d_=d_: e.tensor_tensor(out=mt[:], in0=gm[:, d_, :], in1=dh[:], op=ALU.mult),
                                 reads=[gm_r, dh_r], writes=[mt_r])
                            S.op("pe", lambda e, mt=mt, xd=xd, r=r, h=h, d_=d_: e.matmul(
                                pd[:, r * 64:(r + 1) * 64], lhsT=mt[:], rhs=xd[:, h * 64:(h + 1) * 64], start=(d_ == 0), stop=(d_ == 1)),
                                reads=[mt_r, xd_r], writes=[pd_r], pe_acc=True)
                    S.op("pe", lambda e, g=g, ct=ct: e.matmul(pf[:, 0, :], lhsT=ct[:, g, :], rhs=Hb16[:, g, :], start=True, stop=True),
                         reads=[ct_r, Hb16_r], writes=[pf_r], pe_acc=True)
                    S.op("pe", lambda e, g=g, ct=ct, hb=hb: e.matmul(pf[:, 1, :], lhsT=ct[:, g, :], rhs=hb[:, g * 512:(g + 1) * 512], start=True, stop=True),
                         reads=[ct_r, hb_r], writes=[pf_r], pe_acc=True)
                    S.op("act", lambda e: e.activation(out=yg[:], in_=pd[:], func=AF.Copy), reads=[pd_r], writes=[yg_r])
                    for d_ in range(2):
                        q_, q_r = tq.next()
                        S.op("dve", lambda e, q_=q_, d_=d_, t=t, g=g: e.tensor_tensor(out=v3(q_[:]), in0=v3(pf[:, d_, :]),
                                                                                  in1=bc(expA[:, t, d_ * 32 + g * 8:d_ * 32 + (g + 1) * 8]), op=ALU.mult),
                             reads=[pf_r, expA_r], writes=[q_r])
                        S.op("pool", lambda e, q_=q_: e.tensor_tensor(out=yg[:], in0=yg[:], in1=q_[:], op=ALU.add), reads=[yg_r, q_r], writes=[yg_r])
                    q_, q_r = tq.next()
                    S.op("dve", lambda e, q_=q_, g=g, xs=xs: e.tensor_tensor(out=v3(q_[:]), in0=v3(xs[:, g * 512:(g + 1) * 512]),
                                                                          in1=bc(dsk[:, g * 8:(g + 1) * 8]), op=ALU.mult),
                         reads=[xs_r, dsk_r], writes=[q_r])
                    S.op("pool", lambda e, q_=q_: e.tensor_tensor(out=yg[:], in0=yg[:], in1=q_[:], op=ALU.add), reads=[yg_r, q_r], writes=[yg_r])
                    S.op("dve", lambda e, g=g, zs=zs: e.tensor_tensor(out=yg[:], in0=yg[:], in1=zs[:, g * 512:(g + 1) * 512], op=ALU.mult),
                         reads=[yg_r, zs_r], writes=[yg_r])
                    jk, jk_r = tq.next()
                    S.op("act", lambda e, g=g, jk=jk: e.activation(out=jk[:], in_=yg[:], func=AF.Square, accum_out=ssq[:, g:g + 1]),
                         reads=[yg_r, ssq_r], writes=[jk_r, ssq_r])
                    S.op("pool", lambda e, g=g: e.tensor_copy(out=ybf[:, g * 512:(g + 1) * 512], in_=yg[:]), reads=[yg_r], writes=[ybf_r])
                S.op("dve", lambda e: e.reduce_sum(out=ssq[:, 4:5], in_=ssq[:, 0:4], axis=AX.X), reads=[ssq_r], writes=[ssq_r])
                S.op("act", lambda e: e.activation(out=ssq[:, 5:6], in_=ssq[:, 4:5], func=AF.Sqrt, scale=1.0 / 2048, bias=self.epsb[:, 0:1]),
                     reads=[ssq_r, self.epsb_r], writes=[ssq_r])
                S.op("dve", lambda e: e.reciprocal(out=ssq[:, 6:7], in_=ssq[:, 5:6]), reads=[ssq_r], writes=[ssq_r])
                S.op("dve", lambda e: e.tensor_scalar(out=ybf[:], in0=ybf[:], scalar1=ssq[:, 6:7], scalar2=None, op0=ALU.mult),
                     reads=[ybf_r, ssq_r], writes=[ybf_r])
                for b2 in range(2):
                    for jj in range(8):
                        kc = b2 * 8 + jj
                        S.op("pe", lambda e, jj=jj, kc=kc: e.transpose(out=ptT[:, jj, :], in_=ybf[:, kc * 128:(kc + 1) * 128], identity=self.identb[:]),
                             reads=[ybf_r, self.identb_r], writes=[ptT_r], pe_acc=True)
                    S.op("act", lambda e, b2=b2: e.activation(out=ynT[:, b2 * 8:(b2 + 1) * 8, :], in_=ptT[:], func=AF.Copy), reads=[ptT_r], writes=[ynT_r])
                for oc in range(8):
                    for kc in range(16):
                        S.op("pe", lambda e, oc=oc, kc=kc: e.matmul(pout[:], lhsT=wo_t[:, kc, oc * 128:(oc + 1) * 128], rhs=ynT[:, kc, :],
                                                                    start=(kc == 0), stop=(kc == 15)),
                             reads=[wo_r, ynT_r], writes=[pout_r], pe_acc=True)
                    self.resid(pout, pout_r, 128, oc, t * 128)
                state_update(t, xs, xs_r, bm, bm_r, 0)
                S.op("act", lambda e: e.activation(out=Hb16[:], in_=Hf[:], func=AF.Copy), reads=[Hf_r], writes=[Hb16_r])
            S.barrier()

    def out_raw(self):
        nc, S, M = self.nc, self.S, self.M
        y = nc.dram_tensor("y", [T, D], F32, kind="ExternalOutput").ap()
        y_r = Res("y")
        with ExitStack() as ps:
            pst = Ring([M.ps(ps, [128, 4, 128], F32, "otp") for _ in range(2)])
            stg = Ring([M.sb(ps, [128, D], F32, "ostg") for _ in range(2)])
            for t in range(NT):
                st, st_r = stg.next()
                for h in range(2):
                    pt, pt_r = pst.next()
                    for k in range(4):
                        S.op("pe", lambda e, pt=pt, k=k, h=h, t=t: e.transpose(
                            out=pt[:, k, :], in_=self.xT[:, h * 4 + k, t * 128:(t + 1) * 128], identity=self.identf[:]),
                            reads=[self.xTr, self.identf_r], writes=[pt_r], pe_acc=True)
                    S.op("dve", lambda e, pt=pt, st=st, h=h: e.tensor_copy(out=st[:, h * 512:(h + 1) * 512], in_=pt[:]),
                         reads=[pt_r], writes=[st_r])
                S.dma("sp", y[t * 128:(t + 1) * 128, :], st[:], reads=[st_r], writes=[y_r], key=st_r)
            S.barrier()

    def out_final(self):
        nc, S, M = self.nc, self.S, self.M
        y = nc.dram_tensor("y", [NLAT, D], F32, kind="ExternalOutput").ap()
        gfin = self.inp("g_final", [D])
        y_r = Res("y")
        with ExitStack() as ps:
            gb, gb_r = M.sb(ps, [128, D], F32, "gfin")
            S.dma("sp", gb[:], gfin.partition_broadcast(128), writes=[gb_r], key=gb_r)
            pst = Ring([M.ps(ps, [128, 4, 128], F32, "otp") for _ in range(2)])
            stg = Ring([M.sb(ps, [128, D], F32, "ostg") for _ in range(2)])
            jk = Ring([M.sb(ps, [128, D], F32, "ojk") for _ in range(2)])
            ssq = Ring([M.sb(ps, [128, 1], F32, "ossq") for _ in range(2)])
            for t in range(NCTX // 128, NT):
                st, st_r = stg.next()
                for h in range(2):
                    pt, pt_r = pst.next()
                    for k in range(4):
                        S.op("pe", lambda e, pt=pt, k=k, h=h, t=t: e.transpose(
                            out=pt[:, k, :], in_=self.xT[:, h * 4 + k, t * 128:(t + 1) * 128], identity=self.identf[:]),
                            reads=[self.xTr, self.identf_r], writes=[pt_r], pe_acc=True)
                    S.op("dve", lambda e, pt=pt, st=st, h=h: e.tensor_copy(out=st[:, h * 512:(h + 1) * 512], in_=pt[:]),
                         reads=[pt_r], writes=[st_r])
                j_, j_r = jk.next()
                q, q_r = ssq.next()
                S.op("act", lambda e, j_=j_, st=st, q=q: e.activation(out=j_[:], in_=st[:], func=AF.Square, accum_out=q[:]),
                     reads=[st_r], writes=[j_r, q_r])
                S.op("act", lambda e, q=q: e.activation(out=q[:], in_=q[:], func=AF.Sqrt, scale=1.0 / D, bias=self.epsb[:, 0:1]),
                     reads=[q_r, self.epsb_r], writes=[q_r])
                S.op("dve", lambda e, q=q: e.reciprocal(out=q[:], in_=q[:]), reads=[q_r], writes=[q_r])
                S.op("dve", lambda e, j_=j_, st=st, q=q: e.scalar_tensor_tensor(out=j_[:], in0=st[:], scalar=q[:, 0:1], in1=gb[:],
                                                                              op0=ALU.mult, op1=ALU.mult),
                     reads=[st_r, q_r, gb_r, j_r], writes=[j_r])
                r0 = (t - NCTX // 128) * 128
                S.dma("sp", y[r0:r0 + 128, :], j_[:], reads=[j_r], writes=[y_r], key=j_r)
            S.barrier()


FULL_STEPS = []
for _i in range(4):
    FULL_STEPS += [("mods", _i), ("mixer", _i), ("ffn", _i, _i < 3)]


def make_in_maps(inputs, ncores=8, xs=None, cs=None):
    f32 = np.float32
    shared = {}
    lvec = np.zeros((4, 128, 64), f32)
    for i in range(4):
        lvec[i, :, 0:48] = fm(inputs["ada_b"][i])
        lvec[i, :, 48:56] = fm(inputs["g_mix"][i])
        lvec[i, :, 56:64] = fm(inputs["g_ffn"][i])
    shared["lvec"] = lvec
    shared["ada_w"] = np.ascontiguousarray(inputs["ada_w"], f32)
    shared["g_final"] = np.ascontiguousarray(inputs["g_final"], f32)
    shared["moe_w_router"] = np.ascontiguousarray(inputs["moe_w_router"], f32)
    shared["moe_b_router"] = np.ascontiguousarray(inputs["moe_b_router"], f32)
    wgu = np.ascontiguousarray(inputs["moe_w_gu"], f32)
    wdn = np.ascontiguousarray(inputs["moe_w_down"], f32)
    shared["moe_w_gu"] = wgu
    shared["moe_w_down"] = wdn
    shared["moe_w_gu2d"] = wgu.reshape(-1, 2 * D)
    shared["moe_w_down2d"] = wdn.reshape(-1, D)
    shared["moe_b_down2d"] = np.ascontiguousarray(inputs["moe_b_down"], f32).reshape(-1, D)
    shared["moe_b_down"] = np.ascontiguousarray(inputs["moe_b_down"], f32)
    bgu = np.asarray(inputs["moe_b_gu"], f32)
    shared["moe_b_gu_fm"] = np.ascontiguousarray(bgu.reshape(4, NE, 16, 128).transpose(0, 3, 1, 2))
    shared["moe_b_gu_rows"] = np.ascontiguousarray(bgu.reshape(4, NE, 16, 128).transpose(0, 1, 3, 2)).reshape(4 * NE * 128, 16)
    if "conv_w_pw1" in inputs:
        shared["conv_w_pw1"] = np.ascontiguousarray(inputs["conv_w_pw1"], f32)
        shared["conv_w_pw2"] = np.ascontiguousarray(inputs["conv_w_pw2"], f32)
        cvec = np.zeros((128, 296), f32)
        cvec[:, 0:16] = fm(inputs["conv_b_pw1"][0])
        wdw = np.asarray(inputs["conv_w_dw"][0], f32)
        cvec[:, 16:264] = wdw.reshape(31, 8, 128).transpose(2, 1, 0).reshape(128, 248)
        cvec[:, 264:272] = fm(inputs["conv_b_dw"][0])
        cvec[:, 272:280] = fm(inputs["conv_ln_g"][0])
        cvec[:, 280:288] = fm(inputs["conv_ln_b"][0])
        cvec[:, 288:296] = fm(inputs["conv_b_pw2"][0])
        shared["conv_vec"] = cvec
    if "swa_w_qkv" in inputs:
        shared["swa_w_qkv"] = np.ascontiguousarray(inputs["swa_w_qkv"], f32)
        shared["swa_b_qkv"] = np.ascontiguousarray(inputs["swa_b_qkv"], f32)
        shared["swa_w_o"] = np.ascontiguousarray(inputs["swa_w_o"], f32)
        shared["swa_sinks"] = np.ascontiguousarray(inputs["swa_sinks"], f32)
        bq = np.asarray(inputs["swa_b_qkv"][0], f32).reshape(24, 64).T
        bh = np.zeros((64, 44), f32)
        bh[:, 0:24] = bq
        bh[:, 24:44] = np.roll(bq[:, 0:20], 32, axis=0)
        shared["swa_bh"] = bh
        shared["swa_bo_fm"] = fm(inputs["swa_b_o"][0])
        Ct, St = rope_tables()
        shared["rope_c"] = Ct
        shared["rope_s"] = St
    if "diff_w_qkv" in inputs:
        shared["diff_w_qkv"] = np.ascontiguousarray(inputs["diff_w_qkv"], f32)
        shared["diff_w_o"] = np.ascontiguousarray(inputs["diff_w_o"], f32)
        shared["diff_lam"] = np.ascontiguousarray(np.stack([inputs["diff_lambda_q1"][0], inputs["diff_lambda_k1"][0],
                                                            inputs["diff_lambda_q2"][0], inputs["diff_lambda_k2"][0]], 0), f32)
        shared["diff_subln_g"] = np.ascontiguousarray(np.asarray(inputs["diff_subln_g"][0], f32).reshape(128, 1))
        if "rope_c" not in shared:
            Ct, St = rope_tables()
            shared["rope_c"] = Ct
            shared["rope_s"] = St
    if "ssm_w_in" in inputs:
        shared["ssm_w_in"] = np.ascontiguousarray(inputs["ssm_w_in"], f32)
        shared["ssm_w_out"] = np.ascontiguousarray(inputs["ssm_w_out"], f32)
        svec = np.zeros((128, 160), f32)
        wc = np.asarray(inputs["ssm_w_conv"][0], f32)
        svec[:, 0:120] = wc.reshape(5, 24, 128).transpose(2, 1, 0).reshape(128, 120)
        svec[:, 120:144] = fm(inputs["ssm_b_conv"][0])
        svec[:, 144:160] = fm(inputs["ssm_norm_g"][0])
        shared["ssm_vec"] = svec
        shared["ssm_a_log"] = np.ascontiguousarray(np.asarray(inputs["ssm_a_log"], f32).reshape(1, 64))
        shared["ssm_dt_bias"] = np.ascontiguousarray(np.asarray(inputs["ssm_dt_bias"], f32).reshape(1, 64))
        shared["ssm_d"] = np.ascontiguousarray(np.asarray(inputs["ssm_d"], f32).reshape(1, 32))
    maps = []
    for b in range(ncores):
        m = dict(shared)
        m["x_in"] = np.ascontiguousarray(np.concatenate([inputs["ctx"][b], inputs["x"][b]], axis=0), f32)
        cond = np.stack([fm(inputs["c"][b]), fm(inputs["c_ctx"])], axis=-1)
        m["cond"] = np.ascontiguousarray(cond, f32)
        maps.append(m)
    return maps


def kernel(**inputs):
    prog = Prog(FULL_STEPS)
    nc = prog.build()
    maps = make_in_maps(inputs)
    maps = [{k: v for k, v in m.items() if k in prog.din} for m in maps]
    res = run_bass_kernel_spmd(nc, maps, core_ids=list(range(8)))
    return np.stack([r["y"] for r in res.results], axis=0).astype(np.float32)
```

```python
import math
import os
from contextlib import ExitStack

import numpy as np
import concourse.bass as bass
import concourse.mybir as mybir
from concourse.bass_utils import run_bass_kernel_spmd

F32 = mybir.dt.float32
BF16 = mybir.dt.bfloat16
I32 = mybir.dt.int32
AF = mybir.ActivationFunctionType
ALU = mybir.AluOpType
AX = mybir.AxisListType

D = 1024
NCTX = 256
NLAT = 2048
T = NCTX + NLAT
NT = T // 128
BLKS = [(0, 256), (256, 512), (768, 512), (1280, 512), (1792, 512)]
EPS = 1e-6
NE = 32
KMOE_NE = int(os.environ.get("KMOE_NE", NE))


class Res:
    __slots__ = ("name", "w", "r", "dsem", "dcnt")

    def __init__(self, name):
        self.name = name
        self.w = None
        self.r = []
        self.dsem = None
        self.dcnt = 0


class Sched:
    CE = ("pe", "act", "dve", "pool")
    ALLE = ("pe", "act", "dve", "pool", "sp")

    def __init__(self, nc, es):
        self.nc = nc
        self.es = es
        self.ops = {e: [] for e in self.ALLE}
        self.sems = {}
        for e in self.CE:
            self.sems["c_" + e] = es.enter_context(nc.semaphore("c_" + e))
        self.cnt = {e: 0 for e in self.CE}
        self.waited = {e: {} for e in self.ALLE}
        self.dtot = {}
        self.free_dsems = []
        self.ndsem = 0

    def _dsem(self, res):
        if res.dsem is None:
            if self.free_dsems:
                k = self.free_dsems.pop()
            else:
                k = "d%d" % self.ndsem
                self.ndsem += 1
                self.sems[k] = self.es.enter_context(self.nc.semaphore(k))
                self.dtot[k] = 0
            res.dsem = k
        return res.dsem

    def release(self, ress):
        for r in ress:
            if r.dsem is not None:
                self.free_dsems.append(r.dsem)
                r.dsem = None

    def _collect(self, e, reads, writes, pe_acc=False, skip_key=None):
        deps = {}

        def add(ev):
            if ev is None:
                return
            k, v = ev
            if deps.get(k, 0) < v:
                deps[k] = v
        for r in reads:
            add(r.w)
        for w in writes:
            if not (pe_acc and w.w is not None and w.w[0] == "c_pe") and not (
                    skip_key is not None and w.w is not None and w.w[0] == skip_key):
                add(w.w)
            for ev in w.r:
                add(ev)
        out = []
        wd = self.waited[e]
        for k, v in deps.items():
            if wd.get(k, 0) >= v:
                continue
            wd[k] = v
            out.append((k, v))
        return out

    def op(self, e, fn, reads=(), writes=(), pe_acc=False):
        waits = self._collect(e, reads, writes, pe_acc)
        self.cnt[e] += 1
        ev = ("c_" + e, self.cnt[e])
        for r in reads:
            r.r.append(ev)
        for w in writes:
            w.w = ev
            w.r = []
        self.ops[e].append((waits, fn, ev[0], 1))
        return ev

    def dma(self, q, out, in_, reads=(), writes=(), key=None, **kw):
        k = self._dsem(key)
        waits = self._collect(q, reads, writes, skip_key=k)
        self.dtot[k] += 16
        ev = (k, self.dtot[k])
        for r in reads:
            r.r.append(ev)
        for w in writes:
            w.w = ev
            w.r = []
        self.ops[q].append((waits, lambda eng: eng.dma_start(out=out, in_=in_, **kw), k, 16))
        return ev

    def idma(self, out, out_off, in_, in_off, bound, reads=(), writes=(), key=None):
        k = self._dsem(key)
        waits = self._collect("pool", reads, writes, skip_key=k)
        self.dtot[k] += 16
        ev = (k, self.dtot[k])
        for r in reads:
            r.r.append(ev)
        for w in writes:
            w.w = ev
            w.r = []
        oo = None if out_off is None else bass.IndirectOffsetOnAxis(ap=out_off, axis=0)
        io = None if in_off is None else bass.IndirectOffsetOnAxis(ap=in_off, axis=0)
        self.ops["pool"].append((waits, lambda eng: eng.indirect_dma_start(
            out=out, out_offset=oo, in_=in_, in_offset=io), k, 16))
        return ev

    def barrier(self):
        allev = [("c_" + e, self.cnt[e]) for e in self.CE if self.cnt[e] > 0]
        allev += [(k, v) for k, v in self.dtot.items() if v > 0]
        for e in self.ALLE:
            wd = self.waited[e]
            waits = []
            for k, v in allev:
                if wd.get(k, 0) < v:
                    wd[k] = v
                    waits.append((k, v))
            if waits:
                self.ops[e].append((waits, None, None, 0))

    def replay(self):
        nc = self.nc
        sems = self.sems
        ops = self.ops

        def run(eng, lst):
            for waits, fn, sk, n in lst:
                for k, v in waits:
                    eng.wait_ge(sems[k], v)
                if fn is not None:
                    fn(eng).then_inc(sems[sk], n)
        with nc.Block() as block:
            @block.sync
            def _(e):
                run(e, ops["sp"])

            @block.tensor
            def _(e):
                run(e, ops["pe"])

            @block.scalar
            def _(e):
                run(e, ops["act"])

            @block.vector
            def _(e):
                run(e, ops["dve"])

            @block.gpsimd
            def _(e):
                run(e, ops["pool"])


class Mem:
    def __init__(self, nc):
        self.nc = nc
        self.n = 0

    def sb(self, es, shape, dt, name=None):
        self.n += 1
        name = (name or "sb") + "_%d" % self.n
        t = es.enter_context(self.nc.sbuf_tensor(name, list(shape), dt))
        return t, Res(name)

    def ps(self, es, shape, dt, name=None):
        self.n += 1
        name = (name or "ps") + "_%d" % self.n
        t = es.enter_context(self.nc.psum_tensor(name, list(shape), dt))
        return t, Res(name)


class Ring:
    def __init__(self, items):
        self.items = items
        self.i = 0

    def next(self):
        it = self.items[self.i % len(self.items)]
        self.i += 1
        return it


def fm(v):
    v = np.asarray(v, np.float32)
    return np.ascontiguousarray(v.reshape(-1, 128).T)


def rope_tables():
    t = np.arange(NLAT)
    row = (t // 64).astype(np.float32)
    col = (t % 64).astype(np.float32)
    quarter = 16
    inv = (10000.0 ** (-np.arange(quarter, dtype=np.float32) / quarter)).astype(np.float32)
    ang = np.concatenate([row[:, None] * inv, col[:, None] * inv], axis=-1).astype(np.float32)
    cos = np.cos(ang).T.astype(np.float32)
    sin = np.sin(ang).T.astype(np.float32)
    C = np.ones((64, T), np.float32)
    S = np.zeros((64, T), np.float32)
    C[0:32, NCTX:] = cos
    C[32:64, NCTX:] = cos
    S[0:32, NCTX:] = -sin
    S[32:64, NCTX:] = sin
    return C, S


class Prog:
    def __init__(self, steps, raw_out=False):
        self.steps = steps
        self.raw_out = raw_out
        self.dbg = int(os.environ.get("KDBG", "0"))
        self.nc = bass.Bass("TRN2", target_bir_lowering=False)
        self.din = {}

    def inp(self, name, shape, dt=F32):
        if name not in self.din:
            self.din[name] = self.nc.dram_tensor(name, list(shape), dt, kind="ExternalInput").ap()
        return self.din[name]

    def build(self):
        nc = self.nc
        with ExitStack() as es:
            self.S = S = Sched(nc, es)
            self.M = M = Mem(nc)
            self.es = es
            self.xT, self.xTr = M.sb(es, [128, 8, T], F32, "xT")
            self.identf, self.identf_r = M.sb(es, [128, 128], F32, "identf")
            self.identb, self.identb_r = M.sb(es, [128, 128], BF16, "identb")
            self.onesf, self.onesf_r = M.sb(es, [128, 128], F32, "onesf")
            self.condT, self.condT_r = M.sb(es, [128, 8, 2], F32, "condT")
            self.mv, self.mv_r = M.sb(es, [128, 2, 6, 8], F32, "mv")
            self.epsb, self.epsb_r = M.sb(es, [128, 1], F32, "epsb")
            self.onesb, self.onesb_r = M.sb(es, [128, 128], BF16, "onesb")
            self.setup()
            for st in self.steps:
                kind = st[0]
                if kind == "mods":
                    self.mods(st[1])
                elif kind == "ffn":
                    self.ffn(st[1], with_ctx=st[2])
                elif kind == "mixer":
                    getattr(self, "mixer%d" % st[1])(st[1])
                S.barrier()
            if self.raw_out:
                self.out_raw()
            else:
                self.out_final()
            S.barrier()
            S.replay()
        return nc

    def setup(self):
        nc, S, M = self.nc, self.S, self.M
        x_in = self.inp("x_in", [T, D])
        cond = self.inp("cond", [128, 8, 2])
        identf, ifr = self.identf, self.identf_r
        S.op("pool", lambda e: e.memset(identf[:], 1.0), writes=[ifr])
        S.op("pool", lambda e: e.affine_select(out=identf[:], in_=identf[:], pattern=[[-1, 128]],
                                               compare_op=ALU.is_equal, fill=0.0, base=0, channel_multiplier=1),
             reads=[ifr], writes=[ifr])
        S.op("dve", lambda e: e.tensor_copy(out=self.identb[:], in_=identf[:]), reads=[ifr], writes=[self.identb_r])
        S.op("pool", lambda e: e.memset(self.onesf[:], 1.0), writes=[self.onesf_r])
        S.op("pool", lambda e: e.memset(self.epsb[:], EPS), writes=[self.epsb_r])
        S.op("pool", lambda e: e.memset(self.onesb[:], 1.0), writes=[self.onesb_r])
        with ExitStack() as ps:
            craw, craw_r = M.sb(ps, [128, 8, 2], F32, "craw")
            S.dma("sp", craw[:], cond, writes=[craw_r], key=craw_r)
            S.op("act", lambda e: e.activation(out=self.condT[:], in_=craw[:], func=AF.Silu),
                 reads=[craw_r], writes=[self.condT_r])
            stg = Ring([M.sb(ps, [128, D], F32, "xstg") for _ in range(2)])
            pst = Ring([M.ps(ps, [128, 4, 128], F32, "xtp") for _ in range(2)])
            for t in range(NT):
                st, st_r = stg.next()
                S.dma("sp", st[:], x_in[t * 128:(t + 1) * 128, :], writes=[st_r], key=st_r)
                for h in range(2):
                    pt, pt_r = pst.next()
                    for k in range(4):
                        S.op("pe", lambda e, pt=pt, st=st, k=k, h=h: e.transpose(
                            out=pt[:, k, :], in_=st[:, (h * 4 + k) * 128:(h * 4 + k + 1) * 128], identity=identf[:]),
                            reads=[st_r, ifr], writes=[pt_r], pe_acc=True)
                    eng = "dve" if h == 0 else "act"
                    if eng == "dve":
                        S.op("dve", lambda e, pt=pt, h=h, t=t: e.tensor_copy(
                            out=self.xT[:, h * 4:(h + 1) * 4, t * 128:(t + 1) * 128], in_=pt[:]),
                            reads=[pt_r], writes=[self.xTr])
                    else:
                        S.op("act", lambda e, pt=pt, h=h, t=t: e.activation(
                            out=self.xT[:, h * 4:(h + 1) * 4, t * 128:(t + 1) * 128], in_=pt[:], func=AF.Copy),
                            reads=[pt_r], writes=[self.xTr])
            S.barrier()
            S.release([craw_r] + [r for _, r in stg.items])

    def mods(self, i):
        nc, S, M = self.nc, self.S, self.M
        ada_w = self.inp("ada_w", [4, D, 6 * D])
        lv = self.inp("lvec", [4, 128, 64])
        with ExitStack() as ps:
            lvt, lvt_r = M.sb(ps, [128, 64], F32, "lvt")
            S.dma("sp", lvt[:], lv[i], writes=[lvt_r], key=lvt_r)
            wring = Ring([M.sb(ps, [128, 8, 512], F32, "adaw") for _ in range(2)])
            pm, pm_r = M.ps(ps, [128, 48, 2], F32, "pm")
            md, md_r = M.sb(ps, [128, 2, 48], F32, "md")
            for pi in range(12):
                wt, wt_r = wring.next()
                S.dma("sp", wt[:], ada_w[i, :, pi * 512:(pi + 1) * 512].rearrange("(k p) n -> p k n", p=128),
                      writes=[wt_r], key=wt_r)
                for o4 in range(4):
                    ob = pi * 4 + o4
                    for k in range(8):
                        S.op("pe", lambda e, wt=wt, k=k, o4=o4, ob=ob: e.matmul(
                            pm[:, ob, :], lhsT=wt[:, k, o4 * 128:(o4 + 1) * 128], rhs=self.condT[:, k, :],
                            start=(k == 0), stop=(k == 7)),
                            reads=[wt_r, self.condT_r], writes=[pm_r], pe_acc=True)
            for c in range(2):
                S.op("dve", lambda e, c=c: e.tensor_tensor(out=md[:, c, :], in0=pm[:, :, c], in1=lvt[:, 0:48], op=ALU.add),
                     reads=[pm_r, lvt_r], writes=[md_r])
            mv, mv_r = self.mv, self.mv_r
            for c in range(2):
                S.op("dve", lambda e, c=c: e.scalar_tensor_tensor(out=mv[:, c, 0, :], in0=md[:, c, 8:16], scalar=1.0,
                                                                  in1=lvt[:, 48:56], op0=ALU.add, op1=ALU.mult),
                     reads=[md_r, lvt_r], writes=[mv_r])
                S.op("dve", lambda e, c=c: e.tensor_copy(out=mv[:, c, 1, :], in_=md[:, c, 0:8]), reads=[md_r], writes=[mv_r])
                S.op("dve", lambda e, c=c: e.tensor_copy(out=mv[:, c, 2, :], in_=md[:, c, 16:24]), reads=[md_r], writes=[mv_r])
                S.op("dve", lambda e, c=c: e.scalar_tensor_tensor(out=mv[:, c, 3, :], in0=md[:, c, 32:40], scalar=1.0,
                                                                  in1=lvt[:, 56:64], op0=ALU.add, op1=ALU.mult),
                     reads=[md_r, lvt_r], writes=[mv_r])
                S.op("dve", lambda e, c=c: e.tensor_copy(out=mv[:, c, 4, :], in_=md[:, c, 24:32]), reads=[md_r], writes=[mv_r])
                S.op("dve", lambda e, c=c: e.tensor_copy(out=mv[:, c, 5, :], in_=md[:, c, 40:48]), reads=[md_r], writes=[mv_r])
            S.barrier()
            S.release([lvt_r] + [r for _, r in wring.items])

    def adanorm(self, ps, hT, hT_r, slotA, with_ctx=True, router=None):
        nc, S, M = self.nc, self.S, self.M
        xT, xTr = self.xT, self.xTr
        sq = Ring([M.sb(ps, [128, 512], F32, "nsq") for _ in range(2)])
        if router is not None:
            h32 = Ring([M.sb(ps, [128, 8, 512], F32, "nh32") for _ in range(1)])
        pss = Ring([M.ps(ps, [128, 512], F32, "nss") for _ in range(2)])
        rst = Ring([M.sb(ps, [128, 512], F32, "nrstd") for _ in range(2)])
        tmp = Ring([M.sb(ps, [128, 512], F32, "ntmp") for _ in range(2)])
        if router is not None:
            plg = Ring([M.ps(ps, [128, 32], F32, "rlg") for _ in range(2)])
            pgt = Ring([M.ps(ps, [32, 128], F32, "rgt") for _ in range(2)])
            rt = Ring([[M.sb(ps, [128, 32], F32, "rt%d" % j) for j in range(4)] for _ in range(2)])
            rs = Ring([[M.sb(ps, [128, 8], F32, "rs%d" % j) for j in range(4)] for _ in range(2)])
        for (s, n) in BLKS:
            if s == 0 and not with_ctx:
                continue
            c = 1 if s == 0 else 0
            A = self.mv[:, c, slotA, :]
            B = self.mv[:, c, slotA + 1, :]
            pp, pp_r = pss.next()
            for k in range(8):
                q, q_r = sq.next()
                S.op("act", lambda e, q=q, s=s, n=n, k=k: e.activation(out=q[:, :n], in_=xT[:, k, s:s + n], func=AF.Square),
                     reads=[xTr], writes=[q_r])
                S.op("pe", lambda e, pp=pp, q=q, k=k, n=n: e.matmul(pp[:, :n], lhsT=self.onesf[:], rhs=q[:, :n],
                                                                    start=(k == 0), stop=(k == 7)),
                     reads=[q_r, self.onesf_r], writes=[pp_r], pe_acc=True)
            r, r_r = rst.next()
            S.op("act", lambda e, r=r, pp=pp, n=n: e.activation(out=r[:, :n], in_=pp[:, :n], func=AF.Sqrt, scale=1.0 / D,
                                                                bias=self.epsb[:, 0:1]),
                 reads=[pp_r, self.epsb_r], writes=[r_r])
            S.op("dve", lambda e, r=r, n=n: e.reciprocal(out=r[:, :n], in_=r[:, :n]), reads=[r_r], writes=[r_r])
            if router is not None:
                hh, hh_r = h32.next()
            for k in range(8):
                t_, t_r = tmp.next()
                S.op("dve", lambda e, t_=t_, k=k, s=s, n=n, r=r: e.tensor_tensor(out=t_[:, :n], in0=xT[:, k, s:s + n], in1=r[:, :n],
                                                                                 op=ALU.mult),
                     reads=[xTr, r_r], writes=[t_r])
                if router is not None:
                    S.op("act", lambda e, t_=t_, hh=hh, k=k, n=n, A=A, B=B: e.activation(
                        out=hh[:, k, :n], in_=t_[:, :n], func=AF.Identity, scale=A[:, k:k + 1], bias=B[:, k:k + 1]),
                        reads=[t_r, self.mv_r], writes=[hh_r])
                    S.op("pool", lambda e, hh=hh, k=k, s=s, n=n: e.tensor_copy(out=hT[:, k, s:s + n], in_=hh[:, k, :n]),
                         reads=[hh_r], writes=[hT_r])
                else:
                    S.op("act", lambda e, t_=t_, k=k, s=s, n=n, A=A, B=B: e.activation(
                        out=hT[:, k, s:s + n], in_=t_[:, :n], func=AF.Identity, scale=A[:, k:k + 1], bias=B[:, k:k + 1]),
                        reads=[t_r, self.mv_r], writes=[hT_r])
            if router is not None:
                R = router
                for tt in range(n // 128):
                    lg, lg_r = plg.next()
                    for k in range(8):
                        S.op("pe", lambda e, lg=lg, hh=hh, k=k, tt=tt: e.matmul(
                            lg[:], lhsT=hh[:, k, tt * 128:(tt + 1) * 128], rhs=R["wr"][:, k, :], start=(k == 0), stop=(k == 7)),
                            reads=[hh_r, R["wr_r"]], writes=[lg_r], pe_acc=True)
                    (l, l_r), (ex, ex_r), (mk, mk_r), (gt, gt_r) = rt.next()
                    (m8, m8_r), (ng, ng_r), (sm, sm_r), (rc, rc_r) = rs.next()
                    S.op("dve", lambda e, l=l, lg=lg: e.tensor_tensor(out=l[:], in0=lg[:], in1=R["brb"][:], op=ALU.add),
                         reads=[lg_r, R["brb_r"]], writes=[l_r])
                    S.op("dve", lambda e, m8=m8, l=l: e.max(out=m8[:], in_=l[:]), reads=[l_r], writes=[m8_r])
                    S.op("dve", lambda e, ng=ng, m8=m8: e.tensor_scalar(out=ng[:, 0:1], in0=m8[:, 0:1], scalar1=-1.0, scalar2=None,
                                                                        op0=ALU.mult),
                         reads=[m8_r], writes=[ng_r])
                    S.op("act", lambda e, ex=ex, l=l, ng=ng: e.activation(out=ex[:], in_=l[:], func=AF.Exp, bias=ng[:, 0:1], scale=1.0),
                         reads=[l_r, ng_r], writes=[ex_r])
                    S.op("dve", lambda e, mk=mk, l=l, m8=m8: e.tensor_scalar(out=mk[:], in0=l[:], scalar1=m8[:, 3:4], scalar2=None,
                                                                            op0=ALU.is_ge),
                         reads=[l_r, m8_r], writes=[mk_r])
                    S.op("dve", lambda e, ex=ex, mk=mk: e.tensor_tensor(out=ex[:], in0=ex[:], in1=mk[:], op=ALU.mult),
                         reads=[ex_r, mk_r], writes=[ex_r])
                    S.op("dve", lambda e, sm=sm, ex=ex: e.reduce_sum(out=sm[:, 0:1], in_=ex[:], axis=AX.X), reads=[ex_r], writes=[sm_r])
                    S.op("dve", lambda e, rc=rc, sm=sm: e.reciprocal(out=rc[:, 0:1], in_=sm[:, 0:1]), reads=[sm_r], writes=[rc_r])
                    S.op("dve", lambda e, gt=gt, ex=ex, rc=rc: e.tensor_scalar(out=gt[:], in0=ex[:], scalar1=rc[:, 0:1], scalar2=None,
                                                                              op0=ALU.mult),
                         reads=[ex_r, rc_r], writes=[gt_r])
                    pg, pg_r = pgt.next()
                    S.op("pe", lambda e, pg=pg, gt=gt: e.transpose(out=pg[:], in_=gt[:], identity=self.identf[:]),
                         reads=[gt_r, self.identf_r], writes=[pg_r], pe_acc=True)
                    S.op("act", lambda e, pg=pg, s=s, tt=tt: e.activation(
                        out=R["gateT"][:, s + tt * 128:s + (tt + 1) * 128], in_=pg[:], func=AF.Copy),
                        reads=[pg_r], writes=[R["gateT_r"]])

    def ffn_dense(self, i, with_ctx=True):
        nc, S, M = self.nc, self.S, self.M
        xT, xTr = self.xT, self.xTr
        w_router = self.inp("moe_w_router", [4, D, NE])
        b_router = self.inp("moe_b_router", [4, NE])
        w_gu = self.inp("moe_w_gu", [4, KMOE_NE, D, 2 * D])
        w_dn = self.inp("moe_w_down", [4, KMOE_NE, D, D])
        b_gu = self.inp("moe_b_gu_fm", [4, 128, NE, 16])
        b_dn = self.inp("moe_b_down", [4, NE, D])
        blks = [b for b in BLKS if with_ctx or b[0] != 0]
        with ExitStack() as ps:
            hT, hT_r = M.sb(ps, [128, 8, T], BF16, "hT")
            gateT, gateT_r = M.sb(ps, [NE, T], F32, "gateT")
            bgu, bgu_r = M.sb(ps, [128, NE, 16], F32, "bgu")
            bdn, bdn_r = M.sb(ps, [NE, D], F32, "bdn")
            S.dma("sp", bgu[:], b_gu[i], writes=[bgu_r], key=bgu_r)
            S.op("dve", lambda e: e.tensor_scalar(out=bgu[:, :, 8:16], in0=bgu[:, :, 8:16], scalar1=1.0, scalar2=None, op0=ALU.add),
                 reads=[bgu_r], writes=[bgu_r])
            S.dma("sp", bdn[:], b_dn[i], writes=[bdn_r], key=bdn_r)
            with ExitStack() as ps2:
                wr, wr_r = M.sb(ps2, [128, 8, NE], F32, "wr")
                brb, brb_r = M.sb(ps2, [128, NE], F32, "brb")
                S.dma("sp", wr[:], w_router[i].rearrange("(k p) n -> p k n", p=128), writes=[wr_r], key=wr_r)
                S.dma("sp", brb[:], b_router[i].partition_broadcast(128), writes=[brb_r], key=brb_r)
                self.adanorm(ps2, hT, hT_r, 3, with_ctx=with_ctx,
                             router=dict(wr=wr, wr_r=wr_r, brb=brb, brb_r=brb_r, gateT=gateT, gateT_r=gateT_r))
                S.barrier()
                S.release([wr_r, brb_r])
            wg = Ring([M.sb(ps, [128, 8, 2, 512], BF16, "wg") for _ in range(2)])
            wd = Ring([M.sb(ps, [128, 4, D], BF16, "wd") for _ in range(2)])
            pgl = Ring([M.ps(ps, [128, 2, 512], F32, "pgl") for _ in range(2)])
            pout = Ring([M.ps(ps, [128, 512], F32, "pout") for _ in range(2)])
            pgb = Ring([M.ps(ps, [128, 512], F32, "pgb") for _ in range(2)])
            gB = Ring([M.sb(ps, [128, 512], F32, "gB") for _ in range(2)])
            mg = Ring([M.sb(ps, [NE, 512], F32, "mg") for _ in range(2)])
            aT = Ring([M.sb(ps, [128, 4, 512], BF16, "aT") for _ in range(2)])
            tg = Ring([M.sb(ps, [128, 512], F32, "tg") for _ in range(2)])
            tsg = Ring([M.sb(ps, [128, 512], F32, "tsg") for _ in range(2)])
            tl = Ring([M.sb(ps, [128, 512], F32, "tl") for _ in range(2)])
            for (s, n) in blks:
                c = 1 if s == 0 else 0
                for oc in range(8):
                    po, po_r = pout.next()
                    S.op("pe", lambda e, po=po, oc=oc, s=s, n=n: e.matmul(
                        po[:, :n], lhsT=bdn[:, oc * 128:(oc + 1) * 128], rhs=gateT[:, s:s + n], start=True, stop=True),
                        reads=[bdn_r, gateT_r], writes=[po_r], pe_acc=True)
                    S.op("dve", lambda e, po=po, oc=oc, s=s, n=n, c=c: e.scalar_tensor_tensor(
                        out=xT[:, oc, s:s + n], in0=po[:, :n], scalar=self.mv[:, c, 5, oc:oc + 1], in1=xT[:, oc, s:s + n],
                        op0=ALU.mult, op1=ALU.add),
                        reads=[po_r, self.mv_r, xTr], writes=[xTr])
            pieces = [(ex, half) for ex in range(KMOE_NE) for half in range(2)]
            loaded = {}

            def load_piece(idx):
                ex, half = pieces[idx]
                wgt, wg_r = wg.next()
                wdt, wd_r = wd.next()
                for gl in range(2):
                    S.dma("pool", wgt[:, :, gl, :],
                          w_gu[i, ex, :, gl * D + half * 512: gl * D + half * 512 + 512].rearrange("(k p) n -> p k n", p=128),
                          writes=[wg_r], key=wg_r)
                S.dma("pool", wdt[:], w_dn[i, ex, half * 512:(half + 1) * 512, :].rearrange("(j p) n -> p j n", p=128),
                      writes=[wd_r], key=wd_r)
                loaded[idx] = (wgt, wg_r, wdt, wd_r)

            load_piece(0)
            for idx in range(len(pieces)):
                if True:
                    ex, half = pieces[idx]
                    if idx + 1 < len(pieces):
                        load_piece(idx + 1)
                    wgt, wg_r, wdt, wd_r = loaded.pop(idx)
                    for (s, n) in blks:
                        c = 1 if s == 0 else 0
                        pb, pb_r = pgb.next()
                        mg_, mg_r = mg.next()
                        S.op("act", lambda e, mg_=mg_, ex=ex, s=s, n=n: e.activation(
                            out=mg_[:, :n], in_=gateT[:, s:s + n], func=AF.Copy, scale=self.identf[0:NE, ex:ex + 1]),
                            reads=[gateT_r, self.identf_r], writes=[mg_r])
                        S.op("pe", lambda e, pb=pb, mg_=mg_, n=n: e.matmul(
                            pb[:, :n], lhsT=self.onesf[0:NE, :], rhs=mg_[:, :n], start=True, stop=True),
                            reads=[mg_r, self.onesf_r], writes=[pb_r], pe_acc=True)
                        g_, g_r = gB.next()
                        S.op("act", lambda e, g_=g_, pb=pb, n=n: e.activation(out=g_[:, :n], in_=pb[:, :n], func=AF.Copy),
                             reads=[pb_r], writes=[g_r])
                        a_, a_r = aT.next()
                        for jj in range(4):
                            j = half * 4 + jj
                            pg, pg_r = pgl.next()
                            for gl in range(2):
                                for k in range(8):
                                    S.op("pe", lambda e, pg=pg, gl=gl, k=k, jj=jj, s=s, n=n, wgt=wgt: e.matmul(
                                        pg[:, gl, :n], lhsT=wgt[:, k, gl, jj * 128:(jj + 1) * 128], rhs=hT[:, k, s:s + n],
                                        start=(k == 0), stop=(k == 7)),
                                        reads=[wg_r, hT_r], writes=[pg_r], pe_acc=True)
                            t1, t1_r = tg.next()
                            S.op("dve", lambda e, t1=t1, pg=pg, n=n, ex=ex, j=j: e.tensor_scalar(
                                out=t1[:, :n], in0=pg[:, 0, :n], scalar1=bgu[:, ex, j:j + 1], scalar2=7.0, op0=ALU.add, op1=ALU.min),
                                reads=[pg_r, bgu_r], writes=[t1_r])
                            t2, t2_r = tsg.next()
                            S.op("act", lambda e, t2=t2, t1=t1, n=n: e.activation(out=t2[:, :n], in_=t1[:, :n], func=AF.Sigmoid, scale=1.702),
                                 reads=[t1_r], writes=[t2_r])
                            t3, t3_r = tl.next()
                            S.op("dve", lambda e, t3=t3, pg=pg, n=n, ex=ex, j=j: e.tensor_scalar(
                                out=t3[:, :n], in0=pg[:, 1, :n], scalar1=bgu[:, ex, 8 + j:9 + j], scalar2=-6.0, op0=ALU.add, op1=ALU.max),
                                reads=[pg_r, bgu_r], writes=[t3_r])
                            S.op("pool", lambda e, t1=t1, t2=t2, n=n: e.tensor_tensor(out=t1[:, :n], in0=t1[:, :n], in1=t2[:, :n], op=ALU.mult),
                                 reads=[t1_r, t2_r], writes=[t1_r])
                            S.op("dve", lambda e, t1=t1, t3=t3, n=n: e.scalar_tensor_tensor(
                                out=t3[:, :n], in0=t3[:, :n], scalar=8.0, in1=t1[:, :n], op0=ALU.min, op1=ALU.mult),
                                reads=[t1_r, t3_r], writes=[t3_r])
                            S.op("pool", lambda e, a_=a_, t3=t3, g_=g_, jj=jj, n=n: e.tensor_tensor(out=a_[:, jj, :n], in0=t3[:, :n], in1=g_[:, :n], op=ALU.mult),
                                 reads=[t3_r, g_r], writes=[a_r])
                        for oc in range(8):
                            po, po_r = pout.next()
                            for jj in range(4):
                                S.op("pe", lambda e, po=po, oc=oc, jj=jj, n=n, wdt=wdt, a_=a_: e.matmul(
                                    po[:, :n], lhsT=wdt[:, jj, oc * 128:(oc + 1) * 128], rhs=a_[:, jj, :n],
                                    start=(jj == 0), stop=(jj == 3)),
                                    reads=[wd_r, a_r], writes=[po_r], pe_acc=True)
                            S.op("dve", lambda e, po=po, oc=oc, s=s, n=n, c=c: e.scalar_tensor_tensor(
                                out=xT[:, oc, s:s + n], in0=po[:, :n], scalar=self.mv[:, c, 5, oc:oc + 1], in1=xT[:, oc, s:s + n],
                                op0=ALU.mult, op1=ALU.add),
                                reads=[po_r, self.mv_r, xTr], writes=[xTr])
            S.barrier()
            S.release([bgu_r, bdn_r] + [r for _, r in wg.items] + [r for _, r in wd.items])

    def ffn(self, i, with_ctx=True):
        nc, S, M = self.nc, self.S, self.M
        xT, xTr = self.xT, self.xTr
        w_router = self.inp("moe_w_router", [4, D, NE])
        b_router = self.inp("moe_b_router", [4, NE])
        w_gu = self.inp("moe_w_gu2d", [4 * NE * D, 2 * D])
        w_dn = self.inp("moe_w_down2d", [4 * NE * D, D])
        b_gu = self.inp("moe_b_gu_rows", [4 * NE * 128, 16])
        b_dn = self.inp("moe_b_down2d", [4 * NE, D])
        tiles = list(range(NT)) if with_ctx else list(range(2, NT))
        t0 = tiles[0]
        ntl = len(tiles)
        NB = ntl + NE
        NBMAX = NT + NE
        blks = [b for b in BLKS if with_ctx or b[0] != 0]
        if not hasattr(self, "XE"):
            self.XE = nc.dram_tensor("scr_xe", [NBMAX * 512, D], BF16, kind="Internal").ap()
            self.YE = nc.dram_tensor("scr_ye", [NBMAX * 512, D], F32, kind="Internal").ap()
            self.XE_r, self.YE_r = Res("XE"), Res("YE")
            first = True
        else:
            first = False
        XE, YE, XE_r, YE_r = self.XE, self.YE, self.XE_r, self.YE_r
        with ExitStack() as pA:
            g4, g4_r = M.sb(pA, [128, NT, 4], F32, "g4")
            IDXi, IDXi_r = M.sb(pA, [128, NT * 4], I32, "IDXi")
            idxw, idxw_r = M.sb(pA, [128, NBMAX, 8], I32, "idxw")
            idxb, idxb_r = M.sb(pA, [128, NBMAX], I32, "idxb")
            idxd, idxd_r = M.sb(pA, [128, NBMAX], I32, "idxd")
            if first:
                with ExitStack() as pz:
                    z, z_r = M.sb(pz, [128, 4, D], BF16, "zero")
                    S.op("pool", lambda e: e.memset(z[:], 0.0), writes=[z_r])
                    for b in range(NBMAX):
                        S.dma("sp", XE[b * 512:(b + 1) * 512, :].rearrange("(s p) f -> p s f", p=128), z[:],
                              reads=[z_r], writes=[XE_r], key=z_r)
                    S.barrier()
                    S.release([z_r])
            with ExitStack() as pH:
                hTM, hTM_r = M.sb(pH, [128, NT, D], BF16, "hTM")
                lgs, lgs_r = M.sb(pH, [128, NT, NE], F32, "lgs")
                m8s, m8s_r = M.sb(pH, [128, NT, 8], F32, "m8s")
                MK, MK_r = M.sb(pH, [128, NT, NE], F32, "MK")
                with ExitStack() as pR:
                    wr, wr_r = M.sb(pR, [128, 8, NE], F32, "wr")
                    brb, brb_r = M.sb(pR, [128, NE], F32, "brb")
                    S.dma("sp", wr[:], w_router[i].rearrange("(k p) n -> p k n", p=128), writes=[wr_r], key=wr_r)
                    S.dma("sp", brb[:], b_router[i].partition_broadcast(128), writes=[brb_r], key=brb_r)
                    sq = Ring([M.sb(pR, [128, 512], F32, "nsq") for _ in range(2)])
                    h32, h32_r = M.sb(pR, [128, 8, 512], F32, "nh32")
                    pss = Ring([M.ps(pR, [128, 512], F32, "nss") for _ in range(2)])
                    rst = Ring([M.sb(pR, [128, 512], F32, "nrstd") for _ in range(2)])
                    tmp = Ring([M.sb(pR, [128, 512], F32, "ntmp") for _ in range(2)])
                    plg = Ring([M.ps(pR, [128, NE], F32, "rlg") for _ in range(2)])
                    ptk = Ring([M.ps(pR, [128, 4, 128], F32, "ptk") for _ in range(2)])
                    rsm = Ring([M.sb(pR, [128, 8], F32, "rsm") for _ in range(2)])
                    for (s, n) in blks:
                        c = 1 if s == 0 else 0
                        A = self.mv[:, c, 3, :]
                        B = self.mv[:, c, 4, :]
                        pp, pp_r = pss.next()
                        for k in range(8):
                            q, q_r = sq.next()
                            S.op("act", lambda e, q=q, s=s, n=n, k=k: e.activation(out=q[:, :n], in_=xT[:, k, s:s + n], func=AF.Square),
                                 reads=[xTr], writes=[q_r])
                            S.op("pe", lambda e, pp=pp, q=q, k=k, n=n: e.matmul(pp[:, :n], lhsT=self.onesf[:], rhs=q[:, :n],
                                                                                start=(k == 0), stop=(k == 7)),
                                 reads=[q_r, self.onesf_r], writes=[pp_r], pe_acc=True)
                        r, r_r = rst.next()
                        S.op("act", lambda e, r=r, pp=pp, n=n: e.activation(out=r[:, :n], in_=pp[:, :n], func=AF.Sqrt, scale=1.0 / D,
                                                                            bias=self.epsb[:, 0:1]),
                             reads=[pp_r, self.epsb_r], writes=[r_r])
                        S.op("dve", lambda e, r=r, n=n: e.reciprocal(out=r[:, :n], in_=r[:, :n]), reads=[r_r], writes=[r_r])
                        for k in range(8):
                            t_, t_r = tmp.next()
                            S.op("dve", lambda e, t_=t_, k=k, s=s, n=n, r=r: e.tensor_tensor(out=t_[:, :n], in0=xT[:, k, s:s + n], in1=r[:, :n],
                                                                                             op=ALU.mult),
                                 reads=[xTr, r_r], writes=[t_r])
                            S.op("act", lambda e, t_=t_, k=k, n=n, A=A, B=B: e.activation(
                                out=h32[:, k, :n], in_=t_[:, :n], func=AF.Identity, scale=A[:, k:k + 1], bias=B[:, k:k + 1]),
                                reads=[t_r, self.mv_r], writes=[h32_r])
                        for tt in range(n // 128):
                            t = s // 128 + tt
                            lg, lg_r = plg.next()
                            for k in range(8):
                                S.op("pe", lambda e, lg=lg, k=k, tt=tt: e.matmul(
                                    lg[:], lhsT=h32[:, k, tt * 128:(tt + 1) * 128], rhs=wr[:, k, :], start=(k == 0), stop=(k == 7)),
                                    reads=[h32_r, wr_r], writes=[lg_r], pe_acc=True)
                            sm, sm_r = rsm.next()
                            S.op("dve", lambda e, lg=lg, t=t: e.tensor_tensor(out=lgs[:, t, :], in0=lg[:], in1=brb[:], op=ALU.add),
                                 reads=[lg_r, brb_r], writes=[lgs_r])
                            S.op("dve", lambda e, t=t: e.max(out=m8s[:, t, :], in_=lgs[:, t, :]), reads=[lgs_r], writes=[m8s_r])
                            S.op("dve", lambda e, t=t: e.tensor_scalar(out=MK[:, t, :], in0=lgs[:, t, :], scalar1=m8s[:, t, 3:4], scalar2=None,
                                                                       op0=ALU.is_ge), reads=[lgs_r, m8s_r], writes=[MK_r])
                            S.op("dve", lambda e, sm=sm, t=t: e.tensor_scalar(out=sm[:, 0:1], in0=m8s[:, t, 0:1], scalar1=-1.0, scalar2=None,
                                                                            op0=ALU.mult), reads=[m8s_r], writes=[sm_r])
                            S.op("act", lambda e, sm=sm, t=t: e.activation(out=sm[:, 4:8], in_=m8s[:, t, 0:4], func=AF.Exp, bias=sm[:, 0:1], scale=1.0),
                                 reads=[m8s_r, sm_r], writes=[sm_r])
                            S.op("dve", lambda e, sm=sm: e.reduce_sum(out=sm[:, 1:2], in_=sm[:, 4:8], axis=AX.X), reads=[sm_r], writes=[sm_r])
                            S.op("dve", lambda e, sm=sm: e.reciprocal(out=sm[:, 2:3], in_=sm[:, 1:2]), reads=[sm_r], writes=[sm_r])
                            S.op("dve", lambda e, sm=sm, t=t: e.tensor_scalar(out=g4[:, t, :], in0=sm[:, 4:8], scalar1=sm[:, 2:3], scalar2=None,
                                                                            op0=ALU.mult), reads=[sm_r], writes=[g4_r])
                            for hf in range(2):
                                pk, pk_r = ptk.next()
                                for kk in range(4):
                                    k = hf * 4 + kk
                                    S.op("pe", lambda e, pk=pk, kk=kk, k=k, tt=tt: e.transpose(
                                        out=pk[:, kk, :], in_=h32[:, k, tt * 128:(tt + 1) * 128], identity=self.identf[:]),
                                        reads=[h32_r, self.identf_r], writes=[pk_r], pe_acc=True)
                                if hf == 0:
                                    S.op("act", lambda e, pk=pk, t=t: e.activation(out=hTM[:, t, 0:512], in_=pk[:].rearrange("p a b -> p (a b)"), func=AF.Copy),
                                         reads=[pk_r], writes=[hTM_r])
                                else:
                                    S.op("dve", lambda e, pk=pk, t=t: e.tensor_copy(out=hTM[:, t, 512:1024], in_=pk[:].rearrange("p a b -> p (a b)")),
                                         reads=[pk_r], writes=[hTM_r])
                    S.barrier()
                    S.release([wr_r, brb_r])
                with ExitStack() as pI:
                    f = lambda name, shape: M.sb(pI, shape, F32, name)
                    msum, msum_r = f("msum", [128, NE])
                    cnt, cnt_r = f("cnt", [128, NE])
                    nb, nb_r = f("nb", [128, NE])
                    sz, sz_r = f("sz", [128, NE])
                    sa, sa_r = f("sa", [128, NE])
                    sbb, sbb_r = f("sbb", [128, NE])
                    offX, offX_r = f("offX", [128, NE])
                    run, run_r = f("run", [128, NE])
                    mlt, mlt_r = f("mlt", [128, 128])
                    IDXf, IDXf_r = f("IDXf", [128, NT * 4])
                    ebf, ebf_r = f("ebf", [128, NBMAX])
                    eb2, eb2_r = f("eb2", [128, NBMAX])
                    kp, kp_r = f("kp", [128, 8])
                    pc, pc_r = f("pc", [128, 1])
                    iwf, iwf_r = f("iwf", [128, NBMAX, 8])
                    slr = Ring([f("slot", [128, NE]) for _ in range(2)])
                    ohr = Ring([f("oh", [128, NE]) for _ in range(2)])
                    cmr = Ring([f("cmp", [128, NE]) for _ in range(2)])
                    pcn, pcn_r = M.ps(pI, [128, NE], F32, "pcn")
                    ppr = Ring([M.ps(pI, [128, NE], F32, "ppos") for _ in range(2)])
                    S.op("pool", lambda e: e.memset(mlt[:], 1.0), writes=[mlt_r])
                    S.op("pool", lambda e: e.affine_select(out=mlt[:], in_=mlt[:], pattern=[[1, 128]], compare_op=ALU.is_gt, fill=0.0,
                                                           base=0, channel_multiplier=-1), reads=[mlt_r], writes=[mlt_r])
                    S.op("pool", lambda e: e.iota(kp[:], pattern=[[128, 8]], base=i * NE * D, channel_multiplier=1, allow_small_or_imprecise_dtypes=True),
                         writes=[kp_r])
                    S.op("pool", lambda e: e.iota(pc[:], pattern=[[0, 1]], base=i * NE * 128, channel_multiplier=1, allow_small_or_imprecise_dtypes=True),
                         writes=[pc_r])
                    S.op("pool", lambda e: e.memset(IDXf[:], 0.0), writes=[IDXf_r])
                    S.op("pool", lambda e: e.memset(ebf[:], 31.0), writes=[ebf_r])
                    S.op("dve", lambda e: e.reduce_sum(out=msum[:], in_=MK[:, t0:NT, :].rearrange("p t e -> p e t"), axis=AX.X),
                         reads=[MK_r], writes=[msum_r])
                    S.op("pe", lambda e: e.matmul(pcn[:], lhsT=self.onesf[:], rhs=msum[:], start=True, stop=True),
                         reads=[self.onesf_r, msum_r], writes=[pcn_r], pe_acc=True)
                    S.op("act", lambda e: e.activation(out=cnt[:], in_=pcn[:], func=AF.Copy), reads=[pcn_r], writes=[cnt_r])
                    S.op("dve", lambda e: e.tensor_scalar(out=nb[:], in0=cnt[:], scalar1=0.0, scalar2=None, op0=ALU.is_gt), reads=[cnt_r], writes=[nb_r])
                    for m in range(1, 5):
                        S.op("dve", lambda e, m=m: e.scalar_tensor_tensor(out=nb[:], in0=cnt[:], scalar=512.0 * m, in1=nb[:], op0=ALU.is_gt, op1=ALU.add),
                             reads=[cnt_r, nb_r], writes=[nb_r])
                    S.op("dve", lambda e: e.tensor_scalar(out=sz[:], in0=nb[:], scalar1=512.0, scalar2=None, op0=ALU.mult), reads=[nb_r], writes=[sz_r])
                    S.op("dve", lambda e: e.tensor_copy(out=sa[:], in_=sz[:]), reads=[sz_r], writes=[sa_r])
                    cur, cur_r, oth, oth_r = sa, sa_r, sbb, sbb_r
                    for dd in (1, 2, 4, 8, 16):
                        S.op("dve", lambda e, cur=cur, oth=oth, dd=dd: e.tensor_copy(out=oth[:, 0:dd], in_=cur[:, 0:dd]), reads=[cur_r], writes=[oth_r])
                        S.op("dve", lambda e, cur=cur, oth=oth, dd=dd: e.tensor_tensor(out=oth[:, dd:NE], in0=cur[:, dd:NE], in1=cur[:, 0:NE - dd], op=ALU.add),
                             reads=[cur_r, oth_r], writes=[oth_r])
                        cur, cur_r, oth, oth_r = oth, oth_r, cur, cur_r
                    offE, offE_r = cur, cur_r
                    S.op("dve", lambda e: e.tensor_tensor(out=offX[:], in0=offE[:], in1=sz[:], op=ALU.subtract), reads=[offE_r, sz_r], writes=[offX_r])
                    S.op("pool", lambda e: e.memset(run[:], 0.0), writes=[run_r])
                    for t in tiles:
                        pp, pp_r = ppr.next()
                        S.op("pe", lambda e, pp=pp, t=t: e.matmul(pp[:], lhsT=mlt[:], rhs=MK[:, t, :], start=True, stop=False),
                             reads=[mlt_r, MK_r], writes=[pp_r], pe_acc=True)
                        S.op("pe", lambda e, pp=pp: e.matmul(pp[:], lhsT=self.onesf[:], rhs=run[:], start=False, stop=True),
                             reads=[self.onesf_r, run_r], writes=[pp_r], pe_acc=True)
                        sl, sl_r = slr.next()
                        S.op("dve", lambda e, sl=sl, pp=pp: e.tensor_tensor(out=sl[:], in0=pp[:], in1=offX[:], op=ALU.add),
                             reads=[pp_r, offX_r], writes=[sl_r])
                        S.op("dve", lambda e, t=t: e.tensor_tensor(out=run[:], in0=run[:], in1=MK[:, t, :], op=ALU.add),
                             reads=[run_r, MK_r], writes=[run_r])
                        for j in range(4):
                            oh, oh_r = ohr.next()
                            S.op("dve", lambda e, oh=oh, t=t, j=j: e.tensor_scalar(out=oh[:], in0=lgs[:, t, :], scalar1=m8s[:, t, j:j + 1], scalar2=None,
                                                                                 op0=ALU.is_equal), reads=[lgs_r, m8s_r], writes=[oh_r])
                            S.op("dve", lambda e, oh=oh, sl=sl: e.tensor_tensor(out=oh[:], in0=oh[:], in1=sl[:], op=ALU.mult),
                                 reads=[oh_r, sl_r], writes=[oh_r])
                            S.op("dve", lambda e, oh=oh, t=t, j=j: e.reduce_sum(out=IDXf[:, t * 4 + j:t * 4 + j + 1], in_=oh[:], axis=AX.X),
                                 reads=[oh_r], writes=[IDXf_r])
                    S.op("dve", lambda e: e.tensor_copy(out=IDXi[:], in_=IDXf[:]), reads=[IDXf_r], writes=[IDXi_r])
                    for b in range(NB):
                        cm, cm_r = cmr.next()
                        S.op("dve", lambda e, cm=cm, b=b: e.tensor_scalar(out=cm[:], in0=offE[:], scalar1=512.0 * b, scalar2=None, op0=ALU.is_le),
                             reads=[offE_r], writes=[cm_r])
                        S.op("dve", lambda e, cm=cm, b=b: e.reduce_sum(out=ebf[:, b:b + 1], in_=cm[:], axis=AX.X), reads=[cm_r], writes=[ebf_r])
                    S.op("dve", lambda e: e.tensor_scalar(out=ebf[:], in0=ebf[:], scalar1=31.0, scalar2=None, op0=ALU.min), reads=[ebf_r], writes=[ebf_r])
                    S.op("dve", lambda e: e.tensor_scalar(out=eb2[:], in0=ebf[:], scalar1=float(i * NE), scalar2=None, op0=ALU.add),
                         reads=[ebf_r], writes=[eb2_r])
                    S.op("dve", lambda e: e.tensor_copy(out=idxd[:], in_=eb2[:]), reads=[eb2_r], writes=[idxd_r])
                    S.op("dve", lambda e: e.tensor_scalar(out=eb2[:], in0=ebf[:], scalar1=128.0, scalar2=pc[:, 0:1], op0=ALU.mult, op1=ALU.add),
                         reads=[ebf_r, pc_r, idxd_r], writes=[eb2_r])
                    S.op("dve", lambda e: e.tensor_copy(out=idxb[:], in_=eb2[:]), reads=[eb2_r], writes=[idxb_r])
                    S.op("dve", lambda e: e.tensor_scalar(out=eb2[:], in0=ebf[:], scalar1=1024.0, scalar2=None, op0=ALU.mult),
                         reads=[ebf_r, idxb_r], writes=[eb2_r])
                    S.op("dve", lambda e: e.tensor_tensor(out=iwf[:], in0=eb2[:].unsqueeze(2).to_broadcast([128, NBMAX, 8]),
                                                          in1=kp[:].unsqueeze(1).to_broadcast([128, NBMAX, 8]), op=ALU.add),
                         reads=[eb2_r, kp_r], writes=[iwf_r])
                    S.op("dve", lambda e: e.tensor_copy(out=idxw[:], in_=iwf[:]), reads=[iwf_r], writes=[idxw_r])
                    S.barrier()
                for t in tiles:
                    for j in range(4):
                        S.idma(XE, IDXi[:, t * 4 + j:t * 4 + j + 1], hTM[:, t, :], None, NBMAX * 512 - 1,
                               reads=[hTM_r, IDXi_r], writes=[XE_r], key=hTM_r)
                S.barrier()
                S.release([hTM_r])
            with ExitStack() as pE:
                wgr = Ring([M.sb(pE, [128, 8, 2 * D], BF16, "wgb") for _ in range(2)])
                wd, wd_r = M.sb(pE, [128, 8, D], BF16, "wdb")
                bgr = Ring([M.sb(pE, [128, 16], F32, "bgb") for _ in range(2)])
                bdr = Ring([M.sb(pE, [128, D], F32, "bdb") for _ in range(1)])
                xet = [M.sb(pE, [128, D], BF16, "xe%d" % st) for st in range(4)]
                xeT, xeT_r = M.sb(pE, [128, 8, 512], BF16, "xeT")
                aT, aT_r = M.sb(pE, [128, 8, 512], BF16, "aT")
                aT_rs = [Res("aT%d" % j) for j in range(8)]
                tg = Ring([M.sb(pE, [128, 512], F32, "tg") for _ in range(2)])
                tsg = Ring([M.sb(pE, [128, 512], F32, "tsg") for _ in range(2)])
                tl = Ring([M.sb(pE, [128, 512], F32, "tl") for _ in range(2)])
                yer = Ring([M.sb(pE, [128, D], F32, "ye") for _ in range(2)])
                ptx, ptx_r = M.ps(pE, [128, 8, 128], BF16, "ptx")
                pgl = Ring([M.ps(pE, [128, 2, 512], F32, "pgl") for _ in range(2)])
                pout = Ring([M.ps(pE, [128, 512], F32, "pout") for _ in range(2)])
                loaded = {}

                def load_block(b):
                    wg, wg_r = wgr.next()
                    bg, bg_r = bgr.next()
                    for k in range(8):
                        S.idma(wg[:, k, :], None, w_gu, idxw[:, b, k:k + 1], 4 * NE * D - 1, reads=[idxw_r], writes=[wg_r], key=wg_r)
                    S.idma(bg[:], None, b_gu, idxb[:, b:b + 1], 4 * NE * 128 - 1, reads=[idxb_r], writes=[bg_r], key=bg_r)
                    loaded[b] = (wg, wg_r, bg, bg_r)

                def load_xe(b):
                    for st in range(4):
                        xe, xe_r = xet[st]
                        S.dma("act", xe[:], XE[b * 512 + st * 128:b * 512 + (st + 1) * 128, :], reads=[XE_r], writes=[xe_r], key=xe_r)

                load_block(0)
                load_xe(0)
                for b in range(NB):
                    wg, wg_r, bg, bg_r = loaded.pop(b)
                    bd, bd_r = bdr.next()
                    S.idma(bd[:], None, b_dn, idxd[:, b:b + 1], 4 * NE - 1, reads=[idxd_r], writes=[bd_r], key=bd_r)
                    for k in range(8):
                        S.idma(wd[:, k, :], None, w_dn, idxw[:, b, k:k + 1], 4 * NE * D - 1, reads=[idxw_r], writes=[wd_r], key=wd_r)
                    if b + 1 < NB:
                        load_block(b + 1)
                    for st in range(4):
                        xe, xe_r = xet[st]
                        for k in range(8):
                            S.op("pe", lambda e, st=st, k=k, xe=xe: e.transpose(out=ptx[:, k, :], in_=xe[:, k * 128:(k + 1) * 128], identity=self.identb[:]),
                                 reads=[xe_r, self.identb_r], writes=[ptx_r], pe_acc=True)
                        S.op("act", lambda e, st=st: e.activation(out=xeT[:, :, st * 128:(st + 1) * 128], in_=ptx[:], func=AF.Copy),
                             reads=[ptx_r], writes=[xeT_r])
                    if b + 1 < NB:
                        load_xe(b + 1)
                    S.op("dve", lambda e, bg=bg: e.tensor_scalar(out=bg[:, 8:16], in0=bg[:, 8:16], scalar1=1.0, scalar2=None, op0=ALU.add),
                         reads=[bg_r], writes=[bg_r])
                    pend = None
                    for j in range(8):
                        pg, pg_r = pgl.next()
                        for gl in range(2):
                            for k in range(8):
                                S.op("pe", lambda e, pg=pg, gl=gl, k=k, j=j, wg=wg: e.matmul(
                                    pg[:, gl, :], lhsT=wg[:, k, gl * D + j * 128:gl * D + (j + 1) * 128], rhs=xeT[:, k, :],
                                    start=(k == 0), stop=(k == 7)), reads=[wg_r, xeT_r], writes=[pg_r], pe_acc=True)
                        t1, t1_r = tg.next()
                        t2, t2_r = tsg.next()
                        t3, t3_r = tl.next()
                        S.op("dve", lambda e, t1=t1, pg=pg, j=j, bg=bg: e.tensor_scalar(
                            out=t1[:], in0=pg[:, 0, :], scalar1=bg[:, j:j + 1], scalar2=7.0, op0=ALU.add, op1=ALU.min),
                            reads=[pg_r, bg_r], writes=[t1_r])
                        S.op("act", lambda e, t2=t2, t1=t1: e.activation(out=t2[:], in_=t1[:], func=AF.Sigmoid, scale=1.702), reads=[t1_r], writes=[t2_r])
                        S.op("dve", lambda e, t3=t3, pg=pg, j=j, bg=bg: e.tensor_scalar(
                            out=t3[:], in0=pg[:, 1, :], scalar1=bg[:, 8 + j:9 + j], scalar2=-6.0, op0=ALU.add, op1=ALU.max),
                            reads=[pg_r, bg_r], writes=[t3_r])
                        if pend is not None:
                            pend()

                        def tail(t1=t1, t1_r=t1_r, t2=t2, t2_r=t2_r, t3=t3, t3_r=t3_r, j=j):
                            S.op("dve", lambda e: e.tensor_tensor(out=t1[:], in0=t1[:], in1=t2[:], op=ALU.mult), reads=[t1_r, t2_r], writes=[t1_r])
                            S.op("dve", lambda e: e.scalar_tensor_tensor(
                                out=aT[:, j, :], in0=t3[:], scalar=8.0, in1=t1[:], op0=ALU.min, op1=ALU.mult), reads=[t1_r, t3_r], writes=[aT_rs[j]])
                        pend = tail
                    pend()
                    for st in range(4):
                        ye, ye_r = yer.next()
                        for hf in range(2):
                            po, po_r = pout.next()
                            for j in range(8):
                                S.op("pe", lambda e, po=po, j=j, st=st, hf=hf: e.matmul(
                                    po[:], lhsT=aT[:, j, st * 128:(st + 1) * 128], rhs=wd[:, j, hf * 512:(hf + 1) * 512],
                                    start=(j == 0), stop=(j == 7)), reads=[aT_rs[j], wd_r], writes=[po_r], pe_acc=True)
                            S.op("dve", lambda e, po=po, ye=ye, hf=hf, bd=bd: e.tensor_tensor(
                                out=ye[:, hf * 512:(hf + 1) * 512], in0=po[:], in1=bd[:, hf * 512:(hf + 1) * 512], op=ALU.add),
                                reads=[po_r, bd_r], writes=[ye_r])
                        S.dma("sp", YE[b * 512 + st * 128:b * 512 + (st + 1) * 128, :], ye[:], reads=[ye_r], writes=[YE_r], key=ye_r)
                S.barrier()
                S.release([r for _, r in wgr.items] + [wd_r] + [r for _, r in bgr.items] + [r for _, r in bdr.items]
                          + [r for _, r in xet] + [r for _, r in yer.items])
            with ExitStack() as pC:
                yjr = Ring([M.sb(pC, [128, D], F32, "yj") for _ in range(3)])
                acr = Ring([M.sb(pC, [128, D], F32, "acc") for _ in range(2)])
                pct = Ring([M.ps(pC, [128, 4, 128], F32, "pct") for _ in range(2)])
                for t in tiles:
                    c = 1 if t < 2 else 0
                    ac, ac_r = acr.next()
                    for j in range(4):
                        yj, yj_r = yjr.next()
                        S.idma(yj[:], None, YE, IDXi[:, t * 4 + j:t * 4 + j + 1], NBMAX * 512 - 1, reads=[YE_r, IDXi_r], writes=[yj_r], key=yj_r)
                        if j == 0:
                            S.op("dve", lambda e, ac=ac, yj=yj, t=t: e.tensor_scalar(out=ac[:], in0=yj[:], scalar1=g4[:, t, 0:1], scalar2=None, op0=ALU.mult),
                                 reads=[yj_r, g4_r], writes=[ac_r])
                        else:
                            S.op("dve", lambda e, ac=ac, yj=yj, t=t, j=j: e.scalar_tensor_tensor(
                                out=ac[:], in0=yj[:], scalar=g4[:, t, j:j + 1], in1=ac[:], op0=ALU.mult, op1=ALU.add),
                                reads=[yj_r, g4_r, ac_r], writes=[ac_r])
                    for hf in range(2):
                        pk, pk_r = pct.next()
                        for kk in range(4):
                            k = hf * 4 + kk
                            S.op("pe", lambda e, pk=pk, kk=kk, k=k, ac=ac: e.transpose(out=pk[:, kk, :], in_=ac[:, k * 128:(k + 1) * 128], identity=self.identf[:]),
                                 reads=[ac_r, self.identf_r], writes=[pk_r], pe_acc=True)
                        for kk in range(4):
                            k = hf * 4 + kk
                            S.op("dve", lambda e, pk=pk, kk=kk, k=k, t=t, c=c: e.scalar_tensor_tensor(
                                out=xT[:, k, t * 128:(t + 1) * 128], in0=pk[:, kk, :], scalar=self.mv[:, c, 5, k:k + 1], in1=xT[:, k, t * 128:(t + 1) * 128],
                                op0=ALU.mult, op1=ALU.add), reads=[pk_r, self.mv_r, xTr], writes=[xTr])
                S.barrier()
                S.release([r for _, r in yjr.items])

    def resid(self, po, po_r, n, oc, s, gb=None, tring=None):
        S = self.S
        c = 1 if s < NCTX else 0
        xT, xTr = self.xT, self.xTr
        if gb is None:
            S.op("dve", lambda e: e.scalar_tensor_tensor(
                out=xT[:, oc, s:s + n], in0=po[:, :n], scalar=self.mv[:, c, 2, oc:oc + 1], in1=xT[:, oc, s:s + n],
                op0=ALU.mult, op1=ALU.add), reads=[po_r, self.mv_r, xTr], writes=[xTr])
        else:
            gbt, gbt_r = gb
            t_, t_r = tring.next()
            S.op("act", lambda e: e.activation(out=t_[:, :n], in_=po[:, :n], func=AF.Identity,
                                               scale=self.mv[:, c, 2, oc:oc + 1], bias=gbt[:, c, oc:oc + 1]),
                 reads=[po_r, self.mv_r, gbt_r], writes=[t_r])
            S.op("pool", lambda e: e.tensor_tensor(out=xT[:, oc, s:s + n], in0=xT[:, oc, s:s + n], in1=t_[:, :n], op=ALU.add),
                 reads=[t_r, xTr], writes=[xTr])

    def gate_bias(self, ps, bvec_ap):
        S, M = self.S, self.M
        gb, gb_r = M.sb(ps, [128, 2, 8], F32, "gb")
        for c in range(2):
            S.op("dve", lambda e, c=c: e.tensor_tensor(out=gb[:, c, :], in0=self.mv[:, c, 2, :], in1=bvec_ap, op=ALU.mult),
                 reads=[self.mv_r] + self._vec_deps, writes=[gb_r])
        return gb, gb_r

    def mixer0(self, i):
        nc, S, M = self.nc, self.S, self.M
        w1 = self.inp("conv_w_pw1", [1, D, 2 * D])
        w2 = self.inp("conv_w_pw2", [1, D, D])
        cvd = self.inp("conv_vec", [128, 296])
        UW = 2364

        def ucol(s):
            return 15 if s == 0 else s + 45
        with ExitStack() as pA:
            cv, cv_r = M.sb(pA, [128, 296], F32, "cv")
            S.dma("sp", cv[:], cvd, writes=[cv_r], key=cv_r)
            self._vec_deps = [cv_r]
            V, V_r = M.sb(pA, [128, 8, T], BF16, "V")
            with ExitStack() as pB:
                U, U_r = M.sb(pB, [128, 8, UW], BF16, "U")
                S.op("pool", lambda e: e.memset(U[:], 0.0), writes=[U_r])
                with ExitStack() as pC:
                    hT, hT_r = M.sb(pC, [128, 8, T], BF16, "hT")
                    with ExitStack() as pD:
                        self.adanorm(pD, hT, hT_r, 0)
                        S.barrier()
                    wp = Ring([M.sb(pC, [128, 8, 2, 128], BF16, "wp") for _ in range(2)])
                    pa = Ring([M.ps(pC, [128, 2, 512], F32, "pa") for _ in range(2)])
                    sg = Ring([M.sb(pC, [128, 512], F32, "sg") for _ in range(2)])
                    for oc in range(8):
                        wt, wt_r = wp.next()
                        for gl in range(2):
                            S.dma("pool", wt[:, :, gl, :],
                                  w1[0, :, gl * D + oc * 128: gl * D + (oc + 1) * 128].rearrange("(k p) n -> p k n", p=128),
                                  writes=[wt_r], key=wt_r)
                        for (s, n) in BLKS:
                            p_, p_r = pa.next()
                            for gl in range(2):
                                for k in range(8):
                                    S.op("pe", lambda e, p_=p_, gl=gl, k=k, s=s, n=n, wt=wt: e.matmul(
                                        p_[:, gl, :n], lhsT=wt[:, k, gl, :], rhs=hT[:, k, s:s + n], start=(k == 0), stop=(k == 7)),
                                        reads=[wt_r, hT_r], writes=[p_r], pe_acc=True)
                            g_, g_r = sg.next()
                            S.op("act", lambda e, g_=g_, p_=p_, n=n, oc=oc: e.activation(
                                out=g_[:, :n], in_=p_[:, 1, :n], func=AF.Sigmoid, bias=cv[:, 8 + oc:9 + oc], scale=1.0),
                                reads=[p_r, cv_r], writes=[g_r])
                            S.op("dve", lambda e, g_=g_, p_=p_, n=n, oc=oc, s=s: e.scalar_tensor_tensor(
                                out=U[:, oc, ucol(s):ucol(s) + n], in0=p_[:, 0, :n], scalar=cv[:, oc:oc + 1], in1=g_[:, :n],
                                op0=ALU.add, op1=ALU.mult),
                                reads=[p_r, cv_r, g_r], writes=[U_r])
                    S.barrier()
                    S.release([r for _, r in wp.items])
                dgr = Ring([M.sb(pB, [128, 31, 128], BF16, "dg") for _ in range(2)])
                pc = Ring([M.ps(pB, [128, 512], F32, "pc") for _ in range(2)])
                for oc in range(8):
                    dg, dg_r = dgr.next()
                    for w in range(31):
                        S.op("dve", lambda e, dg=dg, w=w, oc=oc: e.tensor_scalar(
                            out=dg[:, w, :], in0=self.identb[:], scalar1=cv[:, 16 + oc * 31 + w:17 + oc * 31 + w], scalar2=None,
                            op0=ALU.mult), reads=[self.identb_r, cv_r], writes=[dg_r])
                    for (s, n) in BLKS:
                        o0 = 0 if s == 0 else s + 30
                        p_, p_r = pc.next()
                        for w in range(31):
                            S.op("pe", lambda e, p_=p_, w=w, oc=oc, o0=o0, n=n, dg=dg: e.matmul(
                                p_[:, :n], lhsT=dg[:, w, :], rhs=U[:, oc, o0 + w:o0 + w + n], start=(w == 0), stop=(w == 30)),
                                reads=[dg_r, U_r], writes=[p_r], pe_acc=True)
                        S.op("act", lambda e, p_=p_, oc=oc, s=s, n=n: e.activation(
                            out=V[:, oc, s:s + n], in_=p_[:, :n], func=AF.Identity, bias=cv[:, 264 + oc:265 + oc], scale=1.0),
                            reads=[p_r, cv_r], writes=[V_r])
                S.barrier()
            h2, h2_r = M.sb(pA, [128, 8, T], BF16, "h2")
            w2t, w2_r = M.sb(pA, [128, 8, D], BF16, "w2t")
            S.dma("pool", w2t[:], w2[0].rearrange("(k p) n -> p k n", p=128), writes=[w2_r], key=w2_r)
            gb = self.gate_bias(pA, cv[:, 288:296])
            ps1 = Ring([M.ps(pA, [128, 512], F32, "ps1") for _ in range(1)])
            ps2 = Ring([M.ps(pA, [128, 512], F32, "ps2") for _ in range(1)])
            po = Ring([M.ps(pA, [128, 512], F32, "po") for _ in range(2)])
            vsq = Ring([M.sb(pA, [128, 512], BF16, "vsq") for _ in range(2)])
            mu = Ring([M.sb(pA, [128, 512], F32, "mu") for _ in range(1)])
            rs = Ring([M.sb(pA, [128, 512], F32, "rs") for _ in range(1)])
            tt = Ring([M.sb(pA, [128, 512], F32, "tt") for _ in range(2)])
            tr = Ring([M.sb(pA, [128, 512], F32, "tr") for _ in range(2)])
            for (s, n) in BLKS:
                a1, a1_r = ps1.next()
                a2, a2_r = ps2.next()
                for k in range(8):
                    q, q_r = vsq.next()
                    S.op("pool", lambda e, q=q, k=k, s=s, n=n: e.tensor_tensor(out=q[:, :n], in0=V[:, k, s:s + n], in1=V[:, k, s:s + n], op=ALU.mult),
                         reads=[V_r], writes=[q_r])
                    S.op("pe", lambda e, a1=a1, k=k, s=s, n=n: e.matmul(a1[:, :n], lhsT=self.onesb[:], rhs=V[:, k, s:s + n],
                                                                         start=(k == 0), stop=(k == 7)),
                         reads=[V_r, self.onesb_r], writes=[a1_r], pe_acc=True)
                    S.op("pe", lambda e, a2=a2, q=q, k=k, n=n: e.matmul(a2[:, :n], lhsT=self.onesb[:], rhs=q[:, :n],
                                                                        start=(k == 0), stop=(k == 7)),
                         reads=[q_r, self.onesb_r], writes=[a2_r], pe_acc=True)
                m_, m_r = mu.next()
                r_, r_r = rs.next()
                S.op("act", lambda e, m_=m_, a1=a1, n=n: e.activation(out=m_[:, :n], in_=a1[:, :n], func=AF.Copy, scale=1.0 / D),
                     reads=[a1_r], writes=[m_r])
                S.op("dve", lambda e, r_=r_, m_=m_, n=n: e.tensor_tensor(out=r_[:, :n], in0=m_[:, :n], in1=m_[:, :n], op=ALU.mult),
                     reads=[m_r], writes=[r_r])
                S.op("dve", lambda e, r_=r_, a2=a2, n=n: e.scalar_tensor_tensor(out=r_[:, :n], in0=a2[:, :n], scalar=1.0 / D, in1=r_[:, :n],
                                                                               op0=ALU.mult, op1=ALU.subtract),
                     reads=[a2_r, r_r], writes=[r_r])
                S.op("act", lambda e, r_=r_, n=n: e.activation(out=r_[:, :n], in_=r_[:, :n], func=AF.Sqrt, bias=self.epsb[:, 0:1], scale=1.0),
                     reads=[r_r, self.epsb_r], writes=[r_r])
                S.op("dve", lambda e, r_=r_, n=n: e.reciprocal(out=r_[:, :n], in_=r_[:, :n]), reads=[r_r], writes=[r_r])
                for k in range(8):
                    t_, t_r = tt.next()
                    S.op("dve", lambda e, t_=t_, k=k, s=s, n=n, m_=m_: e.tensor_tensor(out=t_[:, :n], in0=V[:, k, s:s + n], in1=m_[:, :n], op=ALU.subtract),
                         reads=[V_r, m_r], writes=[t_r])
                    S.op("pool", lambda e, t_=t_, r_=r_, n=n: e.tensor_tensor(out=t_[:, :n], in0=t_[:, :n], in1=r_[:, :n], op=ALU.mult),
                         reads=[t_r, r_r], writes=[t_r])
                    S.op("act", lambda e, t_=t_, k=k, s=s, n=n: e.activation(
                        out=h2[:, k, s:s + n], in_=t_[:, :n], func=AF.Silu, scale=cv[:, 272 + k:273 + k], bias=cv[:, 280 + k:281 + k]),
                        reads=[t_r, cv_r], writes=[h2_r])
            for (s, n) in BLKS:
                for oc in range(8):
                    p_, p_r = po.next()
                    for k in range(8):
                        S.op("pe", lambda e, p_=p_, k=k, oc=oc, s=s, n=n: e.matmul(
                            p_[:, :n], lhsT=w2t[:, k, oc * 128:(oc + 1) * 128], rhs=h2[:, k, s:s + n], start=(k == 0), stop=(k == 7)),
                            reads=[w2_r, h2_r], writes=[p_r], pe_acc=True)
                    self.resid(p_, p_r, n, oc, s, gb=gb, tring=tr)
            S.barrier()
            S.release([cv_r, w2_r])

    def load_w(self, ps, src2d, kch, n, name, npart=128):
        S, M = self.S, self.M
        t, t_r = M.sb(ps, [npart, kch, n], BF16, name)
        S.dma("pool", t[:], src2d.rearrange("(k p) n -> p k n", p=npart), writes=[t_r], key=t_r)
        return t, t_r

    def swap_halves(self, ps, w, w_r, kch, nh, name):
        S, M = self.S, self.M
        ws, ws_r = M.sb(ps, [128, kch, nh * 64], BF16, name)
        for h in range(nh):
            S.op("pool", lambda e, h=h: e.tensor_copy(out=ws[:, :, h * 64:h * 64 + 32], in_=w[:, :, h * 64 + 32:h * 64 + 64]),
                 reads=[w_r], writes=[ws_r])
            S.op("pool", lambda e, h=h: e.tensor_copy(out=ws[:, :, h * 64 + 32:h * 64 + 64], in_=w[:, :, h * 64:h * 64 + 32]),
                 reads=[w_r], writes=[ws_r])
        return ws, ws_r

    def proj_rope(self, pj, pj_r, w, w_r, ws, ws_r, c0, b, bs, b_r, hT, hT_r, out, out_r, blks, C, Sn, tab_r, t1r, t2r):
        S = self.S
        for (s, n) in blks:
            for a, (ww, ww_r) in enumerate(((w, w_r), (ws, ws_r))):
                for k in range(8):
                    S.op("pe", lambda e, a=a, ww=ww, k=k, s=s, n=n: e.matmul(
                        pj[0:64, a, :n], lhsT=ww[:, k, c0:c0 + 64], rhs=hT[:, k, s:s + n], start=(k == 0), stop=(k == 7)),
                        reads=[ww_r, hT_r], writes=[pj_r], pe_acc=True)
            t1, t1_r = t1r.next()
            t2, t2_r = t2r.next()
            S.op("dve", lambda e, t1=t1, s=s, n=n: e.scalar_tensor_tensor(
                out=t1[0:64, :n], in0=pj[0:64, 0, :n], scalar=b, in1=C[:, s:s + n], op0=ALU.add, op1=ALU.mult),
                reads=[pj_r, b_r, tab_r], writes=[t1_r])
            S.op("dve", lambda e, t2=t2, s=s, n=n: e.scalar_tensor_tensor(
                out=t2[0:64, :n], in0=pj[0:64, 1, :n], scalar=bs, in1=Sn[:, s:s + n], op0=ALU.add, op1=ALU.mult),
                reads=[pj_r, b_r, tab_r], writes=[t2_r])
            S.op("pool", lambda e, t1=t1, t2=t2, s=s, n=n: e.tensor_tensor(out=out[0:64, s:s + n], in0=t1[0:64, :n], in1=t2[0:64, :n], op=ALU.add),
                 reads=[t1_r, t2_r], writes=[out_r])

    def load_tables(self, ps):
        S, M = self.S, self.M
        Cd = self.inp("rope_c", [64, T])
        Sd = self.inp("rope_s", [64, T])
        C, C_r = M.sb(ps, [64, T], F32, "ropeC")
        Sn, _ = M.sb(ps, [64, T], F32, "ropeS")
        S.dma("sp", C[:], Cd, writes=[C_r], key=C_r)
        S.dma("sp", Sn[:], Sd, writes=[C_r], key=C_r)
        return C, Sn, C_r

    def mixer2(self, i):
        nc, S, M = self.nc, self.S, self.M
        wqkv = self.inp("swa_w_qkv", [1, D, 1536])
        bqkv = self.inp("swa_b_qkv", [1, 1536])
        wo = self.inp("swa_w_o", [1, D, D])
        bh = self.inp("swa_bh", [64, 44])
        sinks = self.inp("swa_sinks", [1, 16])
        bo = self.inp("swa_bo_fm", [128, 8])
        NEG = -30000.0
        with ExitStack() as pA:
            hT, hT_r = M.sb(pA, [128, 8, T], BF16, "hT")
            with ExitStack() as pD:
                self.adanorm(pD, hT, hT_r, 0)
                S.barrier()
            C, Sn, tab_r = self.load_tables(pA)
            bht, bht_r = M.sb(pA, [64, 44], F32, "bht")
            S.dma("sp", bht[:], bh, writes=[bht_r], key=bht_r)
            skb, skb_r = M.sb(pA, [128, 16], F32, "skb")
            S.dma("sp", skb[:], sinks[0].partition_broadcast(128), writes=[skb_r], key=skb_r)
            bot, bot_r = M.sb(pA, [128, 8], F32, "bot")
            S.dma("sp", bot[:], bo, writes=[bot_r], key=bot_r)
            self._vec_deps = [bot_r]
            gb = self.gate_bias(pA, bot[:])
            mW, mW_r = M.sb(pA, [128, 384], F32, "mW")
            S.op("pool", lambda e: e.memset(mW[:], 0.0), writes=[mW_r])
            S.op("pool", lambda e: e.affine_select(out=mW[:, 0:128], in_=mW[:, 0:128], pattern=[[1, 128]], compare_op=ALU.is_ge,
                                                   fill=NEG, base=0, channel_multiplier=-1), reads=[mW_r], writes=[mW_r])
            S.op("pool", lambda e: e.affine_select(out=mW[:, 256:384], in_=mW[:, 256:384], pattern=[[-1, 128]], compare_op=ALU.is_ge,
                                                   fill=NEG, base=0, channel_multiplier=1), reads=[mW_r], writes=[mW_r])
            kT, kT_r = M.sb(pA, [64, T], BF16, "kT")
            vs, vs_r = M.sb(pA, [128, NT, 64], BF16, "vs")
            qTr = Ring([M.sb(pA, [64, T], BF16, "qT") for _ in range(2)])
            oTg, oTg_r = M.sb(pA, [64, 4, T], BF16, "oTg")
            bvb, bvb_r = M.sb(pA, [128, 64], F32, "bvb")
            t1r = Ring([M.sb(pA, [64, 512], F32, "rp1") for _ in range(2)])
            t2r = Ring([M.sb(pA, [64, 512], F32, "rp2") for _ in range(2)])
            tr = Ring([M.sb(pA, [128, 512], F32, "tr") for _ in range(2)])
            swr = Ring([M.sb(pA, [128, 384], F32, "sw") for _ in range(2)])
            pwr = Ring([M.sb(pA, [128, 640], BF16, "pw") for _ in range(2)])
            pTsr = Ring([M.sb(pA, [128, 5, 128], BF16, "pTs") for _ in range(2)])
            osr = Ring([M.sb(pA, [128, 64], F32, "osb") for _ in range(2)])
            smr = Ring([M.sb(pA, [128, 8], F32, "sm") for _ in range(3)])
            pj, pj_r = M.ps(pA, [128, 2, 512], F32, "pj")
            pscr = Ring([M.ps(pA, [128, 2, 512], F32, "psc") for _ in range(2)])
            pT, pT_r = M.ps(pA, [128, 5, 128], BF16, "pT")
            pso, pso_r = M.ps(pA, [128, 512], F32, "pso")
            for g in range(4):
                if (self.dbg == 7 and g == 1) or (self.dbg == 17 and g == 2) or (self.dbg == 18 and g == 3):
                    return
                with ExitStack() as pG:
                    wq, wq_r = self.load_w(pG, wqkv[0, :, g * 256:(g + 1) * 256], 8, 256, "wq")
                    wk, wk_r = self.load_w(pG, wqkv[0, :, 1024 + g * 64:1024 + (g + 1) * 64], 8, 64, "wk")
                    wv, wv_r = self.load_w(pG, wqkv[0, :, 1280 + g * 64:1280 + (g + 1) * 64], 8, 64, "wv")
                    wqs, wqs_r = self.swap_halves(pG, wq, wq_r, 8, 4, "wqs")
                    wks, wks_r = self.swap_halves(pG, wk, wk_r, 8, 1, "wks")
                    wog, wog_r = self.load_w(pG, wo[0, g * 256:(g + 1) * 256, :], 4, D, "wog", npart=64)
                    S.dma("sp", bvb[:], bqkv[0, 1280 + g * 64:1280 + (g + 1) * 64].partition_broadcast(128),
                          reads=[], writes=[bvb_r], key=bvb_r)
                    self.proj_rope(pj, pj_r, wk, wk_r, wks, wks_r, 0, bht[:, 16 + g:17 + g], bht[:, 24 + 16 + g:25 + 16 + g], bht_r,
                                   hT, hT_r, kT, kT_r, BLKS, C, Sn, tab_r, t1r, t2r)
                    for t in range(NT):
                        for k in range(8):
                            S.op("pe", lambda e, t=t, k=k: e.matmul(pso[:, 0:64], lhsT=hT[:, k, t * 128:(t + 1) * 128], rhs=wv[:, k, :],
                                                                    start=(k == 0), stop=(k == 7)),
                                 reads=[hT_r, wv_r], writes=[pso_r], pe_acc=True)
                        S.op("dve", lambda e, t=t: e.tensor_tensor(out=vs[:, t, :], in0=pso[:, 0:64], in1=bvb[:], op=ALU.add),
                             reads=[pso_r, bvb_r], writes=[vs_r])
                    if self.dbg == 1 or (self.dbg == 11 and g == 1):
                        return
                    for hh in range(4):
                        h = g * 4 + hh
                        qT, qT_r = qTr.next()
                        self.proj_rope(pj, pj_r, wq, wq_r, wqs, wqs_r, hh * 64, bht[:, h:h + 1], bht[:, 24 + h:25 + h], bht_r,
                                       hT, hT_r, qT, qT_r, BLKS, C, Sn, tab_r, t1r, t2r)
                        if self.dbg == 2 or (self.dbg == 12 and g == 1):
                            return
                        for qt in range(NT):
                            if self.dbg == 3 and qt == 1:
                                return
                            if self.dbg == 4 and qt == 3:
                                return
                            if self.dbg == 5 and hh == 1:
                                return
                            psc, psc_r = pscr.next()
                            sm, sm_r = smr.next()
                            pw, pw_r = pwr.next()
                            lat = qt >= 2
                            S.op("pe", lambda e, psc=psc, qT=qT, qt=qt: e.matmul(
                                psc[:, 1, 0:256], lhsT=qT[:, qt * 128:(qt + 1) * 128], rhs=kT[:, 0:256], start=True, stop=True),
                                reads=[qT_r, kT_r], writes=[psc_r], pe_acc=True)
                            if lat:
                                qb = qt - 2
                                lo = max(0, qb - 1)
                                hi = min(15, qb + 1)
                                c0 = (lo - (qb - 1)) * 128
                                c1 = c0 + (hi - lo + 1) * 128
                                ktiles = list(range(lo + 2, hi + 3))
                                S.op("pe", lambda e, psc=psc, qT=qT, qt=qt, lo=lo, hi=hi, c0=c0, c1=c1: e.matmul(
                                    psc[:, 0, c0:c1], lhsT=qT[:, qt * 128:(qt + 1) * 128], rhs=kT[:, 256 + lo * 128:256 + (hi + 1) * 128],
                                    start=True, stop=True), reads=[qT_r, kT_r], writes=[psc_r], pe_acc=True)
                                sw, sw_r = swr.next()
                                S.op("dve", lambda e, sw=sw, psc=psc, c0=c0, c1=c1: e.tensor_tensor(
                                    out=sw[:, c0:c1], in0=psc[:, 0, c0:c1], in1=mW[:, c0:c1], op=ALU.add),
                                    reads=[psc_r, mW_r], writes=[sw_r])
                                S.op("dve", lambda e, sm=sm, sw=sw, c0=c0, c1=c1: e.reduce_max(out=sm[:, 0:1], in_=sw[:, c0:c1], axis=AX.X),
                                     reads=[sw_r], writes=[sm_r])
                            else:
                                ktiles = []
                            S.op("dve", lambda e, sm=sm, psc=psc: e.reduce_max(out=sm[:, 1:2], in_=psc[:, 1, 0:256], axis=AX.X),
                                 reads=[psc_r], writes=[sm_r])
                            if lat:
                                S.op("dve", lambda e, sm=sm: e.tensor_tensor(out=sm[:, 1:2], in0=sm[:, 0:1], in1=sm[:, 1:2], op=ALU.max),
                                     reads=[sm_r], writes=[sm_r])
                            S.op("dve", lambda e, sm=sm, h=h: e.scalar_tensor_tensor(out=sm[:, 2:3], in0=sm[:, 1:2], scalar=0.125, in1=skb[:, h:h + 1],
                                                                                   op0=ALU.mult, op1=ALU.max),
                                 reads=[sm_r, skb_r], writes=[sm_r])
                            S.op("dve", lambda e, sm=sm: e.tensor_scalar(out=sm[:, 3:4], in0=sm[:, 2:3], scalar1=-1.0, scalar2=None, op0=ALU.mult),
                                 reads=[sm_r], writes=[sm_r])
                            S.op("pool", lambda e, sm=sm: e.memset(sm[:, 4:7], 0.0), reads=[sm_r], writes=[sm_r])
                            if lat:
                                S.op("act", lambda e, pw=pw, sw=sw, sm=sm, c0=c0, c1=c1: e.activation(
                                    out=pw[:, c0:c1], in_=sw[:, c0:c1], func=AF.Exp, scale=0.125, bias=sm[:, 3:4], accum_out=sm[:, 4:5]),
                                    reads=[sw_r, sm_r], writes=[pw_r, sm_r])
                            S.op("act", lambda e, pw=pw, psc=psc, sm=sm: e.activation(
                                out=pw[:, 384:640], in_=psc[:, 1, 0:256], func=AF.Exp, scale=0.125, bias=sm[:, 3:4], accum_out=sm[:, 5:6]),
                                reads=[psc_r, sm_r], writes=[pw_r, sm_r])
                            S.op("act", lambda e, sm=sm, h=h: e.activation(out=sm[:, 6:7], in_=skb[:, h:h + 1], func=AF.Exp, scale=1.0, bias=sm[:, 3:4]),
                                 reads=[skb_r, sm_r], writes=[sm_r])
                            S.op("dve", lambda e, sm=sm: e.tensor_tensor(out=sm[:, 4:5], in0=sm[:, 4:5], in1=sm[:, 5:6], op=ALU.add),
                                 reads=[sm_r], writes=[sm_r])
                            S.op("dve", lambda e, sm=sm: e.tensor_tensor(out=sm[:, 4:5], in0=sm[:, 4:5], in1=sm[:, 6:7], op=ALU.add),
                                 reads=[sm_r], writes=[sm_r])
                            S.op("dve", lambda e, sm=sm: e.reciprocal(out=sm[:, 7:8], in_=sm[:, 4:5]), reads=[sm_r], writes=[sm_r])
                            srcs = []
                            if lat:
                                for j, kt_ in enumerate(ktiles):
                                    srcs.append((c0 + j * 128, kt_))
                            srcs += [(384, 0), (512, 1)]
                            for j, (col, kt_) in enumerate(srcs):
                                S.op("pe", lambda e, j=j, col=col, pw=pw: e.transpose(out=pT[:, j, :], in_=pw[:, col:col + 128], identity=self.identb[:]),
                                     reads=[pw_r, self.identb_r], writes=[pT_r], pe_acc=True)
                            pTs, pTs_r = pTsr.next()
                            nj = len(srcs)
                            S.op("act", lambda e, pTs=pTs, nj=nj: e.activation(out=pTs[:, 0:nj, :], in_=pT[:, 0:nj, :], func=AF.Copy),
                                 reads=[pT_r], writes=[pTs_r])
                            for j, (col, kt_) in enumerate(srcs):
                                S.op("pe", lambda e, j=j, kt_=kt_, pTs=pTs, nj=nj: e.matmul(
                                    pso[:, 64:128], lhsT=pTs[:, j, :], rhs=vs[:, kt_, :], start=(j == 0), stop=(j == nj - 1)),
                                    reads=[pTs_r, vs_r], writes=[pso_r], pe_acc=True)
                            osb, osb_r = osr.next()
                            S.op("dve", lambda e, osb=osb, sm=sm: e.tensor_scalar(out=osb[:], in0=pso[:, 64:128], scalar1=sm[:, 7:8], scalar2=None, op0=ALU.mult),
                                 reads=[pso_r, sm_r], writes=[osb_r])
                            S.op("pe", lambda e, osb=osb: e.transpose(out=pso[0:64, 128:256], in_=osb[:], identity=self.identf[:]),
                                 reads=[osb_r, self.identf_r], writes=[pso_r], pe_acc=True)
                            S.op("act", lambda e, hh=hh, qt=qt: e.activation(out=oTg[:, hh, qt * 128:(qt + 1) * 128], in_=pso[0:64, 128:256], func=AF.Copy),
                                 reads=[pso_r], writes=[oTg_r])
                    if self.dbg == 6 or (self.dbg == 16 and g == 1):
                        return
                    for (s, n) in BLKS:
                        for oc in range(8):
                            for hh in range(4):
                                S.op("pe", lambda e, hh=hh, oc=oc, s=s, n=n: e.matmul(
                                    pj[:, 0, :n], lhsT=wog[:, hh, oc * 128:(oc + 1) * 128], rhs=oTg[:, hh, s:s + n], start=(hh == 0), stop=(hh == 3)),
                                    reads=[wog_r, oTg_r], writes=[pj_r], pe_acc=True)
                            if g == 0:
                                self.resid(pj[:, 0, :], pj_r, n, oc, s, gb=gb, tring=tr)
                            else:
                                self.resid(pj[:, 0, :], pj_r, n, oc, s)
                    S.barrier()
                    S.release([wq_r, wk_r, wv_r, wog_r])

    def mixer3(self, i):
        nc, S, M = self.nc, self.S, self.M
        wqkv = self.inp("diff_w_qkv", [1, D, 3 * D])
        wo = self.inp("diff_w_o", [1, D, D])
        lvec = self.inp("diff_lam", [4, 64])
        sg = self.inp("diff_subln_g", [128, 1])
        lambda_init = 0.8 - 0.6 * math.exp(-0.3 * i)
        LBLK = BLKS[1:]
        with ExitStack() as pA:
            hT, hT_r = M.sb(pA, [128, 8, T], BF16, "hT")
            with ExitStack() as pD:
                self.adanorm(pD, hT, hT_r, 0)
                S.barrier()
            C, Sn, tab_r = self.load_tables(pA)
            zb, zb_r = M.sb(pA, [64, 1], F32, "zb")
            S.op("pool", lambda e: e.memset(zb[:], 0.0), writes=[zb_r])
            sgt, sgt_r = M.sb(pA, [128, 1], F32, "sgt")
            S.dma("sp", sgt[:], sg, writes=[sgt_r], key=sgt_r)
            lv, lv_r = M.sb(pA, [128, 4, 64], F32, "lv")
            for a in range(4):
                S.dma("sp", lv[:, a, :], lvec[a].partition_broadcast(128), writes=[lv_r], key=lv_r)
            lam, lam_r = M.sb(pA, [128, 4], F32, "lam")
            lp, lp_r = M.sb(pA, [128, 2, 64], F32, "lp")
            for a in range(2):
                S.op("dve", lambda e, a=a: e.tensor_tensor(out=lp[:, a, :], in0=lv[:, 2 * a, :], in1=lv[:, 2 * a + 1, :], op=ALU.mult),
                     reads=[lv_r], writes=[lp_r])
                S.op("dve", lambda e, a=a: e.reduce_sum(out=lam[:, a:a + 1], in_=lp[:, a, :], axis=AX.X), reads=[lp_r], writes=[lam_r])
            S.op("act", lambda e: e.activation(out=lam[:, 0:2], in_=lam[:, 0:2], func=AF.Exp), reads=[lam_r], writes=[lam_r])
            S.op("dve", lambda e: e.tensor_tensor(out=lam[:, 2:3], in0=lam[:, 0:1], in1=lam[:, 1:2], op=ALU.subtract), reads=[lam_r], writes=[lam_r])
            S.op("dve", lambda e: e.tensor_scalar(out=lam[:, 3:4], in0=lam[:, 2:3], scalar1=float(lambda_init), scalar2=None, op0=ALU.add),
                 reads=[lam_r], writes=[lam_r])
            kTs = [M.sb(pA, [64, T], BF16, "kT%d" % t) for t in range(2)]
            qTs = [M.sb(pA, [64, T], BF16, "qT%d" % t) for t in range(2)]
            vs, vs_r = M.sb(pA, [128, NT, 128], BF16, "vs")
            oT, oT_r = M.sb(pA, [128, NLAT], BF16, "oT")
            pbr = Ring([M.sb(pA, [128, T], BF16, "pb") for _ in range(2)])
            pTs, pTs_r = M.sb(pA, [128, NT, 128], BF16, "pTs")
            t1r = Ring([M.sb(pA, [64, 512], F32, "rp1") for _ in range(2)])
            t2r = Ring([M.sb(pA, [64, 512], F32, "rp2") for _ in range(2)])
            smr = Ring([M.sb(pA, [128, 16], F32, "sm") for _ in range(4)])
            o1r = Ring([M.sb(pA, [128, 128], F32, "o1") for _ in range(2)])
            o2r = Ring([M.sb(pA, [128, 128], F32, "o2") for _ in range(2)])
            jkr = Ring([M.sb(pA, [128, 128], F32, "jk") for _ in range(2)])
            psc, psc_r = M.ps(pA, [128, 5, 512], F32, "psc")
            pbank = [Res("pscb%d" % j) for j in range(5)]
            pT, pT_r = M.ps(pA, [128, 6, 128], BF16, "pT")
            po, po_r = M.ps(pA, [128, 2, 128], F32, "po")
            pout, pout_r = M.ps(pA, [128, 512], F32, "pout")
            KB = [(j * 512, min(512, T - j * 512)) for j in range(5)]
            for c in range(8):
                with ExitStack() as pG:
                    wq, wq_r = self.load_w(pG, wqkv[0, :, c * 128:(c + 1) * 128], 8, 128, "wq")
                    wk, wk_r = self.load_w(pG, wqkv[0, :, D + c * 128:D + (c + 1) * 128], 8, 128, "wk")
                    wv, wv_r = self.load_w(pG, wqkv[0, :, 2 * D + c * 128:2 * D + (c + 1) * 128], 8, 128, "wv")
                    woc, woc_r = self.load_w(pG, wo[0, c * 128:(c + 1) * 128, :], 1, D, "woc")
                    wqs, wqs_r = self.swap_halves(pG, wq, wq_r, 8, 2, "wqs")
                    wks, wks_r = self.swap_halves(pG, wk, wk_r, 8, 2, "wks")
                    for t in range(2):
                        self.proj_rope(psc, psc_r, wk, wk_r, wks, wks_r, t * 64, zb[:, 0:1], zb[:, 0:1], zb_r,
                                       hT, hT_r, kTs[t][0], kTs[t][1], BLKS, C, Sn, tab_r, t1r, t2r)
                        self.proj_rope(psc, psc_r, wq, wq_r, wqs, wqs_r, t * 64, zb[:, 0:1], zb[:, 0:1], zb_r,
                                       hT, hT_r, qTs[t][0], qTs[t][1], LBLK, C, Sn, tab_r, t1r, t2r)
                    for tt in range(NT):
                        for k in range(8):
                            S.op("pe", lambda e, tt=tt, k=k: e.matmul(pout[:, 0:128], lhsT=hT[:, k, tt * 128:(tt + 1) * 128], rhs=wv[:, k, :],
                                                                      start=(k == 0), stop=(k == 7)),
                                 reads=[hT_r, wv_r], writes=[pout_r], pe_acc=True)
                        S.op("act", lambda e, tt=tt: e.activation(out=vs[:, tt, :], in_=pout[:, 0:128], func=AF.Copy),
                             reads=[pout_r], writes=[vs_r])
                    S.barrier()
                    its = [(qb, t) for qb in range(16) for t in range(2)]
                    cx = {}

                    def A_pe(n_):
                        qb, t = its[n_]
                        q0 = NCTX + qb * 128
                        qT, qT_r = qTs[t]
                        kT, kT_r = kTs[t]
                        sm, sm_r = smr.next()
                        cx[n_] = dict(sm=sm, sm_r=sm_r)
                        for j, (k0, kn) in enumerate(KB):
                            S.op("pe", lambda e, j=j, k0=k0, kn=kn: e.matmul(
                                psc[:, j, :kn], lhsT=qT[:, q0:q0 + 128], rhs=kT[:, k0:k0 + kn], start=True, stop=True),
                                reads=[qT_r, kT_r, psc_r], writes=[pbank[j]], pe_acc=True)

                    def A_rest(n_):
                        sm, sm_r = cx[n_]["sm"], cx[n_]["sm_r"]
                        for j, (k0, kn) in enumerate(KB):
                            S.op("dve", lambda e, j=j, kn=kn: e.reduce_max(out=sm[:, j:j + 1], in_=psc[:, j, :kn], axis=AX.X),
                                 reads=[pbank[j]], writes=[sm_r])
                        S.op("dve", lambda e: e.reduce_max(out=sm[:, 5:6], in_=sm[:, 0:5], axis=AX.X), reads=[sm_r], writes=[sm_r])
                        S.op("dve", lambda e: e.tensor_scalar(out=sm[:, 6:7], in0=sm[:, 5:6], scalar1=-0.125, scalar2=None, op0=ALU.mult),
                             reads=[sm_r], writes=[sm_r])
                        S.op("pool", lambda e: e.memset(sm[:, 8:13], 0.0), reads=[sm_r], writes=[sm_r])
                        pb, pb_r = pbr.next()
                        cx[n_]["pb"] = pb
                        cx[n_]["pb_r"] = pb_r
                        for j, (k0, kn) in enumerate(KB):
                            S.op("act", lambda e, j=j, k0=k0, kn=kn: e.activation(
                                out=pb[:, k0:k0 + kn], in_=psc[:, j, :kn], func=AF.Exp, scale=0.125, bias=sm[:, 6:7], accum_out=sm[:, 8 + j:9 + j]),
                                reads=[pbank[j], sm_r], writes=[pb_r, sm_r])
                        S.op("dve", lambda e: e.reduce_sum(out=sm[:, 13:14], in_=sm[:, 8:13], axis=AX.X), reads=[sm_r], writes=[sm_r])
                        S.op("dve", lambda e: e.reciprocal(out=sm[:, 14:15], in_=sm[:, 13:14]), reads=[sm_r], writes=[sm_r])

                    def B(n_):
                        qb, t = its[n_]
                        pb, pb_r = cx[n_]["pb"], cx[n_]["pb_r"]
                        for b3 in range(3):
                            for jj in range(6):
                                j = b3 * 6 + jj
                                S.op("pe", lambda e, j=j, jj=jj: e.transpose(out=pT[:, jj, :], in_=pb[:, j * 128:(j + 1) * 128], identity=self.identb[:]),
                                     reads=[pb_r, self.identb_r], writes=[pT_r], pe_acc=True)
                            S.op("dve", lambda e, b3=b3: e.tensor_copy(out=pTs[:, b3 * 6:(b3 + 1) * 6, :], in_=pT[:]),
                                 reads=[pT_r], writes=[pTs_r])
                        for j in range(NT):
                            S.op("pe", lambda e, j=j: e.matmul(po[:, t, :], lhsT=pTs[:, j, :], rhs=vs[:, j, :], start=(j == 0), stop=(j == NT - 1)),
                                 reads=[pTs_r, vs_r], writes=[po_r], pe_acc=True)
                        if t == 0:
                            return
                        sm0, sm0_r = cx[n_ - 1]["sm"], cx[n_ - 1]["sm_r"]
                        sm1, sm1_r = cx[n_]["sm"], cx[n_]["sm_r"]
                        del cx[n_ - 1]
                        del cx[n_]
                        o1, o1_r = o1r.next()
                        o2, o2_r = o2r.next()
                        jk, jk_r = jkr.next()
                        S.op("dve", lambda e: e.tensor_tensor(out=sm1[:, 15:16], in0=sm1[:, 14:15], in1=lam[:, 3:4], op=ALU.mult),
                             reads=[sm1_r, lam_r], writes=[sm1_r])
                        S.op("dve", lambda e: e.tensor_scalar(out=o1[:], in0=po[:, 1, :], scalar1=sm1[:, 15:16], scalar2=None, op0=ALU.mult),
                             reads=[po_r, sm1_r], writes=[o1_r])
                        S.op("dve", lambda e: e.scalar_tensor_tensor(out=o2[:], in0=po[:, 0, :], scalar=sm0[:, 14:15], in1=o1[:],
                                                                     op0=ALU.mult, op1=ALU.subtract),
                             reads=[po_r, sm0_r, o1_r], writes=[o2_r])
                        S.op("pool", lambda e: e.memset(sm0[:, 7:8], 0.0), reads=[sm0_r], writes=[sm0_r])
                        S.op("act", lambda e: e.activation(out=jk[:], in_=o2[:], func=AF.Square, accum_out=sm0[:, 7:8]),
                             reads=[o2_r, sm0_r], writes=[jk_r, sm0_r])
                        S.op("act", lambda e: e.activation(out=sm0[:, 7:8], in_=sm0[:, 7:8], func=AF.Sqrt, scale=1.0 / 128, bias=self.epsb[:, 0:1]),
                             reads=[sm0_r, self.epsb_r], writes=[sm0_r])
                        S.op("dve", lambda e: e.reciprocal(out=sm0[:, 7:8], in_=sm0[:, 7:8]), reads=[sm0_r], writes=[sm0_r])
                        S.op("dve", lambda e: e.tensor_scalar(out=o2[:], in0=o2[:], scalar1=sm0[:, 7:8], scalar2=float(1.0 - lambda_init),
                                                              op0=ALU.mult, op1=ALU.mult),
                             reads=[o2_r, sm0_r], writes=[o2_r])
                        S.op("pe", lambda e: e.transpose(out=pout[:, 128:256], in_=o2[:], identity=self.identf[:]),
                             reads=[o2_r, self.identf_r], writes=[pout_r], pe_acc=True)
                        S.op("act", lambda e: e.activation(out=oT[:, qb * 128:(qb + 1) * 128], in_=pout[:, 128:256], func=AF.Identity,
                                                           scale=sgt[:, 0:1]),
                             reads=[pout_r, sgt_r], writes=[oT_r])

                    A_pe(0)
                    A_rest(0)
                    for n_ in range(1, len(its)):
                        A_pe(n_)
                        B(n_ - 1)
                        A_rest(n_)
                    B(len(its) - 1)
                    for (s, n) in LBLK:
                        for oc in range(8):
                            S.op("pe", lambda e, oc=oc, s=s, n=n: e.matmul(
                                pout[:, :n], lhsT=woc[:, 0, oc * 128:(oc + 1) * 128], rhs=oT[:, s - NCTX:s - NCTX + n], start=True, stop=True),
                                reads=[woc_r, oT_r], writes=[pout_r], pe_acc=True)
                            self.resid(pout, pout_r, n, oc, s)
                    S.barrier()
                    S.release([wq_r, wk_r, wv_r, woc_r])

    def mixer1(self, i):
        nc, S, M = self.nc, self.S, self.M
        w_in = self.inp("ssm_w_in", [1, D, 5184])
        w_out = self.inp("ssm_w_out", [1, 2048, D])
        svec = self.inp("ssm_vec", [128, 160])
        a_log = self.inp("ssm_a_log", [1, 64])
        dt_bias = self.inp("ssm_dt_bias", [1, 64])
        d_skip = self.inp("ssm_d", [1, 32])
        dr = lambda name, shape: (nc.dram_tensor(name, list(shape), BF16, kind="Internal").ap(), Res(name))
        XS, XS_r = dr("scr_xs", [NT, 128, 2048])
        BTd, BTd_r = dr("scr_bt", [NT, 128, 4, 128])
        CTd, CTd_r = dr("scr_ct", [NT, 128, 4, 128])
        BMd, BMd_r = dr("scr_bm", [NT, 128, 512])
        ZS, ZS_r = dr("scr_zs", [NT, 128, 2048])
        HB, HB_r = dr("scr_hb", [NT, 128, 2048])
        PW = 2312
        OW = 2308

        def pcol(s):
            return 2 if s == 0 else s + 6
        with ExitStack() as pA:
            sv, sv_r = M.sb(pA, [128, 160], F32, "sv")
            S.dma("sp", sv[:], svec, writes=[sv_r], key=sv_r)
            msk, msk_r = M.sb(pA, [128, 4, 128], F32, "msk")
            S.op("pool", lambda e: e.memset(msk[:], 1.0), writes=[msk_r])
            for a, (pat, cm, op) in enumerate((([[1, 128]], -1, ALU.is_ge), ([[-1, 128]], 1, ALU.is_ge),
                                              ([[-1, 128]], 1, ALU.is_gt), ([[1, 128]], -1, ALU.is_gt))):
                S.op("pool", lambda e, a=a, pat=pat, cm=cm, op=op: e.affine_select(
                    out=msk[:, a, :], in_=msk[:, a, :], pattern=pat, compare_op=op, fill=0.0, base=0, channel_multiplier=cm),
                    reads=[msk_r], writes=[msk_r])
            triF, triB, mltF, mltB = (msk[:, a, :] for a in range(4))
            dt, dt_r = M.sb(pA, [128, NT, 64], F32, "dt")
            loga, loga_r = M.sb(pA, [128, NT, 64], F32, "loga")
            expA, expA_r = M.sb(pA, [128, NT, 64], F32, "expA")
            dec, dec_r = M.sb(pA, [128, NT, 64], F32, "dec")
            wst, wst_r = M.sb(pA, [128, NT, 64], F32, "wst")
            abc, abc_r = M.sb(pA, [128, 64], F32, "abc")
            dtb, dtb_r = M.sb(pA, [128, 64], F32, "dtb")
            dsk, dsk_r = M.sb(pA, [128, 32], F32, "dsk")
            S.dma("sp", abc[:], a_log[0].partition_broadcast(128), writes=[abc_r], key=abc_r)
            S.dma("sp", dtb[:], dt_bias[0].partition_broadcast(128), writes=[dtb_r], key=dtb_r)
            S.dma("sp", dsk[:], d_skip[0].partition_broadcast(128), writes=[dsk_r], key=dsk_r)
            S.op("act", lambda e: e.activation(out=abc[:], in_=abc[:], func=AF.Exp), reads=[abc_r], writes=[abc_r])
            S.op("dve", lambda e: e.tensor_scalar(out=abc[:], in0=abc[:], scalar1=-1.0, scalar2=None, op0=ALU.mult), reads=[abc_r], writes=[abc_r])
            with ExitStack() as pB:
                hT, hT_r = M.sb(pB, [128, 8, T], BF16, "hT")
                with ExitStack() as pD:
                    self.adanorm(pD, hT, hT_r, 0)
                    S.barrier()
                with ExitStack() as pC:
                    wdt, wdt_r = self.load_w(pC, w_in[0, :, 5120:5184], 8, 64, "wdt")
                    pdt = Ring([M.ps(pC, [128, 64], F32, "pdt") for _ in range(2)])
                    pcs = Ring([M.ps(pC, [128, 2, 64], F32, "pcs") for _ in range(2)])
                    tmr = Ring([[M.sb(pC, [128, 64], F32, "sp%d" % j) for j in range(4)] for _ in range(2)])
                    for t in range(NT):
                        p_, p_r = pdt.next()
                        for k in range(8):
                            S.op("pe", lambda e, p_=p_, t=t, k=k: e.matmul(p_[:], lhsT=hT[:, k, t * 128:(t + 1) * 128], rhs=wdt[:, k, :],
                                                                          start=(k == 0), stop=(k == 7)),
                                 reads=[hT_r, wdt_r], writes=[p_r], pe_acc=True)
                        (x_, x_r), (ax, ax_r), (ex, ex_r), (rl, rl_r) = tmr.next()
                        S.op("dve", lambda e, x_=x_, p_=p_: e.tensor_tensor(out=x_[:], in0=p_[:], in1=dtb[:], op=ALU.add),
                             reads=[p_r, dtb_r], writes=[x_r])
                        S.op("act", lambda e, ax=ax, x_=x_: e.activation(out=ax[:], in_=x_[:], func=AF.Abs),
                             reads=[x_r], writes=[ax_r])
                        S.op("act", lambda e, ex=ex, ax=ax: e.activation(out=ex[:], in_=ax[:], func=AF.Exp, scale=-1.0), reads=[ax_r], writes=[ex_r])
                        S.op("act", lambda e, ex=ex: e.activation(out=ex[:], in_=ex[:], func=AF.Ln, bias=self.onesf[:, 0:1], scale=1.0),
                             reads=[ex_r, self.onesf_r], writes=[ex_r])
                        S.op("dve", lambda e, rl=rl, x_=x_: e.tensor_scalar(out=rl[:], in0=x_[:], scalar1=0.0, scalar2=None, op0=ALU.max),
                             reads=[x_r], writes=[rl_r])
                        S.op("dve", lambda e, rl=rl, ex=ex, t=t: e.tensor_tensor(out=dt[:, t, :], in0=rl[:], in1=ex[:], op=ALU.add),
                             reads=[rl_r, ex_r], writes=[dt_r])
                        S.op("dve", lambda e, t=t: e.tensor_tensor(out=loga[:, t, :], in0=dt[:, t, :], in1=abc[:], op=ALU.mult),
                             reads=[dt_r, abc_r], writes=[loga_r])
                        c_, c_r = pcs.next()
                        S.op("pe", lambda e, c_=c_, t=t: e.matmul(c_[:, 0, 0:32], lhsT=triF, rhs=loga[:, t, 0:32], start=True, stop=True),
                             reads=[msk_r, loga_r], writes=[c_r], pe_acc=True)
                        S.op("pe", lambda e, c_=c_, t=t: e.matmul(c_[:, 0, 32:64], lhsT=triB, rhs=loga[:, t, 32:64], start=True, stop=True),
                             reads=[msk_r, loga_r], writes=[c_r], pe_acc=True)
                        S.op("pe", lambda e, c_=c_, t=t: e.matmul(c_[:, 1, :], lhsT=self.onesf[:], rhs=loga[:, t, :], start=True, stop=True),
                             reads=[self.onesf_r, loga_r], writes=[c_r], pe_acc=True)
                        S.op("act", lambda e, c_=c_, t=t: e.activation(out=expA[:, t, :], in_=c_[:, 0, :], func=AF.Exp), reads=[c_r], writes=[expA_r])
                        S.op("act", lambda e, c_=c_, t=t: e.activation(out=dec[:, t, :], in_=c_[:, 1, :], func=AF.Exp), reads=[c_r], writes=[dec_r])
                        S.op("act", lambda e, c_=c_, ax=ax: e.activation(out=ax[:], in_=c_[:, 0, :], func=AF.Copy), reads=[c_r, ax_r], writes=[ax_r])
                        S.op("dve", lambda e, c_=c_, ax=ax: e.tensor_tensor(out=ax[:], in0=c_[:, 1, :], in1=ax[:], op=ALU.subtract),
                             reads=[c_r, ax_r], writes=[ax_r])
                        S.op("act", lambda e, ax=ax: e.activation(out=ax[:], in_=ax[:], func=AF.Exp), reads=[ax_r], writes=[ax_r])
                        S.op("dve", lambda e, ax=ax, t=t: e.tensor_tensor(out=wst[:, t, :], in0=ax[:], in1=dt[:, t, :], op=ALU.mult),
                             reads=[ax_r, dt_r], writes=[wst_r])
                    S.barrier()
                    S.release([wdt_r])
                with ExitStack() as pC:
                    wz, wz_r = self.load_w(pC, w_in[0, :, 0:2048], 8, 2048, "wz")
                    pz, pz_r = M.ps(pC, [128, 4, 512], F32, "pz")
                    zr = Ring([M.sb(pC, [128, 2048], BF16, "zt") for _ in range(2)])
                    for t in range(NT):
                        for nb in range(4):
                            for k in range(8):
                                S.op("pe", lambda e, t=t, nb=nb, k=k: e.matmul(pz[:, nb, :], lhsT=hT[:, k, t * 128:(t + 1) * 128],
                                                                               rhs=wz[:, k, nb * 512:(nb + 1) * 512], start=(k == 0), stop=(k == 7)),
                                     reads=[hT_r, wz_r], writes=[pz_r], pe_acc=True)
                        z_, z_r = zr.next()
                        S.op("act", lambda e, z_=z_: e.activation(out=z_[:].rearrange("p (a b) -> p a b", b=512), in_=pz[:], func=AF.Silu),
                             reads=[pz_r], writes=[z_r])
                        S.dma("sp", ZS[t], z_[:], reads=[z_r], writes=[ZS_r], key=z_r)
                    S.barrier()
                    S.release([wz_r] + [r for _, r in zr.items])
                with ExitStack() as pC:
                    wpr = Ring([M.sb(pC, [128, 8, 128], BF16, "wxp") for _ in range(3)])
                    upr = Ring([M.sb(pC, [128, PW], F32, "upad") for _ in range(1)])
                    acr = Ring([M.sb(pC, [128, OW], F32, "cacc") for _ in range(1)])
                    scr = Ring([M.sb(pC, [128, T], BF16, "scc") for _ in range(2)])
                    xh, xh_r = M.sb(pC, [128, NT, 512], BF16, "xhalf")
                    btm, btm_r = M.sb(pC, [128, NT, 128], BF16, "btm")
                    pp = Ring([M.ps(pC, [128, 512], F32, "pxp") for _ in range(2)])
                    ptr = Ring([M.ps(pC, [128, 6, 128], BF16, "ptx") for _ in range(2)])
                    for u, u_r in upr.items:
                        S.op("pool", lambda e, u=u: e.memset(u[:], 0.0), writes=[u_r])
                    for cc in range(24):
                        wt, wt_r = wpr.next()
                        S.dma("pool", wt[:], w_in[0, :, 2048 + cc * 128:2048 + (cc + 1) * 128].rearrange("(k p) n -> p k n", p=128),
                              writes=[wt_r], key=wt_r)
                        u, u_r = upr.next()
                        for (s, n) in BLKS:
                            p_, p_r = pp.next()
                            for k in range(8):
                                S.op("pe", lambda e, p_=p_, wt=wt, k=k, s=s, n=n: e.matmul(p_[:, :n], lhsT=wt[:, k, :], rhs=hT[:, k, s:s + n],
                                                                                          start=(k == 0), stop=(k == 7)),
                                     reads=[wt_r, hT_r], writes=[p_r], pe_acc=True)
                            S.op("act", lambda e, p_=p_, u=u, s=s, n=n: e.activation(out=u[:, pcol(s):pcol(s) + n], in_=p_[:, :n], func=AF.Copy),
                                 reads=[p_r], writes=[u_r])
                        ac, ac_r = acr.next()
                        eng = "dve"
                        S.op(eng, lambda e, ac=ac, u=u, cc=cc: e.tensor_scalar(
                            out=ac[:], in0=u[:, 0:OW], scalar1=sv[:, cc * 5:cc * 5 + 1], scalar2=sv[:, 120 + cc:121 + cc], op0=ALU.mult, op1=ALU.add),
                            reads=[u_r, sv_r], writes=[ac_r])
                        for w in range(1, 5):
                            S.op(eng, lambda e, ac=ac, u=u, cc=cc, w=w: e.scalar_tensor_tensor(
                                out=ac[:], in0=u[:, w:w + OW], scalar=sv[:, cc * 5 + w:cc * 5 + w + 1], in1=ac[:], op0=ALU.mult, op1=ALU.add),
                                reads=[u_r, sv_r, ac_r], writes=[ac_r])
                        sc, sc_r = scr.next()
                        S.op("act", lambda e, sc=sc, ac=ac: e.activation(out=sc[:, 0:NCTX], in_=ac[:, 0:NCTX], func=AF.Silu), reads=[ac_r], writes=[sc_r])
                        S.op("act", lambda e, sc=sc, ac=ac: e.activation(out=sc[:, NCTX:T], in_=ac[:, 260:260 + NLAT], func=AF.Silu), reads=[ac_r], writes=[sc_r])
                        if cc < 20:
                            for b3 in range(3):
                                pt, pt_r = ptr.next()
                                for jj in range(6):
                                    t = b3 * 6 + jj
                                    S.op("pe", lambda e, pt=pt, jj=jj, t=t, sc=sc: e.transpose(out=pt[:, jj, :], in_=sc[:, t * 128:(t + 1) * 128], identity=self.identb[:]),
                                         reads=[sc_r, self.identb_r], writes=[pt_r], pe_acc=True)
                                if cc < 16:
                                    c8 = cc % 4
                                    S.op("dve", lambda e, pt=pt, b3=b3, c8=c8: e.tensor_copy(out=xh[:, b3 * 6:(b3 + 1) * 6, c8 * 128:(c8 + 1) * 128], in_=pt[:]),
                                         reads=[pt_r], writes=[xh_r])
                                else:
                                    S.op("dve", lambda e, pt=pt, b3=b3: e.tensor_copy(out=btm[:, b3 * 6:(b3 + 1) * 6, :], in_=pt[:]),
                                         reads=[pt_r], writes=[btm_r])
                        if cc < 16 and cc % 4 == 3:
                            half = cc // 4
                            S.dma("sp", XS[:, :, half * 512:(half + 1) * 512].rearrange("t l f -> l t f"), xh[:],
                                  reads=[xh_r], writes=[XS_r], key=xh_r)
                        if 16 <= cc < 20:
                            g = cc - 16
                            S.dma("sp", BTd[:, :, g, :].rearrange("t n l -> n t l"), sc[:].rearrange("p (t l) -> p t l", l=128),
                                  reads=[sc_r], writes=[BTd_r], key=sc_r)
                            S.dma("sp", BMd[:, :, g * 128:(g + 1) * 128].rearrange("t l n -> l t n"), btm[:],
                                  reads=[btm_r], writes=[BMd_r], key=btm_r)
                        if cc >= 20:
                            g = cc - 20
                            S.dma("sp", CTd[:, :, g, :].rearrange("t n l -> n t l"), sc[:].rearrange("p (t l) -> p t l", l=128),
                                  reads=[sc_r], writes=[CTd_r], key=sc_r)
                    S.barrier()
                    S.release([r for _, r in wpr.items] + [r for _, r in scr.items] + [xh_r, btm_r])
            order_b = [1, 0] + list(range(NT - 1, 1, -1))
            Hf, Hf_r = M.sb(pA, [128, 4, 512], F32, "Hst")
            Hb16, Hb16_r = M.sb(pA, [128, 4, 512], BF16, "Hst16")
            pst, pst_r = M.ps(pA, [128, 512], F32, "pst")
            xsr = Ring([M.sb(pA, [128, 2048], BF16, "xs") for _ in range(2)])
            bmr = Ring([M.sb(pA, [128, 512], BF16, "bm") for _ in range(2)])
            xwr = Ring([M.sb(pA, [128, 2048], BF16, "xw") for _ in range(1)])
            hbo = Ring([M.sb(pA, [128, 2048], BF16, "hbo") for _ in range(2)])

            def bc(ap2d):
                return ap2d.unsqueeze(2).to_broadcast([128, 8, 64])

            def v3(ap2d):
                return ap2d.rearrange("p (h d) -> p h d", d=64)

            def state_update(t, xs, xs_r, bm, bm_r, d0):
                xw, xw_r = xwr.next()
                for g in range(4):
                    S.op("pool" if g % 2 else "dve", lambda e, g=g, xw=xw, xs=xs, t=t: e.tensor_tensor(
                        out=v3(xw[:, g * 512:(g + 1) * 512]), in0=v3(xs[:, g * 512:(g + 1) * 512]),
                        in1=bc(wst[:, t, d0 + g * 8:d0 + (g + 1) * 8]), op=ALU.mult),
                        reads=[xs_r, wst_r], writes=[xw_r])
                for g in range(4):
                    S.op("pe", lambda e, g=g, bm=bm, xw=xw: e.matmul(pst[:], lhsT=bm[:, g * 128:(g + 1) * 128], rhs=xw[:, g * 512:(g + 1) * 512],
                                                                     start=True, stop=True),
                         reads=[bm_r, xw_r], writes=[pst_r], pe_acc=True)
                    S.op("dve", lambda e, g=g, t=t: e.tensor_tensor(out=v3(Hf[:, g, :]), in0=v3(Hf[:, g, :]),
                                                                    in1=bc(dec[:, t, d0 + g * 8:d0 + (g + 1) * 8]), op=ALU.mult),
                         reads=[Hf_r, dec_r], writes=[Hf_r])
                    S.op("dve", lambda e, g=g: e.tensor_tensor(out=Hf[:, g, :], in0=Hf[:, g, :], in1=pst[:], op=ALU.add),
                         reads=[Hf_r, pst_r], writes=[Hf_r])
            S.op("pool", lambda e: e.memset(Hf[:], 0.0), writes=[Hf_r])
            for t in order_b:
                xs, xs_r = xsr.next()
                bm, bm_r = bmr.next()
                S.dma("sp", xs[:], XS[t], reads=[XS_r], writes=[xs_r], key=xs_r)
                S.dma("sp", bm[:], BMd[t], reads=[BMd_r], writes=[bm_r], key=bm_r)
                ho, ho_r = hbo.next()
                S.op("act", lambda e, ho=ho: e.activation(out=ho[:].rearrange("p (g f) -> p g f", f=512), in_=Hf[:], func=AF.Copy),
                     reads=[Hf_r], writes=[ho_r])
                S.dma("sp", HB[t], ho[:], reads=[ho_r], writes=[HB_r], key=ho_r)
                state_update(t, xs, xs_r, bm, bm_r, 32)
            S.barrier()
            S.op("pool", lambda e: e.memset(Hf[:], 0.0), writes=[Hf_r])
            S.op("pool", lambda e: e.memset(Hb16[:], 0.0), writes=[Hb16_r])
            wo_t, wo_r = self.load_w(pA, w_out[0], 16, D, "wout")
            for kc in range(16):
                S.op("dve", lambda e, kc=kc: e.tensor_scalar(out=wo_t[:, kc, :], in0=wo_t[:, kc, :], scalar1=sv[:, 144 + kc:145 + kc], scalar2=None,
                                                            op0=ALU.mult), reads=[wo_r, sv_r], writes=[wo_r])
            btr = Ring([M.sb(pA, [128, 4, 128], BF16, "btc") for _ in range(2)])
            ctr = Ring([M.sb(pA, [128, 4, 128], BF16, "ctc") for _ in range(2)])
            zsr = Ring([M.sb(pA, [128, 2048], BF16, "zsc") for _ in range(2)])
            xdf, xdf_r = M.sb(pA, [128, 2048], BF16, "xdf")
            xdb, xdb_r = M.sb(pA, [128, 2048], BF16, "xdb")
            gm, gm_r = M.sb(pA, [128, 2, 128], F32, "gm")
            lhr = Ring([M.sb(pA, [128, 128], F32, "lh") for _ in range(4)])
            dhr = Ring([M.sb(pA, [128, 128], F32, "dh") for _ in range(4)])
            mtr = Ring([M.sb(pA, [128, 128], BF16, "mt") for _ in range(4)])
            yg, yg_r = M.sb(pA, [128, 512], F32, "yg")
            tq = Ring([M.sb(pA, [128, 512], F32, "tq") for _ in range(2)])
            ybf, ybf_r = M.sb(pA, [128, 2048], BF16, "ybf")
            ynT, ynT_r = M.sb(pA, [128, 16, 128], BF16, "ynT")
            ssq, ssq_r = M.sb(pA, [128, 8], F32, "ssq")
            psg = Ring([M.ps(pA, [128, 128], F32, "psg") for _ in range(2)])
            pd, pd_r = M.ps(pA, [128, 512], F32, "pd")
            pf, pf_r = M.ps(pA, [128, 2, 512], F32, "pf")
            ptT, ptT_r = M.ps(pA, [128, 8, 128], BF16, "ptT")
            pout, pout_r = M.ps(pA, [128, 128], F32, "pout")
            for t in range(NT):
                xs, xs_r = xsr.next()
                bm, bm_r = bmr.next()
                bt, bt_r = btr.next()
                ct, ct_r = ctr.next()
                zs, zs_r = zsr.next()
                hb, hb_r = hbo.next()
                S.dma("sp", xs[:], XS[t], reads=[XS_r], writes=[xs_r], key=xs_r)
                S.dma("sp", bm[:], BMd[t], reads=[BMd_r], writes=[bm_r], key=bm_r)
                S.dma("sp", bt[:], BTd[t], reads=[BTd_r], writes=[bt_r], key=bt_r)
                S.dma("sp", ct[:], CTd[t], reads=[CTd_r], writes=[ct_r], key=ct_r)
                S.dma("sp", zs[:], ZS[t], reads=[ZS_r], writes=[zs_r], key=zs_r)
                S.dma("sp", hb[:], HB[t], reads=[HB_r], writes=[hb_r], key=hb_r)
                for g in range(4):
                    S.op("dve", lambda e, g=g, xs=xs, t=t: e.tensor_tensor(out=v3(xdf[:, g * 512:(g + 1) * 512]), in0=v3(xs[:, g * 512:(g + 1) * 512]),
                                                                        in1=bc(dt[:, t, g * 8:(g + 1) * 8]), op=ALU.mult),
                         reads=[xs_r, dt_r], writes=[xdf_r])
                    S.op("pool", lambda e, g=g, xs=xs, t=t: e.tensor_tensor(out=v3(xdb[:, g * 512:(g + 1) * 512]), in0=v3(xs[:, g * 512:(g + 1) * 512]),
                                                                         in1=bc(dt[:, t, 32 + g * 8:32 + (g + 1) * 8]), op=ALU.mult),
                         reads=[xs_r, dt_r], writes=[xdb_r])
                S.op("pool", lambda e: e.memset(ssq[:], 0.0), reads=[ssq_r], writes=[ssq_r])
                for g in range(4):
                    S.op("pe", lambda e, g=g, bt=bt, ct=ct: e.matmul(pst[:, 0:128], lhsT=bt[:, g, :], rhs=ct[:, g, :], start=True, stop=True),
                         reads=[bt_r, ct_r], writes=[pst_r], pe_acc=True)
                    S.op("dve", lambda e: e.tensor_tensor(out=gm[:, 0, :], in0=pst[:, 0:128], in1=triF, op=ALU.mult), reads=[pst_r, msk_r], writes=[gm_r])
                    S.op("dve", lambda e: e.tensor_tensor(out=gm[:, 1, :], in0=pst[:, 0:128], in1=triB, op=ALU.mult), reads=[pst_r, msk_r], writes=[gm_r])
                    units = [(r, d_) for r in range(8) for d_ in range(2)]
                    ctxs = {}

                    def st1(u):
                        r, d_ = units[u]
                        h = g * 8 + r
                        mlt = mltF if d_ == 0 else mltB
                        tri = triF if d_ == 0 else triB
                        lh, lh_r = lhr.next()
                        sc_ap = loga[:, t, d_ * 32 + h:d_ * 32 + h + 1]
                        S.op("act", lambda e: e.activation(out=lh[:], in_=mlt, func=AF.Copy, scale=sc_ap),
                             reads=[msk_r, loga_r], writes=[lh_r])
                        sg_, sg_r = psg.next()
                        S.op("pe", lambda e: e.matmul(sg_[:], lhsT=lh[:], rhs=tri, start=True, stop=True),
                             reads=[lh_r, msk_r], writes=[sg_r], pe_acc=True)
                        ctxs[u] = (sg_, sg_r)

                    def st2(u):
                        r, d_ = units[u]
                        sg_, sg_r = ctxs[u]
                        dh, dh_r = dhr.next()
                        S.op("act", lambda e: e.activation(out=dh[:], in_=sg_[:], func=AF.Exp), reads=[sg_r], writes=[dh_r])
                        mt, mt_r = mtr.next()
                        S.op("dve", lambda e: e.tensor_tensor(out=mt[:], in0=gm[:, d_, :], in1=dh[:], op=ALU.mult),
                             reads=[gm_r, dh_r], writes=[mt_r])
                        ctxs[u] = (mt, mt_r)

                    def st3(u):
                        r, d_ = units[u]
                        h = g * 8 + r
                        mt, mt_r = ctxs.pop(u)
                        xd, xd_r = (xdf, xdf_r) if d_ == 0 else (xdb, xdb_r)
                        S.op("pe", lambda e: e.matmul(
                            pd[:, r * 64:(r + 1) * 64], lhsT=mt[:], rhs=xd[:, h * 64:(h + 1) * 64], start=(d_ == 0), stop=(d_ == 1)),
                            reads=[mt_r, xd_r], writes=[pd_r], pe_acc=True)
                    nu = len(units)
                    for idx in range(nu + 2):
                        if idx < nu:
                            st1(idx)
                        if 0 <= idx - 1 < nu:
                            st2(idx - 1)
                        if 0 <= idx - 2 < nu:
                            st3(idx - 2)
                    S.op("pe", lambda e, g=g, ct=ct: e.matmul(pf[:, 0, :], lhsT=ct[:, g, :], rhs=Hb16[:, g, :], start=True, stop=True),
                         reads=[ct_r, Hb16_r], writes=[pf_r], pe_acc=True)
                    S.op("pe", lambda e, g=g, ct=ct, hb=hb: e.matmul(pf[:, 1, :], lhsT=ct[:, g, :], rhs=hb[:, g * 512:(g + 1) * 512], start=True, stop=True),
                         reads=[ct_r, hb_r], writes=[pf_r], pe_acc=True)
                    S.op("act", lambda e: e.activation(out=yg[:], in_=pd[:], func=AF.Copy), reads=[pd_r], writes=[yg_r])
                    for d_ in range(2):
                        q_, q_r = tq.next()
                        S.op("dve", lambda e, q_=q_, d_=d_, t=t, g=g: e.tensor_tensor(out=v3(q_[:]), in0=v3(pf[:, d_, :]),
                                                                                  in1=bc(expA[:, t, d_ * 32 + g * 8:d_ * 32 + (g + 1) * 8]), op=ALU.mult),
                             reads=[pf_r, expA_r], writes=[q_r])
                        S.op("pool", lambda e, q_=q_: e.tensor_tensor(out=yg[:], in0=yg[:], in1=q_[:], op=ALU.add), reads=[yg_r, q_r], writes=[yg_r])
                    q_, q_r = tq.next()
                    S.op("dve", lambda e, q_=q_, g=g, xs=xs: e.tensor_tensor(out=v3(q_[:]), in0=v3(xs[:, g * 512:(g + 1) * 512]),
                                                                          in1=bc(dsk[:, g * 8:(g + 1) * 8]), op=ALU.mult),
                         reads=[xs_r, dsk_r], writes=[q_r])
                    S.op("pool", lambda e, q_=q_: e.tensor_tensor(out=yg[:], in0=yg[:], in1=q_[:], op=ALU.add), reads=[yg_r, q_r], writes=[yg_r])
                    S.op("dve", lambda e, g=g, zs=zs: e.tensor_tensor(out=yg[:], in0=yg[:], in1=zs[:, g * 512:(g + 1) * 512], op=ALU.mult),
                         reads=[yg_r, zs_r], writes=[yg_r])
                    jk, jk_r = tq.next()
                    S.op("act", lambda e, g=g, jk=jk: e.activation(out=jk[:], in_=yg[:], func=AF.Square, accum_out=ssq[:, g:g + 1]),
                         reads=[yg_r, ssq_r], writes=[jk_r, ssq_r])
                    S.op("pool", lambda e, g=g: e.tensor_copy(out=ybf[:, g * 512:(g + 1) * 512], in_=yg[:]), reads=[yg_r], writes=[ybf_r])
                S.op("dve", lambda e: e.reduce_sum(out=ssq[:, 4:5], in_=ssq[:, 0:4], axis=AX.X), reads=[ssq_r], writes=[ssq_r])
                S.op("act", lambda e: e.activation(out=ssq[:, 5:6], in_=ssq[:, 4:5], func=AF.Sqrt, scale=1.0 / 2048, bias=self.epsb[:, 0:1]),
                     reads=[ssq_r, self.epsb_r], writes=[ssq_r])
                S.op("dve", lambda e: e.reciprocal(out=ssq[:, 6:7], in_=ssq[:, 5:6]), reads=[ssq_r], writes=[ssq_r])
                S.op("dve", lambda e: e.tensor_scalar(out=ybf[:], in0=ybf[:], scalar1=ssq[:, 6:7], scalar2=None, op0=ALU.mult),
                     reads=[ybf_r, ssq_r], writes=[ybf_r])
                for b2 in range(2):
                    for jj in range(8):
                        kc = b2 * 8 + jj
                        S.op("pe", lambda e, jj=jj, kc=kc: e.transpose(out=ptT[:, jj, :], in_=ybf[:, kc * 128:(kc + 1) * 128], identity=self.identb[:]),
                             reads=[ybf_r, self.identb_r], writes=[ptT_r], pe_acc=True)
                    S.op("act", lambda e, b2=b2: e.activation(out=ynT[:, b2 * 8:(b2 + 1) * 8, :], in_=ptT[:], func=AF.Copy), reads=[ptT_r], writes=[ynT_r])
                for oc in range(8):
                    for kc in range(16):
                        S.op("pe", lambda e, oc=oc, kc=kc: e.matmul(pout[:], lhsT=wo_t[:, kc, oc * 128:(oc + 1) * 128], rhs=ynT[:, kc, :],
                                                                    start=(kc == 0), stop=(kc == 15)),
                             reads=[wo_r, ynT_r], writes=[pout_r], pe_acc=True)
                    self.resid(pout, pout_r, 128, oc, t * 128)
                state_update(t, xs, xs_r, bm, bm_r, 0)
                S.op("act", lambda e: e.activation(out=Hb16[:], in_=Hf[:], func=AF.Copy), reads=[Hf_r], writes=[Hb16_r])
            S.barrier()

    def out_raw(self):
        nc, S, M = self.nc, self.S, self.M
        y = nc.dram_tensor("y", [T, D], F32, kind="ExternalOutput").ap()
        y_r = Res("y")
        with ExitStack() as ps:
            pst = Ring([M.ps(ps, [128, 4, 128], F32, "otp") for _ in range(2)])
            stg = Ring([M.sb(ps, [128, D], F32, "ostg") for _ in range(2)])
            for t in range(NT):
                st, st_r = stg.next()
                for h in range(2):
                    pt, pt_r = pst.next()
                    for k in range(4):
                        S.op("pe", lambda e, pt=pt, k=k, h=h, t=t: e.transpose(
                            out=pt[:, k, :], in_=self.xT[:, h * 4 + k, t * 128:(t + 1) * 128], identity=self.identf[:]),
                            reads=[self.xTr, self.identf_r], writes=[pt_r], pe_acc=True)
                    S.op("dve", lambda e, pt=pt, st=st, h=h: e.tensor_copy(out=st[:, h * 512:(h + 1) * 512], in_=pt[:]),
                         reads=[pt_r], writes=[st_r])
                S.dma("sp", y[t * 128:(t + 1) * 128, :], st[:], reads=[st_r], writes=[y_r], key=st_r)
            S.barrier()

    def out_final(self):
        nc, S, M = self.nc, self.S, self.M
        y = nc.dram_tensor("y", [NLAT, D], F32, kind="ExternalOutput").ap()
        gfin = self.inp("g_final", [D])
        y_r = Res("y")
        with ExitStack() as ps:
            gb, gb_r = M.sb(ps, [128, D], F32, "gfin")
            S.dma("sp", gb[:], gfin.partition_broadcast(128), writes=[gb_r], key=gb_r)
            pst = Ring([M.ps(ps, [128, 4, 128], F32, "otp") for _ in range(2)])
            stg = Ring([M.sb(ps, [128, D], F32, "ostg") for _ in range(2)])
            jk = Ring([M.sb(ps, [128, D], F32, "ojk") for _ in range(2)])
            ssq = Ring([M.sb(ps, [128, 1], F32, "ossq") for _ in range(2)])
            for t in range(NCTX // 128, NT):
                st, st_r = stg.next()
                for h in range(2):
                    pt, pt_r = pst.next()
                    for k in range(4):
                        S.op("pe", lambda e, pt=pt, k=k, h=h, t=t: e.transpose(
                            out=pt[:, k, :], in_=self.xT[:, h * 4 + k, t * 128:(t + 1) * 128], identity=self.identf[:]),
                            reads=[self.xTr, self.identf_r], writes=[pt_r], pe_acc=True)
                    S.op("dve", lambda e, pt=pt, st=st, h=h: e.tensor_copy(out=st[:, h * 512:(h + 1) * 512], in_=pt[:]),
                         reads=[pt_r], writes=[st_r])
                j_, j_r = jk.next()
                q, q_r = ssq.next()
                S.op("act", lambda e, j_=j_, st=st, q=q: e.activation(out=j_[:], in_=st[:], func=AF.Square, accum_out=q[:]),
                     reads=[st_r], writes=[j_r, q_r])
                S.op("act", lambda e, q=q: e.activation(out=q[:], in_=q[:], func=AF.Sqrt, scale=1.0 / D, bias=self.epsb[:, 0:1]),
                     reads=[q_r, self.epsb_r], writes=[q_r])
                S.op("dve", lambda e, q=q: e.reciprocal(out=q[:], in_=q[:]), reads=[q_r], writes=[q_r])
                S.op("dve", lambda e, j_=j_, st=st, q=q: e.scalar_tensor_tensor(out=j_[:], in0=st[:], scalar=q[:, 0:1], in1=gb[:],
                                                                              op0=ALU.mult, op1=ALU.mult),
                     reads=[st_r, q_r, gb_r, j_r], writes=[j_r])
                r0 = (t - NCTX // 128) * 128
                S.dma("sp", y[r0:r0 + 128, :], j_[:], reads=[j_r], writes=[y_r], key=j_r)
            S.barrier()


FULL_STEPS = []
for _i in range(4):
    FULL_STEPS += [("mods", _i), ("mixer", _i), ("ffn", _i, _i < 3)]


def make_in_maps(inputs, ncores=8, xs=None, cs=None):
    f32 = np.float32
    shared = {}
    lvec = np.zeros((4, 128, 64), f32)
    for i in range(4):
        lvec[i, :, 0:48] = fm(inputs["ada_b"][i])
        lvec[i, :, 48:56] = fm(inputs["g_mix"][i])
        lvec[i, :, 56:64] = fm(inputs["g_ffn"][i])
    shared["lvec"] = lvec
    shared["ada_w"] = np.ascontiguousarray(inputs["ada_w"], f32)
    shared["g_final"] = np.ascontiguousarray(inputs["g_final"], f32)
    shared["moe_w_router"] = np.ascontiguousarray(inputs["moe_w_router"], f32)
    shared["moe_b_router"] = np.ascontiguousarray(inputs["moe_b_router"], f32)
    wgu = np.ascontiguousarray(inputs["moe_w_gu"], f32)
    wdn = np.ascontiguousarray(inputs["moe_w_down"], f32)
    shared["moe_w_gu"] = wgu
    shared["moe_w_down"] = wdn
    shared["moe_w_gu2d"] = wgu.reshape(-1, 2 * D)
    shared["moe_w_down2d"] = wdn.reshape(-1, D)
    shared["moe_b_down2d"] = np.ascontiguousarray(inputs["moe_b_down"], f32).reshape(-1, D)
    shared["moe_b_down"] = np.ascontiguousarray(inputs["moe_b_down"], f32)
    bgu = np.asarray(inputs["moe_b_gu"], f32)
    shared["moe_b_gu_fm"] = np.ascontiguousarray(bgu.reshape(4, NE, 16, 128).transpose(0, 3, 1, 2))
    shared["moe_b_gu_rows"] = np.ascontiguousarray(bgu.reshape(4, NE, 16, 128).transpose(0, 1, 3, 2)).reshape(4 * NE * 128, 16)
    if "conv_w_pw1" in inputs:
        shared["conv_w_pw1"] = np.ascontiguousarray(inputs["conv_w_pw1"], f32)
        shared["conv_w_pw2"] = np.ascontiguousarray(inputs["conv_w_pw2"], f32)
        cvec = np.zeros((128, 296), f32)
        cvec[:, 0:16] = fm(inputs["conv_b_pw1"][0])
        wdw = np.asarray(inputs["conv_w_dw"][0], f32)
        cvec[:, 16:264] = wdw.reshape(31, 8, 128).transpose(2, 1, 0).reshape(128, 248)
        cvec[:, 264:272] = fm(inputs["conv_b_dw"][0])
        cvec[:, 272:280] = fm(inputs["conv_ln_g"][0])
        cvec[:, 280:288] = fm(inputs["conv_ln_b"][0])
        cvec[:, 288:296] = fm(inputs["conv_b_pw2"][0])
        shared["conv_vec"] = cvec
    if "swa_w_qkv" in inputs:
        shared["swa_w_qkv"] = np.ascontiguousarray(inputs["swa_w_qkv"], f32)
        shared["swa_b_qkv"] = np.ascontiguousarray(inputs["swa_b_qkv"], f32)
        shared["swa_w_o"] = np.ascontiguousarray(inputs["swa_w_o"], f32)
        shared["swa_sinks"] = np.ascontiguousarray(inputs["swa_sinks"], f32)
        bq = np.asarray(inputs["swa_b_qkv"][0], f32).reshape(24, 64).T
        bh = np.zeros((64, 44), f32)
        bh[:, 0:24] = bq
        bh[:, 24:44] = np.roll(bq[:, 0:20], 32, axis=0)
        shared["swa_bh"] = bh
        shared["swa_bo_fm"] = fm(inputs["swa_b_o"][0])
        Ct, St = rope_tables()
        shared["rope_c"] = Ct
        shared["rope_s"] = St
    if "diff_w_qkv" in inputs:
        shared["diff_w_qkv"] = np.ascontiguousarray(inputs["diff_w_qkv"], f32)
        shared["diff_w_o"] = np.ascontiguousarray(inputs["diff_w_o"], f32)
        shared["diff_lam"] = np.ascontiguousarray(np.stack([inputs["diff_lambda_q1"][0], inputs["diff_lambda_k1"][0],
                                                            inputs["diff_lambda_q2"][0], inputs["diff_lambda_k2"][0]], 0), f32)
        shared["diff_subln_g"] = np.ascontiguousarray(np.asarray(inputs["diff_subln_g"][0], f32).reshape(128, 1))
        if "rope_c" not in shared:
            Ct, St = rope_tables()
            shared["rope_c"] = Ct
            shared["rope_s"] = St
    if "ssm_w_in" in inputs:
        shared["ssm_w_in"] = np.ascontiguousarray(inputs["ssm_w_in"], f32)
        shared["ssm_w_out"] = np.ascontiguousarray(inputs["ssm_w_out"], f32)
        svec = np.zeros((128, 160), f32)
        wc = np.asarray(inputs["ssm_w_conv"][0], f32)
        svec[:, 0:120] = wc.reshape(5, 24, 128).transpose(2, 1, 0).reshape(128, 120)
        svec[:, 120:144] = fm(inputs["ssm_b_conv"][0])
        svec[:, 144:160] = fm(inputs["ssm_norm_g"][0])
        shared["ssm_vec"] = svec
        shared["ssm_a_log"] = np.ascontiguousarray(np.asarray(inputs["ssm_a_log"], f32).reshape(1, 64))
        shared["ssm_dt_bias"] = np.ascontiguousarray(np.asarray(inputs["ssm_dt_bias"], f32).reshape(1, 64))
        shared["ssm_d"] = np.ascontiguousarray(np.asarray(inputs["ssm_d"], f32).reshape(1, 32))
    maps = []
    for b in range(ncores):
        m = dict(shared)
        m["x_in"] = np.ascontiguousarray(np.concatenate([inputs["ctx"][b], inputs["x"][b]], axis=0), f32)
        cond = np.stack([fm(inputs["c"][b]), fm(inputs["c_ctx"])], axis=-1)
        m["cond"] = np.ascontiguousarray(cond, f32)
        maps.append(m)
    return maps


def kernel(**inputs):
    prog = Prog(FULL_STEPS)
    nc = prog.build()
    maps = make_in_maps(inputs)
    maps = [{k: v for k, v in m.items() if k in prog.din} for m in maps]
    res = run_bass_kernel_spmd(nc, maps, core_ids=list(range(8)))
    return np.stack([r["y"] for r in res.results], axis=0).astype(np.float32)
```
